# Optimizing a Trainium2 kernel written in Bass

```python
import jax, jax.numpy as jnp
from jax import lax
import numpy as np

D_MODEL = 1024
BATCH = 32
SEQ = 2048
DEPTH = 2

GRID_W = 64
CTX_LEN = 256
EPS = 1e-6

HG_HEADS = 4
HG_KDIM = 128
HG_VDIM = 128
HG_KW = HG_HEADS * HG_KDIM
HG_VW = HG_HEADS * HG_VDIM
HG_CHUNK = 16
POOL_WINDOWS = (2, 4, 8, 16)
POOL_GROUP = 128
POOL_WIDTH = len(POOL_WINDOWS) * POOL_GROUP
EV_SPLITS = (HG_KW, 2 * HG_KW, 3 * HG_KW, 3 * HG_KW + HG_VW, 3 * HG_KW + 2 * HG_VW)
EV_IN = 3 * HG_KW + 2 * HG_VW + POOL_WIDTH
EV_MIX = HG_VW + POOL_WIDTH
CONV_WIDTH = 512
CONV_K = 31
MLA_HEADS = 8
MLA_Q_RANK = 384
MLA_KV_RANK = 256
MLA_NOPE = 64
MLA_ROPE = 32
MLA_V = 64
MLA_QK = MLA_NOPE + MLA_ROPE
ROPE_AXIS_HALF = MLA_ROPE // 4
ROPE_BASE = 10000.0
Q_BLOCK = 128
OD_C1 = 2 * CONV_WIDTH
OD_C2 = OD_C1 + MLA_Q_RANK
OD_C3 = OD_C2 + MLA_KV_RANK
OD_IN = OD_C3 + MLA_ROPE
OD_MIX = CONV_WIDTH + MLA_HEADS * MLA_V
N_EXPERTS = 32
TOP_K = 4
D_FF = 1024
SWIGLU_LIMIT = 7.0
SWIGLU_ALPHA = 1.702
MOE_BLOCK = 256

N_EVEN = (DEPTH + 1) // 2
N_ODD = DEPTH // 2

kernel_name = "hybrid_hgrn2_pool_conformer_mla_moe_dit"


def rmsnorm(h, gain=None):
    h32 = h.astype(jnp.float32)
    y = h32 * lax.rsqrt(jnp.mean(h32 * h32, axis=-1, keepdims=True) + EPS)
    if gain is not None:
        y = y * gain.astype(jnp.float32)
    return y.astype(h.dtype)


def layernorm(h, w, b):
    h32 = h.astype(jnp.float32)
    mu = jnp.mean(h32, axis=-1, keepdims=True)
    d = h32 - mu
    var = jnp.mean(d * d, axis=-1, keepdims=True)
    return (d * lax.rsqrt(var + EPS) * w.astype(jnp.float32) + b.astype(jnp.float32)).astype(h.dtype)


def modulate(h, shift, scale):
    return rmsnorm(h) * (1.0 + scale) + shift


def axial_rope_tables(rows):
    t_row = jnp.repeat(jnp.arange(rows, dtype=jnp.float32), GRID_W)
    t_col = jnp.tile(jnp.arange(GRID_W, dtype=jnp.float32), rows)
    inv = 1.0 / (ROPE_BASE ** (jnp.arange(ROPE_AXIS_HALF, dtype=jnp.float32) / ROPE_AXIS_HALF))
    ang = jnp.stack([t_row[:, None] * inv, t_col[:, None] * inv], axis=1)
    return jnp.cos(ang), jnp.sin(ang)


def rope2d(x, cos, sin):
    shp = x.shape
    xr = x.reshape(shp[:-1] + (2, 2, ROPE_AXIS_HALF))
    x1, x2 = xr[..., 0, :], xr[..., 1, :]
    cos, sin = cos.astype(x.dtype), sin.astype(x.dtype)
    out = jnp.stack([x1 * cos - x2 * sin, x2 * cos + x1 * sin], axis=-2)
    return out.reshape(shp)


def gla_chunkwise(q, k, v, logf, s0):
    B, H, L, K = q.shape
    V = v.shape[-1]
    C = HG_CHUNK
    N = L // C
    q, k, logf = (a.reshape(B, H, N, C, K) for a in (q, k, logf))
    v = v.reshape(B, H, N, C, V)
    b = jnp.cumsum(logf, axis=3)
    b_last = b[:, :, :, -1:, :]
    q_dec = q * jnp.exp(b)
    k_inv = k * jnp.exp(-b)
    k_end = k * jnp.exp(b_last - b)
    mask = jnp.tril(jnp.ones((C, C), dtype=bool))
    att = jnp.where(mask, jnp.einsum('bhnck,bhnsk->bhncs', q_dec, k_inv), 0.0)
    o_intra = jnp.einsum('bhncs,bhnsv->bhncv', att, v)
    chunk_decay = jnp.exp(b_last[:, :, :, 0, :])

    def step(S, inp):
        qd, ke, vv, dec = inp
        o = jnp.einsum('bhck,bhkv->bhcv', qd, S)
        S = S * dec[..., None] + jnp.einsum('bhck,bhcv->bhkv', ke, vv)
        return S, o

    xs = (jnp.moveaxis(q_dec, 2, 0), jnp.moveaxis(k_end, 2, 0),
          jnp.moveaxis(v, 2, 0), jnp.moveaxis(chunk_decay, 2, 0))
    s_final, o_inter = lax.scan(step, s0, xs)
    o = o_intra + jnp.moveaxis(o_inter, 0, 2)
    return o.reshape(B, H, L, V), s_final


def to_heads(a):
    B, L, _ = a.shape
    return a.astype(jnp.float32).reshape(B, L, HG_HEADS, -1).transpose(0, 2, 1, 3)


def hgrn2_direction(qc, zc, vc, ql, zl, vl, lb, reverse):
    lb = lb.reshape(HG_HEADS, 1, HG_KDIM)

    def gates(z):
        f = lb + (1.0 - lb) * jax.nn.sigmoid(z)
        return 1.0 - f, jnp.log(f)

    flip = (lambda a: jnp.flip(a, axis=2)) if reverse else (lambda a: a)
    kc, gc = gates(zc)
    kl, gl = gates(zl)
    s0 = jnp.zeros(qc.shape[:2] + (HG_KDIM, HG_VDIM), jnp.float32)
    oc, s_ctx = gla_chunkwise(flip(qc), flip(kc), flip(vc), flip(gc), s0)
    ol, _ = gla_chunkwise(flip(ql), flip(kl), flip(vl), flip(gl), s_ctx)
    return flip(ol), flip(oc)


def hgrn2_bidir(ctx_parts, lat_parts, lb_f, lb_b):
    qc, zfc, zbc, vc = (to_heads(a) for a in ctx_parts)
    ql, zfl, zbl, vl = (to_heads(a) for a in lat_parts)
    qc, ql = jax.nn.silu(qc), jax.nn.silu(ql)
    ol_f, oc_f = hgrn2_direction(qc, zfc, vc, ql, zfl, vl, lb_f, False)
    ol_b, oc_b = hgrn2_direction(qc, zbc, vc, ql, zbl, vl, lb_b, True)
    return ol_f + ol_b, oc_f + oc_b


def hgrn_readout(o, og, norm_w):
    B, H, L, V = o.shape
    o = rmsnorm(o.transpose(0, 2, 1, 3), norm_w)
    gate = og.reshape(B, L, H, V)
    return (o.astype(og.dtype) * jax.nn.silu(gate)).reshape(B, L, H * V)


def multiscale_pool(u, pool_w, pool_scale):
    B, L, _ = u.shape
    u32 = u.astype(jnp.float32)
    cs = jnp.pad(jnp.cumsum(u32, axis=1), ((0, 0), (1, 0), (0, 0)))
    t = jnp.arange(L)
    outs = []
    for g, w in enumerate(POOL_WINDOWS):
        sl = slice(g * POOL_GROUP, (g + 1) * POOL_GROUP)
        lo = jnp.clip(t - w // 2, 0, L)
        hi = jnp.clip(t + w - w // 2, 0, L)
        seg = cs[..., sl]
        cnt = (hi - lo).astype(jnp.float32)[None, :, None]
        outs.append((seg[:, hi] - seg[:, lo]) / cnt - u32[..., sl])
    d = jnp.stack(outs, axis=2).astype(u.dtype)
    y = jnp.einsum('blgc,gcd->blgd', d, pool_w)
    return y.reshape(B, L, POOL_WIDTH) * pool_scale


def even_mixer(u, uc, w_in, lb_f, lb_b, norm_w, pool_w, pool_scale, w_out, need_ctx):
    q, zf, zb, v, og, pin = jnp.split(u @ w_in, EV_SPLITS, axis=-1)
    qc, zfc, zbc, vc, ogc, pinc = jnp.split(uc @ w_in, EV_SPLITS, axis=-1)
    o, oc = hgrn2_bidir((qc, zfc, zbc, vc), (q, zf, zb, v), lb_f, lb_b)
    y = jnp.concatenate([hgrn_readout(o, og, norm_w), multiscale_pool(pin, pool_w, pool_scale)], axis=-1) @ w_out
    if not need_ctx:
        return y, None
    yc = jnp.concatenate([hgrn_readout(oc, ogc, norm_w), multiscale_pool(pinc, pool_w, pool_scale)], axis=-1) @ w_out
    return y, yc


def conformer_conv(a, dw_w, dw_b, ln_w, ln_b):
    val, gate = jnp.split(a, 2, axis=-1)
    h = val * jax.nn.sigmoid(gate)
    h = lax.conv_general_dilated(h, dw_w[:, None, :], window_strides=(1,),
                                 padding=((CONV_K // 2, CONV_K // 2),),
                                 dimension_numbers=('NWC', 'WIO', 'NWC'),
                                 feature_group_count=CONV_WIDTH) + dw_b
    return jax.nn.silu(layernorm(h, ln_w, ln_b))


def mla_project(cq, ckv, kr, q_a_norm, w_uq, kv_a_norm, w_ukv, q_norm, k_norm, cos, sin):
    B, L, _ = ckv.shape
    kv = (rmsnorm(ckv, kv_a_norm) @ w_ukv).reshape(B, L, MLA_HEADS, MLA_NOPE + MLA_V)
    k_nope, v = kv[..., :MLA_NOPE], kv[..., MLA_NOPE:]
    k = jnp.concatenate([k_nope, jnp.broadcast_to(kr[:, :, None, :], (B, L, MLA_HEADS, MLA_ROPE))], axis=-1)
    k = rmsnorm(k, k_norm)
    q = None
    if cq is not None:
        q = (rmsnorm(cq, q_a_norm) @ w_uq).reshape(B, L, MLA_HEADS, MLA_QK)
        q = rmsnorm(q, q_norm)
    if cos is not None:
        k = jnp.concatenate([k[..., :MLA_NOPE], rope2d(k[..., MLA_NOPE:], cos[:, None], sin[:, None])], axis=-1)
        q = jnp.concatenate([q[..., :MLA_NOPE], rope2d(q[..., MLA_NOPE:], cos[:, None], sin[:, None])], axis=-1)
    return q, k, v


def attend(q, k, v):
    s = jnp.einsum('bqhd,bkhd->bhqk', q, k).astype(jnp.float32) * (MLA_QK ** -0.5)
    p = jax.nn.softmax(s, axis=-1).astype(v.dtype)
    return jnp.einsum('bhqk,bkhd->bqhd', p, v)


def blockwise_attention(q, k, v):
    B, L, H, Dq = q.shape
    nb = L // Q_BLOCK
    qb = q.reshape(B, nb, Q_BLOCK, H, Dq).transpose(1, 0, 2, 3, 4)
    o = lax.map(lambda qq: attend(qq, k, v), qb)
    return o.transpose(1, 0, 2, 3, 4).reshape(B, L, H * MLA_V)


def odd_mixer(u, uc, w_in, dw_w, dw_b, ln_w, ln_b, q_a_norm, w_uq, kv_a_norm, w_ukv,
              q_norm, k_norm, w_out, cos, sin, need_ctx):
    mla_w = (q_a_norm, w_uq, kv_a_norm, w_ukv, q_norm, k_norm)
    p = u @ w_in
    q, k, v = mla_project(p[..., OD_C1:OD_C2], p[..., OD_C2:OD_C3], p[..., OD_C3:], *mla_w, cos, sin)
    if need_ctx:
        pc = uc @ w_in
        qc, kc, vc = mla_project(pc[..., OD_C1:OD_C2], pc[..., OD_C2:OD_C3], pc[..., OD_C3:], *mla_w, None, None)
    else:
        pc = uc @ w_in[:, OD_C2:]
        _, kc, vc = mla_project(None, pc[..., :MLA_KV_RANK], pc[..., MLA_KV_RANK:], *mla_w, None, None)
    k_all = jnp.concatenate([kc, k], axis=1)
    v_all = jnp.concatenate([vc, v], axis=1)
    attn = blockwise_attention(q, k_all, v_all)
    y = jnp.concatenate([conformer_conv(p[..., :OD_C1], dw_w, dw_b, ln_w, ln_b), attn], axis=-1) @ w_out
    if not need_ctx:
        return y, None
    Bc, Lc = uc.shape[:2]
    attn_c = attend(qc, kc, vc).reshape(Bc, Lc, MLA_HEADS * MLA_V)
    yc = jnp.concatenate([conformer_conv(pc[..., :OD_C1], dw_w, dw_b, ln_w, ln_b), attn_c], axis=-1) @ w_out
    return y, yc


def moe_ffn(x2, w_r, b_r, w1, b1, w2, b2):
    T, D = x2.shape
    logits = (x2 @ w_r + b_r).astype(jnp.float32)
    top_v, top_i = lax.top_k(logits, TOP_K)
    gates = jax.nn.softmax(top_v, axis=-1)
    M = T * TOP_K
    flat_e = top_i.reshape(-1)
    flat_t = jnp.repeat(jnp.arange(T, dtype=jnp.int32), TOP_K)
    flat_g = gates.reshape(-1)
    order = jnp.argsort(flat_e)
    se, st, sg = flat_e[order], flat_t[order], flat_g[order]
    counts = jnp.bincount(flat_e, length=N_EXPERTS)
    padded = (counts + MOE_BLOCK - 1) // MOE_BLOCK * MOE_BLOCK
    pad_end = jnp.cumsum(padded)
    pad_start = pad_end - padded
    start = jnp.cumsum(counts) - counts
    dest = pad_start[se] + jnp.arange(M, dtype=jnp.int32) - start[se]
    n_blocks = -(-M // MOE_BLOCK) + N_EXPERTS
    P = n_blocks * MOE_BLOCK
    buf_t = jnp.zeros((P,), jnp.int32).at[dest].set(st)
    buf_g = jnp.zeros((P,), jnp.float32).at[dest].set(sg)
    blk_e = jnp.minimum(jnp.searchsorted(pad_end, jnp.arange(n_blocks) * MOE_BLOCK, side='right'), N_EXPERTS - 1)

    def step(acc, inp):
        idx, g, e = inp
        hb = x2[idx] @ w1[e] + b1[e]
        gl, lin = hb[:, :D_FF], hb[:, D_FF:]
        gl = jnp.minimum(gl, SWIGLU_LIMIT)
        lin = jnp.clip(lin, -SWIGLU_LIMIT, SWIGLU_LIMIT)
        a = gl * jax.nn.sigmoid(SWIGLU_ALPHA * gl) * (lin + 1.0)
        y = a @ w2[e] + b2[e]
        return acc.at[idx].add(y.astype(jnp.float32) * g[:, None]), None

    out, _ = lax.scan(step, jnp.zeros((T, D), jnp.float32),
                      (buf_t.reshape(n_blocks, MOE_BLOCK), buf_g.reshape(n_blocks, MOE_BLOCK), blk_e))
    return out.astype(x2.dtype)


def setup_inputs(seed: int = 0) -> dict:
    key = jax.random.key(seed)
    ks = iter(jax.random.split(key, 40))
    D = D_MODEL

    def nrm(shape, s):
        return jax.random.normal(next(ks), shape, jnp.float32) * s

    def gain(shape):
        return 1.0 + nrm(shape, 0.1)

    return {
        "x": nrm((BATCH, SEQ, D), 1.0),
        "c": nrm((BATCH, D), 1.0),
        "ctx": nrm((BATCH, CTX_LEN, D), 1.0),
        "c_ctx": nrm((D,), 1.0),
        "ada_w": nrm((DEPTH, D, 6 * D), 0.5 * D ** -0.5),
        "ada_b": nrm((DEPTH, 6 * D), 0.02),
        "ev_w_in": nrm((N_EVEN, D, EV_IN), D ** -0.5),
        "hgrn_lb_logits": nrm((DEPTH + 1, 2, HG_KW), 0.1),
        "hgrn_norm_w": gain((N_EVEN, HG_VDIM)),
        "pool_w": nrm((N_EVEN, len(POOL_WINDOWS), POOL_GROUP, POOL_GROUP), POOL_GROUP ** -0.5),
        "pool_scale": gain((N_EVEN, POOL_WIDTH)),
        "ev_w_out": nrm((N_EVEN, EV_MIX, D), EV_MIX ** -0.5),
        "od_w_in": nrm((N_ODD, D, OD_IN), D ** -0.5),
        "conv_dw_w": nrm((N_ODD, CONV_K, CONV_WIDTH), CONV_K ** -0.5),
        "conv_dw_b": nrm((N_ODD, CONV_WIDTH), 0.02),
        "conv_ln_w": gain((N_ODD, CONV_WIDTH)),
        "conv_ln_b": nrm((N_ODD, CONV_WIDTH), 0.02),
        "mla_q_a_norm": gain((N_ODD, MLA_Q_RANK)),
        "mla_w_uq": nrm((N_ODD, MLA_Q_RANK, MLA_HEADS * MLA_QK), MLA_Q_RANK ** -0.5),
        "mla_kv_a_norm": gain((N_ODD, MLA_KV_RANK)),
        "mla_w_ukv": nrm((N_ODD, MLA_KV_RANK, MLA_HEADS * (MLA_NOPE + MLA_V)), MLA_KV_RANK ** -0.5),
        "mla_q_norm": gain((N_ODD, MLA_QK)),
        "mla_k_norm": gain((N_ODD, MLA_QK)),
        "od_w_out": nrm((N_ODD, OD_MIX, D), OD_MIX ** -0.5),
        "moe_router_w": nrm((DEPTH, D, N_EXPERTS), D ** -0.5),
        "moe_router_b": nrm((DEPTH, N_EXPERTS), 0.01),
        "moe_w1": nrm((DEPTH, N_EXPERTS, D, 2 * D_FF), D ** -0.5),
        "moe_b1": nrm((DEPTH, N_EXPERTS, 2 * D_FF), 0.01),
        "moe_w2": nrm((DEPTH, N_EXPERTS, D_FF, D), D_FF ** -0.5),
        "moe_b2": nrm((DEPTH, N_EXPERTS, D), 0.01),
    }


def reference(x, c, ctx, c_ctx, ada_w, ada_b, ev_w_in, hgrn_lb_logits, hgrn_norm_w, pool_w,
              pool_scale, ev_w_out, od_w_in, conv_dw_w, conv_dw_b, conv_ln_w, conv_ln_b,
              mla_q_a_norm, mla_w_uq, mla_kv_a_norm, mla_w_ukv, mla_q_norm, mla_k_norm, od_w_out,
              moe_router_w, moe_router_b, moe_w1, moe_b1, moe_w2, moe_b2):
    B, L, D = x.shape
    rows = L // GRID_W
    cos, sin = axial_rope_tables(rows)
    lb_all = jnp.cumsum(jax.nn.softmax(hgrn_lb_logits.astype(jnp.float32), axis=0), axis=0)
    s_lat = jax.nn.silu(c)
    s_ctx = jax.nn.silu(c_ctx)
    h, hc = x, ctx
    for layer in range(DEPTH):
        need_ctx = layer < DEPTH - 1
        j = layer // 2
        mod = (s_lat @ ada_w[layer] + ada_b[layer])[:, None, :]
        modc = s_ctx @ ada_w[layer] + ada_b[layer]
        sh1, sc1, g1, sh2, sc2, g2 = jnp.split(mod, 6, axis=-1)
        csh1, csc1, cg1, csh2, csc2, cg2 = jnp.split(modc, 6, axis=-1)
        u = modulate(h, sh1, sc1)
        uc = modulate(hc, csh1, csc1)
        if layer % 2 == 0:
            y, yc = even_mixer(u, uc, ev_w_in[j], lb_all[layer, 0], lb_all[layer, 1], hgrn_norm_w[j],
                               pool_w[j], pool_scale[j], ev_w_out[j], need_ctx)
        else:
            y, yc = odd_mixer(u, uc, od_w_in[j], conv_dw_w[j], conv_dw_b[j], conv_ln_w[j], conv_ln_b[j],
                              mla_q_a_norm[j], mla_w_uq[j], mla_kv_a_norm[j], mla_w_ukv[j],
                              mla_q_norm[j], mla_k_norm[j], od_w_out[j], cos, sin, need_ctx)
        h = h + g1 * y
        moe_w = (moe_router_w[layer], moe_router_b[layer], moe_w1[layer], moe_b1[layer],
                 moe_w2[layer], moe_b2[layer])
        m = modulate(h, sh2, sc2).reshape(-1, D)
        if need_ctx:
            hc = hc + cg1 * yc
            mc = modulate(hc, csh2, csc2).reshape(-1, D)
            f = moe_ffn(jnp.concatenate([m, mc], axis=0), *moe_w)
            n_lat = m.shape[0]
            h = h + g2 * f[:n_lat].reshape(h.shape)
            hc = hc + cg2 * f[n_lat:].reshape(hc.shape)
        else:
            h = h + g2 * moe_ffn(m, *moe_w).reshape(h.shape)
    return h
```

```python
import numpy as np
import ml_dtypes
import concourse.bass as bass
import concourse.mybir as mybir
from concourse.bass_utils import run_bass_kernel_spmd

F32 = mybir.dt.float32
BF16 = mybir.dt.bfloat16
I32 = mybir.dt.int32
AF = mybir.ActivationFunctionType
ALU = mybir.AluOpType
AX = mybir.AxisListType

D = 1024
LAT = 2048
CTX = 256
SEQ = LAT + CTX
NT = SEQ // 128
EPS = 1e-6
NE = 32
CAP = 2048
NSLOT = NE * CAP


class Sched:
    EPOCH = 30000

    def __init__(self, nc):
        self.nc = nc
        self.E = {"pe": nc.tensor, "dve": nc.vector, "act": nc.scalar, "pool": nc.gpsimd, "sp": nc.sync}
        self.nsem = 0
        self.esem, self.ecnt = {}, {}
        self.pesems = set()
        for e in self.E:
            self._new_epoch(e)
        self.seen = {e: {} for e in self.E}
        self.W, self.R = {}, {}
        self.dpool = {q: [] for q in ("sp", "pool", "act")}
        self.dnext = {q: 0 for q in self.dpool}
        self.NPOOL = {"sp": 24, "pool": 24, "act": 4}
        self.ninst = 0
        self.psum = set()

    def _sem(self, name):
        self.nsem += 1
        return self.nc.semaphore(f"{name}{self.nsem}").__enter__()

    def _new_epoch(self, e):
        self.esem[e] = self._sem("e" + e)
        self.ecnt[e] = 0
        if e == "pe":
            self.pesems.add(self.esem[e])

    @staticmethod
    def _nm(x):
        if isinstance(x, str):
            return x
        t = getattr(x, "tensor", None)
        return t.name if t is not None else x.name

    @staticmethod
    def _key(x):
        if isinstance(x, tuple):
            return (Sched._nm(x[0]), x[1])
        return (Sched._nm(x), None)

    def _collect(self, table, key, evs):
        name, sub = key
        t = table.get(name)
        if not t:
            return
        subs = t.keys() if sub is None else [s for s in (sub, None) if s in t]
        for s in subs:
            for sem, val in t[s].items():
                if evs.get(sem, 0) < val:
                    evs[sem] = val

    def _deps(self, e, rk, wk):
        evs = {}
        for k in rk:
            self._collect(self.W, k, evs)
        for k in wk:
            self._collect(self.W, k, evs)
            self._collect(self.R, k, evs)
        for sem, val in evs.items():
            if e == "pe" and sem in self.pesems:
                continue
            if self.seen[e].get(sem, 0) >= val:
                continue
            self.E[e].wait_ge(sem, val)
            self.seen[e][sem] = val

    def _commit(self, ev, rk, wk):
        sem, val = ev
        for name, sub in rk:
            d = self.R.setdefault(name, {}).setdefault(sub, {})
            if d.get(sem, 0) < val:
                d[sem] = val
        for name, sub in wk:
            if sub is None:
                self.W[name] = {None: {sem: val}}
                self.R[name] = {}
            else:
                self.W.setdefault(name, {})[sub] = {sem: val}
                self.R.setdefault(name, {}).pop(sub, None)

    def op(self, e, fn, reads=(), writes=()):
        rk = [self._key(x) for x in reads]
        wk = [self._key(x) for x in writes]
        pr = [(k[0], None) for k in rk if k[0] in self.psum]
        if pr:
            rk = [k for k in rk if k[0] not in self.psum]
            wk = wk + pr
        wk = [((k[0], None) if k[0] in self.psum else k) for k in wk]
        self._deps(e, rk, wk)
        if self.ecnt[e] >= self.EPOCH:
            self._new_epoch(e)
        ins = fn()
        self.ecnt[e] += 1
        ins.then_inc(self.esem[e], 1)
        self._commit((self.esem[e], self.ecnt[e]), rk, wk)
        self.ninst += 1
        return ins

    def dma(self, q, out, in_, reads=None, writes=None, indirect=None, **kw):
        rk = [self._key(x) for x in (reads if reads is not None else [in_])]
        wk = [self._key(x) for x in (writes if writes is not None else [out])]
        self._deps(q, rk, wk)
        pool = self.dpool[q]
        if len(pool) < self.NPOOL[q]:
            pool.append([self._sem("d" + q), 0])
            slot = pool[-1]
        else:
            slot = pool[self.dnext[q] % len(pool)]
            self.dnext[q] += 1
            if slot[1] >= 60000:
                if self.seen[q].get(slot[0], 0) < slot[1]:
                    self.E[q].wait_ge(slot[0], slot[1])
                slot[0], slot[1] = self._sem("d" + q), 0
        sem, cnt = slot
        if cnt > 0 and self.seen[q].get(sem, 0) < cnt:
            self.E[q].wait_ge(sem, cnt)
            self.seen[q][sem] = cnt
        if indirect is None:
            ins = self.E[q].dma_start(out=out, in_=in_, **kw)
        else:
            ins = self.E[q].indirect_dma_start(out=out, in_=in_, **indirect)
        slot[1] = cnt + 16
        ins.then_inc(sem, 16)
        self._commit((sem, slot[1]), rk, wk)
        self.ninst += 1
        return ins

    def barrier(self):
        evs = {}
        for e in self.E:
            if self.ecnt[e] > 0:
                evs[self.esem[e]] = self.ecnt[e]
        for q, pool in self.dpool.items():
            for sem, cnt in pool:
                if cnt > 0:
                    evs[sem] = cnt
        for e in self.E:
            for sem, val in evs.items():
                if self.seen[e].get(sem, 0) >= val:
                    continue
                self.E[e].wait_ge(sem, val)
                self.seen[e][sem] = val
        self.W, self.R = {}, {}

    def finish(self):
        self.barrier()


def host_consts():
    c = {}
    c["ident_f"] = np.eye(128, dtype=np.float32)
    c["ident_b"] = np.eye(128).astype(ml_dtypes.bfloat16)
    s = np.arange(128)[:, None]
    t = np.arange(128)[None, :]
    c["mask_f"] = (s <= t).astype(np.float32)
    c["mask_b"] = (s >= t).astype(np.float32)
    c["tri_b"] = (s < t).astype(ml_dtypes.bfloat16)
    c["ones_b"] = np.ones((128, 128), ml_dtypes.bfloat16)
    c["ones_f"] = np.ones((128, 128), np.float32)
    facS = np.ones((4, 8), np.float32)
    facE = np.ones((4, 8), np.float32)
    for g, w in enumerate((2, 4, 8, 16)):
        half = w // 2
        for tt in range(min(half, 8)):
            facS[g, tt] = w / (tt + half)
        for j in range(8):
            i = 7 - j
            if i < half - 1:
                facE[g, j] = w / (half + 1 + i)
    c["facS"] = np.broadcast_to(facS[None], (128, 4, 8)).copy()
    c["facE"] = np.broadcast_to(facE[None], (128, 4, 8)).copy()
    rows = LAT // 64
    t_row = np.repeat(np.arange(rows, dtype=np.float32), 64)
    t_col = np.tile(np.arange(64, dtype=np.float32), rows)
    inv = (1.0 / (10000.0 ** (np.arange(8, dtype=np.float32) / 8))).astype(np.float32)
    ang = np.stack([t_row[:, None] * inv, t_col[:, None] * inv], axis=1).astype(np.float32)
    cos = np.cos(ang).reshape(LAT, 16).astype(np.float32)
    sin = np.sin(ang).reshape(LAT, 16).astype(np.float32)
    c["slotbase"] = np.broadcast_to((np.arange(32, dtype=np.float32) * CAP)[None], (128, 32)).copy()
    c["pidx"] = np.arange(128, dtype=np.float32).reshape(128, 1).copy()
    c["rope_cos"] = cos.reshape(16, 128, 16).transpose(1, 0, 2).copy()
    c["rope_sin"] = sin.reshape(16, 128, 16).transpose(1, 0, 2).copy()
    return c


CONST_SPECS = {
    "ident_f": ([128, 128], F32), "ident_b": ([128, 128], BF16), "mask_f": ([128, 128], F32),
    "mask_b": ([128, 128], F32), "tri_b": ([128, 128], BF16), "ones_b": ([128, 128], BF16),
    "ones_f": ([128, 128], F32), "facS": ([128, 4, 8], F32), "facE": ([128, 4, 8], F32),
    "slotbase": ([128, 32], F32), "pidx": ([128, 1], F32),
    "rope_cos": ([128, 16, 16], F32), "rope_sin": ([128, 16, 16], F32),
}

IN_SPECS = {
    "x": lambda ns: [ns, LAT, D], "c": lambda ns: [ns, D], "ctx": lambda ns: [ns, CTX, D], "c_ctx": lambda ns: [D],
    "ada_w": lambda ns: [2, D, 6 * D], "ada_b": lambda ns: [2, 6 * D], "ev_w_in": lambda ns: [1, D, 3072],
    "hgrn_lb_logits": lambda ns: [3, 2, 512], "hgrn_norm_w": lambda ns: [1, 128], "pool_w": lambda ns: [1, 4, 128, 128],
    "pool_scale": lambda ns: [1, 512], "ev_w_out": lambda ns: [1, D, D], "od_w_in": lambda ns: [1, D, 1696],
    "conv_dw_w": lambda ns: [1, 31, 512], "conv_dw_b": lambda ns: [1, 512], "conv_ln_w": lambda ns: [1, 512],
    "conv_ln_b": lambda ns: [1, 512], "mla_q_a_norm": lambda ns: [1, 384], "mla_w_uq": lambda ns: [1, 384, 768],
    "mla_kv_a_norm": lambda ns: [1, 256], "mla_w_ukv": lambda ns: [1, 256, 1024], "mla_q_norm": lambda ns: [1, 96],
    "mla_k_norm": lambda ns: [1, 96], "od_w_out": lambda ns: [1, D, D], "moe_router_w": lambda ns: [2, D, 32],
    "moe_router_b": lambda ns: [2, 32], "moe_w1": lambda ns: [2, 32, D, 2 * D], "moe_b1": lambda ns: [2, 32, 2 * D],
    "moe_w2": lambda ns: [2, 32, D, D], "moe_b2": lambda ns: [2, 32, D],
}


class StopBuild(Exception):
    pass


class K:
    def __init__(self, ns, stop_after=None, dbg=(), skip_inputs=()):
        self.ns = ns
        self.stop_after = stop_after
        self.dbg = set(dbg)
        nc = self.nc = bass.Bass("TRN2", target_bir_lowering=False)
        self.S = Sched(nc)
        self.I = {k: nc.dram_tensor(k, f(ns), F32, kind="ExternalInput").ap() for k, f in IN_SPECS.items() if k not in skip_inputs}
        self.C = {k: nc.dram_tensor("k_" + k, sh, dt, kind="ExternalInput").ap() for k, (sh, dt) in CONST_SPECS.items()}
        self.out = nc.dram_tensor("out", [ns, LAT, D], F32, kind="ExternalOutput").ap()
        self.T0 = ns * SEQ
        self.T1 = ns * LAT
        self.MOD = nc.dram_tensor("MOD", [10, 6 * D], F32).ap()
        self.H1 = nc.dram_tensor("H1", [self.T0, D], F32).ap()
        self.H2 = nc.dram_tensor("H2", [self.T0, D], F32).ap()
        self.M = nc.dram_tensor("M", [self.T0 + 128, D], BF16).ap()
        self.SLOT = nc.dram_tensor("SLOT", [NSLOT + 128, 2], I32).ap()
        self.YB = nc.dram_tensor("YB", [NSLOT + 128, D], BF16).ap()
        self.dbg_out = {}
        self._stack = []

    def sb(self, name, shape, dt):
        self._uid = getattr(self, "_uid", 0) + 1
        cm = self.nc.sbuf_tensor(f"{name}_{self._uid}", shape, dt)
        t = cm.__enter__()
        self._stack.append(cm)
        return t

    def ps(self, name, shape, dt):
        cm = self.nc.psum_tensor(name, shape, dt)
        t = cm.__enter__()
        self.S.psum.add(name)
        self._stack.append(cm)
        return t

    def mark(self):
        return len(self._stack)

    def release(self, mark):
        self.S.barrier()
        while len(self._stack) > mark:
            self._stack.pop().__exit__(None, None, None)

    def mm(self, out, lhsT, rhs, start=True, stop=True, reads=None, writes=None):
        nc = self.nc
        return self.S.op("pe", lambda: nc.tensor.matmul(out, lhsT, rhs, start=start, stop=stop),
                         reads=reads if reads is not None else [lhsT, rhs], writes=writes if writes is not None else [out])

    def tr(self, out, in_, ident, reads=None, writes=None):
        nc = self.nc
        return self.S.op("pe", lambda: nc.tensor.transpose(out, in_, ident),
                         reads=reads if reads is not None else [in_, ident], writes=writes if writes is not None else [out])

    def act(self, out, in_, func, bias=None, scale=None, accum_out=None, reads=None, writes=None):
        nc = self.nc
        kw = {}
        rd = [in_]
        wr = [out]
        if bias is not None:
            kw["bias"] = bias
            if not isinstance(bias, (int, float)):
                rd.append(bias)
        if scale is not None:
            kw["scale"] = scale
            if not isinstance(scale, (int, float)):
                rd.append(scale)
        if accum_out is not None:
            kw["accum_out"] = accum_out
            wr.append(accum_out)
        return self.S.op("act", lambda: nc.scalar.activation(out, in_, func, **kw),
                         reads=reads if reads is not None else rd, writes=writes if writes is not None else wr)

    def v(self, e, name, *args, reads, writes, **kw):
        eng = self.S.E[e]
        return self.S.op(e, lambda: getattr(eng, name)(*args, **kw), reads=reads, writes=writes)

    def tt(self, e, out, in0, in1, op, reads=None, writes=None):
        return self.v(e, "tensor_tensor", out, in0, in1, op, reads=reads if reads is not None else [in0, in1],
                      writes=writes if writes is not None else [out])

    def ts(self, e, out, in0, s1, s2, op0, op1=None, reads=None, writes=None, accum_out=None):
        rd = [in0] + [s for s in (s1, s2) if s is not None and not isinstance(s, (int, float))]
        kw = {}
        if op1 is not None:
            kw["op1"] = op1
        wr = [out]
        if accum_out is not None:
            kw["accum_out"] = accum_out
            wr.append(accum_out)
        return self.v(e, "tensor_scalar", out, in0, s1, s2, op0, reads=reads if reads is not None else rd,
                      writes=writes if writes is not None else wr, **kw)

    def stt(self, e, out, in0, scalar, in1, op0, op1, reads=None, writes=None):
        rd = [in0, in1] + ([] if isinstance(scalar, (int, float)) else [scalar])
        return self.v(e, "scalar_tensor_tensor", out, in0, scalar, in1, op0, op1,
                      reads=reads if reads is not None else rd, writes=writes if writes is not None else [out])

    def copy(self, e, out, in_, reads=None, writes=None):
        if e == "act":
            return self.act(out, in_, AF.Copy, reads=reads, writes=writes)
        return self.v(e, "tensor_copy", out, in_, reads=reads if reads is not None else [in_],
                      writes=writes if writes is not None else [out])

    def memset(self, e, ap, val):
        return self.v(e, "memset", ap, val, reads=[], writes=[ap])

    def dma(self, q, out, in_, **kw):
        return self.S.dma(q, out, in_, **kw)

    def rstd_from_ss(self, rstd, ss, n):
        self.ts("dve", rstd, ss, 1.0 / n, EPS, ALU.mult, ALU.add)
        self.act(rstd, rstd, AF.Sqrt)
        self.v("dve", "reciprocal", rstd, rstd, reads=[rstd], writes=[rstd])

    def build(self):
        nc, S, I, C = self.nc, self.S, self.I, self.C
        ns = self.ns
        self.cst = {}
        for k in ("ident_f", "ident_b", "mask_f", "mask_b", "tri_b", "ones_b", "ones_f", "slotbase", "pidx"):
            sh, dt = CONST_SPECS[k]
            t = self.sb("c_" + k, sh, dt)
            self.dma("sp", t[:], C[k][:, :])
            self.cst[k] = t
        self.PS = [self.ps(f"ps{i}", [128, 512], F32) for i in range(6)]
        self.PB = [self.ps(f"pb{i}", [128, 1024], BF16) for i in range(2)]
        self.small = self.sb("small", [128, 64], F32)

        self.phase_adaln()
        if self.stop_after == "adaln":
            return self.end()
        mkr = self.mark()
        self.route_init(0)
        self.layer0()
        if self.stop_after == "mixer0":
            return self.end()
        if self.stop_after and self.stop_after.startswith("l1_"):
            self.S.barrier()
            self.dma("sp", self.H2, self.H1)
        else:
            self.experts(0)
            self.combine(0)
        self.release(mkr)
        if self.stop_after == "layer0":
            return self.end()
        mkr = self.mark()
        self.route_init(1)
        try:
            self.layer1()
        except StopBuild:
            return self.end()
        self.experts(1)
        self.combine(1)
        self.release(mkr)
        return self.end()

    def end(self):
        for name in ("MOD", "H1", "H2", "M"):
            if name in self.dbg:
                src = getattr(self, name)
                d = self.dbg_dump(name, list(src.shape), src.dtype)
                self.S.barrier()
                self.dma("sp", d, src)
        self.S.finish()
        return self.nc

    def dbg_dump(self, name, shape, dt=F32):
        t = self.nc.dram_tensor("dbg_" + name, shape, dt, kind="ExternalOutput").ap()
        self.dbg_out[name] = t
        return t

    def phase_adaln(self):
        nc, S, I, C = self.nc, self.S, self.I, self.C
        ns = self.ns
        mk = self.mark()
        cs = self.sb("cs", [128, 8, 8], F32)
        s5 = self.sb("s5", [128, 8, 8], F32)
        self.memset("dve", cs[:], 0.0)
        for b in range(ns):
            self.dma("sp", cs[:, :, b], I["c"][b].rearrange("(kc p) -> p kc", p=128), allow_slow_non_contiguous=True)
        self.dma("sp", cs[:, :, ns], I["c_ctx"].rearrange("(kc p) -> p kc", p=128), allow_slow_non_contiguous=True)
        self.act(s5[:], cs[:], AF.Silu)
        nr = ns + 1
        brow = self.sb("brow", [1, 6 * D], F32)
        wa = [self.sb(f"wa{i}", [128, 8, 512], F32) for i in range(2)]
        mo = [self.sb(f"mo{i}", [8, 512], F32) for i in range(2)]
        onesf = self.cst["ones_f"]
        it = 0
        for layer in range(2):
            self.dma("sp", brow[:], I["ada_b"][layer:layer + 1, :])
            for nb in range(12):
                w = wa[it % 2]
                self.dma("sp" if it % 2 == 0 else "pool", w[:],
                         I["ada_w"][layer][:, nb * 512:(nb + 1) * 512].rearrange("(kc p) n -> p kc n", p=128))
                ps = self.PS[it % 2]
                for kc in range(8):
                    self.mm(ps[0:nr, :], s5[:, kc, 0:nr], w[:, kc, :], start=(kc == 0), stop=False)
                self.mm(ps[0:nr, :], onesf[0:1, 0:nr], brow[0:1, nb * 512:(nb + 1) * 512], start=False, stop=True)
                m = mo[it % 2]
                self.copy("dve", m[0:nr, :], ps[0:nr, :])
                self.dma("sp", self.MOD[layer * 5:layer * 5 + ns, nb * 512:(nb + 1) * 512], m[0:ns, :],
                         writes=[(self.MOD, (layer, nb, 0))])
                self.dma("sp", self.MOD[layer * 5 + 4:layer * 5 + 5, nb * 512:(nb + 1) * 512], m[ns:ns + 1, :],
                         writes=[(self.MOD, (layer, nb, 1))])
                it += 1
        self.release(mk)

    def modrow(self, dst, layer, row, q):
        src = self.MOD[layer * 5 + row:layer * 5 + row + 1, q * D:(q + 1) * D].partition_broadcast(128)
        self.dma("sp", dst, src, reads=[self.MOD])

    def modulate_tile(self, u, ht, sc1p, sh, junk, ss, rstd):
        self.act(junk, ht, AF.Square, accum_out=ss)
        self.rstd_from_ss(rstd, ss, D)
        self.stt("dve", u, ht, rstd, sc1p, ALU.mult, ALU.mult)
        self.tt("pool", u, u, sh, ALU.add)

    def transpose_tile_to(self, dstT, col0, src, nkc, ident_f, psA, psB, dt_note=None):
        for kc in range(nkc):
            ps = psA if kc < 4 else psB
            self.tr(ps[:, (kc % 4) * 128:(kc % 4 + 1) * 128], src[:, kc * 128:(kc + 1) * 128], ident_f[:])
        n0 = min(4, nkc)
        self.copy("act", dstT[:, 0:n0, col0:col0 + 128], psA[:, 0:n0 * 128].rearrange("p (k c) -> p k c", c=128))
        if nkc > 4:
            self.copy("dve", dstT[:, 4:nkc, col0:col0 + 128], psB[:, 0:(nkc - 4) * 128].rearrange("p (k c) -> p k c", c=128))

    def layer0(self):
        nc, S, I, C = self.nc, self.S, self.I, self.C
        ns = self.ns
        cst = self.cst
        mk0 = self.mark()
        lg = self.sb("lg", [128, 3, 8], F32)
        lb = self.sb("lb", [128, 8], F32)
        oml = self.sb("oml", [128, 8], F32)
        for l in range(3):
            self.dma("sp", lg[:, l, :].rearrange("p (d h) -> p d h", d=2),
                     I["hgrn_lb_logits"][l].rearrange("d (h p) -> p d h", p=128), allow_slow_non_contiguous=True)
        self.act(lg[:], lg[:], AF.Exp)
        self.tt("dve", lb[:], lg[:, 0, :], lg[:, 1, :], ALU.add)
        self.tt("dve", lb[:], lb[:], lg[:, 2, :], ALU.add)
        self.v("dve", "reciprocal", lb[:], lb[:], reads=[lb], writes=[lb])
        self.tt("dve", lb[:], lb[:], lg[:, 0, :], ALU.mult)
        self.ts("dve", oml[:], lb[:], -1.0, 1.0, ALU.mult, ALU.add)
        nwb = self.sb("nwb", [128, 128], F32)
        self.dma("sp", nwb[:], I["hgrn_norm_w"][0:1, :].partition_broadcast(128))
        pscale = self.sb("pscale", [128, 4], F32)
        self.dma("sp", pscale[:], I["pool_scale"][0].rearrange("(g p) -> p g", p=128), allow_slow_non_contiguous=True)
        poolw = self.sb("poolw", [128, 4, 128], BF16)
        for g in range(4):
            self.dma("pool", poolw[:, g, :], I["pool_w"][0, g])
        facS = self.sb("facS", [128, 4, 8], F32)
        facE = self.sb("facE", [128, 4, 8], F32)
        self.dma("sp", facS[:], C["facS"])
        self.dma("sp", facE[:], C["facE"])
        wr = self.sb("wr", [128, 8, 32], F32)
        self.dma("sp", wr[:], I["moe_router_w"][0].rearrange("(kc p) e -> p kc e", p=128))
        brr = self.sb("brr", [1, 32], F32)
        self.dma("sp", brr[:], I["moe_router_b"][0:1, :])
        uT = self.sb("uT", [128, 8, SEQ], BF16)
        mixT = self.sb("mixT", [128, 8, SEQ], BF16)
        w_in = I["ev_w_in"][0]
        for b in range(ns):
            mk = self.mark()
            mr = self.sb("mr", [128, 4, D], F32)
            self.modrow(mr[:, 0, :], 0, b, 1)
            self.modrow(mr[:, 1, :], 0, b, 0)
            self.modrow(mr[:, 2, :], 0, 4, 1)
            self.modrow(mr[:, 3, :], 0, 4, 0)
            self.ts("dve", mr[:, 0, :], mr[:, 0, :], 1.0, None, ALU.add)
            self.ts("dve", mr[:, 2, :], mr[:, 2, :], 1.0, None, ALU.add)
            hts = [self.sb(f"ht{i}", [128, D], F32) for i in range(2)]
            us = [self.sb(f"u{i}", [128, D], F32) for i in range(2)]
            junk = self.sb("junk", [128, D], F32)
            for i in range(NT):
                ht, u = hts[i % 2], us[i % 2]
                src = I["ctx"][b, i * 128:(i + 1) * 128, :] if i < 2 else I["x"][b, (i - 2) * 128:(i - 1) * 128, :]
                self.dma("sp", ht[:], src)
                o = 2 if i < 2 else 0
                self.modulate_tile(u[:], ht[:], mr[:, o, :], mr[:, o + 1, :], junk[:], self.small[:, 0:1], self.small[:, 1:2])
                self.transpose_tile_to(uT, i * 128, u, 8, cst["ident_f"], self.PS[0], self.PS[1])
            if "uT" in self.dbg and b == 0:
                d = self.dbg_dump("uT", [128, 8, SEQ], BF16)
                self.dma("sp", d[:, :, :], uT[:])
            self.release(mk)
            mk = self.mark()
            W2 = SEQ + 64
            q32 = self.sb("q32", [128, W2], F32)
            sg = self.sb("sg", [128, W2], F32)
            kin = self.sb("kin", [128, W2], F32)
            A = self.sb("A", [128, W2], F32)
            oacc = self.sb("oacc", [128, NT, 128], F32)
            v16 = self.sb("v16", [128, NT, 128], BF16)
            whs = [self.sb(f"wh{i}", [128, 8, 640], BF16) for i in range(2)]
            S32 = self.sb("S32", [128, 128], F32)
            S16 = self.sb("S16", [128, 128], BF16)
            bt = self.sb("bt", [128, 8], F32)
            Pk = [self.sb(f"Pk{i}", [128, 128], F32) for i in range(5)]
            kiA = [self.sb(f"kiA{i}", [128, 128], BF16) for i in range(2)]
            kiB = [self.sb(f"kiB{i}", [128, 128], BF16) for i in range(2)]
            kiC = [self.sb(f"kiC{i}", [128, 128], BF16) for i in range(2)]
            qdx = [self.sb(f"qdx{i}", [128, 64], BF16) for i in range(2)]
            qd = [self.sb(f"qd{i}", [128, 128], BF16) for i in range(2)]
            ki = [self.sb(f"ki{i}", [128, 128], BF16) for i in range(2)]
            qdS = [self.sb(f"qdS{i}", [128, 128], BF16) for i in range(2)]
            ke = [self.sb(f"ke{i}", [128, 128], BF16) for i in range(2)]
            att16 = [self.sb(f"att{i}", [128, 128], BF16) for i in range(2)]
            ket16 = [self.sb(f"ket{i}", [128, 128], BF16) for i in range(2)]
            ogs = self.sb("ogs", [128, 128], F32)
            rr = self.sb("rr", [128, 128], F32)
            zero1 = self.sb("zero1", [128, 1], F32)
            self.memset("dve", zero1[:], 0.0)
            groups = [(g * 512, min(512, SEQ - g * 512)) for g in range((SEQ + 511) // 512)]
            for h in range(4):
                wh = whs[h % 2]
                for j, base in enumerate((0, 512, 1024, 1536, 2048)):
                    c0 = base + h * 128
                    self.dma("pool", wh[:, :, j * 128:(j + 1) * 128],
                             w_in[:, c0:c0 + 128].rearrange("(kc p) n -> p kc n", p=128))
                for gi, (g0, gn) in enumerate(groups):
                    ps = self.PS[gi % 2]
                    for kc in range(8):
                        self.mm(ps[:, 0:gn], wh[:, kc, 0:128], uT[:, kc, g0:g0 + gn], start=(kc == 0), stop=(kc == 7))
                    self.act(q32[:, g0:g0 + gn], ps[:, 0:gn], AF.Silu)
                for i in range(NT):
                    ps = self.PS[2 + i % 2]
                    for kc in range(8):
                        self.mm(ps[:, 0:128], uT[:, kc, i * 128:(i + 1) * 128], wh[:, kc, 384:512], start=(kc == 0), stop=(kc == 7))
                    self.copy("dve", v16[:, i, :], ps[:, 0:128])
                for di in range(2):
                    for gi, (g0, gn) in enumerate(groups):
                        ps = self.PS[gi % 2]
                        for kc in range(8):
                            self.mm(ps[:, 0:gn], wh[:, kc, (1 + di) * 128:(2 + di) * 128], uT[:, kc, g0:g0 + gn],
                                    start=(kc == 0), stop=(kc == 7))
                        self.act(sg[:, g0:g0 + gn], ps[:, 0:gn], AF.Sigmoid)
                    lbc = lb[:, di * 4 + h:di * 4 + h + 1]
                    omc = oml[:, di * 4 + h:di * 4 + h + 1]
                    self.ts("dve", sg[:, 0:SEQ], sg[:, 0:SEQ], omc, lbc, ALU.mult, ALU.add)
                    self.ts("pool", kin[:, 0:SEQ], sg[:, 0:SEQ], -1.0, 1.0, ALU.mult, ALU.add)
                    self.act(sg[:, 0:SEQ], sg[:, 0:SEQ], AF.Ln)
                    self.S.op("dve", lambda: nc.vector.tensor_tensor_scan(A[:, 0:SEQ], sg[:, 0:SEQ], sg[:, 0:SEQ], 0.0, ALU.add, ALU.add),
                              reads=[sg], writes=[A])
                    if di == 1:
                        self.stt("dve", sg[:, 0:SEQ], sg[:, 0:SEQ], -2.0, A[:, 0:SEQ], ALU.mult, ALU.add)
                    AA = A if di == 0 else sg
                    order = list(range(NT)) if di == 0 else [1, 0] + list(range(NT - 1, 1, -1))
                    self.memset("dve", S32[:], 0.0)
                    self.memset("pool", S16[:], 0.0)
                    for t_ in kiA + kiB + kiC:
                        self.memset("pool", t_[:], 0.0)
                    mask = cst["mask_f"] if di == 0 else cst["mask_b"]
                    for n, i in enumerate(order):
                        c0, c1 = i * 128, (i + 1) * 128
                        E0 = A[:, c0 - 1:c0] if c0 > 0 else zero1[:, 0:1]
                        E1 = A[:, c1 - 1:c1]
                        R0 = AA[:, c0 + 31:c0 + 32]
                        R1 = AA[:, c0 + 95:c0 + 96]
                        Rx = AA[:, c0 + 63:c0 + 64] if di == 0 else AA[:, c0 + 64:c0 + 65]
                        for bc, (src_, sgn) in enumerate(((R0, -0.5), (R0, 0.5), (R1, -0.5), (R1, 0.5), (Rx, -0.5), (Rx, 0.5),
                                                         (E0, -0.5), (E1, 0.5))):
                            self.ts("dve" if bc % 2 == 0 else "pool", bt[:, bc:bc + 1], src_, sgn, None, ALU.mult)
                        lo, hi = slice(0, 64), slice(64, 128)
                        Alo, Ahi, Acol = AA[:, c0:c0 + 64], AA[:, c0 + 64:c1], AA[:, c0:c1]
                        p = n % 2
                        if di == 0:
                            self.act(Pk[0][:, lo], Alo, AF.Exp, bias=bt[:, 0:1], scale=0.5)
                            self.act(Pk[0][:, hi], Ahi, AF.Exp, bias=bt[:, 2:3], scale=0.5)
                            self.act(Pk[1][:, lo], Alo, AF.Exp, bias=bt[:, 1:2], scale=-0.5)
                            self.act(Pk[1][:, hi], Ahi, AF.Exp, bias=bt[:, 3:4], scale=-0.5)
                            self.act(Pk[2][:], Acol, AF.Exp, bias=bt[:, 6:7], scale=0.5)
                            self.act(Pk[3][:], Acol, AF.Exp, bias=bt[:, 7:8], scale=-0.5)
                            self.act(Pk[4][:, lo], Ahi, AF.Exp, bias=bt[:, 4:5], scale=0.5)
                            self.act(Pk[4][:, hi], Alo, AF.Exp, bias=bt[:, 5:6], scale=-0.5)
                            qx_cols, kx_cols = hi, lo
                        else:
                            self.act(Pk[0][:, lo], Alo, AF.Exp, bias=bt[:, 1:2], scale=-0.5)
                            self.act(Pk[0][:, hi], Ahi, AF.Exp, bias=bt[:, 3:4], scale=-0.5)
                            self.act(Pk[1][:, lo], Alo, AF.Exp, bias=bt[:, 0:1], scale=0.5)
                            self.act(Pk[1][:, hi], Ahi, AF.Exp, bias=bt[:, 2:3], scale=0.5)
                            self.act(Pk[2][:], Acol, AF.Exp, bias=bt[:, 7:8], scale=-0.5)
                            self.act(Pk[3][:], Acol, AF.Exp, bias=bt[:, 6:7], scale=0.5)
                            self.act(Pk[4][:, lo], Alo, AF.Exp, bias=bt[:, 5:6], scale=-0.5)
                            self.act(Pk[4][:, hi], Ahi, AF.Exp, bias=bt[:, 4:5], scale=0.5)
                            qx_cols, kx_cols = lo, hi
                        qcs = slice(c0 + qx_cols.start, c0 + qx_cols.stop)
                        kcs = slice(c0 + kx_cols.start, c0 + kx_cols.stop)
                        self.tt("dve", qd[p][:], q32[:, c0:c1], Pk[0][:], ALU.mult)
                        self.tt("pool", kiA[p][:, lo], kin[:, c0:c0 + 64], Pk[1][:, lo], ALU.mult)
                        self.tt("pool", kiB[p][:, hi], kin[:, c0 + 64:c1], Pk[1][:, hi], ALU.mult)
                        self.tt("dve", qdS[p][:], q32[:, c0:c1], Pk[2][:], ALU.mult)
                        self.tt("pool", ke[p][:], kin[:, c0:c1], Pk[3][:], ALU.mult)
                        self.tt("dve", qdx[p][:], q32[:, qcs], Pk[4][:, lo], ALU.mult)
                        self.tt("pool", kiC[p][:, kx_cols], kin[:, kcs], Pk[4][:, hi], ALU.mult)
                        dec = Pk[2][:, 127:128] if di == 0 else Pk[2][:, 0:1]
                        pa, po, pS = self.PS[0 + n % 2], self.PS[2 + n % 2], self.PS[4 + n % 2]
                        pq = 0
                        if di == 0:
                            self.mm(pa[:, 0:64], kiA[p][:], qd[p][:, lo], start=True, stop=True)
                            self.mm(pa[:, 64:128], kiB[p][:], qd[p][:, hi], start=True, stop=False)
                            self.mm(pa[:, 64:128], kiC[p][:], qdx[p][:], start=False, stop=True)
                        else:
                            self.mm(pa[:, 0:64], kiA[p][:], qd[p][:, lo], start=True, stop=False)
                            self.mm(pa[:, 0:64], kiC[p][:], qdx[p][:], start=False, stop=True)
                            self.mm(pa[:, 64:128], kiB[p][:], qd[p][:, hi], start=True, stop=True)
                        self.tt("dve", att16[p][:], pa[:, pq:pq + 128], mask[:], ALU.mult)
                        pbt = self.PB[n % 2]
                        self.tr(pbt[:, pq:pq + 128], ke[p][:], cst["ident_b"][:])
                        self.copy("act", ket16[p][:], pbt[:, pq:pq + 128])
                        self.mm(po[:, pq:pq + 128], att16[p][:], v16[:, i, :], start=True, stop=False)
                        self.mm(po[:, pq:pq + 128], qdS[p][:], S16[:], start=False, stop=True)
                        if di == 0:
                            self.copy("act", oacc[:, i, :], po[:, pq:pq + 128])
                        else:
                            self.tt("dve", oacc[:, i, :], oacc[:, i, :], po[:, pq:pq + 128], ALU.add)
                        self.mm(pS[:, pq:pq + 128], ket16[p][:], v16[:, i, :])
                        self.stt("dve", S32[:], S32[:], dec, pS[:, pq:pq + 128], ALU.mult, ALU.add)
                        self.copy("act", S16[:], S32[:])
                for i in range(NT):
                    ps = self.PS[3 + i % 2]
                    for kc in range(8):
                        self.mm(ps[:, 0:128], uT[:, kc, i * 128:(i + 1) * 128], wh[:, kc, 512:640], start=(kc == 0), stop=(kc == 7))
                    self.act(ogs[:], ps[:, 0:128], AF.Silu)
                    self.act(rr[:], oacc[:, i, :], AF.Square, accum_out=self.small[:, 2:3])
                    self.rstd_from_ss(self.small[:, 3:4], self.small[:, 2:3], 128)
                    self.stt("dve", rr[:], oacc[:, i, :], self.small[:, 3:4], nwb[:], ALU.mult, ALU.mult)
                    self.tt("dve", rr[:], rr[:], ogs[:], ALU.mult)
                    pt = self.PS[5]
                    self.tr(pt[:, (i % 4) * 128:(i % 4 + 1) * 128], rr[:], cst["ident_f"][:])
                    self.copy("act", mixT[:, h, i * 128:(i + 1) * 128], pt[:, (i % 4) * 128:(i % 4 + 1) * 128])
            wp = whs[0]
            self.dma("pool", wp[:, :, 0:512], w_in[:, 2560:3072].rearrange("(kc p) n -> p kc n", p=128))
            OFFC, OFFL = 16, 16 + CTX + 16
            WB = OFFL + LAT + 16
            d16 = self.sb("d16", [128, SEQ], BF16)
            for g in range(4):
                w = 2 << g
                half = w // 2
                PBf, T1, T2 = q32, kin, A
                self.memset("pool", PBf[:, 0:WB], 0.0)
                for gi, (g0, gn) in enumerate(groups):
                    ps = self.PS[gi % 2]
                    for kc in range(8):
                        self.mm(ps[:, 0:gn], wp[:, kc, g * 128:(g + 1) * 128], uT[:, kc, g0:g0 + gn], start=(kc == 0), stop=(kc == 7))
                    a0, a1 = g0, g0 + gn
                    if a0 < CTX:
                        n_c = min(a1, CTX) - a0
                        self.copy("act", PBf[:, OFFC + a0:OFFC + a0 + n_c], ps[:, 0:n_c])
                        if a1 > CTX:
                            self.copy("act", PBf[:, OFFL:OFFL + (a1 - CTX)], ps[:, n_c:gn])
                    else:
                        self.copy("act", PBf[:, OFFL + a0 - CTX:OFFL + a1 - CTX], ps[:, 0:gn])
                cur = PBf
                step = 1
                tmp = [T1, T2]
                ti = 0
                width = WB
                while step < w:
                    nxt = tmp[ti % 2]
                    ti += 1
                    width2 = width - step
                    self.tt("dve", nxt[:, 0:width2], cur[:, 0:width2], cur[:, step:step + width2], ALU.add)
                    cur, width, step = nxt, width2, step * 2
                dst = tmp[ti % 2]
                for off, L, tcol in ((OFFC, CTX, 0), (OFFL, LAT, CTX)):
                    sw = cur[:, off - half:off - half + L]
                    self.tt("pool", sw[:, 0:8], sw[:, 0:8], facS[:, g, :], ALU.mult, reads=[cur, facS], writes=[cur])
                    self.tt("pool", sw[:, L - 8:L], sw[:, L - 8:L], facE[:, g, :], ALU.mult, reads=[cur, facE], writes=[cur])
                    self.stt("dve", d16[:, tcol:tcol + L], sw, 1.0 / w, PBf[:, off:off + L], ALU.mult, ALU.subtract,
                             reads=[cur, PBf], writes=[d16])
                for gi, (g0, gn) in enumerate(groups):
                    ps = self.PS[2 + gi % 2]
                    self.mm(ps[:, 0:gn], poolw[:, g, :], d16[:, g0:g0 + gn])
                    self.ts("dve", mixT[:, 4 + g, g0:g0 + gn], ps[:, 0:gn], pscale[:, g:g + 1], None, ALU.mult)
            if "mixT" in self.dbg and b == 0:
                d = self.dbg_dump("mixT", [128, 8, SEQ], BF16)
                self.dma("sp", d[:, :, :], mixT[:])
            self.release(mk)
            self.post_mixer(0, b, mixT, NT, wr, brr)
        self.release(mk0)

    def rms_rows(self, out, src, n, gain_b, sq_junk, c0):
        sm = self.small
        self.act(sq_junk, src, AF.Square, accum_out=sm[:, c0:c0 + 1])
        self.rstd_from_ss(sm[:, c0 + 1:c0 + 2], sm[:, c0:c0 + 1], n)
        self.stt("dve", out, src, sm[:, c0 + 1:c0 + 2], gain_b, ALU.mult, ALU.mult)

    def head_norm(self, xf, gain_b, tmp, hs, rs):
        self.tt("dve", tmp[:], xf[:], xf[:], ALU.mult)
        self.v("dve", "reduce_sum", hs[:], tmp[:], AX.X, reads=[tmp], writes=[hs])
        self.ts("dve", rs[:], hs[:], 1.0 / 96, EPS, ALU.mult, ALU.add)
        self.act(rs[:], rs[:], AF.Sqrt)
        self.v("dve", "reciprocal", rs[:], rs[:], reads=[rs], writes=[rs])
        for h in range(8):
            self.stt("dve", xf[:, h, :], xf[:, h, :], rs[:, h:h + 1], gain_b[:], ALU.mult, ALU.mult,
                     reads=[xf, rs, gain_b], writes=[xf])

    def rope(self, dst, src, cosr, sinr, t1, t2, eng):
        s4 = src.rearrange("p (a h i) -> p a h i", a=2, h=2)
        d4 = dst.rearrange("p (a h i) -> p a h i", a=2, h=2)
        c3 = cosr.rearrange("p (a i) -> p a i", a=2)
        s3 = sinr.rearrange("p (a i) -> p a i", a=2)
        a3 = t1.rearrange("p (a i) -> p a i", a=2)
        b3 = t2.rearrange("p (a i) -> p a i", a=2)
        x1, x2 = s4[:, :, 0, :], s4[:, :, 1, :]
        self.tt(eng, a3, x1, c3, ALU.mult, reads=[src, cosr], writes=[t1])
        self.tt(eng, b3, x2, s3, ALU.mult, reads=[src, sinr], writes=[t2])
        self.tt(eng, d4[:, :, 0, :], a3, b3, ALU.subtract, reads=[t1, t2], writes=[dst])
        self.tt(eng, a3, x2, c3, ALU.mult, reads=[src, cosr], writes=[t1])
        self.tt(eng, b3, x1, s3, ALU.mult, reads=[src, sinr], writes=[t2])
        self.tt(eng, d4[:, :, 1, :], a3, b3, ALU.add, reads=[t1, t2], writes=[dst])

    def layer1(self):
        nc, S, I, C = self.nc, self.S, self.I, self.C
        ns, cst = self.ns, self.cst
        mk0 = self.mark()
        w_in = I["od_w_in"][0]
        NL = LAT // 128
        wr = self.sb("wr1", [128, 8, 32], F32)
        self.dma("sp", wr[:], I["moe_router_w"][1].rearrange("(kc p) e -> p kc e", p=128))
        brr = self.sb("brr1", [1, 32], F32)
        self.dma("sp", brr[:], I["moe_router_b"][1:2, :])
        rcos = self.sb("rcos", [128, 16, 16], F32)
        rsin = self.sb("rsin", [128, 16, 16], F32)
        self.dma("sp", rcos[:], C["rope_cos"])
        self.dma("sp", rsin[:], C["rope_sin"])
        dww = self.sb("dww", [128, 4, 31], F32)
        dwr = self.sb("dwr", [32, 512], F32)
        self.dma("sp", dwr[0:31, :], I["conv_dw_w"][0])
        for cc in range(4):
            self.tr(self.PS[0][:, cc * 32:cc * 32 + 31], dwr[0:31, cc * 128:(cc + 1) * 128], self.cst["ident_f"][0:31, 0:31])
            self.copy("dve", dww[:, cc, :], self.PS[0][:, cc * 32:cc * 32 + 31])
        cpar = self.sb("cpar", [128, 3, 4], F32)
        for j, nm in enumerate(("conv_dw_b", "conv_ln_w", "conv_ln_b")):
            self.dma("sp", cpar[:, j, :], I[nm][0].rearrange("(c p) -> p c", p=128), allow_slow_non_contiguous=True)
        qan = self.sb("qan", [128, 384], F32)
        kvan = self.sb("kvan", [128, 256], F32)
        qnb = self.sb("qnb", [128, 96], F32)
        knb = self.sb("knb", [128, 96], F32)
        self.dma("sp", qan[:], I["mla_q_a_norm"][0:1, :].partition_broadcast(128))
        self.dma("sp", kvan[:], I["mla_kv_a_norm"][0:1, :].partition_broadcast(128))
        self.dma("sp", qnb[:], I["mla_q_norm"][0:1, :].partition_broadcast(128))
        self.dma("sp", knb[:], I["mla_k_norm"][0:1, :].partition_broadcast(128))
        wq = self.sb("wq", [128, 8, 384], BF16)
        wkv = self.sb("wkv", [128, 8, 288], BF16)
        wuq = self.sb("wuq", [128, 3, 768], BF16)
        wukv = self.sb("wukv", [128, 2, 1024], BF16)
        self.dma("pool", wq[:], w_in[:, 1024:1408].rearrange("(kc p) n -> p kc n", p=128))
        self.dma("pool", wkv[:], w_in[:, 1408:1696].rearrange("(kc p) n -> p kc n", p=128))
        self.dma("pool", wuq[:], I["mla_w_uq"][0].rearrange("(kc p) n -> p kc n", p=128))
        self.dma("pool", wukv[:], I["mla_w_ukv"][0].rearrange("(kc p) n -> p kc n", p=128))
        mixA = self.sb("mixA", [128, 4, LAT], BF16)
        sm = self.small
        SC = 96 ** -0.5
        for b in range(ns):
            mkb = self.mark()

            def make_uT(per_tile=None):
                uT = self.sb("uT1", [128, 8, SEQ], BF16) if per_tile is None else None
                mk = self.mark()
                uTts = [self.sb(f"uTt{i}", [128, 8, 128], BF16) for i in range(2)] if per_tile is not None else None
                mr = self.sb("mr", [128, 4, D], F32)
                self.modrow(mr[:, 0, :], 1, b, 1)
                self.modrow(mr[:, 1, :], 1, b, 0)
                self.modrow(mr[:, 2, :], 1, 4, 1)
                self.modrow(mr[:, 3, :], 1, 4, 0)
                self.ts("dve", mr[:, 0, :], mr[:, 0, :], 1.0, None, ALU.add)
                self.ts("dve", mr[:, 2, :], mr[:, 2, :], 1.0, None, ALU.add)
                hts = [self.sb(f"ht{i}", [128, D], F32) for i in range(2)]
                us = [self.sb(f"u{i}", [128, D], F32) for i in range(1 if per_tile is not None else 2)]
                for i in range(NT):
                    ht, u = hts[i % 2], us[i % len(us)]
                    self.dma("sp", ht[:], self.H2[b * SEQ + i * 128:b * SEQ + (i + 1) * 128, :], reads=[self.H2])
                    o = 2 if i < 2 else 0
                    self.modulate_tile(u[:], ht[:], mr[:, o, :], mr[:, o + 1, :], u[:], sm[:, 0:1], sm[:, 1:2])
                    if per_tile is None:
                        self.transpose_tile_to(uT, i * 128, u, 8, cst["ident_f"], self.PS[0], self.PS[1])
                    else:
                        self.transpose_tile_to(uTts[i % 2], 0, u, 8, cst["ident_f"], self.PS[0], self.PS[1])
                        per_tile(i, uTts[i % 2])
                self.release(mk)
                return uT

            mkA = self.mark()
            uT = make_uT()
            mk = self.mark()
            wc = self.sb("wc", [128, 8, 1024], BF16)
            self.dma("pool", wc[:], w_in[:, 0:1024].rearrange("(kc p) n -> p kc n", p=128))
            hb = self.sb("hb", [128, LAT + 32], F32)
            cv = [self.sb(f"cv{i}", [128, LAT], F32) for i in range(4)]
            sgt = [self.sb(f"sgt{i}", [128, 512], F32) for i in range(2)]
            self.memset("pool", hb[:], 0.0)
            for cc in range(4):
                for tg in range(4):
                    pv, pg = self.PS[(tg % 2) * 2], self.PS[(tg % 2) * 2 + 1]
                    cols = slice(CTX + tg * 512, CTX + (tg + 1) * 512)
                    for kc in range(8):
                        self.mm(pv[:], wc[:, kc, cc * 128:(cc + 1) * 128], uT[:, kc, cols], start=(kc == 0), stop=(kc == 7))
                    for kc in range(8):
                        self.mm(pg[:], wc[:, kc, 512 + cc * 128:512 + (cc + 1) * 128], uT[:, kc, cols], start=(kc == 0), stop=(kc == 7))
                    st = sgt[tg % 2]
                    self.act(st[:], pg[:], AF.Sigmoid)
                    self.tt("dve", hb[:, 15 + tg * 512:15 + (tg + 1) * 512], pv[:], st[:], ALU.mult)
                for hf, eng in ((0, "dve"), (1, "dve")):
                    o0 = hf * 1024
                    acc = cv[cc][:, o0:o0 + 1024]
                    self.ts(eng, acc, hb[:, o0:o0 + 1024], dww[:, cc, 0:1], cpar[:, 0, cc:cc + 1], ALU.mult, ALU.add,
                            reads=[hb, dww, cpar], writes=[(cv[cc], hf)])
                    for j in range(1, 31):
                        self.stt(eng, acc, hb[:, o0 + j:o0 + j + 1024], dww[:, cc, j:j + 1], acc, ALU.mult, ALU.add,
                                 reads=[hb, dww, (cv[cc], hf)], writes=[(cv[cc], hf)])
            mean = self.sb("lnm", [128, 512], F32)
            rstd = self.sb("lnr", [128, 512], F32)
            sq = self.sb("lnsq", [128, 512], F32)
            xn = [self.sb(f"lnx{i}", [128, 512], F32) for i in range(2)]
            for tg in range(4):
                cols = slice(tg * 512, (tg + 1) * 512)
                ps_s, ps_q = self.PS[4], self.PS[5]
                for cc in range(4):
                    self.mm(ps_s[:], cst["ones_f"][:], cv[cc][:, cols], start=(cc == 0), stop=(cc == 3))
                for cc in range(4):
                    self.act(sq[:], cv[cc][:, cols], AF.Square)
                    self.mm(ps_q[:], cst["ones_f"][:], sq[:], start=(cc == 0), stop=(cc == 3))
                self.ts("dve", mean[:], ps_s[:], 1.0 / 512, None, ALU.mult)
                self.tt("dve", rstd[:], mean[:], mean[:], ALU.mult)
                self.stt("dve", rstd[:], ps_q[:], 1.0 / 512, rstd[:], ALU.mult, ALU.subtract)
                self.ts("dve", rstd[:], rstd[:], EPS, None, ALU.add)
                self.act(rstd[:], rstd[:], AF.Sqrt)
                self.v("dve", "reciprocal", rstd[:], rstd[:], reads=[rstd], writes=[rstd])
                for cc in range(4):
                    x = xn[cc % 2]
                    self.tt("pool", x[:], cv[cc][:, cols], mean[:], ALU.subtract)
                    self.tt("dve", x[:], x[:], rstd[:], ALU.mult)
                    self.act(mixA[:, cc, cols], x[:], AF.Silu, bias=cpar[:, 2, cc:cc + 1], scale=cpar[:, 1, cc:cc + 1])
            self.release(mkA)
            if self.stop_after == "l1_conv":
                raise StopBuild()
            mixB = self.sb("mixB", [128, 4, LAT], BF16)
            mkq = self.mark()
            qT = self.sb("qT", [128, 8, LAT], BF16)
            kT = self.sb("kT", [128, 8, SEQ], BF16)
            vaug = self.sb("vaug", [128, NT, 8, 68], BF16)
            mk = self.mark()
            self.memset("pool", vaug[:], 1.0)
            cn = self.sb("cn", [128, 384], F32)
            cnT = self.sb("cnT", [128, 3, 128], BF16)
            xf = self.sb("xf", [128, 8, 96], F32)
            xo = self.sb("xo", [128, 8, 96], F32)
            tmp = self.sb("hn_tmp", [128, 8, 96], F32)
            hs = self.sb("hn_hs", [128, 8], F32)
            rs = self.sb("hn_rs", [128, 8], F32)
            t1 = [self.sb(f"rt1{i}", [128, 16], F32) for i in range(2)]
            t2 = [self.sb(f"rt2{i}", [128, 16], F32) for i in range(2)]
            krr = self.sb("krr", [128, 32], F32)
            krg = self.sb("krg", [128, 32], F32)
            junk2 = self.sb("junk2", [128, 384], F32)
            def proj_tile(i, uTt):
                import os as _os
                _stg = float(_os.environ.get("KSTG", "9"))
                if _stg <= 0:
                    return
                lat_i = i - 2
                tok = slice(i * 128, (i + 1) * 128)
                pk = self.PS[2]
                for kc in range(8):
                    self.mm(pk[:, 0:288], uTt[:, kc, :], wkv[:, kc, :], start=(kc == 0), stop=(kc == 7))
                if _stg <= 0.1:
                    return
                self.rms_rows(cn[:, 0:256], pk[:, 0:256], 256, kvan[:], junk2[:, 0:256], 10)
                if _stg <= 0.2:
                    return
                self.tt("dve", krg[:], pk[:, 256:288], knb[:, 64:96], ALU.mult)
                if _stg <= 0.3:
                    return
                pt = self.PS[3]
                for kc in range(2):
                    self.tr(pt[:, kc * 128:(kc + 1) * 128], cn[:, kc * 128:(kc + 1) * 128], cst["ident_f"][:])
                self.copy("act", cnT[:, 0:2, :], pt[:, 0:256].rearrange("p (k c) -> p k c", c=128))
                if _stg <= 0.4:
                    return
                pkv = [self.PS[4], self.PS[5]]
                for half in range(2):
                    for kc in range(2):
                        self.mm(pkv[half][:], cnT[:, kc, :], wukv[:, kc, half * 512:(half + 1) * 512], start=(kc == 0), stop=(kc == 1))
                    v4 = pkv[half][:, :].rearrange("p (h d) -> p h d", d=128)
                    if _stg <= 0.5:
                        continue
                    if _stg != 0.56:
                        self.copy("act", xf[:, half * 4:(half + 1) * 4, 0:64], v4[:, :, 0:64])
                    if _stg != 0.55:
                        self.copy("act" if _stg == 0.57 else "dve", vaug[:, i, half * 4:(half + 1) * 4, 0:64], v4[:, :, 64:128])
                if _stg <= 0.6:
                    return
                for h in range(8):
                    self.copy("pool", xf[:, h, 64:96], pk[:, 256:288]) if False else self.copy("dve" if h % 2 else "act", xf[:, h, 64:96], pk[:, 256:288])
                if _stg <= 1:
                    return
                self.head_norm(xf, knb, tmp, hs, rs)
                if _stg <= 2:
                    return
                if lat_i >= 0:
                    for h in range(8):
                        self.rope(xo[:, h, 64:96], xf[:, h, 64:96], rcos[:, lat_i, :], rsin[:, lat_i, :], t1[h % 2][:], t2[h % 2][:],
                                  "dve" if h % 2 == 0 else "pool")
                    self.copy("act", xo[:, :, 0:64], xf[:, :, 0:64])
                    src = xo
                else:
                    src = xf
                pa, pb_ = self.PS[4], self.PS[5]
                for h in range(8):
                    pp = pa if h < 4 else pb_
                    self.tr(pp[0:96, (h % 4) * 128:(h % 4 + 1) * 128], src[:, h, :], cst["ident_f"][:])
                self.copy("act", kT[0:96, 0:4, tok], pa[0:96, :].rearrange("p (k c) -> p k c", c=128))
                self.copy("dve", kT[0:96, 4:8, tok], pb_[0:96, :].rearrange("p (k c) -> p k c", c=128))
                if lat_i < 0 or _stg <= 3:
                    return
                ltok = slice(lat_i * 128, (lat_i + 1) * 128)
                pq = self.PS[2]
                for kc in range(8):
                    self.mm(pq[:, 0:384], uTt[:, kc, :], wq[:, kc, :], start=(kc == 0), stop=(kc == 7))
                if _stg <= 3.1:
                    return
                self.rms_rows(cn[:, 0:384], pq[:, 0:384], 384, qan[:], junk2[:, 0:384], 12)
                if _stg <= 3.2:
                    return
                pt = self.PS[3]
                for kc in range(3):
                    self.tr(pt[:, kc * 128:(kc + 1) * 128], cn[:, kc * 128:(kc + 1) * 128], cst["ident_f"][:])
                self.copy("act", cnT[:, 0:3, :], pt[:, 0:384].rearrange("p (k c) -> p k c", c=128))
                if _stg <= 3.3:
                    return
                pq1, pq2 = self.PS[4], self.PS[5]
                for kc in range(3):
                    self.mm(pq1[:, 0:384], cnT[:, kc, :], wuq[:, kc, 0:384], start=(kc == 0), stop=(kc == 2))
                for kc in range(3):
                    self.mm(pq2[:, 0:384], cnT[:, kc, :], wuq[:, kc, 384:768], start=(kc == 0), stop=(kc == 2))
                if _stg <= 3.4:
                    return
                self.copy("act", xf[:, 0:4, :], pq1[:, 0:384].rearrange("p (h d) -> p h d", d=96))
                self.copy("dve", xf[:, 4:8, :], pq2[:, 0:384].rearrange("p (h d) -> p h d", d=96))
                if _stg <= 3.5:
                    return
                self.head_norm(xf, qnb, tmp, hs, rs)
                if _stg <= 3.6:
                    return
                for h in range(8):
                    self.rope(xo[:, h, 64:96], xf[:, h, 64:96], rcos[:, lat_i, :], rsin[:, lat_i, :], t1[h % 2][:], t2[h % 2][:],
                              "dve" if h % 2 == 0 else "pool")
                if _stg <= 3.7:
                    return
                self.copy("act", xo[:, :, 0:64], xf[:, :, 0:64])
                for h in range(8):
                    pp = pa if h < 4 else pb_
                    self.tr(pp[0:96, (h % 4) * 128:(h % 4 + 1) * 128], xo[:, h, :], cst["ident_f"][:])
                if _stg <= 3.8:
                    return
                self.copy("act", qT[0:96, 0:4, ltok], pa[0:96, :].rearrange("p (k c) -> p k c", c=128))
                self.copy("dve", qT[0:96, 4:8, ltok], pb_[0:96, :].rearrange("p (k c) -> p k c", c=128))

            make_uT(per_tile=proj_tile)
            self.release(mk)
            if self.stop_after == "l1_proj":
                raise StopBuild()
            mk = self.mark()
            attn = self.sb("attn", [128, NL, 512], BF16)
            PT = [self.sb(f"PT{i}", [128, 512], BF16) for i in range(3)]
            rden = self.sb("rden", [128, 4], F32)
            n = 0
            for h in range(8):
                for qg in range(4):
                    qcols = slice(qg * 512, (qg + 1) * 512)
                    po = self.PS[4 + (h * 4 + qg) % 2]
                    for kt in range(NT):
                        ps = self.PS[n % 4]
                        self.mm(ps[:], kT[0:96, h, kt * 128:(kt + 1) * 128], qT[0:96, h, qcols])
                        p_ = PT[n % 3]
                        self.act(p_[:], ps[:], AF.Exp, scale=SC)
                        for qt in range(4):
                            self.mm(po[:, qt * 68:(qt + 1) * 68], p_[:, qt * 128:(qt + 1) * 128], vaug[:, kt, h, :],
                                    start=(kt == 0), stop=(kt == NT - 1))
                        n += 1
                    for qt in range(4):
                        self.v("dve", "reciprocal", rden[:, qt:qt + 1], po[:, qt * 68 + 64:qt * 68 + 65], reads=[po], writes=[rden])
                        self.ts("dve", attn[:, qg * 4 + qt, h * 64:(h + 1) * 64], po[:, qt * 68:qt * 68 + 64], rden[:, qt:qt + 1], None, ALU.mult,
                                reads=[po, rden], writes=[(attn, qg * 4 + qt)])
            for i in range(NL):
                pb = self.PB[i % 2]
                for c in range(4):
                    self.tr(pb[:, c * 128:(c + 1) * 128], attn[:, i, c * 128:(c + 1) * 128], cst["ident_b"][:])
                self.copy("act" if i % 2 == 0 else "dve", mixB[:, :, i * 128:(i + 1) * 128], pb[:, 0:512].rearrange("p (k c) -> p k c", c=128))
            self.release(mkq)
            if self.stop_after == "l1_attn":
                raise StopBuild()
            self.post_mixer(1, b, [mixA, mixB], NL, wr, brr)
            self.release(mkb)
        self.release(mk0)

    def post_mixer(self, layer, b, mixT, ntile, wr, brr):
        nc, S, I, C = self.nc, self.S, self.I, self.C
        cst = self.cst
        mk = self.mark()
        w_out = I["ev_w_out"][0] if layer == 0 else I["od_w_out"][0]
        wo = self.sb("wo", [128, 8, D], BF16)
        self.dma("pool", wo[:], w_out.rearrange("(kc p) n -> p kc n", p=128))
        mr = self.sb("mr2", [128, 6, D], F32)
        self.modrow(mr[:, 0, :], layer, b, 2)
        self.modrow(mr[:, 1, :], layer, b, 4)
        self.modrow(mr[:, 2, :], layer, b, 3)
        self.ts("dve", mr[:, 1, :], mr[:, 1, :], 1.0, None, ALU.add)
        if layer == 0:
            self.modrow(mr[:, 3, :], layer, 4, 2)
            self.modrow(mr[:, 4, :], layer, 4, 4)
            self.modrow(mr[:, 5, :], layer, 4, 3)
            self.ts("dve", mr[:, 4, :], mr[:, 4, :], 1.0, None, ALU.add)
        hts = [self.sb(f"pht{i}", [128, D], F32) for i in range(2)]
        h1s = [self.sb(f"ph1{i}", [128, D], F32) for i in range(2)]
        ms = [self.sb(f"pm{i}", [128, D], F32) for i in range(2)]
        m16 = [self.sb(f"pm16{i}", [128, D], BF16) for i in range(2)]
        mT = self.sb("pmT", [128, 8, 128], F32)
        junk = self.sb("pjunk", [128, D], F32)
        sm = self.small
        for i in range(ntile):
            if layer == 0:
                isctx = i < 2
                src = I["ctx"][b, i * 128:(i + 1) * 128, :] if isctx else I["x"][b, (i - 2) * 128:(i - 1) * 128, :]
                grow = b * SEQ + i * 128
                hdst = self.H1
            else:
                isctx = False
                grow = b * LAT + i * 128
                src = self.H2[b * SEQ + CTX + i * 128:b * SEQ + CTX + (i + 1) * 128, :]
                hdst = self.H1
            o = 3 if isctx else 0
            ht, h1, m, mb = hts[i % 2], h1s[i % 2], ms[i % 2], m16[i % 2]
            self.dma("sp", ht[:], src)
            p0, p1 = self.PS[0 + 2 * (i % 2)], self.PS[1 + 2 * (i % 2)]
            for half, ps in enumerate((p0, p1)):
                for kc in range(8):
                    mx = mixT[kc // 4] if isinstance(mixT, (list, tuple)) else mixT
                    kcc = kc % 4 if isinstance(mixT, (list, tuple)) else kc
                    self.mm(ps[:], mx[:, kcc, i * 128:(i + 1) * 128], wo[:, kc, half * 512:(half + 1) * 512],
                            start=(kc == 0), stop=(kc == 7))
                self.tt("dve", h1[:, half * 512:(half + 1) * 512], ps[:], mr[:, o, half * 512:(half + 1) * 512], ALU.mult)
            self.tt("pool", h1[:], h1[:], ht[:], ALU.add)
            self.dma("sp", hdst[grow:grow + 128, :], h1[:], writes=[(hdst, grow)])
            self.modulate_tile(m[:], h1[:], mr[:, o + 1, :], mr[:, o + 2, :], junk[:], sm[:, 4:5], sm[:, 5:6])
            self.copy("act", mb[:], m[:])
            self.dma("sp", self.M[grow:grow + 128, :], mb[:], writes=[(self.M, grow)])
            self.transpose_tile_to(mT, 0, m, 8, cst["ident_f"], self.PS[4], self.PS[5])
            pl = self.PS[4]
            for kc in range(8):
                self.mm(pl[:, 0:32], mT[:, kc, :], wr[:, kc, :], start=(kc == 0), stop=False)
            self.mm(pl[:, 0:32], cst["ones_f"][0:1, :], brr[0:1, :], start=False, stop=True)
            self.route_tile(layer, grow // 128, pl)
        self.release(mk)

    def route_init(self, layer):
        ns = self.ns
        T = self.T0 if layer == 0 else self.T1
        self.rT = T
        ntt = T // 128
        self.cntb = self.sb(f"cntb{layer}", [128, NE], F32)
        self.DEST = self.sb(f"DEST{layer}", [128, ntt, 4], I32)
        self.GATE = self.sb(f"GATE{layer}", [128, ntt, 4], F32)
        self.rt = {n: self.sb(f"rt_{n}{layer}", sh, dt) for n, sh, dt in (
            ("L", [128, NE], F32), ("t8", [128, 8], F32), ("e4", [128, 4], F32), ("mask", [128, NE], F32),
            ("m16", [128, NE], BF16), ("rf", [128, NE], F32), ("rfs", [128, NE], F32), ("tmp", [128, NE], F32),
            ("rk", [128, 4], F32), ("dk", [128, 4], F32), ("ok", [128, 4], F32), ("tok", [128, 2], F32),
            ("toki", [128, 2], I32), ("trash", [128, 1], F32), ("fill", [128, (NSLOT + 128) // 64], I32),
            ("z16", [128, D], BF16))}
        rt = self.rt
        self.memset("dve", self.cntb[:], 0.0)
        self.ts("dve", rt["trash"][:], self.cst["pidx"][:], float(NSLOT), None, ALU.add)
        self.memset("dve", rt["fill"][:], int(T))
        self.dma("sp", self.SLOT.rearrange("(p j) two -> p (j two)", p=128), rt["fill"][:])
        self.memset("pool", rt["z16"][:], 0.0)
        self.dma("sp", self.M[T:T + 128, :], rt["z16"][:], writes=[(self.M, "trash")])
        self.dma("sp", self.YB[NSLOT:NSLOT + 128, :], rt["z16"][:], writes=[(self.YB, "trash")])

    def route_tile(self, layer, gt, pl):
        nc, rt, cst = self.nc, self.rt, self.cst
        L, t8 = rt["L"], rt["t8"]
        self.copy("dve", L[:], pl[:, 0:NE])
        self.v("dve", "max", t8[:], L[:], reads=[L], writes=[t8])
        sm = self.small
        self.ts("dve", sm[:, 8:9], t8[:, 0:1], -1.0, None, ALU.mult)
        self.act(rt["e4"][:], t8[:, 0:4], AF.Exp, bias=sm[:, 8:9], scale=1.0)
        self.v("dve", "reduce_sum", sm[:, 9:10], rt["e4"][:], AX.X, reads=[rt["e4"]], writes=[sm])
        self.v("dve", "reciprocal", sm[:, 9:10], sm[:, 9:10], reads=[sm], writes=[sm])
        self.ts("dve", rt["e4"][:], rt["e4"][:], sm[:, 9:10], None, ALU.mult)
        self.ts("dve", rt["mask"][:], L[:], t8[:, 3:4], None, ALU.is_ge)
        self.copy("dve", rt["m16"][:], rt["mask"][:])
        pr = self.PS[5]
        self.mm(pr[:, 0:NE], cst["tri_b"][:], rt["m16"][:])
        self.mm(pr[:, 64:64 + NE], cst["ones_b"][:], rt["m16"][:])
        self.tt("dve", rt["rf"][:], pr[:, 0:NE], self.cntb[:], ALU.add)
        self.tt("dve", self.cntb[:], self.cntb[:], pr[:, 64:64 + NE], ALU.add)
        self.tt("dve", rt["rfs"][:], rt["rf"][:], cst["slotbase"][:], ALU.add)
        for k in range(4):
            self.stt("dve", rt["tmp"][:], L[:], t8[:, k:k + 1], rt["rf"][:], ALU.is_equal, ALU.mult)
            self.v("dve", "reduce_sum", rt["rk"][:, k:k + 1], rt["tmp"][:], AX.X, reads=[rt["tmp"]], writes=[rt["rk"]])
            self.stt("dve", rt["tmp"][:], L[:], t8[:, k:k + 1], rt["rfs"][:], ALU.is_equal, ALU.mult)
            self.v("dve", "reduce_sum", rt["dk"][:, k:k + 1], rt["tmp"][:], AX.X, reads=[rt["tmp"]], writes=[rt["dk"]])
        self.ts("dve", rt["ok"][:], rt["rk"][:], float(CAP), None, ALU.is_lt)
        self.stt("dve", rt["dk"][:], rt["dk"][:], rt["trash"][:, 0:1], rt["ok"][:], ALU.subtract, ALU.mult)
        self.ts("dve", rt["dk"][:], rt["dk"][:], rt["trash"][:, 0:1], None, ALU.add)
        self.tt("dve", self.GATE[:, gt, :], rt["e4"][:], rt["ok"][:], ALU.mult, writes=[(self.GATE, gt)])
        self.copy("dve", self.DEST[:, gt, :], rt["dk"][:], writes=[(self.DEST, gt)])
        self.ts("dve", rt["tok"][:, 0:1], cst["pidx"][:], float(gt * 128), None, ALU.add)
        self.copy("dve", rt["tok"][:, 1:2], rt["tok"][:, 0:1])
        self.copy("dve", rt["toki"][:], rt["tok"][:])
        for k in range(4):
            self.dma("pool", self.SLOT[:, :], rt["toki"][:, :], reads=[rt["toki"], (self.DEST, gt)], writes=[(self.SLOT, (gt, k))],
                     indirect=dict(out_offset=bass.IndirectOffsetOnAxis(ap=self.DEST[:, gt, k:k + 1], axis=0), in_offset=None))

    def experts(self, layer):
        nc, I, cst = self.nc, self.I, self.cst
        mk = self.mark()
        w1s = [self.sb(f"w1s{i}", [128, 8, 2 * D], BF16) for i in range(2)]
        w2s = [self.sb(f"w2s{i}", [128, 8, D], BF16) for i in range(2)]
        b1s = [self.sb(f"b1s{i}", [128, 16], F32) for i in range(2)]
        b2s = [self.sb(f"b2s{i}", [1, D], F32) for i in range(2)]
        idx = [self.sb(f"idx{i}", [128, 2], I32) for i in range(4)]
        xg = [self.sb(f"xg{i}", [128, D], BF16) for i in range(4)]
        xT = [self.sb(f"xT{i}", [128, 8, 512], BF16) for i in range(2)]
        aT = [self.sb(f"aT{i}", [128, 8, 512], BF16) for i in range(2)]
        glc = [self.sb(f"glc{i}", [128, 512], F32) for i in range(2)]
        sig = [self.sb(f"sig{i}", [128, 512], F32) for i in range(2)]
        lin = [self.sb(f"lin{i}", [128, 512], F32) for i in range(2)]
        yt = [self.sb(f"yt{i}", [128, D], BF16) for i in range(2)]
        for x in xg:
            self.memset("pool", x[:], 0.0)
        it = 0
        for e in range(NE):
            w1, w2, b1, b2 = w1s[e % 2], w2s[e % 2], b1s[e % 2], b2s[e % 2]
            src1 = I["moe_w1"][layer, e].rearrange("(kc p) n -> p kc n", p=128)
            for q in range(4):
                self.dma("pool", w1[:, 2 * q:2 * q + 2, :], src1[:, 2 * q:2 * q + 2, :], writes=[(w1, q)])
            src2 = I["moe_w2"][layer, e].rearrange("(kc p) n -> p kc n", p=128)
            for q in range(2):
                self.dma("pool", w2[:, 4 * q:4 * q + 4, :], src2[:, 4 * q:4 * q + 4, :], writes=[(w2, q)])
            self.dma("sp", b1[:], I["moe_b1"][layer, e].rearrange("(c p) -> p c", p=128), allow_slow_non_contiguous=True)
            self.dma("sp", b2[:], I["moe_b2"][layer, e:e + 1, :])
            for grp in range(CAP // 512):
                x_t, a_t = xT[it % 2], aT[it % 2]
                for j in range(4):
                    r0 = e * CAP + (grp * 4 + j) * 128
                    self.dma("sp", idx[j][:], self.SLOT[r0:r0 + 128, :], reads=[self.SLOT])
                    self.dma("pool", xg[j][:], self.M[:, :], reads=[self.M, idx[j]],
                             indirect=dict(out_offset=None, in_offset=bass.IndirectOffsetOnAxis(ap=idx[j][:, 0:1], axis=0)))
                    pb = self.PB[j % 2]
                    for kc in range(8):
                        self.tr(pb[:, kc * 128:(kc + 1) * 128], xg[j][:, kc * 128:(kc + 1) * 128], cst["ident_b"][:])
                    self.copy("act" if j % 2 == 0 else "dve", x_t[:, :, j * 128:(j + 1) * 128],
                              pb[:, :].rearrange("p (k c) -> p k c", c=128))
                for jc in range(8):
                    pg, plin = self.PS[(jc % 2) * 2], self.PS[(jc % 2) * 2 + 1]
                    for kc in range(8):
                        self.mm(pg[:], w1[:, kc, jc * 128:(jc + 1) * 128], x_t[:, kc, :], start=(kc == 0), stop=(kc == 7))
                    for kc in range(8):
                        self.mm(plin[:], w1[:, kc, D + jc * 128:D + (jc + 1) * 128], x_t[:, kc, :], start=(kc == 0), stop=(kc == 7))
                    g_, s_, l_ = glc[jc % 2], sig[jc % 2], lin[jc % 2]
                    self.ts("dve", g_[:], pg[:], b1[:, jc:jc + 1], 7.0, ALU.add, ALU.min)
                    self.act(s_[:], g_[:], AF.Sigmoid, scale=1.702)
                    self.ts("dve", l_[:], plin[:], b1[:, 8 + jc:9 + jc], 7.0, ALU.add, ALU.min)
                    self.ts("pool", l_[:], l_[:], -7.0, 1.0, ALU.max, ALU.add)
                    self.tt("pool", g_[:], g_[:], s_[:], ALU.mult)
                    self.tt("dve", a_t[:, jc, :], g_[:], l_[:], ALU.mult)
                for jt in range(4):
                    y = yt[jt % 2]
                    for half in range(2):
                        ps = self.PS[4 + half]
                        for jc in range(8):
                            self.mm(ps[:], a_t[:, jc, jt * 128:(jt + 1) * 128], w2[:, jc, half * 512:(half + 1) * 512],
                                    start=(jc == 0), stop=False)
                        self.mm(ps[:], cst["ones_f"][0:1, :], b2[0:1, half * 512:(half + 1) * 512], start=False, stop=True)
                        self.copy("act", y[:, half * 512:(half + 1) * 512], ps[:])
                    r0 = e * CAP + (grp * 4 + jt) * 128
                    self.dma("sp", self.YB[r0:r0 + 128, :], y[:], writes=[(self.YB, r0)])
                it += 1
        self.release(mk)

    def combine(self, layer):
        nc, I, cst = self.nc, self.I, self.cst
        ns = self.ns
        mk = self.mark()
        g2 = self.sb("g2", [128, 2, D], F32)
        yk = [self.sb(f"yk{i}", [128, D], BF16) for i in range(4)]
        acc = [self.sb(f"acc{i}", [128, D], F32) for i in range(2)]
        h1t = [self.sb(f"h1t{i}", [128, D], F32) for i in range(2)]
        if layer == 0:
            self.modrow(g2[:, 1, :], 0, 4, 5)
        nt_seq = NT if layer == 0 else LAT // 128
        for b in range(ns):
            self.modrow(g2[:, 0, :], layer, b, 5)
            for i in range(nt_seq):
                gt = b * nt_seq + i
                grow = gt * 128
                a, h = acc[gt % 2], h1t[gt % 2]
                self.dma("sp", h[:], self.H1[grow:grow + 128, :], reads=[self.H1])
                for k in range(4):
                    self.dma("pool", yk[k][:], self.YB[:, :], reads=[self.YB, (self.DEST, gt)],
                             indirect=dict(out_offset=None, in_offset=bass.IndirectOffsetOnAxis(ap=self.DEST[:, gt, k:k + 1], axis=0)))
                    if k == 0:
                        self.ts("dve", a[:], yk[0][:], self.GATE[:, gt, 0:1], None, ALU.mult, reads=[yk[0], (self.GATE, gt)])
                    else:
                        self.stt("dve", a[:], yk[k][:], self.GATE[:, gt, k:k + 1], a[:], ALU.mult, ALU.add,
                                 reads=[yk[k], a, (self.GATE, gt)])
                gsel = 1 if (layer == 0 and i < 2) else 0
                self.tt("pool", a[:], a[:], g2[:, gsel, :], ALU.mult)
                self.tt("dve", a[:], a[:], h[:], ALU.add)
                if layer == 0:
                    self.dma("sp", self.H2[grow:grow + 128, :], a[:], writes=[(self.H2, grow)])
                else:
                    self.dma("sp", self.out[b, i * 128:(i + 1) * 128, :], a[:], writes=[("out", grow)])
        self.release(mk)


def build_program(ns, **kw):
    k = K(ns, **kw)
    k.build()
    return k


_CACHE = {}


def kernel(**inputs):
    ncores, ns = 8, 4
    if "k" not in _CACHE:
        _CACHE["k"] = build_program(ns)
    k = _CACHE["k"]
    consts = {"k_" + n: v for n, v in host_consts().items()}
    shared = {n: np.ascontiguousarray(np.asarray(inputs[n], dtype=np.float32)) for n in IN_SPECS if n not in ("x", "c", "ctx")}
    in_maps = []
    for c in range(ncores):
        m = dict(shared)
        for n in ("x", "c", "ctx"):
            m[n] = np.ascontiguousarray(np.asarray(inputs[n], dtype=np.float32)[c * ns:(c + 1) * ns])
        m.update(consts)
        in_maps.append(m)
    res = run_bass_kernel_spmd(k.nc, in_maps, core_ids=list(range(ncores)))
    return np.concatenate([r["out"] for r in res.results], axis=0).astype(np.float32)
```

```python
import numpy as np
import ml_dtypes
import concourse.bass as bass
import concourse.mybir as mybir
from concourse.bass_utils import run_bass_kernel_spmd

F32 = mybir.dt.float32
BF16 = mybir.dt.bfloat16
I32 = mybir.dt.int32
AF = mybir.ActivationFunctionType
ALU = mybir.AluOpType
AX = mybir.AxisListType

D = 1024
LAT = 2048
CTX = 256
SEQ = LAT + CTX
NT = SEQ // 128
EPS = 1e-6
NE = 32
CAPS = (2560, 2048)
CAPMAX = max(CAPS)
NSLOTMAX = NE * CAPMAX


class Sched:
    EPOCH = 30000

    def __init__(self, nc):
        self.nc = nc
        self.E = {"pe": nc.tensor, "dve": nc.vector, "act": nc.scalar, "pool": nc.gpsimd, "sp": nc.sync}
        self.nsem = 0
        self.esem, self.ecnt = {}, {}
        self.pesems = set()
        for e in self.E:
            self._new_epoch(e)
        self.seen = {e: {} for e in self.E}
        self.W, self.R = {}, {}
        self.dpool = {q: [] for q in ("sp", "pool", "act")}
        self.dnext = {q: 0 for q in self.dpool}
        self.NPOOL = {"sp": 24, "pool": 24, "act": 4}
        self.ninst = 0
        self.psum = set()

    def _sem(self, name):
        self.nsem += 1
        return self.nc.semaphore(f"{name}{self.nsem}").__enter__()

    def _new_epoch(self, e):
        import os as _os
        self.skip_same = _os.environ.get("KSAME", "1") == "0"
        if not hasattr(self, "own"):
            self.own = {}
        self.esem[e] = self._sem("e" + e)
        self.own.setdefault(e, set()).add(self.esem[e])
        self.ecnt[e] = 0
        if e == "pe":
            self.pesems.add(self.esem[e])

    @staticmethod
    def _nm(x):
        if isinstance(x, str):
            return x
        t = getattr(x, "tensor", None)
        return t.name if t is not None else x.name

    @staticmethod
    def _key(x):
        if isinstance(x, tuple):
            return (Sched._nm(x[0]), x[1])
        return (Sched._nm(x), None)

    def _collect(self, table, key, evs):
        name, sub = key
        t = table.get(name)
        if not t:
            return
        subs = t.keys() if sub is None else [s for s in (sub, None) if s in t]
        for s in subs:
            for sem, val in t[s].items():
                if evs.get(sem, 0) < val:
                    evs[sem] = val

    def _deps(self, e, rk, wk):
        evs = {}
        for k in rk:
            self._collect(self.W, k, evs)
        for k in wk:
            self._collect(self.W, k, evs)
            self._collect(self.R, k, evs)
        for sem, val in evs.items():
            if e == "pe" and sem in self.pesems:
                continue
            if self.skip_same and sem in self.own.get(e, ()):
                continue
            if self.seen[e].get(sem, 0) >= val:
                continue
            self.E[e].wait_ge(sem, val)
            self.seen[e][sem] = val

    def _commit(self, ev, rk, wk):
        sem, val = ev
        for name, sub in rk:
            d = self.R.setdefault(name, {}).setdefault(sub, {})
            if d.get(sem, 0) < val:
                d[sem] = val
        for name, sub in wk:
            if sub is None:
                self.W[name] = {None: {sem: val}}
                self.R[name] = {}
            else:
                self.W.setdefault(name, {})[sub] = {sem: val}
                self.R.setdefault(name, {}).pop(sub, None)

    def op(self, e, fn, reads=(), writes=()):
        rk = [self._key(x) for x in reads]
        wk = [self._key(x) for x in writes]
        pr = [(k[0], None) for k in rk if k[0] in self.psum]
        if pr:
            rk = [k for k in rk if k[0] not in self.psum]
            wk = wk + pr
        wk = [((k[0], None) if k[0] in self.psum else k) for k in wk]
        self._deps(e, rk, wk)
        if self.ecnt[e] >= self.EPOCH:
            self._new_epoch(e)
        ins = fn()
        self.ecnt[e] += 1
        ins.then_inc(self.esem[e], 1)
        self._commit((self.esem[e], self.ecnt[e]), rk, wk)
        self.ninst += 1
        return ins

    def dma(self, q, out, in_, reads=None, writes=None, indirect=None, **kw):
        rk = [self._key(x) for x in (reads if reads is not None else [in_])]
        wk = [self._key(x) for x in (writes if writes is not None else [out])]
        self._deps(q, rk, wk)
        pool = self.dpool[q]
        if len(pool) < self.NPOOL[q]:
            pool.append([self._sem("d" + q), 0])
            slot = pool[-1]
        else:
            slot = pool[self.dnext[q] % len(pool)]
            self.dnext[q] += 1
            if slot[1] >= 60000:
                if self.seen[q].get(slot[0], 0) < slot[1]:
                    self.E[q].wait_ge(slot[0], slot[1])
                slot[0], slot[1] = self._sem("d" + q), 0
        sem, cnt = slot
        if cnt > 0 and self.seen[q].get(sem, 0) < cnt:
            self.E[q].wait_ge(sem, cnt)
            self.seen[q][sem] = cnt
        if indirect is None:
            ins = self.E[q].dma_start(out=out, in_=in_, **kw)
        else:
            ins = self.E[q].indirect_dma_start(out=out, in_=in_, **indirect)
        slot[1] = cnt + 16
        ins.then_inc(sem, 16)
        self._commit((sem, slot[1]), rk, wk)
        self.ninst += 1
        return ins

    def barrier(self):
        evs = {}
        for e in self.E:
            if self.ecnt[e] > 0:
                evs[self.esem[e]] = self.ecnt[e]
        for q, pool in self.dpool.items():
            for sem, cnt in pool:
                if cnt > 0:
                    evs[sem] = cnt
        for e in self.E:
            for sem, val in evs.items():
                if self.seen[e].get(sem, 0) >= val:
                    continue
                self.E[e].wait_ge(sem, val)
                self.seen[e][sem] = val
        self.W, self.R = {}, {}

    def finish(self):
        self.barrier()


def host_consts():
    c = {}
    c["ident_f"] = np.eye(128, dtype=np.float32)
    c["ident_b"] = np.eye(128).astype(ml_dtypes.bfloat16)
    s = np.arange(128)[:, None]
    t = np.arange(128)[None, :]
    c["mask_f"] = (s <= t).astype(np.float32)
    c["mask_b"] = (s >= t).astype(np.float32)
    c["tri_b"] = (s < t).astype(ml_dtypes.bfloat16)
    c["ones_b"] = np.ones((128, 128), ml_dtypes.bfloat16)
    c["ones_f"] = np.ones((128, 128), np.float32)
    facS = np.ones((4, 8), np.float32)
    facE = np.ones((4, 8), np.float32)
    for g, w in enumerate((2, 4, 8, 16)):
        half = w // 2
        for tt in range(min(half, 8)):
            facS[g, tt] = w / (tt + half)
        for j in range(8):
            i = 7 - j
            if i < half - 1:
                facE[g, j] = w / (half + 1 + i)
    c["facS"] = np.broadcast_to(facS[None], (128, 4, 8)).copy()
    c["facE"] = np.broadcast_to(facE[None], (128, 4, 8)).copy()
    rows = LAT // 64
    t_row = np.repeat(np.arange(rows, dtype=np.float32), 64)
    t_col = np.tile(np.arange(64, dtype=np.float32), rows)
    inv = (1.0 / (10000.0 ** (np.arange(8, dtype=np.float32) / 8))).astype(np.float32)
    ang = np.stack([t_row[:, None] * inv, t_col[:, None] * inv], axis=1).astype(np.float32)
    cos = np.cos(ang).reshape(LAT, 16).astype(np.float32)
    sin = np.sin(ang).reshape(LAT, 16).astype(np.float32)
    for l_ in range(2):
        c[f"slotbase{l_}"] = np.broadcast_to((np.arange(32, dtype=np.float32) * CAPS[l_])[None], (128, 32)).copy()
    c["pidx"] = np.arange(128, dtype=np.float32).reshape(128, 1).copy()
    c["rope_cos"] = cos.reshape(16, 128, 16).transpose(1, 0, 2).copy()
    c["rope_sin"] = sin.reshape(16, 128, 16).transpose(1, 0, 2).copy()
    return c


CONST_SPECS = {
    "ident_f": ([128, 128], F32), "ident_b": ([128, 128], BF16), "mask_f": ([128, 128], F32),
    "mask_b": ([128, 128], F32), "tri_b": ([128, 128], BF16), "ones_b": ([128, 128], BF16),
    "ones_f": ([128, 128], F32), "facS": ([128, 4, 8], F32), "facE": ([128, 4, 8], F32),
    "slotbase0": ([128, 32], F32), "slotbase1": ([128, 32], F32), "pidx": ([128, 1], F32),
    "rope_cos": ([128, 16, 16], F32), "rope_sin": ([128, 16, 16], F32),
}

IN_SPECS = {
    "x": lambda ns: [ns, LAT, D], "c": lambda ns: [ns, D], "ctx": lambda ns: [ns, CTX, D], "c_ctx": lambda ns: [D],
    "ada_w": lambda ns: [2, D, 6 * D], "ada_b": lambda ns: [2, 6 * D], "ev_w_in": lambda ns: [1, D, 3072],
    "hgrn_lb_logits": lambda ns: [3, 2, 512], "hgrn_norm_w": lambda ns: [1, 128], "pool_w": lambda ns: [1, 4, 128, 128],
    "pool_scale": lambda ns: [1, 512], "ev_w_out": lambda ns: [1, D, D], "od_w_in": lambda ns: [1, D, 1696],
    "conv_dw_w": lambda ns: [1, 31, 512], "conv_dw_b": lambda ns: [1, 512], "conv_ln_w": lambda ns: [1, 512],
    "conv_ln_b": lambda ns: [1, 512], "mla_q_a_norm": lambda ns: [1, 384], "mla_w_uq": lambda ns: [1, 384, 768],
    "mla_kv_a_norm": lambda ns: [1, 256], "mla_w_ukv": lambda ns: [1, 256, 1024], "mla_q_norm": lambda ns: [1, 96],
    "mla_k_norm": lambda ns: [1, 96], "od_w_out": lambda ns: [1, D, D], "moe_router_w": lambda ns: [2, D, 32],
    "moe_router_b": lambda ns: [2, 32], "moe_w1": lambda ns: [2, 32, D, 2 * D], "moe_b1": lambda ns: [2, 32, 2 * D],
    "moe_w2": lambda ns: [2, 32, D, D], "moe_b2": lambda ns: [2, 32, D],
}


class StopBuild(Exception):
    pass


class K:
    def __init__(self, ns, stop_after=None, dbg=(), skip_inputs=()):
        self.ns = ns
        self.stop_after = stop_after
        self.dbg = set(dbg)
        nc = self.nc = bass.Bass("TRN2", target_bir_lowering=False)
        self.S = Sched(nc)
        self.I = {k: nc.dram_tensor(k, f(ns), F32, kind="ExternalInput").ap() for k, f in IN_SPECS.items() if k not in skip_inputs}
        self.C = {k: nc.dram_tensor("k_" + k, sh, dt, kind="ExternalInput").ap() for k, (sh, dt) in CONST_SPECS.items()}
        self.out = nc.dram_tensor("out", [ns, LAT, D], F32, kind="ExternalOutput").ap()
        self.T0 = ns * SEQ
        self.T1 = ns * LAT
        self.MOD = nc.dram_tensor("MOD", [10, 6 * D], F32).ap()
        self.H1 = nc.dram_tensor("H1", [self.T0, D], F32).ap()
        self.H2 = nc.dram_tensor("H2", [self.T0, D], F32).ap()
        self.M = nc.dram_tensor("M", [self.T0 + 128, D], BF16).ap()
        self.SLOT = nc.dram_tensor("SLOT", [NSLOTMAX + 128, 2], I32).ap()
        self.YB = nc.dram_tensor("YB", [NSLOTMAX + 128, D], BF16).ap()
        self.dbg_out = {}
        self._stack = []

    def sb(self, name, shape, dt):
        self._uid = getattr(self, "_uid", 0) + 1
        cm = self.nc.sbuf_tensor(f"{name}_{self._uid}", shape, dt)
        t = cm.__enter__()
        self._stack.append(cm)
        return t

    def ps(self, name, shape, dt):
        cm = self.nc.psum_tensor(name, shape, dt)
        t = cm.__enter__()
        self.S.psum.add(name)
        self._stack.append(cm)
        return t

    def mark(self):
        return len(self._stack)

    def release(self, mark):
        self.S.barrier()
        while len(self._stack) > mark:
            self._stack.pop().__exit__(None, None, None)

    def mm(self, out, lhsT, rhs, start=True, stop=True, reads=None, writes=None):
        nc = self.nc
        return self.S.op("pe", lambda: nc.tensor.matmul(out, lhsT, rhs, start=start, stop=stop),
                         reads=reads if reads is not None else [lhsT, rhs], writes=writes if writes is not None else [out])

    def tr(self, out, in_, ident, reads=None, writes=None):
        nc = self.nc
        return self.S.op("pe", lambda: nc.tensor.transpose(out, in_, ident),
                         reads=reads if reads is not None else [in_, ident], writes=writes if writes is not None else [out])

    def act(self, out, in_, func, bias=None, scale=None, accum_out=None, reads=None, writes=None):
        nc = self.nc
        kw = {}
        rd = [in_]
        wr = [out]
        if bias is not None:
            kw["bias"] = bias
            if not isinstance(bias, (int, float)):
                rd.append(bias)
        if scale is not None:
            kw["scale"] = scale
            if not isinstance(scale, (int, float)):
                rd.append(scale)
        if accum_out is not None:
            kw["accum_out"] = accum_out
            wr.append(accum_out)
        return self.S.op("act", lambda: nc.scalar.activation(out, in_, func, **kw),
                         reads=reads if reads is not None else rd, writes=writes if writes is not None else wr)

    def v(self, e, name, *args, reads, writes, **kw):
        eng = self.S.E[e]
        return self.S.op(e, lambda: getattr(eng, name)(*args, **kw), reads=reads, writes=writes)

    def tt(self, e, out, in0, in1, op, reads=None, writes=None):
        return self.v(e, "tensor_tensor", out, in0, in1, op, reads=reads if reads is not None else [in0, in1],
                      writes=writes if writes is not None else [out])

    def ts(self, e, out, in0, s1, s2, op0, op1=None, reads=None, writes=None, accum_out=None):
        rd = [in0] + [s for s in (s1, s2) if s is not None and not isinstance(s, (int, float))]
        kw = {}
        if op1 is not None:
            kw["op1"] = op1
        wr = [out]
        if accum_out is not None:
            kw["accum_out"] = accum_out
            wr.append(accum_out)
        return self.v(e, "tensor_scalar", out, in0, s1, s2, op0, reads=reads if reads is not None else rd,
                      writes=writes if writes is not None else wr, **kw)

    def stt(self, e, out, in0, scalar, in1, op0, op1, reads=None, writes=None):
        rd = [in0, in1] + ([] if isinstance(scalar, (int, float)) else [scalar])
        return self.v(e, "scalar_tensor_tensor", out, in0, scalar, in1, op0, op1,
                      reads=reads if reads is not None else rd, writes=writes if writes is not None else [out])

    def copy(self, e, out, in_, reads=None, writes=None):
        if e == "act":
            return self.act(out, in_, AF.Copy, reads=reads, writes=writes)
        return self.v(e, "tensor_copy", out, in_, reads=reads if reads is not None else [in_],
                      writes=writes if writes is not None else [out])

    def memset(self, e, ap, val):
        return self.v(e, "memset", ap, val, reads=[], writes=[ap])

    def dma(self, q, out, in_, **kw):
        return self.S.dma(q, out, in_, **kw)

    def rstd_from_ss(self, rstd, ss, n):
        self.ts("dve", rstd, ss, 1.0 / n, EPS, ALU.mult, ALU.add)
        self.act(rstd, rstd, AF.Sqrt)
        self.v("dve", "reciprocal", rstd, rstd, reads=[rstd], writes=[rstd])

    def build(self):
        nc, S, I, C = self.nc, self.S, self.I, self.C
        ns = self.ns
        self.cst = {}
        for k in ("ident_f", "ident_b", "mask_f", "mask_b", "tri_b", "ones_b", "ones_f", "slotbase0", "slotbase1", "pidx"):
            sh, dt = CONST_SPECS[k]
            t = self.sb("c_" + k, sh, dt)
            self.dma("sp", t[:], C[k][:, :])
            self.cst[k] = t
        self.PS = [self.ps(f"ps{i}", [128, 512], F32) for i in range(6)]
        self.PB = [self.ps(f"pb{i}", [128, 1024], BF16) for i in range(2)]
        self.small = self.sb("small", [128, 64], F32)

        self.phase_adaln()
        if self.stop_after == "adaln":
            return self.end()
        mkr = self.mark()
        self.route_init(0)
        try:
            self.layer0()
        except StopBuild:
            return self.end()
        if self.stop_after == "mixer0":
            return self.end()
        if self.stop_after and self.stop_after.startswith("l1_"):
            self.S.barrier()
            self.dma("sp", self.H2, self.H1)
        else:
            self.experts(0)
            self.combine(0)
        self.release(mkr)
        if self.stop_after == "layer0":
            return self.end()
        mkr = self.mark()
        self.route_init(1)
        try:
            self.layer1()
        except StopBuild:
            return self.end()
        self.experts(1)
        self.combine(1)
        self.release(mkr)
        return self.end()

    def end(self):
        for name in ("MOD", "H1", "H2", "M"):
            if name in self.dbg:
                src = getattr(self, name)
                d = self.dbg_dump(name, list(src.shape), src.dtype)
                self.S.barrier()
                self.dma("sp", d, src)
        self.S.finish()
        return self.nc

    def dbg_dump(self, name, shape, dt=F32):
        t = self.nc.dram_tensor("dbg_" + name, shape, dt, kind="ExternalOutput").ap()
        self.dbg_out[name] = t
        return t

    def phase_adaln(self):
        nc, S, I, C = self.nc, self.S, self.I, self.C
        ns = self.ns
        mk = self.mark()
        cs = self.sb("cs", [128, 8, 8], F32)
        s5 = self.sb("s5", [128, 8, 8], F32)
        self.memset("dve", cs[:], 0.0)
        for b in range(ns):
            self.dma("sp", cs[:, :, b], I["c"][b].rearrange("(kc p) -> p kc", p=128), allow_slow_non_contiguous=True)
        self.dma("sp", cs[:, :, ns], I["c_ctx"].rearrange("(kc p) -> p kc", p=128), allow_slow_non_contiguous=True)
        self.act(s5[:], cs[:], AF.Silu)
        nr = ns + 1
        brow = self.sb("brow", [1, 6 * D], F32)
        wa = [self.sb(f"wa{i}", [128, 8, 512], F32) for i in range(2)]
        mo = [self.sb(f"mo{i}", [8, 512], F32) for i in range(2)]
        onesf = self.cst["ones_f"]
        it = 0
        for layer in range(2):
            self.dma("sp", brow[:], I["ada_b"][layer:layer + 1, :])
            for nb in range(12):
                w = wa[it % 2]
                self.dma("sp" if it % 2 == 0 else "pool", w[:],
                         I["ada_w"][layer][:, nb * 512:(nb + 1) * 512].rearrange("(kc p) n -> p kc n", p=128))
                ps = self.PS[it % 2]
                for kc in range(8):
                    self.mm(ps[0:nr, :], s5[:, kc, 0:nr], w[:, kc, :], start=(kc == 0), stop=False)
                self.mm(ps[0:nr, :], onesf[0:1, 0:nr], brow[0:1, nb * 512:(nb + 1) * 512], start=False, stop=True)
                m = mo[it % 2]
                self.copy("dve", m[0:nr, :], ps[0:nr, :])
                self.dma("sp", self.MOD[layer * 5:layer * 5 + ns, nb * 512:(nb + 1) * 512], m[0:ns, :],
                         writes=[(self.MOD, (layer, nb, 0))])
                self.dma("sp", self.MOD[layer * 5 + 4:layer * 5 + 5, nb * 512:(nb + 1) * 512], m[ns:ns + 1, :],
                         writes=[(self.MOD, (layer, nb, 1))])
                it += 1
        self.release(mk)

    def modrow(self, dst, layer, row, q):
        src = self.MOD[layer * 5 + row:layer * 5 + row + 1, q * D:(q + 1) * D].partition_broadcast(128)
        self.dma("sp", dst, src, reads=[self.MOD])

    def modulate_tile(self, u, ht, sc1p, sh, junk, ss, rstd):
        self.act(junk, ht, AF.Square, accum_out=ss)
        self.rstd_from_ss(rstd, ss, D)
        self.stt("dve", u, ht, rstd, sc1p, ALU.mult, ALU.mult)
        self.tt("pool", u, u, sh, ALU.add)

    def transpose_tile_to(self, dstT, col0, src, nkc, ident_f, psA, psB, dt_note=None):
        for kc in range(nkc):
            ps = psA if kc < 4 else psB
            self.tr(ps[:, (kc % 4) * 128:(kc % 4 + 1) * 128], src[:, kc * 128:(kc + 1) * 128], ident_f[:])
        n0 = min(4, nkc)
        self.copy("act", dstT[:, 0:n0, col0:col0 + 128], psA[:, 0:n0 * 128].rearrange("p (k c) -> p k c", c=128))
        if nkc > 4:
            self.copy("dve", dstT[:, 4:nkc, col0:col0 + 128], psB[:, 0:(nkc - 4) * 128].rearrange("p (k c) -> p k c", c=128))

    def layer0(self):
        nc, S, I, C = self.nc, self.S, self.I, self.C
        ns = self.ns
        cst = self.cst
        mk0 = self.mark()
        lg = self.sb("lg", [128, 3, 8], F32)
        lb = self.sb("lb", [128, 8], F32)
        oml = self.sb("oml", [128, 8], F32)
        for l in range(3):
            self.dma("sp", lg[:, l, :].rearrange("p (d h) -> p d h", d=2),
                     I["hgrn_lb_logits"][l].rearrange("d (h p) -> p d h", p=128), allow_slow_non_contiguous=True)
        self.act(lg[:], lg[:], AF.Exp)
        self.tt("dve", lb[:], lg[:, 0, :], lg[:, 1, :], ALU.add)
        self.tt("dve", lb[:], lb[:], lg[:, 2, :], ALU.add)
        self.v("dve", "reciprocal", lb[:], lb[:], reads=[lb], writes=[lb])
        self.tt("dve", lb[:], lb[:], lg[:, 0, :], ALU.mult)
        self.ts("dve", oml[:], lb[:], -1.0, 1.0, ALU.mult, ALU.add)
        nwb = self.sb("nwb", [128, 128], F32)
        self.dma("sp", nwb[:], I["hgrn_norm_w"][0:1, :].partition_broadcast(128))
        pscale = self.sb("pscale", [128, 4], F32)
        self.dma("sp", pscale[:], I["pool_scale"][0].rearrange("(g p) -> p g", p=128), allow_slow_non_contiguous=True)
        poolw = self.sb("poolw", [128, 4, 128], BF16)
        for g in range(4):
            self.dma("pool", poolw[:, g, :], I["pool_w"][0, g])
        facS = self.sb("facS", [128, 4, 8], F32)
        facE = self.sb("facE", [128, 4, 8], F32)
        self.dma("sp", facS[:], C["facS"])
        self.dma("sp", facE[:], C["facE"])
        wr = self.sb("wr", [128, 8, 32], F32)
        self.dma("sp", wr[:], I["moe_router_w"][0].rearrange("(kc p) e -> p kc e", p=128))
        brr = self.sb("brr", [1, 32], F32)
        self.dma("sp", brr[:], I["moe_router_b"][0:1, :])
        uT = self.sb("uT", [128, 8, SEQ], BF16)
        mixT = self.sb("mixT", [128, 8, SEQ], BF16)
        w_in = I["ev_w_in"][0]
        for b in range(ns):
            mk = self.mark()
            mr = self.sb("mr", [128, 4, D], F32)
            self.modrow(mr[:, 0, :], 0, b, 1)
            self.modrow(mr[:, 1, :], 0, b, 0)
            self.modrow(mr[:, 2, :], 0, 4, 1)
            self.modrow(mr[:, 3, :], 0, 4, 0)
            self.ts("dve", mr[:, 0, :], mr[:, 0, :], 1.0, None, ALU.add)
            self.ts("dve", mr[:, 2, :], mr[:, 2, :], 1.0, None, ALU.add)
            hts = [self.sb(f"ht{i}", [128, D], F32) for i in range(2)]
            us = [self.sb(f"u{i}", [128, D], F32) for i in range(2)]
            junk = self.sb("junk", [128, D], F32)
            for i in range(NT):
                ht, u = hts[i % 2], us[i % 2]
                src = I["ctx"][b, i * 128:(i + 1) * 128, :] if i < 2 else I["x"][b, (i - 2) * 128:(i - 1) * 128, :]
                self.dma("sp", ht[:], src)
                o = 2 if i < 2 else 0
                self.modulate_tile(u[:], ht[:], mr[:, o, :], mr[:, o + 1, :], junk[:], self.small[:, 0:1], self.small[:, 1:2])
                self.transpose_tile_to(uT, i * 128, u, 8, cst["ident_f"], self.PS[0], self.PS[1])
            if "uT" in self.dbg and b == 0:
                d = self.dbg_dump("uT", [128, 8, SEQ], BF16)
                self.dma("sp", d[:, :, :], uT[:])
            self.release(mk)
            if self.stop_after == "l0_p0":
                raise StopBuild()
            mk = self.mark()
            W2 = SEQ + 64
            q32 = self.sb("q32", [128, W2], F32)
            sg = self.sb("sg", [128, W2], F32)
            kin = self.sb("kin", [128, W2], F32)
            A = self.sb("A", [128, W2], F32)
            oacc = self.sb("oacc", [128, NT, 128], F32)
            v16 = self.sb("v16", [128, NT, 128], BF16)
            whs = [self.sb(f"wh{i}", [128, 8, 640], BF16) for i in range(2)]
            S32 = self.sb("S32", [128, 128], F32)
            S16 = self.sb("S16", [128, 128], BF16)
            RING = 3
            bts = [self.sb(f"bt{i}", [128, 8], F32) for i in range(RING)]
            decs = [self.sb(f"dec{i}", [128, 1], F32) for i in range(RING)]
            Pks = [[self.sb(f"Pk{r_}{i}", [128, 128], F32) for i in range(5)] for r_ in range(RING)]
            kiA = [self.sb(f"kiA{i}", [128, 128], BF16) for i in range(RING)]
            kiB = [self.sb(f"kiB{i}", [128, 128], BF16) for i in range(RING)]
            kiC = [self.sb(f"kiC{i}", [128, 128], BF16) for i in range(RING)]
            qdx = [self.sb(f"qdx{i}", [128, 64], BF16) for i in range(RING)]
            S16s = [self.sb(f"S16{i}", [128, 128], BF16) for i in range(2)]
            qd = [self.sb(f"qd{i}", [128, 128], BF16) for i in range(RING)]
            qdS = [self.sb(f"qdS{i}", [128, 128], BF16) for i in range(RING)]
            ke = [self.sb(f"ke{i}", [128, 128], BF16) for i in range(RING)]
            att16 = [self.sb(f"att{i}", [128, 128], BF16) for i in range(2)]
            ket16 = [self.sb(f"ket{i}", [128, 128], BF16) for i in range(2)]
            ogs = self.sb("ogs", [128, 128], F32)
            rr = self.sb("rr", [128, 128], F32)
            zero1 = self.sb("zero1", [128, 1], F32)
            self.memset("dve", zero1[:], 0.0)
            groups = [(g * 512, min(512, SEQ - g * 512)) for g in range((SEQ + 511) // 512)]
            for h in range(4):
                wh = whs[h % 2]
                for j, base in enumerate((0, 512, 1024, 1536, 2048)):
                    c0 = base + h * 128
                    self.dma("pool", wh[:, :, j * 128:(j + 1) * 128],
                             w_in[:, c0:c0 + 128].rearrange("(kc p) n -> p kc n", p=128))
                for gi, (g0, gn) in enumerate(groups):
                    ps = self.PS[gi % 2]
                    for kc in range(8):
                        self.mm(ps[:, 0:gn], wh[:, kc, 0:128], uT[:, kc, g0:g0 + gn], start=(kc == 0), stop=(kc == 7))
                    self.act(q32[:, g0:g0 + gn], ps[:, 0:gn], AF.Silu)
                for i in range(NT):
                    ps = self.PS[2 + i % 2]
                    for kc in range(8):
                        self.mm(ps[:, 0:128], uT[:, kc, i * 128:(i + 1) * 128], wh[:, kc, 384:512], start=(kc == 0), stop=(kc == 7))
                    self.copy("dve", v16[:, i, :], ps[:, 0:128])
                for di in range(2):
                    for gi, (g0, gn) in enumerate(groups):
                        ps = self.PS[gi % 2]
                        for kc in range(8):
                            self.mm(ps[:, 0:gn], wh[:, kc, (1 + di) * 128:(2 + di) * 128], uT[:, kc, g0:g0 + gn],
                                    start=(kc == 0), stop=(kc == 7))
                        self.act(sg[:, g0:g0 + gn], ps[:, 0:gn], AF.Sigmoid)
                    lbc = lb[:, di * 4 + h:di * 4 + h + 1]
                    omc = oml[:, di * 4 + h:di * 4 + h + 1]
                    self.ts("dve", sg[:, 0:SEQ], sg[:, 0:SEQ], omc, lbc, ALU.mult, ALU.add)
                    self.ts("pool", kin[:, 0:SEQ], sg[:, 0:SEQ], -1.0, 1.0, ALU.mult, ALU.add)
                    self.act(sg[:, 0:SEQ], sg[:, 0:SEQ], AF.Ln)
                    self.S.op("dve", lambda: nc.vector.tensor_tensor_scan(A[:, 0:SEQ], sg[:, 0:SEQ], sg[:, 0:SEQ], 0.0, ALU.add, ALU.add),
                              reads=[sg], writes=[A])
                    if di == 1:
                        self.stt("dve", sg[:, 0:SEQ], sg[:, 0:SEQ], -2.0, A[:, 0:SEQ], ALU.mult, ALU.add)
                    AA = A if di == 0 else sg
                    order = list(range(NT)) if di == 0 else [1, 0] + list(range(NT - 1, 1, -1))
                    self.memset("dve", S32[:], 0.0)
                    self.memset("pool", S16s[0][:], 0.0)
                    self.memset("pool", S16s[1][:], 0.0)
                    for t_ in kiA + kiB + kiC:
                        self.memset("pool", t_[:], 0.0)
                    mask = cst["mask_f"] if di == 0 else cst["mask_b"]
                    lo, hi = slice(0, 64), slice(64, 128)

                    def stageA(n, i):
                        c0, c1 = i * 128, (i + 1) * 128
                        r = n % RING
                        bt_, Pk_ = bts[r], Pks[r]
                        E0 = A[:, c0 - 1:c0] if c0 > 0 else zero1[:, 0:1]
                        E1 = A[:, c1 - 1:c1]
                        R0 = AA[:, c0 + 31:c0 + 32]
                        R1 = AA[:, c0 + 95:c0 + 96]
                        Rx = AA[:, c0 + 63:c0 + 64] if di == 0 else AA[:, c0 + 64:c0 + 65]
                        for bc, (src_, sgn) in enumerate(((R0, -0.5), (R0, 0.5), (R1, -0.5), (R1, 0.5), (Rx, -0.5), (Rx, 0.5),
                                                         (E0, -0.5), (E1, 0.5))):
                            self.ts("pool", bt_[:, bc:bc + 1], src_, sgn, None, ALU.mult)
                        Alo, Ahi, Acol = AA[:, c0:c0 + 64], AA[:, c0 + 64:c1], AA[:, c0:c1]
                        if di == 0:
                            self.act(Pk_[0][:, lo], Alo, AF.Exp, bias=bt_[:, 0:1], scale=0.5)
                            self.act(Pk_[0][:, hi], Ahi, AF.Exp, bias=bt_[:, 2:3], scale=0.5)
                            self.act(Pk_[1][:, lo], Alo, AF.Exp, bias=bt_[:, 1:2], scale=-0.5)
                            self.act(Pk_[1][:, hi], Ahi, AF.Exp, bias=bt_[:, 3:4], scale=-0.5)
                            self.act(Pk_[2][:], Acol, AF.Exp, bias=bt_[:, 6:7], scale=0.5)
                            self.act(Pk_[3][:], Acol, AF.Exp, bias=bt_[:, 7:8], scale=-0.5)
                            self.act(Pk_[4][:, lo], Ahi, AF.Exp, bias=bt_[:, 4:5], scale=0.5)
                            self.act(Pk_[4][:, hi], Alo, AF.Exp, bias=bt_[:, 5:6], scale=-0.5)
                            qx_cols, kx_cols = hi, lo
                        else:
                            self.act(Pk_[0][:, lo], Alo, AF.Exp, bias=bt_[:, 1:2], scale=-0.5)
                            self.act(Pk_[0][:, hi], Ahi, AF.Exp, bias=bt_[:, 3:4], scale=-0.5)
                            self.act(Pk_[1][:, lo], Alo, AF.Exp, bias=bt_[:, 0:1], scale=0.5)
                            self.act(Pk_[1][:, hi], Ahi, AF.Exp, bias=bt_[:, 2:3], scale=0.5)
                            self.act(Pk_[2][:], Acol, AF.Exp, bias=bt_[:, 7:8], scale=-0.5)
                            self.act(Pk_[3][:], Acol, AF.Exp, bias=bt_[:, 6:7], scale=0.5)
                            self.act(Pk_[4][:, lo], Alo, AF.Exp, bias=bt_[:, 5:6], scale=-0.5)
                            self.act(Pk_[4][:, hi], Ahi, AF.Exp, bias=bt_[:, 4:5], scale=0.5)
                            qx_cols, kx_cols = lo, hi
                        qcs = slice(c0 + qx_cols.start, c0 + qx_cols.stop)
                        kcs = slice(c0 + kx_cols.start, c0 + kx_cols.stop)
                        self.tt("dve", qd[r][:], q32[:, c0:c1], Pk_[0][:], ALU.mult)
                        self.tt("pool", kiA[r][:, lo], kin[:, c0:c0 + 64], Pk_[1][:, lo], ALU.mult)
                        self.tt("pool", kiB[r][:, hi], kin[:, c0 + 64:c1], Pk_[1][:, hi], ALU.mult)
                        self.tt("dve", qdS[r][:], q32[:, c0:c1], Pk_[2][:], ALU.mult)
                        self.tt("pool", ke[r][:], kin[:, c0:c1], Pk_[3][:], ALU.mult)
                        self.tt("dve", qdx[r][:], q32[:, qcs], Pk_[4][:, lo], ALU.mult)
                        self.tt("pool", kiC[r][:, kx_cols], kin[:, kcs], Pk_[4][:, hi], ALU.mult)
                        self.copy("pool", decs[r][:], Pk_[2][:, 127:128] if di == 0 else Pk_[2][:, 0:1])

                    def stageB(n, i):
                        r = n % RING
                        p = n % 2
                        pa, po, pS = self.PS[0 + p], self.PS[2 + p], self.PS[4 + p]
                        pbt = self.PB[p]
                        if di == 0:
                            self.mm(pa[:, 0:64], kiA[r][:], qd[r][:, lo], start=True, stop=True)
                            self.mm(pa[:, 64:128], kiB[r][:], qd[r][:, hi], start=True, stop=False)
                            self.mm(pa[:, 64:128], kiC[r][:], qdx[r][:], start=False, stop=True)
                        else:
                            self.mm(pa[:, 0:64], kiA[r][:], qd[r][:, lo], start=True, stop=False)
                            self.mm(pa[:, 0:64], kiC[r][:], qdx[r][:], start=False, stop=True)
                            self.mm(pa[:, 64:128], kiB[r][:], qd[r][:, hi], start=True, stop=True)
                        self.tr(pbt[:, 0:128], ke[r][:], cst["ident_b"][:])
                        self.tt("dve", att16[p][:], pa[:, 0:128], mask[:], ALU.mult)
                        self.copy("act", ket16[p][:], pbt[:, 0:128])
                        self.mm(pS[:, 0:128], ket16[p][:], v16[:, i, :])
                        self.mm(po[:, 0:128], att16[p][:], v16[:, i, :], start=True, stop=False)
                        self.mm(po[:, 0:128], qdS[r][:], S16s[(n + 1) % 2][:], start=False, stop=True)
                        self.stt("dve", S32[:], S32[:], decs[r][:, 0:1], pS[:, 0:128], ALU.mult, ALU.add)
                        self.copy("act", S16s[n % 2][:], S32[:])
                        if di == 0:
                            self.copy("act", oacc[:, i, :], po[:, 0:128])
                        else:
                            self.tt("dve", oacc[:, i, :], oacc[:, i, :], po[:, 0:128], ALU.add)

                    LOOK = 2
                    for n in range(min(LOOK, NT)):
                        stageA(n, order[n])
                    for n, i in enumerate(order):
                        if n + LOOK < NT:
                            stageA(n + LOOK, order[n + LOOK])
                        stageB(n, i)
                for i in range(NT):
                    ps = self.PS[3 + i % 2]
                    for kc in range(8):
                        self.mm(ps[:, 0:128], uT[:, kc, i * 128:(i + 1) * 128], wh[:, kc, 512:640], start=(kc == 0), stop=(kc == 7))
                    self.act(ogs[:], ps[:, 0:128], AF.Silu)
                    self.act(rr[:], oacc[:, i, :], AF.Square, accum_out=self.small[:, 2:3])
                    self.rstd_from_ss(self.small[:, 3:4], self.small[:, 2:3], 128)
                    self.stt("dve", rr[:], oacc[:, i, :], self.small[:, 3:4], nwb[:], ALU.mult, ALU.mult)
                    self.tt("dve", rr[:], rr[:], ogs[:], ALU.mult)
                    pt = self.PS[5]
                    self.tr(pt[:, (i % 4) * 128:(i % 4 + 1) * 128], rr[:], cst["ident_f"][:])
                    self.copy("act", mixT[:, h, i * 128:(i + 1) * 128], pt[:, (i % 4) * 128:(i % 4 + 1) * 128])
            if self.stop_after == "l0_p1":
                raise StopBuild()
            wp = whs[0]
            self.dma("pool", wp[:, :, 0:512], w_in[:, 2560:3072].rearrange("(kc p) n -> p kc n", p=128))
            OFFC, OFFL = 16, 16 + CTX + 16
            WB = OFFL + LAT + 16
            d16 = self.sb("d16", [128, SEQ], BF16)
            for g in range(4):
                w = 2 << g
                half = w // 2
                PBf, T1, T2 = q32, kin, A
                self.memset("pool", PBf[:, 0:WB], 0.0)
                for gi, (g0, gn) in enumerate(groups):
                    ps = self.PS[gi % 2]
                    for kc in range(8):
                        self.mm(ps[:, 0:gn], wp[:, kc, g * 128:(g + 1) * 128], uT[:, kc, g0:g0 + gn], start=(kc == 0), stop=(kc == 7))
                    a0, a1 = g0, g0 + gn
                    if a0 < CTX:
                        n_c = min(a1, CTX) - a0
                        self.copy("act", PBf[:, OFFC + a0:OFFC + a0 + n_c], ps[:, 0:n_c])
                        if a1 > CTX:
                            self.copy("act", PBf[:, OFFL:OFFL + (a1 - CTX)], ps[:, n_c:gn])
                    else:
                        self.copy("act", PBf[:, OFFL + a0 - CTX:OFFL + a1 - CTX], ps[:, 0:gn])
                cur = PBf
                step = 1
                tmp = [T1, T2]
                ti = 0
                width = WB
                while step < w:
                    nxt = tmp[ti % 2]
                    ti += 1
                    width2 = width - step
                    self.tt("dve", nxt[:, 0:width2], cur[:, 0:width2], cur[:, step:step + width2], ALU.add)
                    cur, width, step = nxt, width2, step * 2
                dst = tmp[ti % 2]
                for off, L, tcol in ((OFFC, CTX, 0), (OFFL, LAT, CTX)):
                    sw = cur[:, off - half:off - half + L]
                    self.tt("pool", sw[:, 0:8], sw[:, 0:8], facS[:, g, :], ALU.mult, reads=[cur, facS], writes=[cur])
                    self.tt("pool", sw[:, L - 8:L], sw[:, L - 8:L], facE[:, g, :], ALU.mult, reads=[cur, facE], writes=[cur])
                    self.stt("dve", d16[:, tcol:tcol + L], sw, 1.0 / w, PBf[:, off:off + L], ALU.mult, ALU.subtract,
                             reads=[cur, PBf], writes=[d16])
                for gi, (g0, gn) in enumerate(groups):
                    ps = self.PS[2 + gi % 2]
                    self.mm(ps[:, 0:gn], poolw[:, g, :], d16[:, g0:g0 + gn])
                    self.ts("dve", mixT[:, 4 + g, g0:g0 + gn], ps[:, 0:gn], pscale[:, g:g + 1], None, ALU.mult)
            if "mixT" in self.dbg and b == 0:
                d = self.dbg_dump("mixT", [128, 8, SEQ], BF16)
                self.dma("sp", d[:, :, :], mixT[:])
            self.release(mk)
            if self.stop_after == "l0_p3":
                raise StopBuild()
            self.post_mixer(0, b, mixT, NT, wr, brr)
        self.release(mk0)

    def rms_rows(self, out, src, n, gain_b, sq_junk, c0):
        sm = self.small
        self.act(sq_junk, src, AF.Square, accum_out=sm[:, c0:c0 + 1])
        self.rstd_from_ss(sm[:, c0 + 1:c0 + 2], sm[:, c0:c0 + 1], n)
        self.stt("dve", out, src, sm[:, c0 + 1:c0 + 2], gain_b, ALU.mult, ALU.mult)

    def head_norm(self, xf, gain_b, tmp, hs, rs):
        self.tt("dve", tmp[:], xf[:], xf[:], ALU.mult)
        self.v("dve", "reduce_sum", hs[:], tmp[:], AX.X, reads=[tmp], writes=[hs])
        self.ts("dve", rs[:], hs[:], 1.0 / 96, EPS, ALU.mult, ALU.add)
        self.act(rs[:], rs[:], AF.Sqrt)
        self.v("dve", "reciprocal", rs[:], rs[:], reads=[rs], writes=[rs])
        for h in range(8):
            self.stt("dve", xf[:, h, :], xf[:, h, :], rs[:, h:h + 1], gain_b[:], ALU.mult, ALU.mult,
                     reads=[xf, rs, gain_b], writes=[xf])

    def rope(self, dst, src, cosr, sinr, t1, t2, eng):
        s4 = src.rearrange("p (a h i) -> p a h i", a=2, h=2)
        d4 = dst.rearrange("p (a h i) -> p a h i", a=2, h=2)
        c3 = cosr.rearrange("p (a i) -> p a i", a=2)
        s3 = sinr.rearrange("p (a i) -> p a i", a=2)
        a3 = t1.rearrange("p (a i) -> p a i", a=2)
        b3 = t2.rearrange("p (a i) -> p a i", a=2)
        x1, x2 = s4[:, :, 0, :], s4[:, :, 1, :]
        self.tt(eng, a3, x1, c3, ALU.mult, reads=[src, cosr], writes=[t1])
        self.tt(eng, b3, x2, s3, ALU.mult, reads=[src, sinr], writes=[t2])
        self.tt(eng, d4[:, :, 0, :], a3, b3, ALU.subtract, reads=[t1, t2], writes=[dst])
        self.tt(eng, a3, x2, c3, ALU.mult, reads=[src, cosr], writes=[t1])
        self.tt(eng, b3, x1, s3, ALU.mult, reads=[src, sinr], writes=[t2])
        self.tt(eng, d4[:, :, 1, :], a3, b3, ALU.add, reads=[t1, t2], writes=[dst])

    def layer1(self):
        nc, S, I, C = self.nc, self.S, self.I, self.C
        ns, cst = self.ns, self.cst
        mk0 = self.mark()
        w_in = I["od_w_in"][0]
        NL = LAT // 128
        wr = self.sb("wr1", [128, 8, 32], F32)
        self.dma("sp", wr[:], I["moe_router_w"][1].rearrange("(kc p) e -> p kc e", p=128))
        brr = self.sb("brr1", [1, 32], F32)
        self.dma("sp", brr[:], I["moe_router_b"][1:2, :])
        rcos = self.sb("rcos", [128, 16, 16], F32)
        rsin = self.sb("rsin", [128, 16, 16], F32)
        self.dma("sp", rcos[:], C["rope_cos"])
        self.dma("sp", rsin[:], C["rope_sin"])
        dww = self.sb("dww", [128, 4, 31], F32)
        dwr = self.sb("dwr", [32, 512], F32)
        self.dma("sp", dwr[0:31, :], I["conv_dw_w"][0])
        for cc in range(4):
            self.tr(self.PS[0][:, cc * 32:cc * 32 + 31], dwr[0:31, cc * 128:(cc + 1) * 128], self.cst["ident_f"][0:31, 0:31])
            self.copy("dve", dww[:, cc, :], self.PS[0][:, cc * 32:cc * 32 + 31])
        cpar = self.sb("cpar", [128, 3, 4], F32)
        for j, nm in enumerate(("conv_dw_b", "conv_ln_w", "conv_ln_b")):
            self.dma("sp", cpar[:, j, :], I[nm][0].rearrange("(c p) -> p c", p=128), allow_slow_non_contiguous=True)
        qan = self.sb("qan", [128, 384], F32)
        kvan = self.sb("kvan", [128, 256], F32)
        qnb = self.sb("qnb", [128, 96], F32)
        knb = self.sb("knb", [128, 96], F32)
        self.dma("sp", qan[:], I["mla_q_a_norm"][0:1, :].partition_broadcast(128))
        self.dma("sp", kvan[:], I["mla_kv_a_norm"][0:1, :].partition_broadcast(128))
        self.dma("sp", qnb[:], I["mla_q_norm"][0:1, :].partition_broadcast(128))
        self.dma("sp", knb[:], I["mla_k_norm"][0:1, :].partition_broadcast(128))
        wq = self.sb("wq", [128, 8, 384], BF16)
        wkv = self.sb("wkv", [128, 8, 288], BF16)
        wuq = self.sb("wuq", [128, 3, 768], BF16)
        wukv = self.sb("wukv", [128, 2, 1024], BF16)
        self.dma("pool", wq[:], w_in[:, 1024:1408].rearrange("(kc p) n -> p kc n", p=128))
        self.dma("pool", wkv[:], w_in[:, 1408:1696].rearrange("(kc p) n -> p kc n", p=128))
        self.dma("pool", wuq[:], I["mla_w_uq"][0].rearrange("(kc p) n -> p kc n", p=128))
        self.dma("pool", wukv[:], I["mla_w_ukv"][0].rearrange("(kc p) n -> p kc n", p=128))
        mixA = self.sb("mixA", [128, 4, LAT], BF16)
        sm = self.small
        SC = 96 ** -0.5
        for b in range(ns):
            mkb = self.mark()

            def make_uT(per_tile=None):
                uT = self.sb("uT1", [128, 8, SEQ], BF16) if per_tile is None else None
                mk = self.mark()
                uTts = [self.sb(f"uTt{i}", [128, 8, 128], BF16) for i in range(2)] if per_tile is not None else None
                mr = self.sb("mr", [128, 4, D], F32)
                self.modrow(mr[:, 0, :], 1, b, 1)
                self.modrow(mr[:, 1, :], 1, b, 0)
                self.modrow(mr[:, 2, :], 1, 4, 1)
                self.modrow(mr[:, 3, :], 1, 4, 0)
                self.ts("dve", mr[:, 0, :], mr[:, 0, :], 1.0, None, ALU.add)
                self.ts("dve", mr[:, 2, :], mr[:, 2, :], 1.0, None, ALU.add)
                hts = [self.sb(f"ht{i}", [128, D], F32) for i in range(2)]
                us = [self.sb(f"u{i}", [128, D], F32) for i in range(1 if per_tile is not None else 2)]
                for i in range(NT):
                    ht, u = hts[i % 2], us[i % len(us)]
                    self.dma("sp", ht[:], self.H2[b * SEQ + i * 128:b * SEQ + (i + 1) * 128, :], reads=[self.H2])
                    o = 2 if i < 2 else 0
                    self.modulate_tile(u[:], ht[:], mr[:, o, :], mr[:, o + 1, :], u[:], sm[:, 0:1], sm[:, 1:2])
                    if per_tile is None:
                        self.transpose_tile_to(uT, i * 128, u, 8, cst["ident_f"], self.PS[0], self.PS[1])
                    else:
                        self.transpose_tile_to(uTts[i % 2], 0, u, 8, cst["ident_f"], self.PS[0], self.PS[1])
                        per_tile(i, uTts[i % 2])
                self.release(mk)
                return uT

            mkA = self.mark()
            uT = make_uT()
            mk = self.mark()
            wc = self.sb("wc", [128, 8, 1024], BF16)
            self.dma("pool", wc[:], w_in[:, 0:1024].rearrange("(kc p) n -> p kc n", p=128))
            hb = self.sb("hb", [128, LAT + 32], F32)
            cv = [self.sb(f"cv{i}", [128, LAT], F32) for i in range(4)]
            sgt = [self.sb(f"sgt{i}", [128, 512], F32) for i in range(2)]
            self.memset("pool", hb[:], 0.0)
            for cc in range(4):
                for tg in range(4):
                    pv, pg = self.PS[(tg % 2) * 2], self.PS[(tg % 2) * 2 + 1]
                    cols = slice(CTX + tg * 512, CTX + (tg + 1) * 512)
                    for kc in range(8):
                        self.mm(pv[:], wc[:, kc, cc * 128:(cc + 1) * 128], uT[:, kc, cols], start=(kc == 0), stop=(kc == 7))
                    for kc in range(8):
                        self.mm(pg[:], wc[:, kc, 512 + cc * 128:512 + (cc + 1) * 128], uT[:, kc, cols], start=(kc == 0), stop=(kc == 7))
                    st = sgt[tg % 2]
                    self.act(st[:], pg[:], AF.Sigmoid)
                    self.tt("dve", hb[:, 15 + tg * 512:15 + (tg + 1) * 512], pv[:], st[:], ALU.mult)
                NH = 4
                HW_ = LAT // NH
                for j in range(31):
                    for hf in range(NH):
                        o0 = hf * HW_
                        acc = cv[cc][:, o0:o0 + HW_]
                        if j == 0:
                            self.ts("dve", acc, hb[:, o0:o0 + HW_], dww[:, cc, 0:1], cpar[:, 0, cc:cc + 1], ALU.mult, ALU.add,
                                    reads=[hb, dww, cpar], writes=[(cv[cc], hf)])
                        else:
                            self.stt("dve", acc, hb[:, o0 + j:o0 + j + HW_], dww[:, cc, j:j + 1], acc, ALU.mult, ALU.add,
                                     reads=[hb, dww, (cv[cc], hf)], writes=[(cv[cc], hf)])
            mean = self.sb("lnm", [128, 512], F32)
            rstd = self.sb("lnr", [128, 512], F32)
            sq = self.sb("lnsq", [128, 512], F32)
            xn = [self.sb(f"lnx{i}", [128, 512], F32) for i in range(2)]
            for tg in range(4):
                cols = slice(tg * 512, (tg + 1) * 512)
                ps_s, ps_q = self.PS[4], self.PS[5]
                for cc in range(4):
                    self.mm(ps_s[:], cst["ones_f"][:], cv[cc][:, cols], start=(cc == 0), stop=(cc == 3))
                for cc in range(4):
                    self.act(sq[:], cv[cc][:, cols], AF.Square)
                    self.mm(ps_q[:], cst["ones_f"][:], sq[:], start=(cc == 0), stop=(cc == 3))
                self.ts("dve", mean[:], ps_s[:], 1.0 / 512, None, ALU.mult)
                self.tt("dve", rstd[:], mean[:], mean[:], ALU.mult)
                self.stt("dve", rstd[:], ps_q[:], 1.0 / 512, rstd[:], ALU.mult, ALU.subtract)
                self.ts("dve", rstd[:], rstd[:], EPS, None, ALU.add)
                self.act(rstd[:], rstd[:], AF.Sqrt)
                self.v("dve", "reciprocal", rstd[:], rstd[:], reads=[rstd], writes=[rstd])
                for cc in range(4):
                    x = xn[cc % 2]
                    self.tt("pool", x[:], cv[cc][:, cols], mean[:], ALU.subtract)
                    self.tt("dve", x[:], x[:], rstd[:], ALU.mult)
                    self.act(mixA[:, cc, cols], x[:], AF.Silu, bias=cpar[:, 2, cc:cc + 1], scale=cpar[:, 1, cc:cc + 1])
            self.release(mkA)
            if self.stop_after == "l1_conv":
                raise StopBuild()
            mixB = self.sb("mixB", [128, 4, LAT], BF16)
            mkq = self.mark()
            qT = self.sb("qT", [128, 8, LAT], BF16)
            kT = self.sb("kT", [128, 8, SEQ], BF16)
            vaug = self.sb("vaug", [128, NT, 8, 68], BF16)
            mk = self.mark()
            self.memset("pool", vaug[:], 1.0)
            cn = self.sb("cn", [128, 384], F32)
            cnT = self.sb("cnT", [128, 3, 128], BF16)
            xf = self.sb("xf", [128, 8, 96], F32)
            xo = self.sb("xo", [128, 8, 96], F32)
            tmp = self.sb("hn_tmp", [128, 8, 96], F32)
            hs = self.sb("hn_hs", [128, 8], F32)
            rs = self.sb("hn_rs", [128, 8], F32)
            t1 = [self.sb(f"rt1{i}", [128, 16], F32) for i in range(2)]
            t2 = [self.sb(f"rt2{i}", [128, 16], F32) for i in range(2)]
            krr = self.sb("krr", [128, 32], F32)
            krg = self.sb("krg", [128, 32], F32)
            junk2 = self.sb("junk2", [128, 384], F32)
            def proj_tile(i, uTt):
                import os as _os
                _stg = float(_os.environ.get("KSTG", "9"))
                if _stg <= 0:
                    return
                lat_i = i - 2
                tok = slice(i * 128, (i + 1) * 128)
                pk = self.PS[2]
                for kc in range(8):
                    self.mm(pk[:, 0:288], uTt[:, kc, :], wkv[:, kc, :], start=(kc == 0), stop=(kc == 7))
                if _stg <= 0.1:
                    return
                self.rms_rows(cn[:, 0:256], pk[:, 0:256], 256, kvan[:], junk2[:, 0:256], 10)
                if _stg <= 0.2:
                    return
                self.tt("dve", krg[:], pk[:, 256:288], knb[:, 64:96], ALU.mult)
                if _stg <= 0.3:
                    return
                pt = self.PS[3]
                for kc in range(2):
                    self.tr(pt[:, kc * 128:(kc + 1) * 128], cn[:, kc * 128:(kc + 1) * 128], cst["ident_f"][:])
                self.copy("act", cnT[:, 0:2, :], pt[:, 0:256].rearrange("p (k c) -> p k c", c=128))
                if _stg <= 0.4:
                    return
                pkv = [self.PS[4], self.PS[5]]
                for half in range(2):
                    for kc in range(2):
                        self.mm(pkv[half][:], cnT[:, kc, :], wukv[:, kc, half * 512:(half + 1) * 512], start=(kc == 0), stop=(kc == 1))
                    v4 = pkv[half][:, :].rearrange("p (h d) -> p h d", d=128)
                    if _stg <= 0.5:
                        continue
                    if _stg != 0.56:
                        self.copy("act", xf[:, half * 4:(half + 1) * 4, 0:64], v4[:, :, 0:64])
                    if _stg != 0.55:
                        self.copy("act" if _stg == 0.57 else "dve", vaug[:, i, half * 4:(half + 1) * 4, 0:64], v4[:, :, 64:128])
                if _stg <= 0.6:
                    return
                for h in range(8):
                    self.copy("pool", xf[:, h, 64:96], pk[:, 256:288]) if False else self.copy("dve" if h % 2 else "act", xf[:, h, 64:96], pk[:, 256:288])
                if _stg <= 1:
                    return
                self.head_norm(xf, knb, tmp, hs, rs)
                if _stg <= 2:
                    return
                if lat_i >= 0:
                    for h in range(8):
                        self.rope(xo[:, h, 64:96], xf[:, h, 64:96], rcos[:, lat_i, :], rsin[:, lat_i, :], t1[h % 2][:], t2[h % 2][:],
                                  "dve" if h % 2 == 0 else "pool")
                    self.copy("act", xo[:, :, 0:64], xf[:, :, 0:64])
                    src = xo
                else:
                    src = xf
                pa, pb_ = self.PS[4], self.PS[5]
                for h in range(8):
                    pp = pa if h < 4 else pb_
                    self.tr(pp[0:96, (h % 4) * 128:(h % 4 + 1) * 128], src[:, h, :], cst["ident_f"][:])
                self.copy("act", kT[0:96, 0:4, tok], pa[0:96, :].rearrange("p (k c) -> p k c", c=128))
                self.copy("dve", kT[0:96, 4:8, tok], pb_[0:96, :].rearrange("p (k c) -> p k c", c=128))
                if lat_i < 0 or _stg <= 3:
                    return
                ltok = slice(lat_i * 128, (lat_i + 1) * 128)
                pq = self.PS[2]
                for kc in range(8):
                    self.mm(pq[:, 0:384], uTt[:, kc, :], wq[:, kc, :], start=(kc == 0), stop=(kc == 7))
                if _stg <= 3.1:
                    return
                self.rms_rows(cn[:, 0:384], pq[:, 0:384], 384, qan[:], junk2[:, 0:384], 12)
                if _stg <= 3.2:
                    return
                pt = self.PS[3]
                for kc in range(3):
                    self.tr(pt[:, kc * 128:(kc + 1) * 128], cn[:, kc * 128:(kc + 1) * 128], cst["ident_f"][:])
                self.copy("act", cnT[:, 0:3, :], pt[:, 0:384].rearrange("p (k c) -> p k c", c=128))
                if _stg <= 3.3:
                    return
                pq1, pq2 = self.PS[4], self.PS[5]
                for kc in range(3):
                    self.mm(pq1[:, 0:384], cnT[:, kc, :], wuq[:, kc, 0:384], start=(kc == 0), stop=(kc == 2))
                for kc in range(3):
                    self.mm(pq2[:, 0:384], cnT[:, kc, :], wuq[:, kc, 384:768], start=(kc == 0), stop=(kc == 2))
                if _stg <= 3.4:
                    return
                self.copy("act", xf[:, 0:4, :], pq1[:, 0:384].rearrange("p (h d) -> p h d", d=96))
                self.copy("dve", xf[:, 4:8, :], pq2[:, 0:384].rearrange("p (h d) -> p h d", d=96))
                if _stg <= 3.5:
                    return
                self.head_norm(xf, qnb, tmp, hs, rs)
                if _stg <= 3.6:
                    return
                for h in range(8):
                    self.rope(xo[:, h, 64:96], xf[:, h, 64:96], rcos[:, lat_i, :], rsin[:, lat_i, :], t1[h % 2][:], t2[h % 2][:],
                              "dve" if h % 2 == 0 else "pool")
                if _stg <= 3.7:
                    return
                self.copy("act", xo[:, :, 0:64], xf[:, :, 0:64])
                for h in range(8):
                    pp = pa if h < 4 else pb_
                    self.tr(pp[0:96, (h % 4) * 128:(h % 4 + 1) * 128], xo[:, h, :], cst["ident_f"][:])
                if _stg <= 3.8:
                    return
                self.copy("act", qT[0:96, 0:4, ltok], pa[0:96, :].rearrange("p (k c) -> p k c", c=128))
                self.copy("dve", qT[0:96, 4:8, ltok], pb_[0:96, :].rearrange("p (k c) -> p k c", c=128))

            make_uT(per_tile=proj_tile)
            self.release(mk)
            if self.stop_after == "l1_proj":
                raise StopBuild()
            mk = self.mark()
            attn = self.sb("attn", [128, NL, 512], BF16)
            PT = [self.sb(f"PT{i}", [128, 512], BF16) for i in range(3)]
            rden = self.sb("rden", [128, 4], F32)
            n = 0
            for h in range(8):
                for qg in range(4):
                    qcols = slice(qg * 512, (qg + 1) * 512)
                    po = self.PS[4 + (h * 4 + qg) % 2]
                    def s_mm(kt_, n_):
                        ps_ = self.PS[n_ % 4]
                        self.mm(ps_[:], kT[0:96, h, kt_ * 128:(kt_ + 1) * 128], qT[0:96, h, qcols])
                        self.act(PT[n_ % 3][:], ps_[:], AF.Exp, scale=SC)

                    s_mm(0, n)
                    for kt in range(NT):
                        if kt + 1 < NT:
                            s_mm(kt + 1, n + 1)
                        p_ = PT[n % 3]
                        for qt in range(4):
                            self.mm(po[:, qt * 68:(qt + 1) * 68], p_[:, qt * 128:(qt + 1) * 128], vaug[:, kt, h, :],
                                    start=(kt == 0), stop=(kt == NT - 1))
                        n += 1
                    for qt in range(4):
                        self.v("dve", "reciprocal", rden[:, qt:qt + 1], po[:, qt * 68 + 64:qt * 68 + 65], reads=[po], writes=[rden])
                        self.ts("dve", attn[:, qg * 4 + qt, h * 64:(h + 1) * 64], po[:, qt * 68:qt * 68 + 64], rden[:, qt:qt + 1], None, ALU.mult,
                                reads=[po, rden], writes=[(attn, qg * 4 + qt)])
            for i in range(NL):
                pb = self.PB[i % 2]
                for c in range(4):
                    self.tr(pb[:, c * 128:(c + 1) * 128], attn[:, i, c * 128:(c + 1) * 128], cst["ident_b"][:])
                self.copy("act" if i % 2 == 0 else "dve", mixB[:, :, i * 128:(i + 1) * 128], pb[:, 0:512].rearrange("p (k c) -> p k c", c=128))
            self.release(mkq)
            if self.stop_after == "l1_attn":
                raise StopBuild()
            self.post_mixer(1, b, [mixA, mixB], NL, wr, brr)
            self.release(mkb)
        self.release(mk0)

    def post_mixer(self, layer, b, mixT, ntile, wr, brr):
        nc, S, I, C = self.nc, self.S, self.I, self.C
        cst = self.cst
        mk = self.mark()
        w_out = I["ev_w_out"][0] if layer == 0 else I["od_w_out"][0]
        wo = self.sb("wo", [128, 8, D], BF16)
        self.dma("pool", wo[:], w_out.rearrange("(kc p) n -> p kc n", p=128))
        mr = self.sb("mr2", [128, 6, D], F32)
        self.modrow(mr[:, 0, :], layer, b, 2)
        self.modrow(mr[:, 1, :], layer, b, 4)
        self.modrow(mr[:, 2, :], layer, b, 3)
        self.ts("dve", mr[:, 1, :], mr[:, 1, :], 1.0, None, ALU.add)
        if layer == 0:
            self.modrow(mr[:, 3, :], layer, 4, 2)
            self.modrow(mr[:, 4, :], layer, 4, 4)
            self.modrow(mr[:, 5, :], layer, 4, 3)
            self.ts("dve", mr[:, 4, :], mr[:, 4, :], 1.0, None, ALU.add)
        hts = [self.sb(f"pht{i}", [128, D], F32) for i in range(2)]
        h1s = [self.sb(f"ph1{i}", [128, D], F32) for i in range(2)]
        ms = [self.sb(f"pm{i}", [128, D], F32) for i in range(2)]
        m16 = [self.sb(f"pm16{i}", [128, D], BF16) for i in range(2)]
        mT = self.sb("pmT", [128, 8, 128], F32)
        junk = self.sb("pjunk", [128, D], F32)
        sm = self.small
        for i in range(ntile):
            if layer == 0:
                isctx = i < 2
                src = I["ctx"][b, i * 128:(i + 1) * 128, :] if isctx else I["x"][b, (i - 2) * 128:(i - 1) * 128, :]
                grow = b * SEQ + i * 128
                hdst = self.H1
            else:
                isctx = False
                grow = b * LAT + i * 128
                src = self.H2[b * SEQ + CTX + i * 128:b * SEQ + CTX + (i + 1) * 128, :]
                hdst = self.H1
            o = 3 if isctx else 0
            ht, h1, m, mb = hts[i % 2], h1s[i % 2], ms[i % 2], m16[i % 2]
            self.dma("sp", ht[:], src)
            p0, p1 = self.PS[0 + 2 * (i % 2)], self.PS[1 + 2 * (i % 2)]
            for half, ps in enumerate((p0, p1)):
                for kc in range(8):
                    mx = mixT[kc // 4] if isinstance(mixT, (list, tuple)) else mixT
                    kcc = kc % 4 if isinstance(mixT, (list, tuple)) else kc
                    self.mm(ps[:], mx[:, kcc, i * 128:(i + 1) * 128], wo[:, kc, half * 512:(half + 1) * 512],
                            start=(kc == 0), stop=(kc == 7))
                self.tt("dve", h1[:, half * 512:(half + 1) * 512], ps[:], mr[:, o, half * 512:(half + 1) * 512], ALU.mult)
            self.tt("pool", h1[:], h1[:], ht[:], ALU.add)
            self.dma("sp", hdst[grow:grow + 128, :], h1[:], writes=[(hdst, grow)])
            self.modulate_tile(m[:], h1[:], mr[:, o + 1, :], mr[:, o + 2, :], junk[:], sm[:, 4:5], sm[:, 5:6])
            self.copy("act", mb[:], m[:])
            self.dma("sp", self.M[grow:grow + 128, :], mb[:], writes=[(self.M, grow)])
            self.transpose_tile_to(mT, 0, m, 8, cst["ident_f"], self.PS[4], self.PS[5])
            pl = self.PS[4]
            for kc in range(8):
                self.mm(pl[:, 0:32], mT[:, kc, :], wr[:, kc, :], start=(kc == 0), stop=False)
            self.mm(pl[:, 0:32], cst["ones_f"][0:1, :], brr[0:1, :], start=False, stop=True)
            self.route_tile(layer, grow // 128, pl)
        self.release(mk)

    def route_init(self, layer):
        ns = self.ns
        T = self.T0 if layer == 0 else self.T1
        self.rT = T
        CAP = self.CAP = CAPS[layer]
        NSLOT = self.NSLOT = NE * CAP
        ntt = T // 128
        self.cntb = self.sb(f"cntb{layer}", [128, NE], F32)
        self.DEST = self.sb(f"DEST{layer}", [128, ntt, 4], I32)
        self.GATE = self.sb(f"GATE{layer}", [128, ntt, 4], F32)
        self.rt = {n: self.sb(f"rt_{n}{layer}", sh, dt) for n, sh, dt in (
            ("L", [128, NE], F32), ("t8", [128, 8], F32), ("e4", [128, 4], F32), ("mask", [128, NE], F32),
            ("m16", [128, NE], BF16), ("rf", [128, NE], F32), ("rfs", [128, NE], F32), ("tmp", [128, NE], F32),
            ("rk", [128, 4], F32), ("dk", [128, 4], F32), ("ok", [128, 4], F32), ("tok", [128, 2], F32),
            ("toki", [128, 2], I32), ("trash", [128, 1], F32), ("fill", [128, (NSLOT + 128) // 64], I32),
            ("z16", [128, D], BF16))}
        rt = self.rt
        self.memset("dve", self.cntb[:], 0.0)
        self.ts("dve", rt["trash"][:], self.cst["pidx"][:], float(NSLOT), None, ALU.add)
        self.memset("dve", rt["fill"][:], int(T))
        self.dma("sp", self.SLOT[0:NSLOT + 128, :].rearrange("(p j) two -> p (j two)", p=128), rt["fill"][:])
        self.memset("pool", rt["z16"][:], 0.0)
        self.dma("sp", self.M[T:T + 128, :], rt["z16"][:], writes=[(self.M, "trash")])
        self.dma("sp", self.YB[NSLOT:NSLOT + 128, :], rt["z16"][:], writes=[(self.YB, "trash")])

    def route_tile(self, layer, gt, pl):
        nc, rt, cst = self.nc, self.rt, self.cst
        CAP, NSLOT = self.CAP, self.NSLOT
        L, t8 = rt["L"], rt["t8"]
        self.copy("dve", L[:], pl[:, 0:NE])
        self.v("dve", "max", t8[:], L[:], reads=[L], writes=[t8])
        sm = self.small
        self.ts("dve", sm[:, 8:9], t8[:, 0:1], -1.0, None, ALU.mult)
        self.act(rt["e4"][:], t8[:, 0:4], AF.Exp, bias=sm[:, 8:9], scale=1.0)
        self.v("dve", "reduce_sum", sm[:, 9:10], rt["e4"][:], AX.X, reads=[rt["e4"]], writes=[sm])
        self.v("dve", "reciprocal", sm[:, 9:10], sm[:, 9:10], reads=[sm], writes=[sm])
        self.ts("dve", rt["e4"][:], rt["e4"][:], sm[:, 9:10], None, ALU.mult)
        self.ts("dve", rt["mask"][:], L[:], t8[:, 3:4], None, ALU.is_ge)
        self.copy("dve", rt["m16"][:], rt["mask"][:])
        pr = self.PS[5]
        self.mm(pr[:, 0:NE], cst["tri_b"][:], rt["m16"][:])
        self.mm(pr[:, 64:64 + NE], cst["ones_b"][:], rt["m16"][:])
        self.tt("dve", rt["rf"][:], pr[:, 0:NE], self.cntb[:], ALU.add)
        self.tt("dve", self.cntb[:], self.cntb[:], pr[:, 64:64 + NE], ALU.add)
        self.tt("dve", rt["rfs"][:], rt["rf"][:], cst[f"slotbase{layer}"][:], ALU.add)
        for k in range(4):
            self.stt("dve", rt["tmp"][:], L[:], t8[:, k:k + 1], rt["rf"][:], ALU.is_equal, ALU.mult)
            self.v("dve", "reduce_sum", rt["rk"][:, k:k + 1], rt["tmp"][:], AX.X, reads=[rt["tmp"]], writes=[rt["rk"]])
            self.stt("dve", rt["tmp"][:], L[:], t8[:, k:k + 1], rt["rfs"][:], ALU.is_equal, ALU.mult)
            self.v("dve", "reduce_sum", rt["dk"][:, k:k + 1], rt["tmp"][:], AX.X, reads=[rt["tmp"]], writes=[rt["dk"]])
        self.ts("dve", rt["ok"][:], rt["rk"][:], float(CAP), None, ALU.is_lt)
        self.stt("dve", rt["dk"][:], rt["dk"][:], rt["trash"][:, 0:1], rt["ok"][:], ALU.subtract, ALU.mult)
        self.ts("dve", rt["dk"][:], rt["dk"][:], rt["trash"][:, 0:1], None, ALU.add)
        self.tt("dve", self.GATE[:, gt, :], rt["e4"][:], rt["ok"][:], ALU.mult, writes=[(self.GATE, gt)])
        self.copy("dve", self.DEST[:, gt, :], rt["dk"][:], writes=[(self.DEST, gt)])
        self.ts("dve", rt["tok"][:, 0:1], cst["pidx"][:], float(gt * 128), None, ALU.add)
        self.copy("dve", rt["tok"][:, 1:2], rt["tok"][:, 0:1])
        self.copy("dve", rt["toki"][:], rt["tok"][:])
        for k in range(4):
            self.dma("pool", self.SLOT[:, :], rt["toki"][:, :], reads=[rt["toki"], (self.DEST, gt)], writes=[(self.SLOT, (gt, k))],
                     indirect=dict(out_offset=bass.IndirectOffsetOnAxis(ap=self.DEST[:, gt, k:k + 1], axis=0), in_offset=None))

    def experts(self, layer):
        nc, I, cst = self.nc, self.I, self.cst
        CAP, NSLOT = self.CAP, self.NSLOT
        mk = self.mark()
        w1s = [self.sb(f"w1s{i}", [128, 8, 2 * D], BF16) for i in range(2)]
        w2s = [self.sb(f"w2s{i}", [128, 8, D], BF16) for i in range(2)]
        b1s = [self.sb(f"b1s{i}", [128, 16], F32) for i in range(2)]
        b2s = [self.sb(f"b2s{i}", [1, D], F32) for i in range(2)]
        idx = [[self.sb(f"idx{s_}{i}", [128, 2], I32) for i in range(4)] for s_ in range(2)]
        xg = [[self.sb(f"xg{s_}{i}", [128, D], BF16) for i in range(4)] for s_ in range(2)]
        xT = [self.sb(f"xT{i}", [128, 8, 512], BF16) for i in range(2)]
        aT = [self.sb(f"aT{i}", [128, 8, 512], BF16) for i in range(2)]
        glc = [self.sb(f"glc{i}", [128, 512], F32) for i in range(2)]
        sig = [self.sb(f"sig{i}", [128, 512], F32) for i in range(2)]
        lin = [self.sb(f"lin{i}", [128, 512], F32) for i in range(2)]
        yt = [self.sb(f"yt{i}", [128, D], BF16) for i in range(2)]
        for xs in xg:
            for x in xs:
                self.memset("dve", x[:], 0.0)
        NG = CAP // 512
        items = [(e, g) for e in range(NE) for g in range(NG)]

        def load_weights(e):
            w1, w2, b1, b2 = w1s[e % 2], w2s[e % 2], b1s[e % 2], b2s[e % 2]
            src1 = I["moe_w1"][layer, e].rearrange("(kc p) n -> p kc n", p=128)
            for q in range(4):
                self.dma("pool", w1[:, 2 * q:2 * q + 2, :], src1[:, 2 * q:2 * q + 2, :], writes=[(w1, q)])
            src2 = I["moe_w2"][layer, e].rearrange("(kc p) n -> p kc n", p=128)
            for q in range(2):
                self.dma("pool", w2[:, 4 * q:4 * q + 4, :], src2[:, 4 * q:4 * q + 4, :], writes=[(w2, q)])
            self.dma("sp", b1[:], I["moe_b1"][layer, e].rearrange("(c p) -> p c", p=128), allow_slow_non_contiguous=True)
            self.dma("sp", b2[:], I["moe_b2"][layer, e:e + 1, :])
            self.ts("dve", b1[:, 8:16], b1[:, 8:16], 1.0, None, ALU.add)

        def load_tokens(k):
            e, grp = items[k]
            st = k % 2
            for j in range(4):
                r0 = e * CAP + (grp * 4 + j) * 128
                self.dma("sp", idx[st][j][:], self.SLOT[r0:r0 + 128, :], reads=[self.SLOT])
                self.dma("pool", xg[st][j][:], self.M[:, :], reads=[self.M, idx[st][j]],
                         indirect=dict(out_offset=None, in_offset=bass.IndirectOffsetOnAxis(ap=idx[st][j][:, 0:1], axis=0)))

        def compute(k):
            e, grp = items[k]
            st = k % 2
            w1, w2, b1, b2 = w1s[e % 2], w2s[e % 2], b1s[e % 2], b2s[e % 2]
            x_t, a_t = xT[k % 2], aT[k % 2]
            for j in range(4):
                pb = self.PB[j % 2]
                for kc in range(8):
                    self.tr(pb[:, kc * 128:(kc + 1) * 128], xg[st][j][:, kc * 128:(kc + 1) * 128], cst["ident_b"][:])
                self.copy("act", x_t[:, :, j * 128:(j + 1) * 128], pb[:, :].rearrange("p (k c) -> p k c", c=128))
            for jc in range(8):
                pg, plin = self.PS[(jc % 2) * 2], self.PS[(jc % 2) * 2 + 1]
                for kc in range(8):
                    self.mm(pg[:], w1[:, kc, jc * 128:(jc + 1) * 128], x_t[:, kc, :], start=(kc == 0), stop=(kc == 7))
                for kc in range(8):
                    self.mm(plin[:], w1[:, kc, D + jc * 128:D + (jc + 1) * 128], x_t[:, kc, :], start=(kc == 0), stop=(kc == 7))
                g_, s_, l_ = glc[jc % 2], sig[jc % 2], lin[jc % 2]
                self.ts("dve", g_[:], pg[:], b1[:, jc:jc + 1], 7.0, ALU.add, ALU.min)
                self.act(s_[:], g_[:], AF.Sigmoid, scale=1.702)
                self.ts("dve", l_[:], plin[:], b1[:, 8 + jc:9 + jc], 8.0, ALU.add, ALU.min)
                self.tt("dve", g_[:], g_[:], s_[:], ALU.mult)
                self.stt("dve", a_t[:, jc, :], l_[:], -6.0, g_[:], ALU.max, ALU.mult)
            for jt in range(4):
                y = yt[jt % 2]
                for half in range(2):
                    ps = self.PS[4 + half]
                    for jc in range(8):
                        self.mm(ps[:], a_t[:, jc, jt * 128:(jt + 1) * 128], w2[:, jc, half * 512:(half + 1) * 512],
                                start=(jc == 0), stop=False)
                    self.mm(ps[:], cst["ones_f"][0:1, :], b2[0:1, half * 512:(half + 1) * 512], start=False, stop=True)
                    self.copy("act", y[:, half * 512:(half + 1) * 512], ps[:])
                r0 = e * CAP + (grp * 4 + jt) * 128
                self.dma("sp", self.YB[r0:r0 + 128, :], y[:], writes=[(self.YB, r0)])

        load_weights(0)
        load_tokens(0)
        for k in range(len(items)):
            e, grp = items[k]
            if grp == 0 and e + 1 < NE:
                load_weights(e + 1)
            if k + 1 < len(items):
                load_tokens(k + 1)
            compute(k)
        self.release(mk)

    def combine(self, layer):
        nc, I, cst = self.nc, self.I, self.cst
        ns = self.ns
        mk = self.mark()
        g2 = self.sb("g2", [128, 2, D], F32)
        yk = [self.sb(f"yk{i}", [128, D], BF16) for i in range(4)]
        acc = [self.sb(f"acc{i}", [128, D], F32) for i in range(2)]
        h1t = [self.sb(f"h1t{i}", [128, D], F32) for i in range(2)]
        if layer == 0:
            self.modrow(g2[:, 1, :], 0, 4, 5)
        nt_seq = NT if layer == 0 else LAT // 128
        for b in range(ns):
            self.modrow(g2[:, 0, :], layer, b, 5)
            for i in range(nt_seq):
                gt = b * nt_seq + i
                grow = gt * 128
                a, h = acc[gt % 2], h1t[gt % 2]
                self.dma("sp", h[:], self.H1[grow:grow + 128, :], reads=[self.H1])
                for k in range(4):
                    self.dma("pool", yk[k][:], self.YB[:, :], reads=[self.YB, (self.DEST, gt)],
                             indirect=dict(out_offset=None, in_offset=bass.IndirectOffsetOnAxis(ap=self.DEST[:, gt, k:k + 1], axis=0)))
                    if k == 0:
                        self.ts("dve", a[:], yk[0][:], self.GATE[:, gt, 0:1], None, ALU.mult, reads=[yk[0], (self.GATE, gt)])
                    else:
                        self.stt("dve", a[:], yk[k][:], self.GATE[:, gt, k:k + 1], a[:], ALU.mult, ALU.add,
                                 reads=[yk[k], a, (self.GATE, gt)])
                gsel = 1 if (layer == 0 and i < 2) else 0
                self.tt("pool", a[:], a[:], g2[:, gsel, :], ALU.mult)
                self.tt("dve", a[:], a[:], h[:], ALU.add)
                if layer == 0:
                    self.dma("sp", self.H2[grow:grow + 128, :], a[:], writes=[(self.H2, grow)])
                else:
                    self.dma("sp", self.out[b, i * 128:(i + 1) * 128, :], a[:], writes=[("out", grow)])
        self.release(mk)


def build_program(ns, **kw):
    k = K(ns, **kw)
    k.build()
    return k


_CACHE = {}


def kernel(**inputs):
    ncores, ns = 8, 4
    if "k" not in _CACHE:
        _CACHE["k"] = build_program(ns)
    k = _CACHE["k"]
    consts = {"k_" + n: v for n, v in host_consts().items()}
    shared = {n: np.ascontiguousarray(np.asarray(inputs[n], dtype=np.float32)) for n in IN_SPECS if n not in ("x", "c", "ctx")}
    in_maps = []
    for c in range(ncores):
        m = dict(shared)
        for n in ("x", "c", "ctx"):
            m[n] = np.ascontiguousarray(np.asarray(inputs[n], dtype=np.float32)[c * ns:(c + 1) * ns])
        m.update(consts)
        in_maps.append(m)
    res = run_bass_kernel_spmd(k.nc, in_maps, core_ids=list(range(ncores)))
    return np.concatenate([r["out"] for r in res.results], axis=0).astype(np.float32)
```

```python
import numpy as np
import ml_dtypes
import concourse.bass as bass
import concourse.mybir as mybir
from concourse.bass_utils import run_bass_kernel_spmd

F32 = mybir.dt.float32
BF16 = mybir.dt.bfloat16
I32 = mybir.dt.int32
AF = mybir.ActivationFunctionType
ALU = mybir.AluOpType
AX = mybir.AxisListType

D = 1024
LAT = 2048
CTX = 256
SEQ = LAT + CTX
NT = SEQ // 128
EPS = 1e-6
NE = 32
CAPS = (2560, 2048)
CAPMAX = max(CAPS)
NSLOTMAX = NE * CAPMAX


class Sched:
    EPOCH = 30000

    def __init__(self, nc):
        self.nc = nc
        self.E = {"pe": nc.tensor, "dve": nc.vector, "act": nc.scalar, "pool": nc.gpsimd, "sp": nc.sync}
        self.nsem = 0
        self.esem, self.ecnt = {}, {}
        self.pesems = set()
        for e in self.E:
            self._new_epoch(e)
        self.seen = {e: {} for e in self.E}
        self.W, self.R = {}, {}
        self.dpool = {q: [] for q in ("sp", "pool", "act")}
        self.dnext = {q: 0 for q in self.dpool}
        self.NPOOL = {"sp": 24, "pool": 24, "act": 4}
        self.ninst = 0
        self.psum = set()

    def _sem(self, name):
        self.nsem += 1
        return self.nc.semaphore(f"{name}{self.nsem}").__enter__()

    def _new_epoch(self, e):
        import os as _os
        self.skip_same = _os.environ.get("KSAME", "1") == "0"
        if not hasattr(self, "own"):
            self.own = {}
        self.esem[e] = self._sem("e" + e)
        self.own.setdefault(e, set()).add(self.esem[e])
        self.ecnt[e] = 0
        if e == "pe":
            self.pesems.add(self.esem[e])

    @staticmethod
    def _nm(x):
        if isinstance(x, str):
            return x
        t = getattr(x, "tensor", None)
        return t.name if t is not None else x.name

    @staticmethod
    def _key(x):
        if isinstance(x, tuple):
            return (Sched._nm(x[0]), x[1])
        return (Sched._nm(x), None)

    def _collect(self, table, key, evs):
        name, sub = key
        t = table.get(name)
        if not t:
            return
        subs = t.keys() if sub is None else [s for s in (sub, None) if s in t]
        for s in subs:
            for sem, val in t[s].items():
                if evs.get(sem, 0) < val:
                    evs[sem] = val

    def _deps(self, e, rk, wk):
        evs = {}
        for k in rk:
            self._collect(self.W, k, evs)
        for k in wk:
            self._collect(self.W, k, evs)
            self._collect(self.R, k, evs)
        for sem, val in evs.items():
            if e == "pe" and sem in self.pesems:
                continue
            if self.skip_same and sem in self.own.get(e, ()):
                continue
            if self.seen[e].get(sem, 0) >= val:
                continue
            self.E[e].wait_ge(sem, val)
            self.seen[e][sem] = val

    def _commit(self, ev, rk, wk):
        sem, val = ev
        for name, sub in rk:
            d = self.R.setdefault(name, {}).setdefault(sub, {})
            if d.get(sem, 0) < val:
                d[sem] = val
        for name, sub in wk:
            if sub is None:
                self.W[name] = {None: {sem: val}}
                self.R[name] = {}
            else:
                self.W.setdefault(name, {})[sub] = {sem: val}
                self.R.setdefault(name, {}).pop(sub, None)

    def op(self, e, fn, reads=(), writes=()):
        rk = [self._key(x) for x in reads]
        wk = [self._key(x) for x in writes]
        pr = [(k[0], None) for k in rk if k[0] in self.psum]
        if pr:
            rk = [k for k in rk if k[0] not in self.psum]
            wk = wk + pr
        wk = [((k[0], None) if k[0] in self.psum else k) for k in wk]
        self._deps(e, rk, wk)
        if self.ecnt[e] >= self.EPOCH:
            self._new_epoch(e)
        ins = fn()
        self.ecnt[e] += 1
        ins.then_inc(self.esem[e], 1)
        self._commit((self.esem[e], self.ecnt[e]), rk, wk)
        self.ninst += 1
        return ins

    def dma(self, q, out, in_, reads=None, writes=None, indirect=None, **kw):
        rk = [self._key(x) for x in (reads if reads is not None else [in_])]
        wk = [self._key(x) for x in (writes if writes is not None else [out])]
        self._deps(q, rk, wk)
        pool = self.dpool[q]
        if len(pool) < self.NPOOL[q]:
            pool.append([self._sem("d" + q), 0])
            slot = pool[-1]
        else:
            slot = pool[self.dnext[q] % len(pool)]
            self.dnext[q] += 1
            if slot[1] >= 60000:
                if self.seen[q].get(slot[0], 0) < slot[1]:
                    self.E[q].wait_ge(slot[0], slot[1])
                slot[0], slot[1] = self._sem("d" + q), 0
        sem, cnt = slot
        if cnt > 0 and self.seen[q].get(sem, 0) < cnt:
            self.E[q].wait_ge(sem, cnt)
            self.seen[q][sem] = cnt
        if indirect is None:
            ins = self.E[q].dma_start(out=out, in_=in_, **kw)
        else:
            ins = self.E[q].indirect_dma_start(out=out, in_=in_, **indirect)
        slot[1] = cnt + 16
        ins.then_inc(sem, 16)
        self._commit((sem, slot[1]), rk, wk)
        self.ninst += 1
        return ins

    def barrier(self):
        evs = {}
        for e in self.E:
            if self.ecnt[e] > 0:
                evs[self.esem[e]] = self.ecnt[e]
        for q, pool in self.dpool.items():
            for sem, cnt in pool:
                if cnt > 0:
                    evs[sem] = cnt
        for e in self.E:
            for sem, val in evs.items():
                if self.seen[e].get(sem, 0) >= val:
                    continue
                self.E[e].wait_ge(sem, val)
                self.seen[e][sem] = val
        self.W, self.R = {}, {}

    def finish(self):
        self.barrier()


def host_consts():
    c = {}
    c["ident_f"] = np.eye(128, dtype=np.float32)
    c["ident_b"] = np.eye(128).astype(ml_dtypes.bfloat16)
    s = np.arange(128)[:, None]
    t = np.arange(128)[None, :]
    c["mask_f"] = (s <= t).astype(np.float32)
    c["mask_b"] = (s >= t).astype(np.float32)
    c["tri_b"] = (s < t).astype(ml_dtypes.bfloat16)
    c["ones_b"] = np.ones((128, 128), ml_dtypes.bfloat16)
    c["ones_f"] = np.ones((128, 128), np.float32)
    facS = np.ones((4, 8), np.float32)
    facE = np.ones((4, 8), np.float32)
    for g, w in enumerate((2, 4, 8, 16)):
        half = w // 2
        for tt in range(min(half, 8)):
            facS[g, tt] = w / (tt + half)
        for j in range(8):
            i = 7 - j
            if i < half - 1:
                facE[g, j] = w / (half + 1 + i)
    c["facS"] = np.broadcast_to(facS[None], (128, 4, 8)).copy()
    c["facE"] = np.broadcast_to(facE[None], (128, 4, 8)).copy()
    rows = LAT // 64
    t_row = np.repeat(np.arange(rows, dtype=np.float32), 64)
    t_col = np.tile(np.arange(64, dtype=np.float32), rows)
    inv = (1.0 / (10000.0 ** (np.arange(8, dtype=np.float32) / 8))).astype(np.float32)
    ang = np.stack([t_row[:, None] * inv, t_col[:, None] * inv], axis=1).astype(np.float32)
    cos = np.cos(ang).reshape(LAT, 16).astype(np.float32)
    sin = np.sin(ang).reshape(LAT, 16).astype(np.float32)
    for l_ in range(2):
        c[f"slotbase{l_}"] = np.broadcast_to((np.arange(32, dtype=np.float32) * CAPS[l_])[None], (128, 32)).copy()
    c["pidx"] = np.arange(128, dtype=np.float32).reshape(128, 1).copy()
    c["rope_cos"] = cos.reshape(16, 128, 16).transpose(1, 0, 2).copy()
    c["rope_sin"] = sin.reshape(16, 128, 16).transpose(1, 0, 2).copy()
    return c


CONST_SPECS = {
    "ident_f": ([128, 128], F32), "ident_b": ([128, 128], BF16), "mask_f": ([128, 128], F32),
    "mask_b": ([128, 128], F32), "tri_b": ([128, 128], BF16), "ones_b": ([128, 128], BF16),
    "ones_f": ([128, 128], F32), "facS": ([128, 4, 8], F32), "facE": ([128, 4, 8], F32),
    "slotbase0": ([128, 32], F32), "slotbase1": ([128, 32], F32), "pidx": ([128, 1], F32),
    "rope_cos": ([128, 16, 16], F32), "rope_sin": ([128, 16, 16], F32),
}

IN_SPECS = {
    "x": lambda ns: [ns, LAT, D], "c": lambda ns: [ns, D], "ctx": lambda ns: [ns, CTX, D], "c_ctx": lambda ns: [D],
    "ada_w": lambda ns: [2, D, 6 * D], "ada_b": lambda ns: [2, 6 * D], "ev_w_in": lambda ns: [1, D, 3072],
    "hgrn_lb_logits": lambda ns: [3, 2, 512], "hgrn_norm_w": lambda ns: [1, 128], "pool_w": lambda ns: [1, 4, 128, 128],
    "pool_scale": lambda ns: [1, 512], "ev_w_out": lambda ns: [1, D, D], "od_w_in": lambda ns: [1, D, 1696],
    "conv_dw_w": lambda ns: [1, 31, 512], "conv_dw_b": lambda ns: [1, 512], "conv_ln_w": lambda ns: [1, 512],
    "conv_ln_b": lambda ns: [1, 512], "mla_q_a_norm": lambda ns: [1, 384], "mla_w_uq": lambda ns: [1, 384, 768],
    "mla_kv_a_norm": lambda ns: [1, 256], "mla_w_ukv": lambda ns: [1, 256, 1024], "mla_q_norm": lambda ns: [1, 96],
    "mla_k_norm": lambda ns: [1, 96], "od_w_out": lambda ns: [1, D, D], "moe_router_w": lambda ns: [2, D, 32],
    "moe_router_b": lambda ns: [2, 32], "moe_w1": lambda ns: [2, 32, D, 2 * D], "moe_b1": lambda ns: [2, 32, 2 * D],
    "moe_w2": lambda ns: [2, 32, D, D], "moe_b2": lambda ns: [2, 32, D],
}


class StopBuild(Exception):
    pass


class K:
    def __init__(self, ns, stop_after=None, dbg=(), skip_inputs=()):
        self.ns = ns
        self.stop_after = stop_after
        self.dbg = set(dbg)
        nc = self.nc = bass.Bass("TRN2", target_bir_lowering=False)
        self.S = Sched(nc)
        self.I = {k: nc.dram_tensor(k, f(ns), F32, kind="ExternalInput").ap() for k, f in IN_SPECS.items() if k not in skip_inputs}
        self.C = {k: nc.dram_tensor("k_" + k, sh, dt, kind="ExternalInput").ap() for k, (sh, dt) in CONST_SPECS.items()}
        self.out = nc.dram_tensor("out", [ns, LAT, D], F32, kind="ExternalOutput").ap()
        self.T0 = ns * SEQ
        self.T1 = ns * LAT
        self.MOD = nc.dram_tensor("MOD", [10, 6 * D], F32).ap()
        self.H1 = nc.dram_tensor("H1", [self.T0, D], F32).ap()
        self.H2 = nc.dram_tensor("H2", [self.T0, D], F32).ap()
        self.M = nc.dram_tensor("M", [self.T0 + 128, D], BF16).ap()
        self.SLOT = nc.dram_tensor("SLOT", [NSLOTMAX + 128, 2], I32).ap()
        self.YB = nc.dram_tensor("YB", [NSLOTMAX + 128, D], BF16).ap()
        self.dbg_out = {}
        self._stack = []

    def sb(self, name, shape, dt):
        self._uid = getattr(self, "_uid", 0) + 1
        cm = self.nc.sbuf_tensor(f"{name}_{self._uid}", shape, dt)
        t = cm.__enter__()
        self._stack.append(cm)
        return t

    def ps(self, name, shape, dt):
        cm = self.nc.psum_tensor(name, shape, dt)
        t = cm.__enter__()
        self.S.psum.add(name)
        self._stack.append(cm)
        return t

    def mark(self):
        return len(self._stack)

    def release(self, mark):
        self.S.barrier()
        while len(self._stack) > mark:
            self._stack.pop().__exit__(None, None, None)

    def mm(self, out, lhsT, rhs, start=True, stop=True, reads=None, writes=None):
        nc = self.nc
        return self.S.op("pe", lambda: nc.tensor.matmul(out, lhsT, rhs, start=start, stop=stop),
                         reads=reads if reads is not None else [lhsT, rhs], writes=writes if writes is not None else [out])

    def tr(self, out, in_, ident, reads=None, writes=None):
        nc = self.nc
        return self.S.op("pe", lambda: nc.tensor.transpose(out, in_, ident),
                         reads=reads if reads is not None else [in_, ident], writes=writes if writes is not None else [out])

    def act(self, out, in_, func, bias=None, scale=None, accum_out=None, reads=None, writes=None):
        nc = self.nc
        kw = {}
        rd = [in_]
        wr = [out]
        if bias is not None:
            kw["bias"] = bias
            if not isinstance(bias, (int, float)):
                rd.append(bias)
        if scale is not None:
            kw["scale"] = scale
            if not isinstance(scale, (int, float)):
                rd.append(scale)
        if accum_out is not None:
            kw["accum_out"] = accum_out
            wr.append(accum_out)
        return self.S.op("act", lambda: nc.scalar.activation(out, in_, func, **kw),
                         reads=reads if reads is not None else rd, writes=writes if writes is not None else wr)

    def v(self, e, name, *args, reads, writes, **kw):
        eng = self.S.E[e]
        return self.S.op(e, lambda: getattr(eng, name)(*args, **kw), reads=reads, writes=writes)

    def tt(self, e, out, in0, in1, op, reads=None, writes=None):
        return self.v(e, "tensor_tensor", out, in0, in1, op, reads=reads if reads is not None else [in0, in1],
                      writes=writes if writes is not None else [out])

    def ts(self, e, out, in0, s1, s2, op0, op1=None, reads=None, writes=None, accum_out=None):
        rd = [in0] + [s for s in (s1, s2) if s is not None and not isinstance(s, (int, float))]
        kw = {}
        if op1 is not None:
            kw["op1"] = op1
        wr = [out]
        if accum_out is not None:
            kw["accum_out"] = accum_out
            wr.append(accum_out)
        return self.v(e, "tensor_scalar", out, in0, s1, s2, op0, reads=reads if reads is not None else rd,
                      writes=writes if writes is not None else wr, **kw)

    def stt(self, e, out, in0, scalar, in1, op0, op1, reads=None, writes=None):
        rd = [in0, in1] + ([] if isinstance(scalar, (int, float)) else [scalar])
        return self.v(e, "scalar_tensor_tensor", out, in0, scalar, in1, op0, op1,
                      reads=reads if reads is not None else rd, writes=writes if writes is not None else [out])

    def copy(self, e, out, in_, reads=None, writes=None):
        if e == "act":
            return self.act(out, in_, AF.Copy, reads=reads, writes=writes)
        return self.v(e, "tensor_copy", out, in_, reads=reads if reads is not None else [in_],
                      writes=writes if writes is not None else [out])

    def memset(self, e, ap, val):
        return self.v(e, "memset", ap, val, reads=[], writes=[ap])

    def dma(self, q, out, in_, **kw):
        return self.S.dma(q, out, in_, **kw)

    def rstd_from_ss(self, rstd, ss, n):
        self.ts("dve", rstd, ss, 1.0 / n, EPS, ALU.mult, ALU.add)
        self.act(rstd, rstd, AF.Sqrt)
        self.v("dve", "reciprocal", rstd, rstd, reads=[rstd], writes=[rstd])

    def build(self):
        nc, S, I, C = self.nc, self.S, self.I, self.C
        ns = self.ns
        self.cst = {}
        for k in ("ident_f", "ident_b", "mask_f", "mask_b", "tri_b", "ones_b", "ones_f", "slotbase0", "slotbase1", "pidx"):
            sh, dt = CONST_SPECS[k]
            t = self.sb("c_" + k, sh, dt)
            self.dma("sp", t[:], C[k][:, :])
            self.cst[k] = t
        self.PS = [self.ps(f"ps{i}", [128, 512], F32) for i in range(6)]
        self.PB = [self.ps(f"pb{i}", [128, 1024], BF16) for i in range(2)]
        self.small = self.sb("small", [128, 64], F32)

        self.phase_adaln()
        if self.stop_after == "adaln":
            return self.end()
        mkr = self.mark()
        self.route_init(0)
        try:
            self.layer0()
        except StopBuild:
            return self.end()
        if self.stop_after == "mixer0":
            return self.end()
        if self.stop_after and self.stop_after.startswith("l1_"):
            self.S.barrier()
            self.dma("sp", self.H2, self.H1)
        else:
            self.experts(0)
            self.combine(0)
        self.release(mkr)
        if self.stop_after == "layer0":
            return self.end()
        mkr = self.mark()
        self.route_init(1)
        try:
            self.layer1()
        except StopBuild:
            return self.end()
        self.experts(1)
        self.combine(1)
        self.release(mkr)
        return self.end()

    def end(self):
        for name in ("MOD", "H1", "H2", "M"):
            if name in self.dbg:
                src = getattr(self, name)
                d = self.dbg_dump(name, list(src.shape), src.dtype)
                self.S.barrier()
                self.dma("sp", d, src)
        self.S.finish()
        return self.nc

    def dbg_dump(self, name, shape, dt=F32):
        t = self.nc.dram_tensor("dbg_" + name, shape, dt, kind="ExternalOutput").ap()
        self.dbg_out[name] = t
        return t

    def phase_adaln(self):
        nc, S, I, C = self.nc, self.S, self.I, self.C
        ns = self.ns
        mk = self.mark()
        cs = self.sb("cs", [128, 8, 8], F32)
        s5 = self.sb("s5", [128, 8, 8], F32)
        self.memset("dve", cs[:], 0.0)
        for b in range(ns):
            self.dma("sp", cs[:, :, b], I["c"][b].rearrange("(kc p) -> p kc", p=128), allow_slow_non_contiguous=True)
        self.dma("sp", cs[:, :, ns], I["c_ctx"].rearrange("(kc p) -> p kc", p=128), allow_slow_non_contiguous=True)
        self.act(s5[:], cs[:], AF.Silu)
        nr = ns + 1
        brow = self.sb("brow", [1, 6 * D], F32)
        wa = [self.sb(f"wa{i}", [128, 8, 512], F32) for i in range(2)]
        mo = [self.sb(f"mo{i}", [8, 512], F32) for i in range(2)]
        onesf = self.cst["ones_f"]
        it = 0
        for layer in range(2):
            self.dma("sp", brow[:], I["ada_b"][layer:layer + 1, :])
            for nb in range(12):
                w = wa[it % 2]
                self.dma("sp" if it % 2 == 0 else "pool", w[:],
                         I["ada_w"][layer][:, nb * 512:(nb + 1) * 512].rearrange("(kc p) n -> p kc n", p=128))
                ps = self.PS[it % 2]
                for kc in range(8):
                    self.mm(ps[0:nr, :], s5[:, kc, 0:nr], w[:, kc, :], start=(kc == 0), stop=False)
                self.mm(ps[0:nr, :], onesf[0:1, 0:nr], brow[0:1, nb * 512:(nb + 1) * 512], start=False, stop=True)
                m = mo[it % 2]
                self.copy("dve", m[0:nr, :], ps[0:nr, :])
                self.dma("sp", self.MOD[layer * 5:layer * 5 + ns, nb * 512:(nb + 1) * 512], m[0:ns, :],
                         writes=[(self.MOD, (layer, nb, 0))])
                self.dma("sp", self.MOD[layer * 5 + 4:layer * 5 + 5, nb * 512:(nb + 1) * 512], m[ns:ns + 1, :],
                         writes=[(self.MOD, (layer, nb, 1))])
                it += 1
        self.release(mk)

    def modrow(self, dst, layer, row, q):
        src = self.MOD[layer * 5 + row:layer * 5 + row + 1, q * D:(q + 1) * D].partition_broadcast(128)
        self.dma("sp", dst, src, reads=[self.MOD])

    def modulate_tile(self, u, ht, sc1p, sh, junk, ss, rstd):
        self.act(junk, ht, AF.Square, accum_out=ss)
        self.rstd_from_ss(rstd, ss, D)
        self.stt("dve", u, ht, rstd, sc1p, ALU.mult, ALU.mult)
        self.tt("pool", u, u, sh, ALU.add)

    def transpose_tile_to(self, dstT, col0, src, nkc, ident_f, psA, psB, dt_note=None):
        for kc in range(nkc):
            ps = psA if kc < 4 else psB
            self.tr(ps[:, (kc % 4) * 128:(kc % 4 + 1) * 128], src[:, kc * 128:(kc + 1) * 128], ident_f[:])
        n0 = min(4, nkc)
        self.copy("act", dstT[:, 0:n0, col0:col0 + 128], psA[:, 0:n0 * 128].rearrange("p (k c) -> p k c", c=128))
        if nkc > 4:
            self.copy("dve", dstT[:, 4:nkc, col0:col0 + 128], psB[:, 0:(nkc - 4) * 128].rearrange("p (k c) -> p k c", c=128))

    def layer0(self):
        nc, S, I, C = self.nc, self.S, self.I, self.C
        ns = self.ns
        cst = self.cst
        mk0 = self.mark()
        lg = self.sb("lg", [128, 3, 8], F32)
        lb = self.sb("lb", [128, 8], F32)
        oml = self.sb("oml", [128, 8], F32)
        for l in range(3):
            self.dma("sp", lg[:, l, :].rearrange("p (d h) -> p d h", d=2),
                     I["hgrn_lb_logits"][l].rearrange("d (h p) -> p d h", p=128), allow_slow_non_contiguous=True)
        self.act(lg[:], lg[:], AF.Exp)
        self.tt("dve", lb[:], lg[:, 0, :], lg[:, 1, :], ALU.add)
        self.tt("dve", lb[:], lb[:], lg[:, 2, :], ALU.add)
        self.v("dve", "reciprocal", lb[:], lb[:], reads=[lb], writes=[lb])
        self.tt("dve", lb[:], lb[:], lg[:, 0, :], ALU.mult)
        self.ts("dve", oml[:], lb[:], -1.0, 1.0, ALU.mult, ALU.add)
        nwb = self.sb("nwb", [128, 128], F32)
        self.dma("sp", nwb[:], I["hgrn_norm_w"][0:1, :].partition_broadcast(128))
        pscale = self.sb("pscale", [128, 4], F32)
        self.dma("sp", pscale[:], I["pool_scale"][0].rearrange("(g p) -> p g", p=128), allow_slow_non_contiguous=True)
        poolw = self.sb("poolw", [128, 4, 128], BF16)
        for g in range(4):
            self.dma("pool", poolw[:, g, :], I["pool_w"][0, g])
        facS = self.sb("facS", [128, 4, 8], F32)
        facE = self.sb("facE", [128, 4, 8], F32)
        self.dma("sp", facS[:], C["facS"])
        self.dma("sp", facE[:], C["facE"])
        wr = self.sb("wr", [128, 8, 32], F32)
        self.dma("sp", wr[:], I["moe_router_w"][0].rearrange("(kc p) e -> p kc e", p=128))
        brr = self.sb("brr", [1, 32], F32)
        self.dma("sp", brr[:], I["moe_router_b"][0:1, :])
        uT = self.sb("uT", [128, 8, SEQ], BF16)
        mixT = self.sb("mixT", [128, 8, SEQ], BF16)
        w_in = I["ev_w_in"][0]
        for b in range(ns):
            mk = self.mark()
            mr = self.sb("mr", [128, 4, D], F32)
            self.modrow(mr[:, 0, :], 0, b, 1)
            self.modrow(mr[:, 1, :], 0, b, 0)
            self.modrow(mr[:, 2, :], 0, 4, 1)
            self.modrow(mr[:, 3, :], 0, 4, 0)
            self.ts("dve", mr[:, 0, :], mr[:, 0, :], 1.0, None, ALU.add)
            self.ts("dve", mr[:, 2, :], mr[:, 2, :], 1.0, None, ALU.add)
            hts = [self.sb(f"ht{i}", [128, D], F32) for i in range(2)]
            us = [self.sb(f"u{i}", [128, D], F32) for i in range(2)]
            junk = self.sb("junk", [128, D], F32)
            for i in range(NT):
                ht, u = hts[i % 2], us[i % 2]
                src = I["ctx"][b, i * 128:(i + 1) * 128, :] if i < 2 else I["x"][b, (i - 2) * 128:(i - 1) * 128, :]
                self.dma("sp", ht[:], src)
                o = 2 if i < 2 else 0
                self.modulate_tile(u[:], ht[:], mr[:, o, :], mr[:, o + 1, :], junk[:], self.small[:, 0:1], self.small[:, 1:2])
                self.transpose_tile_to(uT, i * 128, u, 8, cst["ident_f"], self.PS[0], self.PS[1])
            if "uT" in self.dbg and b == 0:
                d = self.dbg_dump("uT", [128, 8, SEQ], BF16)
                self.dma("sp", d[:, :, :], uT[:])
            self.release(mk)
            if self.stop_after == "l0_p0":
                raise StopBuild()
            mk = self.mark()
            W2 = SEQ + 64
            q32 = self.sb("q32", [128, W2], F32)
            sg = self.sb("sg", [128, W2], F32)
            kin = self.sb("kin", [128, W2], F32)
            A = self.sb("A", [128, W2], F32)
            oacc = self.sb("oacc", [128, NT, 128], F32)
            v16 = self.sb("v16", [128, NT, 128], BF16)
            whs = [self.sb(f"wh{i}", [128, 8, 640], BF16) for i in range(2)]
            S32 = self.sb("S32", [128, 128], F32)
            S16 = self.sb("S16", [128, 128], BF16)
            RING = 3
            btall = self.sb("btall", [128, NT, 8], F32)
            decs = [self.sb(f"dec{i}", [128, 1], F32) for i in range(RING)]
            Pks = [[self.sb(f"Pk{r_}{i}", [128, 128], F32) for i in range(5)] for r_ in range(RING)]
            kiA = [self.sb(f"kiA{i}", [128, 128], BF16) for i in range(RING)]
            kiB = [self.sb(f"kiB{i}", [128, 128], BF16) for i in range(RING)]
            kiC = [self.sb(f"kiC{i}", [128, 128], BF16) for i in range(RING)]
            qdx = [self.sb(f"qdx{i}", [128, 64], BF16) for i in range(RING)]
            S16s = [self.sb(f"S16{i}", [128, 128], BF16) for i in range(2)]
            qd = [self.sb(f"qd{i}", [128, 128], BF16) for i in range(RING)]
            qdS = [self.sb(f"qdS{i}", [128, 128], BF16) for i in range(RING)]
            ke = [self.sb(f"ke{i}", [128, 128], BF16) for i in range(RING)]
            att16 = [self.sb(f"att{i}", [128, 128], BF16) for i in range(2)]
            ket16 = [self.sb(f"ket{i}", [128, 128], BF16) for i in range(2)]
            ogs = self.sb("ogs", [128, 128], F32)
            rr = self.sb("rr", [128, 128], F32)
            zero1 = self.sb("zero1", [128, 1], F32)
            self.memset("dve", zero1[:], 0.0)
            groups = [(g * 512, min(512, SEQ - g * 512)) for g in range((SEQ + 511) // 512)]
            for h in range(4):
                wh = whs[h % 2]
                for j, base in enumerate((0, 512, 1024, 1536, 2048)):
                    c0 = base + h * 128
                    self.dma("pool", wh[:, :, j * 128:(j + 1) * 128],
                             w_in[:, c0:c0 + 128].rearrange("(kc p) n -> p kc n", p=128))
                for gi, (g0, gn) in enumerate(groups):
                    ps = self.PS[gi % 2]
                    for kc in range(8):
                        self.mm(ps[:, 0:gn], wh[:, kc, 0:128], uT[:, kc, g0:g0 + gn], start=(kc == 0), stop=(kc == 7))
                    self.act(q32[:, g0:g0 + gn], ps[:, 0:gn], AF.Silu)
                for i in range(NT):
                    ps = self.PS[2 + i % 2]
                    for kc in range(8):
                        self.mm(ps[:, 0:128], uT[:, kc, i * 128:(i + 1) * 128], wh[:, kc, 384:512], start=(kc == 0), stop=(kc == 7))
                    self.copy("dve", v16[:, i, :], ps[:, 0:128])
                for di in range(2):
                    for gi, (g0, gn) in enumerate(groups):
                        ps = self.PS[gi % 2]
                        for kc in range(8):
                            self.mm(ps[:, 0:gn], wh[:, kc, (1 + di) * 128:(2 + di) * 128], uT[:, kc, g0:g0 + gn],
                                    start=(kc == 0), stop=(kc == 7))
                        self.act(sg[:, g0:g0 + gn], ps[:, 0:gn], AF.Sigmoid)
                    lbc = lb[:, di * 4 + h:di * 4 + h + 1]
                    omc = oml[:, di * 4 + h:di * 4 + h + 1]
                    self.ts("dve", sg[:, 0:SEQ], sg[:, 0:SEQ], omc, lbc, ALU.mult, ALU.add)
                    self.ts("pool", kin[:, 0:SEQ], sg[:, 0:SEQ], -1.0, 1.0, ALU.mult, ALU.add)
                    self.act(sg[:, 0:SEQ], sg[:, 0:SEQ], AF.Ln)
                    self.S.op("dve", lambda: nc.vector.tensor_tensor_scan(A[:, 0:SEQ], sg[:, 0:SEQ], sg[:, 0:SEQ], 0.0, ALU.add, ALU.add),
                              reads=[sg], writes=[A])
                    if di == 1:
                        self.stt("dve", sg[:, 0:SEQ], sg[:, 0:SEQ], -2.0, A[:, 0:SEQ], ALU.mult, ALU.add)
                    AA = A if di == 0 else sg
                    order = list(range(NT)) if di == 0 else [1, 0] + list(range(NT - 1, 1, -1))
                    self.memset("dve", S32[:], 0.0)
                    self.memset("pool", S16s[0][:], 0.0)
                    self.memset("pool", S16s[1][:], 0.0)
                    for t_ in kiA + kiB + kiC:
                        self.memset("pool", t_[:], 0.0)
                    mask = cst["mask_f"] if di == 0 else cst["mask_b"]
                    lo, hi = slice(0, 64), slice(64, 128)
                    AAv = AA[:, 0:SEQ].rearrange("p (t c) -> p t c", c=128)
                    Av = A[:, 0:SEQ].rearrange("p (t c) -> p t c", c=128)
                    xcol = 63 if di == 0 else 64
                    for bc, (src_, sgn) in enumerate(((AAv[:, :, 31], -0.5), (AAv[:, :, 31], 0.5), (AAv[:, :, 95], -0.5), (AAv[:, :, 95], 0.5),
                                                     (AAv[:, :, xcol], -0.5), (AAv[:, :, xcol], 0.5))):
                        self.ts("dve", btall[:, :, bc], src_, sgn, None, ALU.mult, reads=[AA], writes=[btall])
                    self.memset("dve", btall[:, 0:1, 6], 0.0)
                    self.ts("dve", btall[:, 1:NT, 6], Av[:, 0:NT - 1, 127], -0.5, None, ALU.mult, reads=[A], writes=[btall])
                    self.ts("dve", btall[:, :, 7], Av[:, :, 127], 0.5, None, ALU.mult, reads=[A], writes=[btall])

                    def stageA(n, i):
                        c0, c1 = i * 128, (i + 1) * 128
                        r = n % RING
                        Pk_ = Pks[r]
                        bt_ = btall[:, i, :]
                        Alo, Ahi, Acol = AA[:, c0:c0 + 64], AA[:, c0 + 64:c1], AA[:, c0:c1]
                        if di == 0:
                            self.act(Pk_[0][:, lo], Alo, AF.Exp, bias=btall[:, i, 0:1], scale=0.5)
                            self.act(Pk_[0][:, hi], Ahi, AF.Exp, bias=btall[:, i, 2:3], scale=0.5)
                            self.act(Pk_[1][:, lo], Alo, AF.Exp, bias=btall[:, i, 1:2], scale=-0.5)
                            self.act(Pk_[1][:, hi], Ahi, AF.Exp, bias=btall[:, i, 3:4], scale=-0.5)
                            self.act(Pk_[2][:], Acol, AF.Exp, bias=btall[:, i, 6:7], scale=0.5)
                            self.act(Pk_[3][:], Acol, AF.Exp, bias=btall[:, i, 7:8], scale=-0.5)
                            self.act(Pk_[4][:, lo], Ahi, AF.Exp, bias=btall[:, i, 4:5], scale=0.5)
                            self.act(Pk_[4][:, hi], Alo, AF.Exp, bias=btall[:, i, 5:6], scale=-0.5)
                            qx_cols, kx_cols = hi, lo
                        else:
                            self.act(Pk_[0][:, lo], Alo, AF.Exp, bias=btall[:, i, 1:2], scale=-0.5)
                            self.act(Pk_[0][:, hi], Ahi, AF.Exp, bias=btall[:, i, 3:4], scale=-0.5)
                            self.act(Pk_[1][:, lo], Alo, AF.Exp, bias=btall[:, i, 0:1], scale=0.5)
                            self.act(Pk_[1][:, hi], Ahi, AF.Exp, bias=btall[:, i, 2:3], scale=0.5)
                            self.act(Pk_[2][:], Acol, AF.Exp, bias=btall[:, i, 7:8], scale=-0.5)
                            self.act(Pk_[3][:], Acol, AF.Exp, bias=btall[:, i, 6:7], scale=0.5)
                            self.act(Pk_[4][:, lo], Alo, AF.Exp, bias=btall[:, i, 5:6], scale=-0.5)
                            self.act(Pk_[4][:, hi], Ahi, AF.Exp, bias=btall[:, i, 4:5], scale=0.5)
                            qx_cols, kx_cols = lo, hi
                        qcs = slice(c0 + qx_cols.start, c0 + qx_cols.stop)
                        kcs = slice(c0 + kx_cols.start, c0 + kx_cols.stop)
                        self.tt("dve", qd[r][:], q32[:, c0:c1], Pk_[0][:], ALU.mult)
                        self.tt("pool", kiA[r][:, lo], kin[:, c0:c0 + 64], Pk_[1][:, lo], ALU.mult)
                        self.tt("pool", kiB[r][:, hi], kin[:, c0 + 64:c1], Pk_[1][:, hi], ALU.mult)
                        self.tt("dve", qdS[r][:], q32[:, c0:c1], Pk_[2][:], ALU.mult)
                        self.tt("pool", ke[r][:], kin[:, c0:c1], Pk_[3][:], ALU.mult)
                        self.tt("dve", qdx[r][:], q32[:, qcs], Pk_[4][:, lo], ALU.mult)
                        self.tt("pool", kiC[r][:, kx_cols], kin[:, kcs], Pk_[4][:, hi], ALU.mult)
                        self.copy("pool", decs[r][:], Pk_[2][:, 127:128] if di == 0 else Pk_[2][:, 0:1])

                    def stageB(n, i):
                        r = n % RING
                        p = n % 2
                        pa, po, pS = self.PS[0 + p], self.PS[2 + p], self.PS[4 + p]
                        pbt = self.PB[p]
                        if di == 0:
                            self.mm(pa[:, 0:64], kiA[r][:], qd[r][:, lo], start=True, stop=True)
                            self.mm(pa[:, 64:128], kiB[r][:], qd[r][:, hi], start=True, stop=False)
                            self.mm(pa[:, 64:128], kiC[r][:], qdx[r][:], start=False, stop=True)
                        else:
                            self.mm(pa[:, 0:64], kiA[r][:], qd[r][:, lo], start=True, stop=False)
                            self.mm(pa[:, 0:64], kiC[r][:], qdx[r][:], start=False, stop=True)
                            self.mm(pa[:, 64:128], kiB[r][:], qd[r][:, hi], start=True, stop=True)
                        self.tr(pbt[:, 0:128], ke[r][:], cst["ident_b"][:])
                        self.tt("dve", att16[p][:], pa[:, 0:128], mask[:], ALU.mult)
                        self.copy("act", ket16[p][:], pbt[:, 0:128])
                        self.mm(pS[:, 0:128], ket16[p][:], v16[:, i, :])
                        self.mm(po[:, 0:128], att16[p][:], v16[:, i, :], start=True, stop=False)
                        self.mm(po[:, 0:128], qdS[r][:], S16s[(n + 1) % 2][:], start=False, stop=True)
                        self.stt("dve", S32[:], S32[:], decs[r][:, 0:1], pS[:, 0:128], ALU.mult, ALU.add)
                        self.copy("act", S16s[n % 2][:], S32[:])
                        if di == 0:
                            self.copy("act", oacc[:, i, :], po[:, 0:128])
                        else:
                            self.tt("dve", oacc[:, i, :], oacc[:, i, :], po[:, 0:128], ALU.add)

                    LOOK = 2
                    for n in range(min(LOOK, NT)):
                        stageA(n, order[n])
                    for n, i in enumerate(order):
                        if n + LOOK < NT:
                            stageA(n + LOOK, order[n + LOOK])
                        stageB(n, i)
                for i in range(NT):
                    ps = self.PS[3 + i % 2]
                    for kc in range(8):
                        self.mm(ps[:, 0:128], uT[:, kc, i * 128:(i + 1) * 128], wh[:, kc, 512:640], start=(kc == 0), stop=(kc == 7))
                    self.act(ogs[:], ps[:, 0:128], AF.Silu)
                    self.act(rr[:], oacc[:, i, :], AF.Square, accum_out=self.small[:, 2:3])
                    self.rstd_from_ss(self.small[:, 3:4], self.small[:, 2:3], 128)
                    self.stt("dve", rr[:], oacc[:, i, :], self.small[:, 3:4], nwb[:], ALU.mult, ALU.mult)
                    self.tt("dve", rr[:], rr[:], ogs[:], ALU.mult)
                    pt = self.PS[5]
                    self.tr(pt[:, (i % 4) * 128:(i % 4 + 1) * 128], rr[:], cst["ident_f"][:])
                    self.copy("act", mixT[:, h, i * 128:(i + 1) * 128], pt[:, (i % 4) * 128:(i % 4 + 1) * 128])
            if self.stop_after == "l0_p1":
                raise StopBuild()
            wp = whs[0]
            self.dma("pool", wp[:, :, 0:512], w_in[:, 2560:3072].rearrange("(kc p) n -> p kc n", p=128))
            OFFC, OFFL = 16, 16 + CTX + 16
            WB = OFFL + LAT + 16
            d16 = self.sb("d16", [128, SEQ], BF16)
            for g in range(4):
                w = 2 << g
                half = w // 2
                PBf, T1, T2 = q32, kin, A
                self.memset("pool", PBf[:, 0:WB], 0.0)
                for gi, (g0, gn) in enumerate(groups):
                    ps = self.PS[gi % 2]
                    for kc in range(8):
                        self.mm(ps[:, 0:gn], wp[:, kc, g * 128:(g + 1) * 128], uT[:, kc, g0:g0 + gn], start=(kc == 0), stop=(kc == 7))
                    a0, a1 = g0, g0 + gn
                    if a0 < CTX:
                        n_c = min(a1, CTX) - a0
                        self.copy("act", PBf[:, OFFC + a0:OFFC + a0 + n_c], ps[:, 0:n_c])
                        if a1 > CTX:
                            self.copy("act", PBf[:, OFFL:OFFL + (a1 - CTX)], ps[:, n_c:gn])
                    else:
                        self.copy("act", PBf[:, OFFL + a0 - CTX:OFFL + a1 - CTX], ps[:, 0:gn])
                cur = PBf
                step = 1
                tmp = [T1, T2]
                ti = 0
                width = WB
                while step < w:
                    nxt = tmp[ti % 2]
                    ti += 1
                    width2 = width - step
                    self.tt("dve", nxt[:, 0:width2], cur[:, 0:width2], cur[:, step:step + width2], ALU.add)
                    cur, width, step = nxt, width2, step * 2
                dst = tmp[ti % 2]
                for off, L, tcol in ((OFFC, CTX, 0), (OFFL, LAT, CTX)):
                    sw = cur[:, off - half:off - half + L]
                    self.tt("pool", sw[:, 0:8], sw[:, 0:8], facS[:, g, :], ALU.mult, reads=[cur, facS], writes=[cur])
                    self.tt("pool", sw[:, L - 8:L], sw[:, L - 8:L], facE[:, g, :], ALU.mult, reads=[cur, facE], writes=[cur])
                    self.stt("dve", d16[:, tcol:tcol + L], sw, 1.0 / w, PBf[:, off:off + L], ALU.mult, ALU.subtract,
                             reads=[cur, PBf], writes=[d16])
                for gi, (g0, gn) in enumerate(groups):
                    ps = self.PS[2 + gi % 2]
                    self.mm(ps[:, 0:gn], poolw[:, g, :], d16[:, g0:g0 + gn])
                    self.ts("dve", mixT[:, 4 + g, g0:g0 + gn], ps[:, 0:gn], pscale[:, g:g + 1], None, ALU.mult)
            if "mixT" in self.dbg and b == 0:
                d = self.dbg_dump("mixT", [128, 8, SEQ], BF16)
                self.dma("sp", d[:, :, :], mixT[:])
            self.release(mk)
            if self.stop_after == "l0_p3":
                raise StopBuild()
            self.post_mixer(0, b, mixT, NT, wr, brr)
        self.release(mk0)

    def rms_rows(self, out, src, n, gain_b, sq_junk, c0):
        sm = self.small
        self.act(sq_junk, src, AF.Square, accum_out=sm[:, c0:c0 + 1])
        self.rstd_from_ss(sm[:, c0 + 1:c0 + 2], sm[:, c0:c0 + 1], n)
        self.stt("dve", out, src, sm[:, c0 + 1:c0 + 2], gain_b, ALU.mult, ALU.mult)

    def head_norm(self, xf, gain_b, tmp, hs, rs):
        self.tt("dve", tmp[:], xf[:], xf[:], ALU.mult)
        self.v("dve", "reduce_sum", hs[:], tmp[:], AX.X, reads=[tmp], writes=[hs])
        self.ts("dve", rs[:], hs[:], 1.0 / 96, EPS, ALU.mult, ALU.add)
        self.act(rs[:], rs[:], AF.Sqrt)
        self.v("dve", "reciprocal", rs[:], rs[:], reads=[rs], writes=[rs])
        self.tt("dve", xf[:], xf[:], rs[:].unsqueeze(2).to_broadcast([128, 8, 96]), ALU.mult, reads=[xf, rs], writes=[xf])
        self.tt("dve", xf[:], xf[:], gain_b[:].unsqueeze(1).to_broadcast([128, 8, 96]), ALU.mult, reads=[xf, gain_b], writes=[xf])

    def rope_all(self, xo, xf, cosr, sinr, tt_):
        s5 = xf[:, :, 64:96].rearrange("p h (a t i) -> p h a t i", a=2, t=2)
        d5 = xo[:, :, 64:96].rearrange("p h (a t i) -> p h a t i", a=2, t=2)
        cb = cosr.rearrange("p (a i) -> p a i", a=2).unsqueeze(1).to_broadcast([128, 8, 2, 8])
        sb_ = sinr.rearrange("p (a i) -> p a i", a=2).unsqueeze(1).to_broadcast([128, 8, 2, 8])
        t = [x[:].rearrange("p h (a i) -> p h a i", a=2) for x in tt_]
        x1, x2 = s5[:, :, :, 0, :], s5[:, :, :, 1, :]
        self.tt("dve", t[0], x1, cb, ALU.mult, reads=[xf, cosr], writes=[tt_[0]])
        self.tt("dve", t[1], x2, sb_, ALU.mult, reads=[xf, sinr], writes=[tt_[1]])
        self.tt("dve", t[2], x2, cb, ALU.mult, reads=[xf, cosr], writes=[tt_[2]])
        self.tt("dve", t[3], x1, sb_, ALU.mult, reads=[xf, sinr], writes=[tt_[3]])
        self.tt("dve", d5[:, :, :, 0, :], t[0], t[1], ALU.subtract, reads=[tt_[0], tt_[1]], writes=[xo])
        self.tt("dve", d5[:, :, :, 1, :], t[2], t[3], ALU.add, reads=[tt_[2], tt_[3]], writes=[xo])

    def rope(self, dst, src, cosr, sinr, t1, t2, eng):
        s4 = src.rearrange("p (a h i) -> p a h i", a=2, h=2)
        d4 = dst.rearrange("p (a h i) -> p a h i", a=2, h=2)
        c3 = cosr.rearrange("p (a i) -> p a i", a=2)
        s3 = sinr.rearrange("p (a i) -> p a i", a=2)
        a3 = t1.rearrange("p (a i) -> p a i", a=2)
        b3 = t2.rearrange("p (a i) -> p a i", a=2)
        x1, x2 = s4[:, :, 0, :], s4[:, :, 1, :]
        self.tt(eng, a3, x1, c3, ALU.mult, reads=[src, cosr], writes=[t1])
        self.tt(eng, b3, x2, s3, ALU.mult, reads=[src, sinr], writes=[t2])
        self.tt(eng, d4[:, :, 0, :], a3, b3, ALU.subtract, reads=[t1, t2], writes=[dst])
        self.tt(eng, a3, x2, c3, ALU.mult, reads=[src, cosr], writes=[t1])
        self.tt(eng, b3, x1, s3, ALU.mult, reads=[src, sinr], writes=[t2])
        self.tt(eng, d4[:, :, 1, :], a3, b3, ALU.add, reads=[t1, t2], writes=[dst])

    def layer1(self):
        nc, S, I, C = self.nc, self.S, self.I, self.C
        ns, cst = self.ns, self.cst
        mk0 = self.mark()
        w_in = I["od_w_in"][0]
        NL = LAT // 128
        wr = self.sb("wr1", [128, 8, 32], F32)
        self.dma("sp", wr[:], I["moe_router_w"][1].rearrange("(kc p) e -> p kc e", p=128))
        brr = self.sb("brr1", [1, 32], F32)
        self.dma("sp", brr[:], I["moe_router_b"][1:2, :])
        rcos = self.sb("rcos", [128, 16, 16], F32)
        rsin = self.sb("rsin", [128, 16, 16], F32)
        self.dma("sp", rcos[:], C["rope_cos"])
        self.dma("sp", rsin[:], C["rope_sin"])
        dww = self.sb("dww", [128, 4, 31], F32)
        dwr = self.sb("dwr", [32, 512], F32)
        self.dma("sp", dwr[0:31, :], I["conv_dw_w"][0])
        for cc in range(4):
            self.tr(self.PS[0][:, cc * 32:cc * 32 + 31], dwr[0:31, cc * 128:(cc + 1) * 128], self.cst["ident_f"][0:31, 0:31])
            self.copy("dve", dww[:, cc, :], self.PS[0][:, cc * 32:cc * 32 + 31])
        cpar = self.sb("cpar", [128, 3, 4], F32)
        for j, nm in enumerate(("conv_dw_b", "conv_ln_w", "conv_ln_b")):
            self.dma("sp", cpar[:, j, :], I[nm][0].rearrange("(c p) -> p c", p=128), allow_slow_non_contiguous=True)
        qan = self.sb("qan", [128, 384], F32)
        kvan = self.sb("kvan", [128, 256], F32)
        qnb = self.sb("qnb", [128, 96], F32)
        knb = self.sb("knb", [128, 96], F32)
        self.dma("sp", qan[:], I["mla_q_a_norm"][0:1, :].partition_broadcast(128))
        self.dma("sp", kvan[:], I["mla_kv_a_norm"][0:1, :].partition_broadcast(128))
        self.dma("sp", qnb[:], I["mla_q_norm"][0:1, :].partition_broadcast(128))
        self.dma("sp", knb[:], I["mla_k_norm"][0:1, :].partition_broadcast(128))
        wq = self.sb("wq", [128, 8, 384], BF16)
        wkv = self.sb("wkv", [128, 8, 288], BF16)
        wuq = self.sb("wuq", [128, 3, 768], BF16)
        wukv = self.sb("wukv", [128, 2, 1024], BF16)
        self.dma("pool", wq[:], w_in[:, 1024:1408].rearrange("(kc p) n -> p kc n", p=128))
        self.dma("pool", wkv[:], w_in[:, 1408:1696].rearrange("(kc p) n -> p kc n", p=128))
        self.dma("pool", wuq[:], I["mla_w_uq"][0].rearrange("(kc p) n -> p kc n", p=128))
        self.dma("pool", wukv[:], I["mla_w_ukv"][0].rearrange("(kc p) n -> p kc n", p=128))
        mixA = self.sb("mixA", [128, 4, LAT], BF16)
        sm = self.small
        SC = 96 ** -0.5
        for b in range(ns):
            mkb = self.mark()

            def make_uT(per_tile=None):
                uT = self.sb("uT1", [128, 8, SEQ], BF16) if per_tile is None else None
                mk = self.mark()
                uTts = [self.sb(f"uTt{i}", [128, 8, 128], BF16) for i in range(2)] if per_tile is not None else None
                mr = self.sb("mr", [128, 4, D], F32)
                self.modrow(mr[:, 0, :], 1, b, 1)
                self.modrow(mr[:, 1, :], 1, b, 0)
                self.modrow(mr[:, 2, :], 1, 4, 1)
                self.modrow(mr[:, 3, :], 1, 4, 0)
                self.ts("dve", mr[:, 0, :], mr[:, 0, :], 1.0, None, ALU.add)
                self.ts("dve", mr[:, 2, :], mr[:, 2, :], 1.0, None, ALU.add)
                hts = [self.sb(f"ht{i}", [128, D], F32) for i in range(2)]
                us = [self.sb(f"u{i}", [128, D], F32) for i in range(1 if per_tile is not None else 2)]
                for i in range(NT):
                    ht, u = hts[i % 2], us[i % len(us)]
                    self.dma("sp", ht[:], self.H2[b * SEQ + i * 128:b * SEQ + (i + 1) * 128, :], reads=[self.H2])
                    o = 2 if i < 2 else 0
                    self.modulate_tile(u[:], ht[:], mr[:, o, :], mr[:, o + 1, :], u[:], sm[:, 0:1], sm[:, 1:2])
                    if per_tile is None:
                        self.transpose_tile_to(uT, i * 128, u, 8, cst["ident_f"], self.PS[0], self.PS[1])
                    else:
                        self.transpose_tile_to(uTts[i % 2], 0, u, 8, cst["ident_f"], self.PS[0], self.PS[1])
                        per_tile(i, uTts[i % 2])
                self.release(mk)
                return uT

            mkA = self.mark()
            uT = make_uT()
            mk = self.mark()
            wc = self.sb("wc", [128, 8, 1024], BF16)
            self.dma("pool", wc[:], w_in[:, 0:1024].rearrange("(kc p) n -> p kc n", p=128))
            hb = self.sb("hb", [128, LAT + 32], F32)
            cv = [self.sb(f"cv{i}", [128, LAT], F32) for i in range(4)]
            sgt = [self.sb(f"sgt{i}", [128, 512], F32) for i in range(2)]
            self.memset("pool", hb[:], 0.0)
            for cc in range(4):
                for tg in range(4):
                    pv, pg = self.PS[(tg % 2) * 2], self.PS[(tg % 2) * 2 + 1]
                    cols = slice(CTX + tg * 512, CTX + (tg + 1) * 512)
                    for kc in range(8):
                        self.mm(pv[:], wc[:, kc, cc * 128:(cc + 1) * 128], uT[:, kc, cols], start=(kc == 0), stop=(kc == 7))
                    for kc in range(8):
                        self.mm(pg[:], wc[:, kc, 512 + cc * 128:512 + (cc + 1) * 128], uT[:, kc, cols], start=(kc == 0), stop=(kc == 7))
                    st = sgt[tg % 2]
                    self.act(st[:], pg[:], AF.Sigmoid)
                    self.tt("dve", hb[:, 15 + tg * 512:15 + (tg + 1) * 512], pv[:], st[:], ALU.mult)
                NH = 4
                HW_ = LAT // NH
                for j in range(31):
                    for hf in range(NH):
                        o0 = hf * HW_
                        acc = cv[cc][:, o0:o0 + HW_]
                        if j == 0:
                            self.ts("dve", acc, hb[:, o0:o0 + HW_], dww[:, cc, 0:1], cpar[:, 0, cc:cc + 1], ALU.mult, ALU.add,
                                    reads=[hb, dww, cpar], writes=[(cv[cc], hf)])
                        else:
                            self.stt("dve", acc, hb[:, o0 + j:o0 + j + HW_], dww[:, cc, j:j + 1], acc, ALU.mult, ALU.add,
                                     reads=[hb, dww, (cv[cc], hf)], writes=[(cv[cc], hf)])
            mean = self.sb("lnm", [128, 512], F32)
            rstd = self.sb("lnr", [128, 512], F32)
            sq = self.sb("lnsq", [128, 512], F32)
            xn = [self.sb(f"lnx{i}", [128, 512], F32) for i in range(2)]
            for tg in range(4):
                cols = slice(tg * 512, (tg + 1) * 512)
                ps_s, ps_q = self.PS[4], self.PS[5]
                for cc in range(4):
                    self.mm(ps_s[:], cst["ones_f"][:], cv[cc][:, cols], start=(cc == 0), stop=(cc == 3))
                for cc in range(4):
                    self.act(sq[:], cv[cc][:, cols], AF.Square)
                    self.mm(ps_q[:], cst["ones_f"][:], sq[:], start=(cc == 0), stop=(cc == 3))
                self.ts("dve", mean[:], ps_s[:], 1.0 / 512, None, ALU.mult)
                self.tt("dve", rstd[:], mean[:], mean[:], ALU.mult)
                self.stt("dve", rstd[:], ps_q[:], 1.0 / 512, rstd[:], ALU.mult, ALU.subtract)
                self.ts("dve", rstd[:], rstd[:], EPS, None, ALU.add)
                self.act(rstd[:], rstd[:], AF.Sqrt)
                self.v("dve", "reciprocal", rstd[:], rstd[:], reads=[rstd], writes=[rstd])
                for cc in range(4):
                    x = xn[cc % 2]
                    self.tt("pool", x[:], cv[cc][:, cols], mean[:], ALU.subtract)
                    self.tt("dve", x[:], x[:], rstd[:], ALU.mult)
                    self.act(mixA[:, cc, cols], x[:], AF.Silu, bias=cpar[:, 2, cc:cc + 1], scale=cpar[:, 1, cc:cc + 1])
            self.release(mkA)
            if self.stop_after == "l1_conv":
                raise StopBuild()
            mixB = self.sb("mixB", [128, 4, LAT], BF16)
            mkq = self.mark()
            qT = self.sb("qT", [128, 8, LAT], BF16)
            kT = self.sb("kT", [128, 8, SEQ], BF16)
            vaug = self.sb("vaug", [128, NT, 8, 68], BF16)
            mk = self.mark()
            self.memset("pool", vaug[:], 1.0)
            cn = self.sb("cn", [128, 384], F32)
            cnT = self.sb("cnT", [128, 3, 128], BF16)
            xf = self.sb("xf", [128, 8, 96], F32)
            xo = self.sb("xo", [128, 8, 96], F32)
            tmp = self.sb("hn_tmp", [128, 8, 96], F32)
            hs = self.sb("hn_hs", [128, 8], F32)
            rs = self.sb("hn_rs", [128, 8], F32)
            rtt = [self.sb(f"rtt{i}", [128, 8, 16], F32) for i in range(4)]
            krr = self.sb("krr", [128, 32], F32)
            krg = self.sb("krg", [128, 32], F32)
            junk2 = self.sb("junk2", [128, 384], F32)
            def proj_tile(i, uTt):
                import os as _os
                _stg = float(_os.environ.get("KSTG", "9"))
                if _stg <= 0:
                    return
                lat_i = i - 2
                tok = slice(i * 128, (i + 1) * 128)
                pk = self.PS[2]
                for kc in range(8):
                    self.mm(pk[:, 0:288], uTt[:, kc, :], wkv[:, kc, :], start=(kc == 0), stop=(kc == 7))
                if _stg <= 0.1:
                    return
                self.rms_rows(cn[:, 0:256], pk[:, 0:256], 256, kvan[:], junk2[:, 0:256], 10)
                if _stg <= 0.2:
                    return
                self.tt("dve", krg[:], pk[:, 256:288], knb[:, 64:96], ALU.mult)
                if _stg <= 0.3:
                    return
                pt = self.PS[3]
                for kc in range(2):
                    self.tr(pt[:, kc * 128:(kc + 1) * 128], cn[:, kc * 128:(kc + 1) * 128], cst["ident_f"][:])
                self.copy("act", cnT[:, 0:2, :], pt[:, 0:256].rearrange("p (k c) -> p k c", c=128))
                if _stg <= 0.4:
                    return
                pkv = [self.PS[4], self.PS[5]]
                for half in range(2):
                    for kc in range(2):
                        self.mm(pkv[half][:], cnT[:, kc, :], wukv[:, kc, half * 512:(half + 1) * 512], start=(kc == 0), stop=(kc == 1))
                    v4 = pkv[half][:, :].rearrange("p (h d) -> p h d", d=128)
                    if _stg <= 0.5:
                        continue
                    if _stg != 0.56:
                        self.copy("act", xf[:, half * 4:(half + 1) * 4, 0:64], v4[:, :, 0:64])
                    if _stg != 0.55:
                        self.copy("act" if _stg == 0.57 else "dve", vaug[:, i, half * 4:(half + 1) * 4, 0:64], v4[:, :, 64:128])
                if _stg <= 0.6:
                    return
                self.copy("dve", xf[:, :, 64:96], pk[:, 256:288].unsqueeze(1).to_broadcast([128, 8, 32]), reads=[pk], writes=[xf])
                if _stg <= 1:
                    return
                self.head_norm(xf, knb, tmp, hs, rs)
                if _stg <= 2:
                    return
                if lat_i >= 0:
                    self.rope_all(xo, xf, rcos[:, lat_i, :], rsin[:, lat_i, :], rtt)
                    self.copy("act", xo[:, :, 0:64], xf[:, :, 0:64])
                    src = xo
                else:
                    src = xf
                pa, pb_ = self.PS[4], self.PS[5]
                for h in range(8):
                    pp = pa if h < 4 else pb_
                    self.tr(pp[0:96, (h % 4) * 128:(h % 4 + 1) * 128], src[:, h, :], cst["ident_f"][:])
                self.copy("act", kT[0:96, 0:4, tok], pa[0:96, :].rearrange("p (k c) -> p k c", c=128))
                self.copy("dve", kT[0:96, 4:8, tok], pb_[0:96, :].rearrange("p (k c) -> p k c", c=128))
                if lat_i < 0 or _stg <= 3:
                    return
                ltok = slice(lat_i * 128, (lat_i + 1) * 128)
                pq = self.PS[2]
                for kc in range(8):
                    self.mm(pq[:, 0:384], uTt[:, kc, :], wq[:, kc, :], start=(kc == 0), stop=(kc == 7))
                if _stg <= 3.1:
                    return
                self.rms_rows(cn[:, 0:384], pq[:, 0:384], 384, qan[:], junk2[:, 0:384], 12)
                if _stg <= 3.2:
                    return
                pt = self.PS[3]
                for kc in range(3):
                    self.tr(pt[:, kc * 128:(kc + 1) * 128], cn[:, kc * 128:(kc + 1) * 128], cst["ident_f"][:])
                self.copy("act", cnT[:, 0:3, :], pt[:, 0:384].rearrange("p (k c) -> p k c", c=128))
                if _stg <= 3.3:
                    return
                pq1, pq2 = self.PS[4], self.PS[5]
                for kc in range(3):
                    self.mm(pq1[:, 0:384], cnT[:, kc, :], wuq[:, kc, 0:384], start=(kc == 0), stop=(kc == 2))
                for kc in range(3):
                    self.mm(pq2[:, 0:384], cnT[:, kc, :], wuq[:, kc, 384:768], start=(kc == 0), stop=(kc == 2))
                if _stg <= 3.4:
                    return
                self.copy("act", xf[:, 0:4, :], pq1[:, 0:384].rearrange("p (h d) -> p h d", d=96))
                self.copy("dve", xf[:, 4:8, :], pq2[:, 0:384].rearrange("p (h d) -> p h d", d=96))
                if _stg <= 3.5:
                    return
                self.head_norm(xf, qnb, tmp, hs, rs)
                if _stg <= 3.6:
                    return
                self.rope_all(xo, xf, rcos[:, lat_i, :], rsin[:, lat_i, :], rtt)
                if _stg <= 3.7:
                    return
                self.copy("act", xo[:, :, 0:64], xf[:, :, 0:64])
                for h in range(8):
                    pp = pa if h < 4 else pb_
                    self.tr(pp[0:96, (h % 4) * 128:(h % 4 + 1) * 128], xo[:, h, :], cst["ident_f"][:])
                if _stg <= 3.8:
                    return
                self.copy("act", qT[0:96, 0:4, ltok], pa[0:96, :].rearrange("p (k c) -> p k c", c=128))
                self.copy("dve", qT[0:96, 4:8, ltok], pb_[0:96, :].rearrange("p (k c) -> p k c", c=128))

            make_uT(per_tile=proj_tile)
            self.release(mk)
            if self.stop_after == "l1_proj":
                raise StopBuild()
            mk = self.mark()
            attn = self.sb("attn", [128, NL, 512], BF16)
            PT = [self.sb(f"PT{i}", [128, 512], BF16) for i in range(3)]
            rden = self.sb("rden", [128, 4], F32)
            n = 0
            for h in range(8):
                for qg in range(4):
                    qcols = slice(qg * 512, (qg + 1) * 512)
                    po = self.PS[4 + (h * 4 + qg) % 2]
                    def s_mm(kt_, n_):
                        ps_ = self.PS[n_ % 4]
                        self.mm(ps_[:], kT[0:96, h, kt_ * 128:(kt_ + 1) * 128], qT[0:96, h, qcols])
                        self.act(PT[n_ % 3][:], ps_[:], AF.Exp, scale=SC)

                    s_mm(0, n)
                    for kt in range(NT):
                        if kt + 1 < NT:
                            s_mm(kt + 1, n + 1)
                        p_ = PT[n % 3]
                        for qt in range(4):
                            self.mm(po[:, qt * 68:(qt + 1) * 68], p_[:, qt * 128:(qt + 1) * 128], vaug[:, kt, h, :],
                                    start=(kt == 0), stop=(kt == NT - 1))
                        n += 1
                    for qt in range(4):
                        self.v("dve", "reciprocal", rden[:, qt:qt + 1], po[:, qt * 68 + 64:qt * 68 + 65], reads=[po], writes=[rden])
                        self.ts("dve", attn[:, qg * 4 + qt, h * 64:(h + 1) * 64], po[:, qt * 68:qt * 68 + 64], rden[:, qt:qt + 1], None, ALU.mult,
                                reads=[po, rden], writes=[(attn, qg * 4 + qt)])
            for i in range(NL):
                pb = self.PB[i % 2]
                for c in range(4):
                    self.tr(pb[:, c * 128:(c + 1) * 128], attn[:, i, c * 128:(c + 1) * 128], cst["ident_b"][:])
                self.copy("act" if i % 2 == 0 else "dve", mixB[:, :, i * 128:(i + 1) * 128], pb[:, 0:512].rearrange("p (k c) -> p k c", c=128))
            self.release(mkq)
            if self.stop_after == "l1_attn":
                raise StopBuild()
            self.post_mixer(1, b, [mixA, mixB], NL, wr, brr)
            self.release(mkb)
        self.release(mk0)

    def post_mixer(self, layer, b, mixT, ntile, wr, brr):
        nc, S, I, C = self.nc, self.S, self.I, self.C
        cst = self.cst
        mk = self.mark()
        w_out = I["ev_w_out"][0] if layer == 0 else I["od_w_out"][0]
        wo = self.sb("wo", [128, 8, D], BF16)
        self.dma("pool", wo[:], w_out.rearrange("(kc p) n -> p kc n", p=128))
        mr = self.sb("mr2", [128, 6, D], F32)
        self.modrow(mr[:, 0, :], layer, b, 2)
        self.modrow(mr[:, 1, :], layer, b, 4)
        self.modrow(mr[:, 2, :], layer, b, 3)
        self.ts("dve", mr[:, 1, :], mr[:, 1, :], 1.0, None, ALU.add)
        if layer == 0:
            self.modrow(mr[:, 3, :], layer, 4, 2)
            self.modrow(mr[:, 4, :], layer, 4, 4)
            self.modrow(mr[:, 5, :], layer, 4, 3)
            self.ts("dve", mr[:, 4, :], mr[:, 4, :], 1.0, None, ALU.add)
        hts = [self.sb(f"pht{i}", [128, D], F32) for i in range(2)]
        h1s = [self.sb(f"ph1{i}", [128, D], F32) for i in range(2)]
        ms = [self.sb(f"pm{i}", [128, D], F32) for i in range(2)]
        m16 = [self.sb(f"pm16{i}", [128, D], BF16) for i in range(2)]
        mT = self.sb("pmT", [128, 8, 128], F32)
        junk = self.sb("pjunk", [128, D], F32)
        sm = self.small
        for i in range(ntile):
            if layer == 0:
                isctx = i < 2
                src = I["ctx"][b, i * 128:(i + 1) * 128, :] if isctx else I["x"][b, (i - 2) * 128:(i - 1) * 128, :]
                grow = b * SEQ + i * 128
                hdst = self.H1
            else:
                isctx = False
                grow = b * LAT + i * 128
                src = self.H2[b * SEQ + CTX + i * 128:b * SEQ + CTX + (i + 1) * 128, :]
                hdst = self.H1
            o = 3 if isctx else 0
            ht, h1, m, mb = hts[i % 2], h1s[i % 2], ms[i % 2], m16[i % 2]
            self.dma("sp", ht[:], src)
            p0, p1 = self.PS[0 + 2 * (i % 2)], self.PS[1 + 2 * (i % 2)]
            for half, ps in enumerate((p0, p1)):
                for kc in range(8):
                    mx = mixT[kc // 4] if isinstance(mixT, (list, tuple)) else mixT
                    kcc = kc % 4 if isinstance(mixT, (list, tuple)) else kc
                    self.mm(ps[:], mx[:, kcc, i * 128:(i + 1) * 128], wo[:, kc, half * 512:(half + 1) * 512],
                            start=(kc == 0), stop=(kc == 7))
                self.tt("dve", h1[:, half * 512:(half + 1) * 512], ps[:], mr[:, o, half * 512:(half + 1) * 512], ALU.mult)
            self.tt("pool", h1[:], h1[:], ht[:], ALU.add)
            self.dma("sp", hdst[grow:grow + 128, :], h1[:], writes=[(hdst, grow)])
            self.modulate_tile(m[:], h1[:], mr[:, o + 1, :], mr[:, o + 2, :], junk[:], sm[:, 4:5], sm[:, 5:6])
            self.copy("act", mb[:], m[:])
            self.dma("sp", self.M[grow:grow + 128, :], mb[:], writes=[(self.M, grow)])
            self.transpose_tile_to(mT, 0, m, 8, cst["ident_f"], self.PS[4], self.PS[5])
            pl = self.PS[4]
            for kc in range(8):
                self.mm(pl[:, 0:32], mT[:, kc, :], wr[:, kc, :], start=(kc == 0), stop=False)
            self.mm(pl[:, 0:32], cst["ones_f"][0:1, :], brr[0:1, :], start=False, stop=True)
            self.route_tile(layer, grow // 128, pl)
        self.release(mk)

    def route_init(self, layer):
        ns = self.ns
        T = self.T0 if layer == 0 else self.T1
        self.rT = T
        CAP = self.CAP = CAPS[layer]
        NSLOT = self.NSLOT = NE * CAP
        ntt = T // 128
        self.cntb = self.sb(f"cntb{layer}", [128, NE], F32)
        self.DEST = self.sb(f"DEST{layer}", [128, ntt, 4], I32)
        self.GATE = self.sb(f"GATE{layer}", [128, ntt, 4], F32)
        self.rt = {n: self.sb(f"rt_{n}{layer}", sh, dt) for n, sh, dt in (
            ("L", [128, NE], F32), ("t8", [128, 8], F32), ("e4", [128, 4], F32), ("mask", [128, NE], F32),
            ("m16", [128, NE], BF16), ("rf", [128, NE], F32), ("rfs", [128, NE], F32), ("tmp", [128, NE], F32),
            ("rk", [128, 4], F32), ("dk", [128, 4], F32), ("ok", [128, 4], F32), ("tok", [128, 2], F32),
            ("toki", [128, 2], I32), ("trash", [128, 1], F32), ("fill", [128, (NSLOT + 128) // 64], I32),
            ("z16", [128, D], BF16))}
        rt = self.rt
        self.memset("dve", self.cntb[:], 0.0)
        self.ts("dve", rt["trash"][:], self.cst["pidx"][:], float(NSLOT), None, ALU.add)
        self.memset("dve", rt["fill"][:], int(T))
        self.dma("sp", self.SLOT[0:NSLOT + 128, :].rearrange("(p j) two -> p (j two)", p=128), rt["fill"][:])
        self.memset("pool", rt["z16"][:], 0.0)
        self.dma("sp", self.M[T:T + 128, :], rt["z16"][:], writes=[(self.M, "trash")])
        self.dma("sp", self.YB[NSLOT:NSLOT + 128, :], rt["z16"][:], writes=[(self.YB, "trash")])

    def route_tile(self, layer, gt, pl):
        nc, rt, cst = self.nc, self.rt, self.cst
        CAP, NSLOT = self.CAP, self.NSLOT
        L, t8 = rt["L"], rt["t8"]
        self.copy("dve", L[:], pl[:, 0:NE])
        self.v("dve", "max", t8[:], L[:], reads=[L], writes=[t8])
        sm = self.small
        self.ts("dve", sm[:, 8:9], t8[:, 0:1], -1.0, None, ALU.mult)
        self.act(rt["e4"][:], t8[:, 0:4], AF.Exp, bias=sm[:, 8:9], scale=1.0)
        self.v("dve", "reduce_sum", sm[:, 9:10], rt["e4"][:], AX.X, reads=[rt["e4"]], writes=[sm])
        self.v("dve", "reciprocal", sm[:, 9:10], sm[:, 9:10], reads=[sm], writes=[sm])
        self.ts("dve", rt["e4"][:], rt["e4"][:], sm[:, 9:10], None, ALU.mult)
        self.ts("dve", rt["mask"][:], L[:], t8[:, 3:4], None, ALU.is_ge)
        self.copy("dve", rt["m16"][:], rt["mask"][:])
        pr = self.PS[5]
        self.mm(pr[:, 0:NE], cst["tri_b"][:], rt["m16"][:])
        self.mm(pr[:, 64:64 + NE], cst["ones_b"][:], rt["m16"][:])
        self.tt("dve", rt["rf"][:], pr[:, 0:NE], self.cntb[:], ALU.add)
        self.tt("dve", self.cntb[:], self.cntb[:], pr[:, 64:64 + NE], ALU.add)
        self.tt("dve", rt["rfs"][:], rt["rf"][:], cst[f"slotbase{layer}"][:], ALU.add)
        for k in range(4):
            self.stt("dve", rt["tmp"][:], L[:], t8[:, k:k + 1], rt["rf"][:], ALU.is_equal, ALU.mult)
            self.v("dve", "reduce_sum", rt["rk"][:, k:k + 1], rt["tmp"][:], AX.X, reads=[rt["tmp"]], writes=[rt["rk"]])
            self.stt("dve", rt["tmp"][:], L[:], t8[:, k:k + 1], rt["rfs"][:], ALU.is_equal, ALU.mult)
            self.v("dve", "reduce_sum", rt["dk"][:, k:k + 1], rt["tmp"][:], AX.X, reads=[rt["tmp"]], writes=[rt["dk"]])
        self.ts("dve", rt["ok"][:], rt["rk"][:], float(CAP), None, ALU.is_lt)
        self.stt("dve", rt["dk"][:], rt["dk"][:], rt["trash"][:, 0:1], rt["ok"][:], ALU.subtract, ALU.mult)
        self.ts("dve", rt["dk"][:], rt["dk"][:], rt["trash"][:, 0:1], None, ALU.add)
        self.tt("dve", self.GATE[:, gt, :], rt["e4"][:], rt["ok"][:], ALU.mult, writes=[(self.GATE, gt)])
        self.copy("dve", self.DEST[:, gt, :], rt["dk"][:], writes=[(self.DEST, gt)])
        self.ts("dve", rt["tok"][:, 0:1], cst["pidx"][:], float(gt * 128), None, ALU.add)
        self.copy("dve", rt["tok"][:, 1:2], rt["tok"][:, 0:1])
        self.copy("dve", rt["toki"][:], rt["tok"][:])
        for k in range(4):
            self.dma("pool", self.SLOT[:, :], rt["toki"][:, :], reads=[rt["toki"], (self.DEST, gt)], writes=[(self.SLOT, (gt, k))],
                     indirect=dict(out_offset=bass.IndirectOffsetOnAxis(ap=self.DEST[:, gt, k:k + 1], axis=0), in_offset=None))

    def experts(self, layer):
        nc, I, cst = self.nc, self.I, self.cst
        CAP, NSLOT = self.CAP, self.NSLOT
        mk = self.mark()
        w1s = [self.sb(f"w1s{i}", [128, 8, 2 * D], BF16) for i in range(2)]
        w2s = [self.sb(f"w2s{i}", [128, 8, D], BF16) for i in range(2)]
        b1s = [self.sb(f"b1s{i}", [128, 16], F32) for i in range(2)]
        b2s = [self.sb(f"b2s{i}", [128, D], F32) for i in range(2)]
        idx = [[self.sb(f"idx{s_}{i}", [128, 2], I32) for i in range(4)] for s_ in range(2)]
        xg = [[self.sb(f"xg{s_}{i}", [128, D], BF16) for i in range(4)] for s_ in range(2)]
        xT = [self.sb(f"xT{i}", [128, 8, 512], BF16) for i in range(2)]
        aT = [self.sb(f"aT{i}", [128, 8, 512], BF16) for i in range(2)]
        glc = [self.sb(f"glc{i}", [128, 512], F32) for i in range(2)]
        sig = [self.sb(f"sig{i}", [128, 512], F32) for i in range(2)]
        lin = [self.sb(f"lin{i}", [128, 512], F32) for i in range(2)]
        yt = [self.sb(f"yt{i}", [128, D], BF16) for i in range(2)]
        for xs in xg:
            for x in xs:
                self.memset("dve", x[:], 0.0)
        NG = CAP // 512
        items = [(e, g) for e in range(NE) for g in range(NG)]

        def load_weights(e):
            w1, w2, b1, b2 = w1s[e % 2], w2s[e % 2], b1s[e % 2], b2s[e % 2]
            src1 = I["moe_w1"][layer, e].rearrange("(kc p) n -> p kc n", p=128)
            for q in range(4):
                self.dma("pool", w1[:, 2 * q:2 * q + 2, :], src1[:, 2 * q:2 * q + 2, :], writes=[(w1, q)])
            src2 = I["moe_w2"][layer, e].rearrange("(kc p) n -> p kc n", p=128)
            for q in range(2):
                self.dma("pool", w2[:, 4 * q:4 * q + 4, :], src2[:, 4 * q:4 * q + 4, :], writes=[(w2, q)])
            self.dma("sp", b1[:], I["moe_b1"][layer, e].rearrange("(c p) -> p c", p=128), allow_slow_non_contiguous=True)
            self.dma("sp", b2[:], I["moe_b2"][layer, e:e + 1, :].partition_broadcast(128))
            self.ts("dve", b1[:, 8:16], b1[:, 8:16], 1.0, None, ALU.add)

        def load_tokens(k):
            e, grp = items[k]
            st = k % 2
            for j in range(4):
                r0 = e * CAP + (grp * 4 + j) * 128
                self.dma("sp", idx[st][j][:], self.SLOT[r0:r0 + 128, :], reads=[self.SLOT])
                self.dma("pool", xg[st][j][:], self.M[:, :], reads=[self.M, idx[st][j]],
                         indirect=dict(out_offset=None, in_offset=bass.IndirectOffsetOnAxis(ap=idx[st][j][:, 0:1], axis=0)))

        def stageT(k):
            e, grp = items[k]
            st = k % 2
            x_t = xT[k % 2]
            for j in range(4):
                pb = self.PB[j % 2]
                for kc in range(8):
                    self.tr(pb[:, kc * 128:(kc + 1) * 128], xg[st][j][:, kc * 128:(kc + 1) * 128], cst["ident_b"][:])
                self.copy("act", x_t[:, :, j * 128:(j + 1) * 128], pb[:, :].rearrange("p (k c) -> p k c", c=128))

        def stageH(k):
            e, grp = items[k]
            w1, b1 = w1s[e % 2], b1s[e % 2]
            x_t, a_t = xT[k % 2], aT[k % 2]
            for jc in range(8):
                pg, plin = self.PS[(jc % 2) * 2], self.PS[(jc % 2) * 2 + 1]
                for kc in range(8):
                    self.mm(pg[:], w1[:, kc, jc * 128:(jc + 1) * 128], x_t[:, kc, :], start=(kc == 0), stop=(kc == 7))
                for kc in range(8):
                    self.mm(plin[:], w1[:, kc, D + jc * 128:D + (jc + 1) * 128], x_t[:, kc, :], start=(kc == 0), stop=(kc == 7))
                g_, s_, l_ = glc[jc % 2], sig[jc % 2], lin[jc % 2]
                self.ts("dve", g_[:], pg[:], b1[:, jc:jc + 1], 7.0, ALU.add, ALU.min)
                self.act(s_[:], g_[:], AF.Sigmoid, scale=1.702)
                self.ts("dve", l_[:], plin[:], b1[:, 8 + jc:9 + jc], 8.0, ALU.add, ALU.min)
                self.tt("dve", g_[:], g_[:], s_[:], ALU.mult)
                self.stt("dve", a_t[:, jc, :], l_[:], -6.0, g_[:], ALU.max, ALU.mult)

        def stageY(k):
            e, grp = items[k]
            w2, b2 = w2s[e % 2], b2s[e % 2]
            a_t = aT[k % 2]
            for jt in range(4):
                y = yt[jt % 2]
                for half in range(2):
                    ps = self.PS[4 + half]
                    for jc in range(8):
                        self.mm(ps[:], a_t[:, jc, jt * 128:(jt + 1) * 128], w2[:, jc, half * 512:(half + 1) * 512],
                                start=(jc == 0), stop=(jc == 7))
                    self.tt("dve", y[:, half * 512:(half + 1) * 512], ps[:], b2[:, half * 512:(half + 1) * 512], ALU.add)
                r0 = e * CAP + (grp * 4 + jt) * 128
                self.dma("sp", self.YB[r0:r0 + 128, :], y[:], writes=[(self.YB, r0)])

        load_weights(0)
        load_tokens(0)
        if len(items) > 1:
            load_tokens(1)
        stageT(0)
        for k in range(len(items)):
            e, grp = items[k]
            if grp == 0 and e + 1 < NE:
                load_weights(e + 1)
            if k + 2 < len(items):
                load_tokens(k + 2)
            stageH(k)
            if k + 1 < len(items):
                stageT(k + 1)
            stageY(k)
        self.release(mk)

    def combine(self, layer):
        nc, I, cst = self.nc, self.I, self.cst
        ns = self.ns
        mk = self.mark()
        g2 = self.sb("g2", [128, 2, D], F32)
        yk = [self.sb(f"yk{i}", [128, D], BF16) for i in range(4)]
        acc = [self.sb(f"acc{i}", [128, D], F32) for i in range(2)]
        h1t = [self.sb(f"h1t{i}", [128, D], F32) for i in range(2)]
        if layer == 0:
            self.modrow(g2[:, 1, :], 0, 4, 5)
        nt_seq = NT if layer == 0 else LAT // 128
        for b in range(ns):
            self.modrow(g2[:, 0, :], layer, b, 5)
            for i in range(nt_seq):
                gt = b * nt_seq + i
                grow = gt * 128
                a, h = acc[gt % 2], h1t[gt % 2]
                self.dma("sp", h[:], self.H1[grow:grow + 128, :], reads=[self.H1])
                for k in range(4):
                    self.dma("pool", yk[k][:], self.YB[:, :], reads=[self.YB, (self.DEST, gt)],
                             indirect=dict(out_offset=None, in_offset=bass.IndirectOffsetOnAxis(ap=self.DEST[:, gt, k:k + 1], axis=0)))
                    if k == 0:
                        self.ts("dve", a[:], yk[0][:], self.GATE[:, gt, 0:1], None, ALU.mult, reads=[yk[0], (self.GATE, gt)])
                    else:
                        self.stt("dve", a[:], yk[k][:], self.GATE[:, gt, k:k + 1], a[:], ALU.mult, ALU.add,
                                 reads=[yk[k], a, (self.GATE, gt)])
                gsel = 1 if (layer == 0 and i < 2) else 0
                self.tt("pool", a[:], a[:], g2[:, gsel, :], ALU.mult)
                self.tt("dve", a[:], a[:], h[:], ALU.add)
                if layer == 0:
                    self.dma("sp", self.H2[grow:grow + 128, :], a[:], writes=[(self.H2, grow)])
                else:
                    self.dma("sp", self.out[b, i * 128:(i + 1) * 128, :], a[:], writes=[("out", grow)])
        self.release(mk)


def build_program(ns, **kw):
    k = K(ns, **kw)
    k.build()
    return k


_CACHE = {}


def kernel(**inputs):
    ncores, ns = 8, 4
    if "k" not in _CACHE:
        _CACHE["k"] = build_program(ns)
    k = _CACHE["k"]
    consts = {"k_" + n: v for n, v in host_consts().items()}
    shared = {n: np.ascontiguousarray(np.asarray(inputs[n], dtype=np.float32)) for n in IN_SPECS if n not in ("x", "c", "ctx")}
    in_maps = []
    for c in range(ncores):
        m = dict(shared)
        for n in ("x", "c", "ctx"):
            m[n] = np.ascontiguousarray(np.asarray(inputs[n], dtype=np.float32)[c * ns:(c + 1) * ns])
        m.update(consts)
        in_maps.append(m)
    res = run_bass_kernel_spmd(k.nc, in_maps, core_ids=list(range(ncores)))
    return np.concatenate([r["out"] for r in res.results], axis=0).astype(np.float32)
```

```python
import numpy as np
import ml_dtypes
import concourse.bass as bass
import concourse.mybir as mybir
from concourse.bass_utils import run_bass_kernel_spmd

F32 = mybir.dt.float32
BF16 = mybir.dt.bfloat16
I32 = mybir.dt.int32
AF = mybir.ActivationFunctionType
ALU = mybir.AluOpType
AX = mybir.AxisListType

D = 1024
LAT = 2048
CTX = 256
SEQ = LAT + CTX
NT = SEQ // 128
EPS = 1e-6
NE = 32
CAPS = (2560, 2048)
CAPMAX = max(CAPS)
NSLOTMAX = NE * CAPMAX


class Sched:
    EPOCH = 30000

    def __init__(self, nc):
        self.nc = nc
        self.E = {"pe": nc.tensor, "dve": nc.vector, "act": nc.scalar, "pool": nc.gpsimd, "sp": nc.sync}
        self.nsem = 0
        self.esem, self.ecnt = {}, {}
        self.pesems = set()
        for e in self.E:
            self._new_epoch(e)
        self.seen = {e: {} for e in self.E}
        self.W, self.R = {}, {}
        self.dpool = {q: [] for q in ("sp", "pool", "act")}
        self.dnext = {q: 0 for q in self.dpool}
        self.NPOOL = {"sp": 24, "pool": 24, "act": 4}
        self.ninst = 0
        self.psum = set()

    def _sem(self, name):
        self.nsem += 1
        return self.nc.semaphore(f"{name}{self.nsem}").__enter__()

    def _new_epoch(self, e):
        import os as _os
        self.skip_same = _os.environ.get("KSAME", "1") == "0"
        if not hasattr(self, "own"):
            self.own = {}
        self.esem[e] = self._sem("e" + e)
        self.own.setdefault(e, set()).add(self.esem[e])
        self.ecnt[e] = 0
        if e == "pe":
            self.pesems.add(self.esem[e])

    @staticmethod
    def _nm(x):
        if isinstance(x, str):
            return x
        t = getattr(x, "tensor", None)
        return t.name if t is not None else x.name

    @staticmethod
    def _key(x):
        if isinstance(x, tuple):
            return (Sched._nm(x[0]), x[1])
        return (Sched._nm(x), None)

    def _collect(self, table, key, evs):
        name, sub = key
        t = table.get(name)
        if not t:
            return
        subs = t.keys() if sub is None else [s for s in (sub, None) if s in t]
        for s in subs:
            for sem, val in t[s].items():
                if evs.get(sem, 0) < val:
                    evs[sem] = val

    def _deps(self, e, rk, wk):
        evs = {}
        for k in rk:
            self._collect(self.W, k, evs)
        for k in wk:
            self._collect(self.W, k, evs)
            self._collect(self.R, k, evs)
        for sem, val in evs.items():
            if e == "pe" and sem in self.pesems:
                continue
            if self.skip_same and sem in self.own.get(e, ()):
                continue
            if self.seen[e].get(sem, 0) >= val:
                continue
            self.E[e].wait_ge(sem, val)
            self.seen[e][sem] = val

    def _commit(self, ev, rk, wk):
        sem, val = ev
        for name, sub in rk:
            d = self.R.setdefault(name, {}).setdefault(sub, {})
            if d.get(sem, 0) < val:
                d[sem] = val
        for name, sub in wk:
            if sub is None:
                self.W[name] = {None: {sem: val}}
                self.R[name] = {}
            else:
                self.W.setdefault(name, {})[sub] = {sem: val}
                self.R.setdefault(name, {}).pop(sub, None)

    def op(self, e, fn, reads=(), writes=()):
        rk = [self._key(x) for x in reads]
        wk = [self._key(x) for x in writes]
        pr = [(k[0], None) for k in rk if k[0] in self.psum]
        if pr:
            rk = [k for k in rk if k[0] not in self.psum]
            wk = wk + pr
        wk = [((k[0], None) if k[0] in self.psum else k) for k in wk]
        self._deps(e, rk, wk)
        if self.ecnt[e] >= self.EPOCH:
            self._new_epoch(e)
        ins = fn()
        self.ecnt[e] += 1
        ins.then_inc(self.esem[e], 1)
        self._commit((self.esem[e], self.ecnt[e]), rk, wk)
        self.ninst += 1
        return ins

    def dma(self, q, out, in_, reads=None, writes=None, indirect=None, **kw):
        rk = [self._key(x) for x in (reads if reads is not None else [in_])]
        wk = [self._key(x) for x in (writes if writes is not None else [out])]
        self._deps(q, rk, wk)
        pool = self.dpool[q]
        if len(pool) < self.NPOOL[q]:
            pool.append([self._sem("d" + q), 0])
            slot = pool[-1]
        else:
            slot = pool[self.dnext[q] % len(pool)]
            self.dnext[q] += 1
            if slot[1] >= 60000:
                if self.seen[q].get(slot[0], 0) < slot[1]:
                    self.E[q].wait_ge(slot[0], slot[1])
                slot[0], slot[1] = self._sem("d" + q), 0
        sem, cnt = slot
        if cnt > 0 and self.seen[q].get(sem, 0) < cnt:
            self.E[q].wait_ge(sem, cnt)
            self.seen[q][sem] = cnt
        if indirect is None:
            ins = self.E[q].dma_start(out=out, in_=in_, **kw)
        else:
            ins = self.E[q].indirect_dma_start(out=out, in_=in_, **indirect)
        slot[1] = cnt + 16
        ins.then_inc(sem, 16)
        self._commit((sem, slot[1]), rk, wk)
        self.ninst += 1
        return ins

    def barrier(self):
        evs = {}
        for e in self.E:
            if self.ecnt[e] > 0:
                evs[self.esem[e]] = self.ecnt[e]
        for q, pool in self.dpool.items():
            for sem, cnt in pool:
                if cnt > 0:
                    evs[sem] = cnt
        for e in self.E:
            for sem, val in evs.items():
                if self.seen[e].get(sem, 0) >= val:
                    continue
                self.E[e].wait_ge(sem, val)
                self.seen[e][sem] = val
        self.W, self.R = {}, {}

    def finish(self):
        self.barrier()


def host_consts():
    c = {}
    c["ident_f"] = np.eye(128, dtype=np.float32)
    c["ident_b"] = np.eye(128).astype(ml_dtypes.bfloat16)
    s = np.arange(128)[:, None]
    t = np.arange(128)[None, :]
    c["mask_f"] = (s <= t).astype(np.float32)
    c["mask_b"] = (s >= t).astype(np.float32)
    c["tri_b"] = (s < t).astype(ml_dtypes.bfloat16)
    c["ones_b"] = np.ones((128, 128), ml_dtypes.bfloat16)
    c["ones_f"] = np.ones((128, 128), np.float32)
    facS = np.ones((4, 8), np.float32)
    facE = np.ones((4, 8), np.float32)
    for g, w in enumerate((2, 4, 8, 16)):
        half = w // 2
        for tt in range(min(half, 8)):
            facS[g, tt] = w / (tt + half)
        for j in range(8):
            i = 7 - j
            if i < half - 1:
                facE[g, j] = w / (half + 1 + i)
    c["facS"] = np.broadcast_to(facS[None], (128, 4, 8)).copy()
    c["facE"] = np.broadcast_to(facE[None], (128, 4, 8)).copy()
    rows = LAT // 64
    t_row = np.repeat(np.arange(rows, dtype=np.float32), 64)
    t_col = np.tile(np.arange(64, dtype=np.float32), rows)
    inv = (1.0 / (10000.0 ** (np.arange(8, dtype=np.float32) / 8))).astype(np.float32)
    ang = np.stack([t_row[:, None] * inv, t_col[:, None] * inv], axis=1).astype(np.float32)
    cos = np.cos(ang).reshape(LAT, 16).astype(np.float32)
    sin = np.sin(ang).reshape(LAT, 16).astype(np.float32)
    for l_ in range(2):
        c[f"slotbase{l_}"] = np.broadcast_to((np.arange(32, dtype=np.float32) * CAPS[l_])[None], (128, 32)).copy()
    c["pidx"] = np.arange(128, dtype=np.float32).reshape(128, 1).copy()
    c["rope_cos"] = cos.reshape(16, 128, 16).transpose(1, 0, 2).copy()
    c["rope_sin"] = sin.reshape(16, 128, 16).transpose(1, 0, 2).copy()
    return c


CONST_SPECS = {
    "ident_f": ([128, 128], F32), "ident_b": ([128, 128], BF16), "mask_f": ([128, 128], F32),
    "mask_b": ([128, 128], F32), "tri_b": ([128, 128], BF16), "ones_b": ([128, 128], BF16),
    "ones_f": ([128, 128], F32), "facS": ([128, 4, 8], F32), "facE": ([128, 4, 8], F32),
    "slotbase0": ([128, 32], F32), "slotbase1": ([128, 32], F32), "pidx": ([128, 1], F32),
    "rope_cos": ([128, 16, 16], F32), "rope_sin": ([128, 16, 16], F32),
}

IN_SPECS = {
    "x": lambda ns: [ns, LAT, D], "c": lambda ns: [ns, D], "ctx": lambda ns: [ns, CTX, D], "c_ctx": lambda ns: [D],
    "ada_w": lambda ns: [2, D, 6 * D], "ada_b": lambda ns: [2, 6 * D], "ev_w_in": lambda ns: [1, D, 3072],
    "hgrn_lb_logits": lambda ns: [3, 2, 512], "hgrn_norm_w": lambda ns: [1, 128], "pool_w": lambda ns: [1, 4, 128, 128],
    "pool_scale": lambda ns: [1, 512], "ev_w_out": lambda ns: [1, D, D], "od_w_in": lambda ns: [1, D, 1696],
    "conv_dw_w": lambda ns: [1, 31, 512], "conv_dw_b": lambda ns: [1, 512], "conv_ln_w": lambda ns: [1, 512],
    "conv_ln_b": lambda ns: [1, 512], "mla_q_a_norm": lambda ns: [1, 384], "mla_w_uq": lambda ns: [1, 384, 768],
    "mla_kv_a_norm": lambda ns: [1, 256], "mla_w_ukv": lambda ns: [1, 256, 1024], "mla_q_norm": lambda ns: [1, 96],
    "mla_k_norm": lambda ns: [1, 96], "od_w_out": lambda ns: [1, D, D], "moe_router_w": lambda ns: [2, D, 32],
    "moe_router_b": lambda ns: [2, 32], "moe_w1": lambda ns: [2, 32, D, 2 * D], "moe_b1": lambda ns: [2, 32, 2 * D],
    "moe_w2": lambda ns: [2, 32, D, D], "moe_b2": lambda ns: [2, 32, D],
}


class StopBuild(Exception):
    pass


class K:
    def __init__(self, ns, stop_after=None, dbg=(), skip_inputs=()):
        self.ns = ns
        self.stop_after = stop_after
        self.dbg = set(dbg)
        nc = self.nc = bass.Bass("TRN2", target_bir_lowering=False)
        self.S = Sched(nc)
        self.I = {k: nc.dram_tensor(k, f(ns), F32, kind="ExternalInput").ap() for k, f in IN_SPECS.items() if k not in skip_inputs}
        self.C = {k: nc.dram_tensor("k_" + k, sh, dt, kind="ExternalInput").ap() for k, (sh, dt) in CONST_SPECS.items()}
        self.out = nc.dram_tensor("out", [ns, LAT, D], F32, kind="ExternalOutput").ap()
        self.T0 = ns * SEQ
        self.T1 = ns * LAT
        self.MOD = nc.dram_tensor("MOD", [10, 6 * D], F32).ap()
        self.H1 = nc.dram_tensor("H1", [self.T0, D], F32).ap()
        self.H2 = nc.dram_tensor("H2", [self.T0, D], F32).ap()
        self.M = nc.dram_tensor("M", [self.T0 + 128, D], BF16).ap()
        self.SLOT = nc.dram_tensor("SLOT", [NSLOTMAX + 128, 2], I32).ap()
        self.YB = nc.dram_tensor("YB", [NSLOTMAX + 128, D], BF16).ap()
        self.dbg_out = {}
        self._stack = []

    def sb(self, name, shape, dt):
        self._uid = getattr(self, "_uid", 0) + 1
        cm = self.nc.sbuf_tensor(f"{name}_{self._uid}", shape, dt)
        t = cm.__enter__()
        self._stack.append(cm)
        return t

    def ps(self, name, shape, dt):
        cm = self.nc.psum_tensor(name, shape, dt)
        t = cm.__enter__()
        self.S.psum.add(name)
        self._stack.append(cm)
        return t

    def mark(self):
        return len(self._stack)

    def release(self, mark):
        self.S.barrier()
        while len(self._stack) > mark:
            self._stack.pop().__exit__(None, None, None)

    def mm(self, out, lhsT, rhs, start=True, stop=True, reads=None, writes=None):
        nc = self.nc
        return self.S.op("pe", lambda: nc.tensor.matmul(out, lhsT, rhs, start=start, stop=stop),
                         reads=reads if reads is not None else [lhsT, rhs], writes=writes if writes is not None else [out])

    def tr(self, out, in_, ident, reads=None, writes=None):
        nc = self.nc
        return self.S.op("pe", lambda: nc.tensor.transpose(out, in_, ident),
                         reads=reads if reads is not None else [in_, ident], writes=writes if writes is not None else [out])

    def act(self, out, in_, func, bias=None, scale=None, accum_out=None, reads=None, writes=None):
        nc = self.nc
        kw = {}
        rd = [in_]
        wr = [out]
        if bias is not None:
            kw["bias"] = bias
            if not isinstance(bias, (int, float)):
                rd.append(bias)
        if scale is not None:
            kw["scale"] = scale
            if not isinstance(scale, (int, float)):
                rd.append(scale)
        if accum_out is not None:
            kw["accum_out"] = accum_out
            wr.append(accum_out)
        return self.S.op("act", lambda: nc.scalar.activation(out, in_, func, **kw),
                         reads=reads if reads is not None else rd, writes=writes if writes is not None else wr)

    def v(self, e, name, *args, reads, writes, **kw):
        eng = self.S.E[e]
        return self.S.op(e, lambda: getattr(eng, name)(*args, **kw), reads=reads, writes=writes)

    def tt(self, e, out, in0, in1, op, reads=None, writes=None):
        return self.v(e, "tensor_tensor", out, in0, in1, op, reads=reads if reads is not None else [in0, in1],
                      writes=writes if writes is not None else [out])

    def ts(self, e, out, in0, s1, s2, op0, op1=None, reads=None, writes=None, accum_out=None):
        rd = [in0] + [s for s in (s1, s2) if s is not None and not isinstance(s, (int, float))]
        kw = {}
        if op1 is not None:
            kw["op1"] = op1
        wr = [out]
        if accum_out is not None:
            kw["accum_out"] = accum_out
            wr.append(accum_out)
        return self.v(e, "tensor_scalar", out, in0, s1, s2, op0, reads=reads if reads is not None else rd,
                      writes=writes if writes is not None else wr, **kw)

    def stt(self, e, out, in0, scalar, in1, op0, op1, reads=None, writes=None):
        rd = [in0, in1] + ([] if isinstance(scalar, (int, float)) else [scalar])
        return self.v(e, "scalar_tensor_tensor", out, in0, scalar, in1, op0, op1,
                      reads=reads if reads is not None else rd, writes=writes if writes is not None else [out])

    def copy(self, e, out, in_, reads=None, writes=None):
        if e == "act":
            return self.act(out, in_, AF.Copy, reads=reads, writes=writes)
        return self.v(e, "tensor_copy", out, in_, reads=reads if reads is not None else [in_],
                      writes=writes if writes is not None else [out])

    def memset(self, e, ap, val):
        return self.v(e, "memset", ap, val, reads=[], writes=[ap])

    def dma(self, q, out, in_, **kw):
        return self.S.dma(q, out, in_, **kw)

    def rstd_from_ss(self, rstd, ss, n):
        self.ts("dve", rstd, ss, 1.0 / n, EPS, ALU.mult, ALU.add)
        self.act(rstd, rstd, AF.Sqrt)
        self.v("dve", "reciprocal", rstd, rstd, reads=[rstd], writes=[rstd])

    def build(self):
        nc, S, I, C = self.nc, self.S, self.I, self.C
        ns = self.ns
        self.cst = {}
        for k in ("ident_f", "ident_b", "mask_f", "mask_b", "tri_b", "ones_b", "ones_f", "slotbase0", "slotbase1", "pidx"):
            sh, dt = CONST_SPECS[k]
            t = self.sb("c_" + k, sh, dt)
            self.dma("sp", t[:], C[k][:, :])
            self.cst[k] = t
        self.PS = [self.ps(f"ps{i}", [128, 512], F32) for i in range(6)]
        self.PB = [self.ps(f"pb{i}", [128, 1024], BF16) for i in range(2)]
        self.small = self.sb("small", [128, 64], F32)

        self.phase_adaln()
        if self.stop_after == "adaln":
            return self.end()
        mkr = self.mark()
        self.route_init(0)
        try:
            self.layer0()
        except StopBuild:
            return self.end()
        if self.stop_after == "mixer0":
            return self.end()
        if self.stop_after and self.stop_after.startswith("l1_"):
            self.S.barrier()
            self.dma("sp", self.H2, self.H1)
        else:
            self.experts(0)
            self.combine(0)
        self.release(mkr)
        if self.stop_after == "layer0":
            return self.end()
        mkr = self.mark()
        self.route_init(1)
        try:
            self.layer1()
        except StopBuild:
            return self.end()
        self.experts(1)
        self.combine(1)
        self.release(mkr)
        return self.end()

    def end(self):
        for name in ("MOD", "H1", "H2", "M"):
            if name in self.dbg:
                src = getattr(self, name)
                d = self.dbg_dump(name, list(src.shape), src.dtype)
                self.S.barrier()
                self.dma("sp", d, src)
        self.S.finish()
        return self.nc

    def dbg_dump(self, name, shape, dt=F32):
        t = self.nc.dram_tensor("dbg_" + name, shape, dt, kind="ExternalOutput").ap()
        self.dbg_out[name] = t
        return t

    def phase_adaln(self):
        nc, S, I, C = self.nc, self.S, self.I, self.C
        ns = self.ns
        mk = self.mark()
        cs = self.sb("cs", [128, 8, 8], F32)
        s5 = self.sb("s5", [128, 8, 8], F32)
        self.memset("dve", cs[:], 0.0)
        for b in range(ns):
            self.dma("sp", cs[:, :, b], I["c"][b].rearrange("(kc p) -> p kc", p=128), allow_slow_non_contiguous=True)
        self.dma("sp", cs[:, :, ns], I["c_ctx"].rearrange("(kc p) -> p kc", p=128), allow_slow_non_contiguous=True)
        self.act(s5[:], cs[:], AF.Silu)
        nr = ns + 1
        brow = self.sb("brow", [1, 6 * D], F32)
        wa = [self.sb(f"wa{i}", [128, 8, 512], F32) for i in range(2)]
        mo = [self.sb(f"mo{i}", [8, 512], F32) for i in range(2)]
        onesf = self.cst["ones_f"]
        it = 0
        for layer in range(2):
            self.dma("sp", brow[:], I["ada_b"][layer:layer + 1, :])
            for nb in range(12):
                w = wa[it % 2]
                self.dma("sp" if it % 2 == 0 else "pool", w[:],
                         I["ada_w"][layer][:, nb * 512:(nb + 1) * 512].rearrange("(kc p) n -> p kc n", p=128))
                ps = self.PS[it % 2]
                for kc in range(8):
                    self.mm(ps[0:nr, :], s5[:, kc, 0:nr], w[:, kc, :], start=(kc == 0), stop=False)
                self.mm(ps[0:nr, :], onesf[0:1, 0:nr], brow[0:1, nb * 512:(nb + 1) * 512], start=False, stop=True)
                m = mo[it % 2]
                self.copy("dve", m[0:nr, :], ps[0:nr, :])
                self.dma("sp", self.MOD[layer * 5:layer * 5 + ns, nb * 512:(nb + 1) * 512], m[0:ns, :],
                         writes=[(self.MOD, (layer, nb, 0))])
                self.dma("sp", self.MOD[layer * 5 + 4:layer * 5 + 5, nb * 512:(nb + 1) * 512], m[ns:ns + 1, :],
                         writes=[(self.MOD, (layer, nb, 1))])
                it += 1
        self.release(mk)

    def modrow(self, dst, layer, row, q):
        src = self.MOD[layer * 5 + row:layer * 5 + row + 1, q * D:(q + 1) * D].partition_broadcast(128)
        self.dma("sp", dst, src, reads=[self.MOD])

    def modulate_tile(self, u, ht, sc1p, sh, junk, ss, rstd):
        self.act(junk, ht, AF.Square, accum_out=ss)
        self.rstd_from_ss(rstd, ss, D)
        self.stt("dve", u, ht, rstd, sc1p, ALU.mult, ALU.mult)
        self.tt("pool", u, u, sh, ALU.add)

    def transpose_tile_to(self, dstT, col0, src, nkc, ident_f, psA, psB, dt_note=None):
        for kc in range(nkc):
            ps = psA if kc < 4 else psB
            self.tr(ps[:, (kc % 4) * 128:(kc % 4 + 1) * 128], src[:, kc * 128:(kc + 1) * 128], ident_f[:])
        n0 = min(4, nkc)
        self.copy("act", dstT[:, 0:n0, col0:col0 + 128], psA[:, 0:n0 * 128].rearrange("p (k c) -> p k c", c=128))
        if nkc > 4:
            self.copy("dve", dstT[:, 4:nkc, col0:col0 + 128], psB[:, 0:(nkc - 4) * 128].rearrange("p (k c) -> p k c", c=128))

    def layer0(self):
        nc, S, I, C = self.nc, self.S, self.I, self.C
        ns = self.ns
        cst = self.cst
        mk0 = self.mark()
        lg = self.sb("lg", [128, 3, 8], F32)
        lb = self.sb("lb", [128, 8], F32)
        oml = self.sb("oml", [128, 8], F32)
        for l in range(3):
            self.dma("sp", lg[:, l, :].rearrange("p (d h) -> p d h", d=2),
                     I["hgrn_lb_logits"][l].rearrange("d (h p) -> p d h", p=128), allow_slow_non_contiguous=True)
        self.act(lg[:], lg[:], AF.Exp)
        self.tt("dve", lb[:], lg[:, 0, :], lg[:, 1, :], ALU.add)
        self.tt("dve", lb[:], lb[:], lg[:, 2, :], ALU.add)
        self.v("dve", "reciprocal", lb[:], lb[:], reads=[lb], writes=[lb])
        self.tt("dve", lb[:], lb[:], lg[:, 0, :], ALU.mult)
        self.ts("dve", oml[:], lb[:], -1.0, 1.0, ALU.mult, ALU.add)
        nwb = self.sb("nwb", [128, 128], F32)
        self.dma("sp", nwb[:], I["hgrn_norm_w"][0:1, :].partition_broadcast(128))
        pscale = self.sb("pscale", [128, 4], F32)
        self.dma("sp", pscale[:], I["pool_scale"][0].rearrange("(g p) -> p g", p=128), allow_slow_non_contiguous=True)
        poolw = self.sb("poolw", [128, 4, 128], BF16)
        for g in range(4):
            self.dma("pool", poolw[:, g, :], I["pool_w"][0, g])
        facS = self.sb("facS", [128, 4, 8], F32)
        facE = self.sb("facE", [128, 4, 8], F32)
        self.dma("sp", facS[:], C["facS"])
        self.dma("sp", facE[:], C["facE"])
        wr = self.sb("wr", [128, 8, 32], F32)
        self.dma("sp", wr[:], I["moe_router_w"][0].rearrange("(kc p) e -> p kc e", p=128))
        brr = self.sb("brr", [1, 32], F32)
        self.dma("sp", brr[:], I["moe_router_b"][0:1, :])
        uT = self.sb("uT", [128, 8, SEQ], BF16)
        mixT = self.sb("mixT", [128, 8, SEQ], BF16)
        w_in = I["ev_w_in"][0]
        for b in range(ns):
            mk = self.mark()
            mr = self.sb("mr", [128, 4, D], F32)
            self.modrow(mr[:, 0, :], 0, b, 1)
            self.modrow(mr[:, 1, :], 0, b, 0)
            self.modrow(mr[:, 2, :], 0, 4, 1)
            self.modrow(mr[:, 3, :], 0, 4, 0)
            self.ts("dve", mr[:, 0, :], mr[:, 0, :], 1.0, None, ALU.add)
            self.ts("dve", mr[:, 2, :], mr[:, 2, :], 1.0, None, ALU.add)
            hts = [self.sb(f"ht{i}", [128, D], F32) for i in range(2)]
            us = [self.sb(f"u{i}", [128, D], F32) for i in range(2)]
            junk = self.sb("junk", [128, D], F32)
            for i in range(NT):
                ht, u = hts[i % 2], us[i % 2]
                src = I["ctx"][b, i * 128:(i + 1) * 128, :] if i < 2 else I["x"][b, (i - 2) * 128:(i - 1) * 128, :]
                self.dma("sp", ht[:], src)
                o = 2 if i < 2 else 0
                self.modulate_tile(u[:], ht[:], mr[:, o, :], mr[:, o + 1, :], junk[:], self.small[:, 0:1], self.small[:, 1:2])
                self.transpose_tile_to(uT, i * 128, u, 8, cst["ident_f"], self.PS[0], self.PS[1])
            if "uT" in self.dbg and b == 0:
                d = self.dbg_dump("uT", [128, 8, SEQ], BF16)
                self.dma("sp", d[:, :, :], uT[:])
            self.release(mk)
            if self.stop_after == "l0_p0":
                raise StopBuild()
            mk = self.mark()
            W2 = SEQ + 64
            q32 = self.sb("q32", [128, W2], F32)
            sg = self.sb("sg", [128, W2], F32)
            kin = self.sb("kin", [128, W2], F32)
            A = self.sb("A", [128, W2], F32)
            oacc = self.sb("oacc", [128, NT, 128], F32)
            v16 = self.sb("v16", [128, NT, 128], BF16)
            whs = [self.sb(f"wh{i}", [128, 8, 640], BF16) for i in range(2)]
            S32 = self.sb("S32", [128, 128], F32)
            S16 = self.sb("S16", [128, 128], BF16)
            RING = 3
            btall = self.sb("btall", [128, NT, 8], F32)
            decs = [self.sb(f"dec{i}", [128, 1], F32) for i in range(RING)]
            Pks = [[self.sb(f"Pk{r_}{i}", [128, 128], F32) for i in range(5)] for r_ in range(RING)]
            kiA = [self.sb(f"kiA{i}", [128, 128], BF16) for i in range(RING)]
            kiB = [self.sb(f"kiB{i}", [128, 128], BF16) for i in range(RING)]
            kiC = [self.sb(f"kiC{i}", [128, 128], BF16) for i in range(RING)]
            qdx = [self.sb(f"qdx{i}", [128, 64], BF16) for i in range(RING)]
            S16s = [self.sb(f"S16{i}", [128, 128], BF16) for i in range(2)]
            qd = [self.sb(f"qd{i}", [128, 128], BF16) for i in range(RING)]
            qdS = [self.sb(f"qdS{i}", [128, 128], BF16) for i in range(RING)]
            ke = [self.sb(f"ke{i}", [128, 128], BF16) for i in range(RING)]
            att16 = [self.sb(f"att{i}", [128, 128], BF16) for i in range(2)]
            ket16 = [self.sb(f"ket{i}", [128, 128], BF16) for i in range(2)]
            rrs = [self.sb(f"rr{i}", [128, 128], F32) for i in range(2)]
            rdo = self.sb("rdo", [128, 2 * NT], F32)
            zero1 = self.sb("zero1", [128, 1], F32)
            self.memset("dve", zero1[:], 0.0)
            groups = [(g * 512, min(512, SEQ - g * 512)) for g in range((SEQ + 511) // 512)]
            for h in range(4):
                wh = whs[h % 2]
                for j, base in enumerate((0, 512, 1024, 1536, 2048)):
                    c0 = base + h * 128
                    self.dma("pool", wh[:, :, j * 128:(j + 1) * 128],
                             w_in[:, c0:c0 + 128].rearrange("(kc p) n -> p kc n", p=128))
                for gi, (g0, gn) in enumerate(groups):
                    ps = self.PS[gi % 2]
                    for kc in range(8):
                        self.mm(ps[:, 0:gn], wh[:, kc, 0:128], uT[:, kc, g0:g0 + gn], start=(kc == 0), stop=(kc == 7))
                    self.act(q32[:, g0:g0 + gn], ps[:, 0:gn], AF.Silu)
                for i in range(NT):
                    ps = self.PS[2 + i % 2]
                    for kc in range(8):
                        self.mm(ps[:, 0:128], uT[:, kc, i * 128:(i + 1) * 128], wh[:, kc, 384:512], start=(kc == 0), stop=(kc == 7))
                    self.copy("dve", v16[:, i, :], ps[:, 0:128])
                for di in range(2):
                    for gi, (g0, gn) in enumerate(groups):
                        ps = self.PS[gi % 2]
                        for kc in range(8):
                            self.mm(ps[:, 0:gn], wh[:, kc, (1 + di) * 128:(2 + di) * 128], uT[:, kc, g0:g0 + gn],
                                    start=(kc == 0), stop=(kc == 7))
                        self.act(sg[:, g0:g0 + gn], ps[:, 0:gn], AF.Sigmoid)
                    lbc = lb[:, di * 4 + h:di * 4 + h + 1]
                    omc = oml[:, di * 4 + h:di * 4 + h + 1]
                    self.ts("dve", sg[:, 0:SEQ], sg[:, 0:SEQ], omc, lbc, ALU.mult, ALU.add)
                    self.ts("pool", kin[:, 0:SEQ], sg[:, 0:SEQ], -1.0, 1.0, ALU.mult, ALU.add)
                    self.act(sg[:, 0:SEQ], sg[:, 0:SEQ], AF.Ln)
                    self.S.op("dve", lambda: nc.vector.tensor_tensor_scan(A[:, 0:SEQ], sg[:, 0:SEQ], sg[:, 0:SEQ], 0.0, ALU.add, ALU.add),
                              reads=[sg], writes=[A])
                    if di == 1:
                        self.stt("dve", sg[:, 0:SEQ], sg[:, 0:SEQ], -2.0, A[:, 0:SEQ], ALU.mult, ALU.add)
                    AA = A if di == 0 else sg
                    order = list(range(NT)) if di == 0 else [1, 0] + list(range(NT - 1, 1, -1))
                    self.memset("dve", S32[:], 0.0)
                    self.memset("pool", S16s[0][:], 0.0)
                    self.memset("pool", S16s[1][:], 0.0)
                    for t_ in kiA + kiB + kiC:
                        self.memset("pool", t_[:], 0.0)
                    mask = cst["mask_f"] if di == 0 else cst["mask_b"]
                    lo, hi = slice(0, 64), slice(64, 128)
                    AAv = AA[:, 0:SEQ].rearrange("p (t c) -> p t c", c=128)
                    Av = A[:, 0:SEQ].rearrange("p (t c) -> p t c", c=128)
                    xcol = 63 if di == 0 else 64
                    for bc, (src_, sgn) in enumerate(((AAv[:, :, 31], -0.5), (AAv[:, :, 31], 0.5), (AAv[:, :, 95], -0.5), (AAv[:, :, 95], 0.5),
                                                     (AAv[:, :, xcol], -0.5), (AAv[:, :, xcol], 0.5))):
                        self.ts("dve", btall[:, :, bc], src_, sgn, None, ALU.mult, reads=[AA], writes=[btall])
                    self.memset("dve", btall[:, 0:1, 6], 0.0)
                    self.ts("dve", btall[:, 1:NT, 6], Av[:, 0:NT - 1, 127], -0.5, None, ALU.mult, reads=[A], writes=[btall])
                    self.ts("dve", btall[:, :, 7], Av[:, :, 127], 0.5, None, ALU.mult, reads=[A], writes=[btall])

                    def stageA(n, i):
                        c0, c1 = i * 128, (i + 1) * 128
                        r = n % RING
                        Pk_ = Pks[r]
                        bt_ = btall[:, i, :]
                        Alo, Ahi, Acol = AA[:, c0:c0 + 64], AA[:, c0 + 64:c1], AA[:, c0:c1]
                        if di == 0:
                            self.act(Pk_[0][:, lo], Alo, AF.Exp, bias=btall[:, i, 0:1], scale=0.5)
                            self.act(Pk_[0][:, hi], Ahi, AF.Exp, bias=btall[:, i, 2:3], scale=0.5)
                            self.act(Pk_[1][:, lo], Alo, AF.Exp, bias=btall[:, i, 1:2], scale=-0.5)
                            self.act(Pk_[1][:, hi], Ahi, AF.Exp, bias=btall[:, i, 3:4], scale=-0.5)
                            self.act(Pk_[2][:], Acol, AF.Exp, bias=btall[:, i, 6:7], scale=0.5)
                            self.act(Pk_[3][:], Acol, AF.Exp, bias=btall[:, i, 7:8], scale=-0.5)
                            self.act(Pk_[4][:, lo], Ahi, AF.Exp, bias=btall[:, i, 4:5], scale=0.5)
                            self.act(Pk_[4][:, hi], Alo, AF.Exp, bias=btall[:, i, 5:6], scale=-0.5)
                            qx_cols, kx_cols = hi, lo
                        else:
                            self.act(Pk_[0][:, lo], Alo, AF.Exp, bias=btall[:, i, 1:2], scale=-0.5)
                            self.act(Pk_[0][:, hi], Ahi, AF.Exp, bias=btall[:, i, 3:4], scale=-0.5)
                            self.act(Pk_[1][:, lo], Alo, AF.Exp, bias=btall[:, i, 0:1], scale=0.5)
                            self.act(Pk_[1][:, hi], Ahi, AF.Exp, bias=btall[:, i, 2:3], scale=0.5)
                            self.act(Pk_[2][:], Acol, AF.Exp, bias=btall[:, i, 7:8], scale=-0.5)
                            self.act(Pk_[3][:], Acol, AF.Exp, bias=btall[:, i, 6:7], scale=0.5)
                            self.act(Pk_[4][:, lo], Alo, AF.Exp, bias=btall[:, i, 5:6], scale=-0.5)
                            self.act(Pk_[4][:, hi], Ahi, AF.Exp, bias=btall[:, i, 4:5], scale=0.5)
                            qx_cols, kx_cols = lo, hi
                        qcs = slice(c0 + qx_cols.start, c0 + qx_cols.stop)
                        kcs = slice(c0 + kx_cols.start, c0 + kx_cols.stop)
                        self.tt("dve", qd[r][:], q32[:, c0:c1], Pk_[0][:], ALU.mult)
                        self.tt("pool", kiA[r][:, lo], kin[:, c0:c0 + 64], Pk_[1][:, lo], ALU.mult)
                        self.tt("pool", kiB[r][:, hi], kin[:, c0 + 64:c1], Pk_[1][:, hi], ALU.mult)
                        self.tt("dve", qdS[r][:], q32[:, c0:c1], Pk_[2][:], ALU.mult)
                        self.tt("pool", ke[r][:], kin[:, c0:c1], Pk_[3][:], ALU.mult)
                        self.tt("dve", qdx[r][:], q32[:, qcs], Pk_[4][:, lo], ALU.mult)
                        self.tt("pool", kiC[r][:, kx_cols], kin[:, kcs], Pk_[4][:, hi], ALU.mult)
                        self.copy("pool", decs[r][:], Pk_[2][:, 127:128] if di == 0 else Pk_[2][:, 0:1])

                    def stageB(n, i):
                        r = n % RING
                        p = n % 2
                        pa, po, pS = self.PS[0 + p], self.PS[2 + p], self.PS[4 + p]
                        pbt = self.PB[p]
                        if di == 0:
                            self.mm(pa[:, 0:64], kiA[r][:], qd[r][:, lo], start=True, stop=True)
                            self.mm(pa[:, 64:128], kiB[r][:], qd[r][:, hi], start=True, stop=False)
                            self.mm(pa[:, 64:128], kiC[r][:], qdx[r][:], start=False, stop=True)
                        else:
                            self.mm(pa[:, 0:64], kiA[r][:], qd[r][:, lo], start=True, stop=False)
                            self.mm(pa[:, 0:64], kiC[r][:], qdx[r][:], start=False, stop=True)
                            self.mm(pa[:, 64:128], kiB[r][:], qd[r][:, hi], start=True, stop=True)
                        self.tr(pbt[:, 0:128], ke[r][:], cst["ident_b"][:])
                        self.tt("dve", att16[p][:], pa[:, 0:128], mask[:], ALU.mult)
                        self.copy("act", ket16[p][:], pbt[:, 0:128])
                        self.mm(pS[:, 0:128], ket16[p][:], v16[:, i, :])
                        self.mm(po[:, 0:128], att16[p][:], v16[:, i, :], start=True, stop=False)
                        self.mm(po[:, 0:128], qdS[r][:], S16s[(n + 1) % 2][:], start=False, stop=True)
                        self.stt("dve", S32[:], S32[:], decs[r][:, 0:1], pS[:, 0:128], ALU.mult, ALU.add)
                        self.copy("act", S16s[n % 2][:], S32[:])
                        if di == 0:
                            self.copy("act", oacc[:, i, :], po[:, 0:128])
                        else:
                            self.tt("dve", oacc[:, i, :], oacc[:, i, :], po[:, 0:128], ALU.add)

                    LOOK = 2
                    for n in range(min(LOOK, NT)):
                        stageA(n, order[n])
                    for n, i in enumerate(order):
                        if n + LOOK < NT:
                            stageA(n + LOOK, order[n + LOOK])
                        stageB(n, i)
                ogall, rrall = sg, kin
                for i in range(NT):
                    ps = self.PS[i % 4]
                    for kc in range(8):
                        self.mm(ps[:, 0:128], uT[:, kc, i * 128:(i + 1) * 128], wh[:, kc, 512:640], start=(kc == 0), stop=(kc == 7))
                    self.act(ogall[:, i * 128:(i + 1) * 128], ps[:, 0:128], AF.Silu, writes=[(ogall, i)])
                    self.act(rrall[:, i * 128:(i + 1) * 128], oacc[:, i, :], AF.Square, accum_out=rdo[:, i:i + 1],
                             writes=[(rrall, i), (rdo, i)])
                self.ts("dve", rdo[:, NT:2 * NT], rdo[:, 0:NT], 1.0 / 128, EPS, ALU.mult, ALU.add, reads=[rdo], writes=[rdo])
                self.act(rdo[:, NT:2 * NT], rdo[:, NT:2 * NT], AF.Sqrt, reads=[rdo], writes=[rdo])
                self.v("dve", "reciprocal", rdo[:, NT:2 * NT], rdo[:, NT:2 * NT], reads=[rdo], writes=[rdo])
                self.tt("dve", rrall[:, 0:SEQ], oacc[:].rearrange("p t c -> p (t c)"), ogall[:, 0:SEQ], ALU.mult,
                        reads=[oacc, ogall], writes=[rrall])
                for i in range(NT):
                    r_ = rrs[i % 2]
                    self.stt("dve", r_[:], rrall[:, i * 128:(i + 1) * 128], rdo[:, NT + i:NT + i + 1], nwb[:], ALU.mult, ALU.mult,
                             reads=[rrall, rdo, nwb], writes=[r_])
                    pt = self.PS[4 + i % 2]
                    self.tr(pt[:, 0:128], r_[:], cst["ident_f"][:])
                    self.copy("act", mixT[:, h, i * 128:(i + 1) * 128], pt[:, 0:128])
            if self.stop_after == "l0_p1":
                raise StopBuild()
            wp = whs[0]
            self.dma("pool", wp[:, :, 0:512], w_in[:, 2560:3072].rearrange("(kc p) n -> p kc n", p=128))
            OFFC, OFFL = 16, 16 + CTX + 16
            WB = OFFL + LAT + 16
            d16 = self.sb("d16", [128, SEQ], BF16)
            for g in range(4):
                w = 2 << g
                half = w // 2
                PBf, T1, T2 = q32, kin, A
                self.memset("pool", PBf[:, 0:WB], 0.0)
                for gi, (g0, gn) in enumerate(groups):
                    ps = self.PS[gi % 2]
                    for kc in range(8):
                        self.mm(ps[:, 0:gn], wp[:, kc, g * 128:(g + 1) * 128], uT[:, kc, g0:g0 + gn], start=(kc == 0), stop=(kc == 7))
                    a0, a1 = g0, g0 + gn
                    if a0 < CTX:
                        n_c = min(a1, CTX) - a0
                        self.copy("act", PBf[:, OFFC + a0:OFFC + a0 + n_c], ps[:, 0:n_c])
                        if a1 > CTX:
                            self.copy("act", PBf[:, OFFL:OFFL + (a1 - CTX)], ps[:, n_c:gn])
                    else:
                        self.copy("act", PBf[:, OFFL + a0 - CTX:OFFL + a1 - CTX], ps[:, 0:gn])
                cur = PBf
                step = 1
                tmp = [T1, T2]
                ti = 0
                width = WB
                while step < w:
                    nxt = tmp[ti % 2]
                    ti += 1
                    width2 = width - step
                    self.tt("dve", nxt[:, 0:width2], cur[:, 0:width2], cur[:, step:step + width2], ALU.add)
                    cur, width, step = nxt, width2, step * 2
                dst = tmp[ti % 2]
                for off, L, tcol in ((OFFC, CTX, 0), (OFFL, LAT, CTX)):
                    sw = cur[:, off - half:off - half + L]
                    self.tt("pool", sw[:, 0:8], sw[:, 0:8], facS[:, g, :], ALU.mult, reads=[cur, facS], writes=[cur])
                    self.tt("pool", sw[:, L - 8:L], sw[:, L - 8:L], facE[:, g, :], ALU.mult, reads=[cur, facE], writes=[cur])
                    self.stt("dve", d16[:, tcol:tcol + L], sw, 1.0 / w, PBf[:, off:off + L], ALU.mult, ALU.subtract,
                             reads=[cur, PBf], writes=[d16])
                for gi, (g0, gn) in enumerate(groups):
                    ps = self.PS[2 + gi % 2]
                    self.mm(ps[:, 0:gn], poolw[:, g, :], d16[:, g0:g0 + gn])
                    self.ts("dve", mixT[:, 4 + g, g0:g0 + gn], ps[:, 0:gn], pscale[:, g:g + 1], None, ALU.mult)
            if "mixT" in self.dbg and b == 0:
                d = self.dbg_dump("mixT", [128, 8, SEQ], BF16)
                self.dma("sp", d[:, :, :], mixT[:])
            self.release(mk)
            if self.stop_after == "l0_p3":
                raise StopBuild()
            self.post_mixer(0, b, mixT, NT, wr, brr)
        self.release(mk0)

    def rms_rows(self, out, src, n, gain_b, sq_junk, c0):
        sm = self.small
        self.act(sq_junk, src, AF.Square, accum_out=sm[:, c0:c0 + 1])
        self.rstd_from_ss(sm[:, c0 + 1:c0 + 2], sm[:, c0:c0 + 1], n)
        self.stt("dve", out, src, sm[:, c0 + 1:c0 + 2], gain_b, ALU.mult, ALU.mult)

    def head_norm(self, xf, gain_b, tmp, hs, rs):
        self.tt("dve", tmp[:], xf[:], xf[:], ALU.mult)
        self.v("dve", "reduce_sum", hs[:], tmp[:], AX.X, reads=[tmp], writes=[hs])
        self.ts("dve", rs[:], hs[:], 1.0 / 96, EPS, ALU.mult, ALU.add)
        self.act(rs[:], rs[:], AF.Sqrt)
        self.v("dve", "reciprocal", rs[:], rs[:], reads=[rs], writes=[rs])
        self.tt("dve", xf[:], xf[:], rs[:].unsqueeze(2).to_broadcast([128, 8, 96]), ALU.mult, reads=[xf, rs], writes=[xf])
        self.tt("dve", xf[:], xf[:], gain_b[:].unsqueeze(1).to_broadcast([128, 8, 96]), ALU.mult, reads=[xf, gain_b], writes=[xf])

    def rope_all(self, xo, xf, cosr, sinr, tt_):
        s5 = xf[:, :, 64:96].rearrange("p h (a t i) -> p h a t i", a=2, t=2)
        d5 = xo[:, :, 64:96].rearrange("p h (a t i) -> p h a t i", a=2, t=2)
        cb = cosr.rearrange("p (a i) -> p a i", a=2).unsqueeze(1).to_broadcast([128, 8, 2, 8])
        sb_ = sinr.rearrange("p (a i) -> p a i", a=2).unsqueeze(1).to_broadcast([128, 8, 2, 8])
        t = [x[:].rearrange("p h (a i) -> p h a i", a=2) for x in tt_]
        x1, x2 = s5[:, :, :, 0, :], s5[:, :, :, 1, :]
        self.tt("dve", t[0], x1, cb, ALU.mult, reads=[xf, cosr], writes=[tt_[0]])
        self.tt("dve", t[1], x2, sb_, ALU.mult, reads=[xf, sinr], writes=[tt_[1]])
        self.tt("dve", t[2], x2, cb, ALU.mult, reads=[xf, cosr], writes=[tt_[2]])
        self.tt("dve", t[3], x1, sb_, ALU.mult, reads=[xf, sinr], writes=[tt_[3]])
        self.tt("dve", d5[:, :, :, 0, :], t[0], t[1], ALU.subtract, reads=[tt_[0], tt_[1]], writes=[xo])
        self.tt("dve", d5[:, :, :, 1, :], t[2], t[3], ALU.add, reads=[tt_[2], tt_[3]], writes=[xo])

    def rope(self, dst, src, cosr, sinr, t1, t2, eng):
        s4 = src.rearrange("p (a h i) -> p a h i", a=2, h=2)
        d4 = dst.rearrange("p (a h i) -> p a h i", a=2, h=2)
        c3 = cosr.rearrange("p (a i) -> p a i", a=2)
        s3 = sinr.rearrange("p (a i) -> p a i", a=2)
        a3 = t1.rearrange("p (a i) -> p a i", a=2)
        b3 = t2.rearrange("p (a i) -> p a i", a=2)
        x1, x2 = s4[:, :, 0, :], s4[:, :, 1, :]
        self.tt(eng, a3, x1, c3, ALU.mult, reads=[src, cosr], writes=[t1])
        self.tt(eng, b3, x2, s3, ALU.mult, reads=[src, sinr], writes=[t2])
        self.tt(eng, d4[:, :, 0, :], a3, b3, ALU.subtract, reads=[t1, t2], writes=[dst])
        self.tt(eng, a3, x2, c3, ALU.mult, reads=[src, cosr], writes=[t1])
        self.tt(eng, b3, x1, s3, ALU.mult, reads=[src, sinr], writes=[t2])
        self.tt(eng, d4[:, :, 1, :], a3, b3, ALU.add, reads=[t1, t2], writes=[dst])

    def layer1(self):
        nc, S, I, C = self.nc, self.S, self.I, self.C
        ns, cst = self.ns, self.cst
        mk0 = self.mark()
        w_in = I["od_w_in"][0]
        NL = LAT // 128
        wr = self.sb("wr1", [128, 8, 32], F32)
        self.dma("sp", wr[:], I["moe_router_w"][1].rearrange("(kc p) e -> p kc e", p=128))
        brr = self.sb("brr1", [1, 32], F32)
        self.dma("sp", brr[:], I["moe_router_b"][1:2, :])
        rcos = self.sb("rcos", [128, 16, 16], F32)
        rsin = self.sb("rsin", [128, 16, 16], F32)
        self.dma("sp", rcos[:], C["rope_cos"])
        self.dma("sp", rsin[:], C["rope_sin"])
        dww = self.sb("dww", [128, 4, 31], F32)
        dwr = self.sb("dwr", [32, 512], F32)
        self.dma("sp", dwr[0:31, :], I["conv_dw_w"][0])
        for cc in range(4):
            self.tr(self.PS[0][:, cc * 32:cc * 32 + 31], dwr[0:31, cc * 128:(cc + 1) * 128], self.cst["ident_f"][0:31, 0:31])
            self.copy("dve", dww[:, cc, :], self.PS[0][:, cc * 32:cc * 32 + 31])
        cpar = self.sb("cpar", [128, 3, 4], F32)
        for j, nm in enumerate(("conv_dw_b", "conv_ln_w", "conv_ln_b")):
            self.dma("sp", cpar[:, j, :], I[nm][0].rearrange("(c p) -> p c", p=128), allow_slow_non_contiguous=True)
        qan = self.sb("qan", [128, 384], F32)
        kvan = self.sb("kvan", [128, 256], F32)
        qnb = self.sb("qnb", [128, 96], F32)
        knb = self.sb("knb", [128, 96], F32)
        self.dma("sp", qan[:], I["mla_q_a_norm"][0:1, :].partition_broadcast(128))
        self.dma("sp", kvan[:], I["mla_kv_a_norm"][0:1, :].partition_broadcast(128))
        self.dma("sp", qnb[:], I["mla_q_norm"][0:1, :].partition_broadcast(128))
        self.dma("sp", knb[:], I["mla_k_norm"][0:1, :].partition_broadcast(128))
        wq = self.sb("wq", [128, 8, 384], BF16)
        wkv = self.sb("wkv", [128, 8, 288], BF16)
        wuq = self.sb("wuq", [128, 3, 768], BF16)
        wukv = self.sb("wukv", [128, 2, 1024], BF16)
        self.dma("pool", wq[:], w_in[:, 1024:1408].rearrange("(kc p) n -> p kc n", p=128))
        self.dma("pool", wkv[:], w_in[:, 1408:1696].rearrange("(kc p) n -> p kc n", p=128))
        self.dma("pool", wuq[:], I["mla_w_uq"][0].rearrange("(kc p) n -> p kc n", p=128))
        self.dma("pool", wukv[:], I["mla_w_ukv"][0].rearrange("(kc p) n -> p kc n", p=128))
        mixA = self.sb("mixA", [128, 4, LAT], BF16)
        sm = self.small
        SC = 96 ** -0.5
        for b in range(ns):
            mkb = self.mark()

            def make_uT(per_tile=None):
                uT = self.sb("uT1", [128, 8, SEQ], BF16) if per_tile is None else None
                mk = self.mark()
                uTts = [self.sb(f"uTt{i}", [128, 8, 128], BF16) for i in range(2)] if per_tile is not None else None
                mr = self.sb("mr", [128, 4, D], F32)
                self.modrow(mr[:, 0, :], 1, b, 1)
                self.modrow(mr[:, 1, :], 1, b, 0)
                self.modrow(mr[:, 2, :], 1, 4, 1)
                self.modrow(mr[:, 3, :], 1, 4, 0)
                self.ts("dve", mr[:, 0, :], mr[:, 0, :], 1.0, None, ALU.add)
                self.ts("dve", mr[:, 2, :], mr[:, 2, :], 1.0, None, ALU.add)
                hts = [self.sb(f"ht{i}", [128, D], F32) for i in range(2)]
                us = [self.sb(f"u{i}", [128, D], F32) for i in range(1 if per_tile is not None else 2)]
                for i in range(NT):
                    ht, u = hts[i % 2], us[i % len(us)]
                    self.dma("sp", ht[:], self.H2[b * SEQ + i * 128:b * SEQ + (i + 1) * 128, :], reads=[self.H2])
                    o = 2 if i < 2 else 0
                    self.modulate_tile(u[:], ht[:], mr[:, o, :], mr[:, o + 1, :], u[:], sm[:, 0:1], sm[:, 1:2])
                    if per_tile is None:
                        self.transpose_tile_to(uT, i * 128, u, 8, cst["ident_f"], self.PS[0], self.PS[1])
                    else:
                        self.transpose_tile_to(uTts[i % 2], 0, u, 8, cst["ident_f"], self.PS[0], self.PS[1])
                        per_tile(i, uTts[i % 2])
                self.release(mk)
                return uT

            mkA = self.mark()
            uT = make_uT()
            mk = self.mark()
            wc = self.sb("wc", [128, 8, 1024], BF16)
            self.dma("pool", wc[:], w_in[:, 0:1024].rearrange("(kc p) n -> p kc n", p=128))
            hb = self.sb("hb", [128, LAT + 32], F32)
            cv = [self.sb(f"cv{i}", [128, LAT], F32) for i in range(4)]
            sgt = [self.sb(f"sgt{i}", [128, 512], F32) for i in range(2)]
            self.memset("pool", hb[:], 0.0)
            for cc in range(4):
                for tg in range(4):
                    pv, pg = self.PS[(tg % 2) * 2], self.PS[(tg % 2) * 2 + 1]
                    cols = slice(CTX + tg * 512, CTX + (tg + 1) * 512)
                    for kc in range(8):
                        self.mm(pv[:], wc[:, kc, cc * 128:(cc + 1) * 128], uT[:, kc, cols], start=(kc == 0), stop=(kc == 7))
                    for kc in range(8):
                        self.mm(pg[:], wc[:, kc, 512 + cc * 128:512 + (cc + 1) * 128], uT[:, kc, cols], start=(kc == 0), stop=(kc == 7))
                    st = sgt[tg % 2]
                    self.act(st[:], pg[:], AF.Sigmoid)
                    self.tt("dve", hb[:, 15 + tg * 512:15 + (tg + 1) * 512], pv[:], st[:], ALU.mult)
                NH = 4
                HW_ = LAT // NH
                for j in range(31):
                    for hf in range(NH):
                        o0 = hf * HW_
                        acc = cv[cc][:, o0:o0 + HW_]
                        if j == 0:
                            self.ts("dve", acc, hb[:, o0:o0 + HW_], dww[:, cc, 0:1], cpar[:, 0, cc:cc + 1], ALU.mult, ALU.add,
                                    reads=[hb, dww, cpar], writes=[(cv[cc], hf)])
                        else:
                            self.stt("dve", acc, hb[:, o0 + j:o0 + j + HW_], dww[:, cc, j:j + 1], acc, ALU.mult, ALU.add,
                                     reads=[hb, dww, (cv[cc], hf)], writes=[(cv[cc], hf)])
            mean = self.sb("lnm", [128, 512], F32)
            rstd = self.sb("lnr", [128, 512], F32)
            sq = self.sb("lnsq", [128, 512], F32)
            xn = [self.sb(f"lnx{i}", [128, 512], F32) for i in range(2)]
            for tg in range(4):
                cols = slice(tg * 512, (tg + 1) * 512)
                ps_s, ps_q = self.PS[4], self.PS[5]
                for cc in range(4):
                    self.mm(ps_s[:], cst["ones_f"][:], cv[cc][:, cols], start=(cc == 0), stop=(cc == 3))
                for cc in range(4):
                    self.act(sq[:], cv[cc][:, cols], AF.Square)
                    self.mm(ps_q[:], cst["ones_f"][:], sq[:], start=(cc == 0), stop=(cc == 3))
                self.ts("dve", mean[:], ps_s[:], 1.0 / 512, None, ALU.mult)
                self.tt("dve", rstd[:], mean[:], mean[:], ALU.mult)
                self.stt("dve", rstd[:], ps_q[:], 1.0 / 512, rstd[:], ALU.mult, ALU.subtract)
                self.ts("dve", rstd[:], rstd[:], EPS, None, ALU.add)
                self.act(rstd[:], rstd[:], AF.Sqrt)
                self.v("dve", "reciprocal", rstd[:], rstd[:], reads=[rstd], writes=[rstd])
                for cc in range(4):
                    x = xn[cc % 2]
                    self.tt("pool", x[:], cv[cc][:, cols], mean[:], ALU.subtract)
                    self.tt("dve", x[:], x[:], rstd[:], ALU.mult)
                    self.act(mixA[:, cc, cols], x[:], AF.Silu, bias=cpar[:, 2, cc:cc + 1], scale=cpar[:, 1, cc:cc + 1])
            self.release(mkA)
            if self.stop_after == "l1_conv":
                raise StopBuild()
            mixB = self.sb("mixB", [128, 4, LAT], BF16)
            mkq = self.mark()
            qT = self.sb("qT", [128, 8, LAT], BF16)
            kT = self.sb("kT", [128, 8, SEQ], BF16)
            vaug = self.sb("vaug", [128, NT, 8, 68], BF16)
            mk = self.mark()
            self.memset("pool", vaug[:], 1.0)
            cn = self.sb("cn", [128, 384], F32)
            cnT = self.sb("cnT", [128, 3, 128], BF16)
            xf = self.sb("xf", [128, 8, 96], F32)
            xo = self.sb("xo", [128, 8, 96], F32)
            tmp = self.sb("hn_tmp", [128, 8, 96], F32)
            hs = self.sb("hn_hs", [128, 8], F32)
            rs = self.sb("hn_rs", [128, 8], F32)
            rtt = [self.sb(f"rtt{i}", [128, 8, 16], F32) for i in range(4)]
            krr = self.sb("krr", [128, 32], F32)
            krg = self.sb("krg", [128, 32], F32)
            junk2 = self.sb("junk2", [128, 384], F32)
            def proj_tile(i, uTt):
                import os as _os
                _stg = float(_os.environ.get("KSTG", "9"))
                if _stg <= 0:
                    return
                lat_i = i - 2
                tok = slice(i * 128, (i + 1) * 128)
                pk = self.PS[2]
                for kc in range(8):
                    self.mm(pk[:, 0:288], uTt[:, kc, :], wkv[:, kc, :], start=(kc == 0), stop=(kc == 7))
                if _stg <= 0.1:
                    return
                self.rms_rows(cn[:, 0:256], pk[:, 0:256], 256, kvan[:], junk2[:, 0:256], 10)
                if _stg <= 0.2:
                    return
                self.tt("dve", krg[:], pk[:, 256:288], knb[:, 64:96], ALU.mult)
                if _stg <= 0.3:
                    return
                pt = self.PS[3]
                for kc in range(2):
                    self.tr(pt[:, kc * 128:(kc + 1) * 128], cn[:, kc * 128:(kc + 1) * 128], cst["ident_f"][:])
                self.copy("act", cnT[:, 0:2, :], pt[:, 0:256].rearrange("p (k c) -> p k c", c=128))
                if _stg <= 0.4:
                    return
                pkv = [self.PS[4], self.PS[5]]
                for half in range(2):
                    for kc in range(2):
                        self.mm(pkv[half][:], cnT[:, kc, :], wukv[:, kc, half * 512:(half + 1) * 512], start=(kc == 0), stop=(kc == 1))
                    v4 = pkv[half][:, :].rearrange("p (h d) -> p h d", d=128)
                    if _stg <= 0.5:
                        continue
                    if _stg != 0.56:
                        self.copy("act", xf[:, half * 4:(half + 1) * 4, 0:64], v4[:, :, 0:64])
                    if _stg != 0.55:
                        self.copy("act" if _stg == 0.57 else "dve", vaug[:, i, half * 4:(half + 1) * 4, 0:64], v4[:, :, 64:128])
                if _stg <= 0.6:
                    return
                self.copy("dve", xf[:, :, 64:96], pk[:, 256:288].unsqueeze(1).to_broadcast([128, 8, 32]), reads=[pk], writes=[xf])
                if _stg <= 1:
                    return
                self.head_norm(xf, knb, tmp, hs, rs)
                if _stg <= 2:
                    return
                if lat_i >= 0:
                    self.rope_all(xo, xf, rcos[:, lat_i, :], rsin[:, lat_i, :], rtt)
                    self.copy("act", xo[:, :, 0:64], xf[:, :, 0:64])
                    src = xo
                else:
                    src = xf
                pa, pb_ = self.PS[4], self.PS[5]
                for h in range(8):
                    pp = pa if h < 4 else pb_
                    self.tr(pp[0:96, (h % 4) * 128:(h % 4 + 1) * 128], src[:, h, :], cst["ident_f"][:])
                self.copy("act", kT[0:96, 0:4, tok], pa[0:96, :].rearrange("p (k c) -> p k c", c=128))
                self.copy("dve", kT[0:96, 4:8, tok], pb_[0:96, :].rearrange("p (k c) -> p k c", c=128))
                if lat_i < 0 or _stg <= 3:
                    return
                ltok = slice(lat_i * 128, (lat_i + 1) * 128)
                pq = self.PS[2]
                for kc in range(8):
                    self.mm(pq[:, 0:384], uTt[:, kc, :], wq[:, kc, :], start=(kc == 0), stop=(kc == 7))
                if _stg <= 3.1:
                    return
                self.rms_rows(cn[:, 0:384], pq[:, 0:384], 384, qan[:], junk2[:, 0:384], 12)
                if _stg <= 3.2:
                    return
                pt = self.PS[3]
                for kc in range(3):
                    self.tr(pt[:, kc * 128:(kc + 1) * 128], cn[:, kc * 128:(kc + 1) * 128], cst["ident_f"][:])
                self.copy("act", cnT[:, 0:3, :], pt[:, 0:384].rearrange("p (k c) -> p k c", c=128))
                if _stg <= 3.3:
                    return
                pq1, pq2 = self.PS[4], self.PS[5]
                for kc in range(3):
                    self.mm(pq1[:, 0:384], cnT[:, kc, :], wuq[:, kc, 0:384], start=(kc == 0), stop=(kc == 2))
                for kc in range(3):
                    self.mm(pq2[:, 0:384], cnT[:, kc, :], wuq[:, kc, 384:768], start=(kc == 0), stop=(kc == 2))
                if _stg <= 3.4:
                    return
                self.copy("act", xf[:, 0:4, :], pq1[:, 0:384].rearrange("p (h d) -> p h d", d=96))
                self.copy("dve", xf[:, 4:8, :], pq2[:, 0:384].rearrange("p (h d) -> p h d", d=96))
                if _stg <= 3.5:
                    return
                self.head_norm(xf, qnb, tmp, hs, rs)
                if _stg <= 3.6:
                    return
                self.rope_all(xo, xf, rcos[:, lat_i, :], rsin[:, lat_i, :], rtt)
                if _stg <= 3.7:
                    return
                self.copy("act", xo[:, :, 0:64], xf[:, :, 0:64])
                for h in range(8):
                    pp = pa if h < 4 else pb_
                    self.tr(pp[0:96, (h % 4) * 128:(h % 4 + 1) * 128], xo[:, h, :], cst["ident_f"][:])
                if _stg <= 3.8:
                    return
                self.copy("act", qT[0:96, 0:4, ltok], pa[0:96, :].rearrange("p (k c) -> p k c", c=128))
                self.copy("dve", qT[0:96, 4:8, ltok], pb_[0:96, :].rearrange("p (k c) -> p k c", c=128))

            make_uT(per_tile=proj_tile)
            self.release(mk)
            if self.stop_after == "l1_proj":
                raise StopBuild()
            mk = self.mark()
            attn = self.sb("attn", [128, NL, 512], BF16)
            PT = [self.sb(f"PT{i}", [128, 512], BF16) for i in range(3)]
            rden = self.sb("rden", [128, 4], F32)
            n = 0
            for h in range(8):
                for qg in range(4):
                    qcols = slice(qg * 512, (qg + 1) * 512)
                    po = self.PS[4 + (h * 4 + qg) % 2]
                    def s_mm(kt_, n_):
                        ps_ = self.PS[n_ % 4]
                        self.mm(ps_[:], kT[0:96, h, kt_ * 128:(kt_ + 1) * 128], qT[0:96, h, qcols])
                        self.act(PT[n_ % 3][:], ps_[:], AF.Exp, scale=SC)

                    s_mm(0, n)
                    for kt in range(NT):
                        if kt + 1 < NT:
                            s_mm(kt + 1, n + 1)
                        p_ = PT[n % 3]
                        for qt in range(4):
                            self.mm(po[:, qt * 68:(qt + 1) * 68], p_[:, qt * 128:(qt + 1) * 128], vaug[:, kt, h, :],
                                    start=(kt == 0), stop=(kt == NT - 1))
                        n += 1
                    for qt in range(4):
                        self.v("dve", "reciprocal", rden[:, qt:qt + 1], po[:, qt * 68 + 64:qt * 68 + 65], reads=[po], writes=[rden])
                        self.ts("dve", attn[:, qg * 4 + qt, h * 64:(h + 1) * 64], po[:, qt * 68:qt * 68 + 64], rden[:, qt:qt + 1], None, ALU.mult,
                                reads=[po, rden], writes=[(attn, qg * 4 + qt)])
            for i in range(NL):
                pb = self.PB[i % 2]
                for c in range(4):
                    self.tr(pb[:, c * 128:(c + 1) * 128], attn[:, i, c * 128:(c + 1) * 128], cst["ident_b"][:])
                self.copy("act" if i % 2 == 0 else "dve", mixB[:, :, i * 128:(i + 1) * 128], pb[:, 0:512].rearrange("p (k c) -> p k c", c=128))
            self.release(mkq)
            if self.stop_after == "l1_attn":
                raise StopBuild()
            self.post_mixer(1, b, [mixA, mixB], NL, wr, brr)
            self.release(mkb)
        self.release(mk0)

    def post_mixer(self, layer, b, mixT, ntile, wr, brr):
        nc, S, I, C = self.nc, self.S, self.I, self.C
        cst = self.cst
        mk = self.mark()
        w_out = I["ev_w_out"][0] if layer == 0 else I["od_w_out"][0]
        wo = self.sb("wo", [128, 8, D], BF16)
        self.dma("pool", wo[:], w_out.rearrange("(kc p) n -> p kc n", p=128))
        mr = self.sb("mr2", [128, 6, D], F32)
        self.modrow(mr[:, 0, :], layer, b, 2)
        self.modrow(mr[:, 1, :], layer, b, 4)
        self.modrow(mr[:, 2, :], layer, b, 3)
        self.ts("dve", mr[:, 1, :], mr[:, 1, :], 1.0, None, ALU.add)
        if layer == 0:
            self.modrow(mr[:, 3, :], layer, 4, 2)
            self.modrow(mr[:, 4, :], layer, 4, 4)
            self.modrow(mr[:, 5, :], layer, 4, 3)
            self.ts("dve", mr[:, 4, :], mr[:, 4, :], 1.0, None, ALU.add)
        hts = [self.sb(f"pht{i}", [128, D], F32) for i in range(2)]
        h1s = [self.sb(f"ph1{i}", [128, D], F32) for i in range(2)]
        ms = [self.sb(f"pm{i}", [128, D], F32) for i in range(2)]
        m16 = [self.sb(f"pm16{i}", [128, D], BF16) for i in range(2)]
        mT = self.sb("pmT", [128, 8, 128], F32)
        junk = self.sb("pjunk", [128, D], F32)
        smf = self.sb("smf", [128, 8], F32)

        def front(i):
            if layer == 0:
                isctx = i < 2
                src = I["ctx"][b, i * 128:(i + 1) * 128, :] if isctx else I["x"][b, (i - 2) * 128:(i - 1) * 128, :]
                grow = b * SEQ + i * 128
            else:
                isctx = False
                grow = b * LAT + i * 128
                src = self.H2[b * SEQ + CTX + i * 128:b * SEQ + CTX + (i + 1) * 128, :]
            hdst = self.H1
            o = 3 if isctx else 0
            ht, h1, m, mb = hts[i % 2], h1s[i % 2], ms[i % 2], m16[i % 2]
            self.dma("sp", ht[:], src)
            p0, p1 = self.PS[0], self.PS[1]
            for half, ps in enumerate((p0, p1)):
                for kc in range(8):
                    mx = mixT[kc // 4] if isinstance(mixT, (list, tuple)) else mixT
                    kcc = kc % 4 if isinstance(mixT, (list, tuple)) else kc
                    self.mm(ps[:], mx[:, kcc, i * 128:(i + 1) * 128], wo[:, kc, half * 512:(half + 1) * 512],
                            start=(kc == 0), stop=(kc == 7))
                self.tt("dve", h1[:, half * 512:(half + 1) * 512], ps[:], mr[:, o, half * 512:(half + 1) * 512], ALU.mult)
            self.tt("pool", h1[:], h1[:], ht[:], ALU.add)
            self.dma("sp", hdst[grow:grow + 128, :], h1[:], writes=[(hdst, grow)])
            self.modulate_tile(m[:], h1[:], mr[:, o + 1, :], mr[:, o + 2, :], junk[:], smf[:, 0:1], smf[:, 1:2])
            self.copy("act", mb[:], m[:])
            self.dma("sp", self.M[grow:grow + 128, :], mb[:], writes=[(self.M, grow)])
            self.transpose_tile_to(mT, 0, m, 8, cst["ident_f"], self.PS[2], self.PS[3])
            pl = self.PS[4 + i % 2]
            for kc in range(8):
                self.mm(pl[:, 0:32], mT[:, kc, :], wr[:, kc, :], start=(kc == 0), stop=False)
            self.mm(pl[:, 0:32], cst["ones_f"][0:1, :], brr[0:1, :], start=False, stop=True)
            return grow // 128, pl

        pend = front(0)
        for i in range(ntile):
            cur = pend
            if i + 1 < ntile:
                pend = front(i + 1)
            self.route_tile(layer, cur[0], cur[1])
        self.release(mk)

    def route_init(self, layer):
        ns = self.ns
        T = self.T0 if layer == 0 else self.T1
        self.rT = T
        CAP = self.CAP = CAPS[layer]
        NSLOT = self.NSLOT = NE * CAP
        ntt = T // 128
        self.cntb = self.sb(f"cntb{layer}", [128, NE], F32)
        self.smr = self.sb(f"smr{layer}", [128, 16], F32)
        self.DEST = self.sb(f"DEST{layer}", [128, ntt, 4], I32)
        self.GATE = self.sb(f"GATE{layer}", [128, ntt, 4], F32)
        self.rt = {n: self.sb(f"rt_{n}{layer}", sh, dt) for n, sh, dt in (
            ("L", [128, NE], F32), ("t8", [128, 8], F32), ("e4", [128, 4], F32), ("mask", [128, NE], F32),
            ("m16", [128, NE], BF16), ("rf", [128, NE], F32), ("rfs", [128, NE], F32), ("tmp", [128, NE], F32),
            ("rk", [128, 4], F32), ("dk", [128, 4], F32), ("ok", [128, 4], F32), ("tok", [128, 2], F32),
            ("toki", [128, 2], I32), ("trash", [128, 1], F32), ("fill", [128, (NSLOT + 128) // 64], I32),
            ("z16", [128, D], BF16))}
        rt = self.rt
        self.memset("dve", self.cntb[:], 0.0)
        self.ts("dve", rt["trash"][:], self.cst["pidx"][:], float(NSLOT), None, ALU.add)
        self.memset("dve", rt["fill"][:], int(T))
        self.dma("sp", self.SLOT[0:NSLOT + 128, :].rearrange("(p j) two -> p (j two)", p=128), rt["fill"][:])
        self.memset("pool", rt["z16"][:], 0.0)
        self.dma("sp", self.M[T:T + 128, :], rt["z16"][:], writes=[(self.M, "trash")])
        self.dma("sp", self.YB[NSLOT:NSLOT + 128, :], rt["z16"][:], writes=[(self.YB, "trash")])

    def route_tile(self, layer, gt, pl):
        nc, rt, cst = self.nc, self.rt, self.cst
        CAP, NSLOT = self.CAP, self.NSLOT
        L, t8 = rt["L"], rt["t8"]
        self.copy("dve", L[:], pl[:, 0:NE])
        self.v("dve", "max", t8[:], L[:], reads=[L], writes=[t8])
        sm = self.smr
        self.ts("dve", sm[:, 8:9], t8[:, 0:1], -1.0, None, ALU.mult)
        self.act(rt["e4"][:], t8[:, 0:4], AF.Exp, bias=sm[:, 8:9], scale=1.0)
        self.v("dve", "reduce_sum", sm[:, 9:10], rt["e4"][:], AX.X, reads=[rt["e4"]], writes=[sm])
        self.v("dve", "reciprocal", sm[:, 9:10], sm[:, 9:10], reads=[sm], writes=[sm])
        self.ts("dve", rt["e4"][:], rt["e4"][:], sm[:, 9:10], None, ALU.mult)
        self.ts("dve", rt["mask"][:], L[:], t8[:, 3:4], None, ALU.is_ge)
        self.copy("dve", rt["m16"][:], rt["mask"][:])
        pr = pl
        self.mm(pr[:, 128:128 + NE], cst["tri_b"][:], rt["m16"][:])
        self.mm(pr[:, 64:64 + NE], cst["ones_b"][:], rt["m16"][:])
        self.tt("dve", rt["rf"][:], pr[:, 128:128 + NE], self.cntb[:], ALU.add)
        self.tt("dve", self.cntb[:], self.cntb[:], pr[:, 64:64 + NE], ALU.add)
        self.tt("dve", rt["rfs"][:], rt["rf"][:], cst[f"slotbase{layer}"][:], ALU.add)
        for k in range(4):
            self.stt("dve", rt["tmp"][:], L[:], t8[:, k:k + 1], rt["rf"][:], ALU.is_equal, ALU.mult)
            self.v("dve", "reduce_sum", rt["rk"][:, k:k + 1], rt["tmp"][:], AX.X, reads=[rt["tmp"]], writes=[rt["rk"]])
            self.stt("dve", rt["tmp"][:], L[:], t8[:, k:k + 1], rt["rfs"][:], ALU.is_equal, ALU.mult)
            self.v("dve", "reduce_sum", rt["dk"][:, k:k + 1], rt["tmp"][:], AX.X, reads=[rt["tmp"]], writes=[rt["dk"]])
        self.ts("dve", rt["ok"][:], rt["rk"][:], float(CAP), None, ALU.is_lt)
        self.stt("dve", rt["dk"][:], rt["dk"][:], rt["trash"][:, 0:1], rt["ok"][:], ALU.subtract, ALU.mult)
        self.ts("dve", rt["dk"][:], rt["dk"][:], rt["trash"][:, 0:1], None, ALU.add)
        self.tt("dve", self.GATE[:, gt, :], rt["e4"][:], rt["ok"][:], ALU.mult, writes=[(self.GATE, gt)])
        self.copy("dve", self.DEST[:, gt, :], rt["dk"][:], writes=[(self.DEST, gt)])
        self.ts("dve", rt["tok"][:, 0:1], cst["pidx"][:], float(gt * 128), None, ALU.add)
        self.copy("dve", rt["tok"][:, 1:2], rt["tok"][:, 0:1])
        self.copy("dve", rt["toki"][:], rt["tok"][:])
        for k in range(4):
            self.dma("pool", self.SLOT[:, :], rt["toki"][:, :], reads=[rt["toki"], (self.DEST, gt)], writes=[(self.SLOT, (gt, k))],
                     indirect=dict(out_offset=bass.IndirectOffsetOnAxis(ap=self.DEST[:, gt, k:k + 1], axis=0), in_offset=None))

    def experts(self, layer):
        nc, I, cst = self.nc, self.I, self.cst
        CAP, NSLOT = self.CAP, self.NSLOT
        mk = self.mark()
        w1s = [self.sb(f"w1s{i}", [128, 8, 2 * D], BF16) for i in range(2)]
        w2s = [self.sb(f"w2s{i}", [128, 8, D], BF16) for i in range(2)]
        b1s = [self.sb(f"b1s{i}", [128, 16], F32) for i in range(2)]
        b2s = [self.sb(f"b2s{i}", [128, D], F32) for i in range(2)]
        idx = [[self.sb(f"idx{s_}{i}", [128, 2], I32) for i in range(4)] for s_ in range(2)]
        xg = [[self.sb(f"xg{s_}{i}", [128, D], BF16) for i in range(4)] for s_ in range(2)]
        xT = [self.sb(f"xT{i}", [128, 8, 512], BF16) for i in range(2)]
        aT = [self.sb(f"aT{i}", [128, 8, 512], BF16) for i in range(2)]
        glc = [self.sb(f"glc{i}", [128, 512], F32) for i in range(2)]
        sig = [self.sb(f"sig{i}", [128, 512], F32) for i in range(2)]
        lin = [self.sb(f"lin{i}", [128, 512], F32) for i in range(2)]
        yt = [self.sb(f"yt{i}", [128, D], BF16) for i in range(2)]
        for xs in xg:
            for x in xs:
                self.memset("dve", x[:], 0.0)
        NG = CAP // 512
        items = [(e, g) for e in range(NE) for g in range(NG)]

        def load_weights(e):
            w1, w2, b1, b2 = w1s[e % 2], w2s[e % 2], b1s[e % 2], b2s[e % 2]
            src1 = I["moe_w1"][layer, e].rearrange("(kc p) n -> p kc n", p=128)
            for q in range(4):
                self.dma("pool", w1[:, 2 * q:2 * q + 2, :], src1[:, 2 * q:2 * q + 2, :], writes=[(w1, q)])
            src2 = I["moe_w2"][layer, e].rearrange("(kc p) n -> p kc n", p=128)
            for q in range(2):
                self.dma("pool", w2[:, 4 * q:4 * q + 4, :], src2[:, 4 * q:4 * q + 4, :], writes=[(w2, q)])
            self.dma("sp", b1[:], I["moe_b1"][layer, e].rearrange("(c p) -> p c", p=128), allow_slow_non_contiguous=True)
            self.dma("sp", b2[:], I["moe_b2"][layer, e:e + 1, :].partition_broadcast(128))
            self.ts("dve", b1[:, 8:16], b1[:, 8:16], 1.0, None, ALU.add)

        def load_tokens(k):
            e, grp = items[k]
            st = k % 2
            for j in range(4):
                r0 = e * CAP + (grp * 4 + j) * 128
                self.dma("sp", idx[st][j][:], self.SLOT[r0:r0 + 128, :], reads=[self.SLOT])
                self.dma("pool", xg[st][j][:], self.M[:, :], reads=[self.M, idx[st][j]],
                         indirect=dict(out_offset=None, in_offset=bass.IndirectOffsetOnAxis(ap=idx[st][j][:, 0:1], axis=0)))

        def stageT(k):
            e, grp = items[k]
            st = k % 2
            x_t = xT[k % 2]
            for j in range(4):
                pb = self.PB[j % 2]
                for kc in range(8):
                    self.tr(pb[:, kc * 128:(kc + 1) * 128], xg[st][j][:, kc * 128:(kc + 1) * 128], cst["ident_b"][:])
                self.copy("act", x_t[:, :, j * 128:(j + 1) * 128], pb[:, :].rearrange("p (k c) -> p k c", c=128))

        def stageH(k):
            e, grp = items[k]
            w1, b1 = w1s[e % 2], b1s[e % 2]
            x_t, a_t = xT[k % 2], aT[k % 2]
            for jc in range(8):
                pg, plin = self.PS[(jc % 2) * 2], self.PS[(jc % 2) * 2 + 1]
                for kc in range(8):
                    self.mm(pg[:], w1[:, kc, jc * 128:(jc + 1) * 128], x_t[:, kc, :], start=(kc == 0), stop=(kc == 7))
                for kc in range(8):
                    self.mm(plin[:], w1[:, kc, D + jc * 128:D + (jc + 1) * 128], x_t[:, kc, :], start=(kc == 0), stop=(kc == 7))
                g_, s_, l_ = glc[jc % 2], sig[jc % 2], lin[jc % 2]
                self.ts("dve", g_[:], pg[:], b1[:, jc:jc + 1], 7.0, ALU.add, ALU.min)
                self.act(s_[:], g_[:], AF.Sigmoid, scale=1.702)
                self.ts("dve", l_[:], plin[:], b1[:, 8 + jc:9 + jc], 8.0, ALU.add, ALU.min)
                self.tt("dve", g_[:], g_[:], s_[:], ALU.mult)
                self.stt("dve", a_t[:, jc, :], l_[:], -6.0, g_[:], ALU.max, ALU.mult)

        def stageY(k):
            e, grp = items[k]
            w2, b2 = w2s[e % 2], b2s[e % 2]
            a_t = aT[k % 2]
            for jt in range(4):
                y = yt[jt % 2]
                for half in range(2):
                    ps = self.PS[4 + half]
                    for jc in range(8):
                        self.mm(ps[:], a_t[:, jc, jt * 128:(jt + 1) * 128], w2[:, jc, half * 512:(half + 1) * 512],
                                start=(jc == 0), stop=(jc == 7))
                    self.tt("dve", y[:, half * 512:(half + 1) * 512], ps[:], b2[:, half * 512:(half + 1) * 512], ALU.add)
                r0 = e * CAP + (grp * 4 + jt) * 128
                self.dma("sp", self.YB[r0:r0 + 128, :], y[:], writes=[(self.YB, r0)])

        load_weights(0)
        load_tokens(0)
        if len(items) > 1:
            load_tokens(1)
        stageT(0)
        for k in range(len(items)):
            e, grp = items[k]
            if grp == 0 and e + 1 < NE:
                load_weights(e + 1)
            if k + 2 < len(items):
                load_tokens(k + 2)
            stageH(k)
            if k + 1 < len(items):
                stageT(k + 1)
            stageY(k)
        self.release(mk)

    def combine(self, layer):
        nc, I, cst = self.nc, self.I, self.cst
        ns = self.ns
        mk = self.mark()
        g2 = self.sb("g2", [128, 2, D], F32)
        yk = [self.sb(f"yk{i}", [128, D], BF16) for i in range(4)]
        acc = [self.sb(f"acc{i}", [128, D], F32) for i in range(2)]
        h1t = [self.sb(f"h1t{i}", [128, D], F32) for i in range(2)]
        if layer == 0:
            self.modrow(g2[:, 1, :], 0, 4, 5)
        nt_seq = NT if layer == 0 else LAT // 128
        for b in range(ns):
            self.modrow(g2[:, 0, :], layer, b, 5)
            for i in range(nt_seq):
                gt = b * nt_seq + i
                grow = gt * 128
                a, h = acc[gt % 2], h1t[gt % 2]
                self.dma("sp", h[:], self.H1[grow:grow + 128, :], reads=[self.H1])
                for k in range(4):
                    self.dma("pool", yk[k][:], self.YB[:, :], reads=[self.YB, (self.DEST, gt)],
                             indirect=dict(out_offset=None, in_offset=bass.IndirectOffsetOnAxis(ap=self.DEST[:, gt, k:k + 1], axis=0)))
                    if k == 0:
                        self.ts("dve", a[:], yk[0][:], self.GATE[:, gt, 0:1], None, ALU.mult, reads=[yk[0], (self.GATE, gt)])
                    else:
                        self.stt("dve", a[:], yk[k][:], self.GATE[:, gt, k:k + 1], a[:], ALU.mult, ALU.add,
                                 reads=[yk[k], a, (self.GATE, gt)])
                gsel = 1 if (layer == 0 and i < 2) else 0
                self.tt("pool", a[:], a[:], g2[:, gsel, :], ALU.mult)
                self.tt("dve", a[:], a[:], h[:], ALU.add)
                if layer == 0:
                    self.dma("sp", self.H2[grow:grow + 128, :], a[:], writes=[(self.H2, grow)])
                else:
                    self.dma("sp", self.out[b, i * 128:(i + 1) * 128, :], a[:], writes=[("out", grow)])
        self.release(mk)


def build_program(ns, **kw):
    k = K(ns, **kw)
    k.build()
    return k


_CACHE = {}


def kernel(**inputs):
    ncores, ns = 8, 4
    if "k" not in _CACHE:
        _CACHE["k"] = build_program(ns)
    k = _CACHE["k"]
    consts = {"k_" + n: v for n, v in host_consts().items()}
    shared = {n: np.ascontiguousarray(np.asarray(inputs[n], dtype=np.float32)) for n in IN_SPECS if n not in ("x", "c", "ctx")}
    in_maps = []
    for c in range(ncores):
        m = dict(shared)
        for n in ("x", "c", "ctx"):
            m[n] = np.ascontiguousarray(np.asarray(inputs[n], dtype=np.float32)[c * ns:(c + 1) * ns])
        m.update(consts)
        in_maps.append(m)
    res = run_bass_kernel_spmd(k.nc, in_maps, core_ids=list(range(ncores)))
    return np.concatenate([r["out"] for r in res.results], axis=0).astype(np.float32)
```

```python
import numpy as np
import ml_dtypes
import concourse.bass as bass
import concourse.mybir as mybir
from concourse.bass_utils import run_bass_kernel_spmd

F32 = mybir.dt.float32
BF16 = mybir.dt.bfloat16
I32 = mybir.dt.int32
AF = mybir.ActivationFunctionType
ALU = mybir.AluOpType
AX = mybir.AxisListType

D = 1024
LAT = 2048
CTX = 256
SEQ = LAT + CTX
NT = SEQ // 128
EPS = 1e-6
NE = 32
CAPS = (2560, 2048)
CAPMAX = max(CAPS)
NSLOTMAX = NE * CAPMAX


class Sched:
    EPOCH = 30000

    def __init__(self, nc):
        self.nc = nc
        self.E = {"pe": nc.tensor, "dve": nc.vector, "act": nc.scalar, "pool": nc.gpsimd, "sp": nc.sync}
        self.nsem = 0
        self.esem, self.ecnt = {}, {}
        self.pesems = set()
        for e in self.E:
            self._new_epoch(e)
        self.seen = {e: {} for e in self.E}
        self.W, self.R = {}, {}
        self.dpool = {q: [] for q in ("sp", "pool", "act")}
        self.dnext = {q: 0 for q in self.dpool}
        self.NPOOL = {"sp": 24, "pool": 24, "act": 4}
        self.ninst = 0
        self.psum = set()

    def _sem(self, name):
        self.nsem += 1
        return self.nc.semaphore(f"{name}{self.nsem}").__enter__()

    def _new_epoch(self, e):
        import os as _os
        self.skip_same = _os.environ.get("KSAME", "1") == "0"
        if not hasattr(self, "own"):
            self.own = {}
        self.esem[e] = self._sem("e" + e)
        self.own.setdefault(e, set()).add(self.esem[e])
        self.ecnt[e] = 0
        if e == "pe":
            self.pesems.add(self.esem[e])

    @staticmethod
    def _nm(x):
        if isinstance(x, str):
            return x
        t = getattr(x, "tensor", None)
        return t.name if t is not None else x.name

    @staticmethod
    def _key(x):
        if isinstance(x, tuple):
            return (Sched._nm(x[0]), x[1])
        return (Sched._nm(x), None)

    def _collect(self, table, key, evs):
        name, sub = key
        t = table.get(name)
        if not t:
            return
        subs = t.keys() if sub is None else [s for s in (sub, None) if s in t]
        for s in subs:
            for sem, val in t[s].items():
                if evs.get(sem, 0) < val:
                    evs[sem] = val

    def _deps(self, e, rk, wk):
        evs = {}
        for k in rk:
            self._collect(self.W, k, evs)
        for k in wk:
            self._collect(self.W, k, evs)
            self._collect(self.R, k, evs)
        for sem, val in evs.items():
            if e == "pe" and sem in self.pesems:
                continue
            if self.skip_same and sem in self.own.get(e, ()):
                continue
            if self.seen[e].get(sem, 0) >= val:
                continue
            self.E[e].wait_ge(sem, val)
            self.seen[e][sem] = val

    def _commit(self, ev, rk, wk):
        sem, val = ev
        for name, sub in rk:
            d = self.R.setdefault(name, {}).setdefault(sub, {})
            if d.get(sem, 0) < val:
                d[sem] = val
        for name, sub in wk:
            if sub is None:
                self.W[name] = {None: {sem: val}}
                self.R[name] = {}
            else:
                self.W.setdefault(name, {})[sub] = {sem: val}
                self.R.setdefault(name, {}).pop(sub, None)

    def op(self, e, fn, reads=(), writes=()):
        rk = [self._key(x) for x in reads]
        wk = [self._key(x) for x in writes]
        pr = [(k[0], None) for k in rk if k[0] in self.psum]
        if pr:
            rk = [k for k in rk if k[0] not in self.psum]
            wk = wk + pr
        wk = [((k[0], None) if k[0] in self.psum else k) for k in wk]
        self._deps(e, rk, wk)
        if self.ecnt[e] >= self.EPOCH:
            self._new_epoch(e)
        ins = fn()
        self.ecnt[e] += 1
        ins.then_inc(self.esem[e], 1)
        self._commit((self.esem[e], self.ecnt[e]), rk, wk)
        self.ninst += 1
        return ins

    def dma(self, q, out, in_, reads=None, writes=None, indirect=None, **kw):
        rk = [self._key(x) for x in (reads if reads is not None else [in_])]
        wk = [self._key(x) for x in (writes if writes is not None else [out])]
        self._deps(q, rk, wk)
        pool = self.dpool[q]
        if len(pool) < self.NPOOL[q]:
            pool.append([self._sem("d" + q), 0])
            slot = pool[-1]
        else:
            slot = pool[self.dnext[q] % len(pool)]
            self.dnext[q] += 1
            if slot[1] >= 60000:
                if self.seen[q].get(slot[0], 0) < slot[1]:
                    self.E[q].wait_ge(slot[0], slot[1])
                slot[0], slot[1] = self._sem("d" + q), 0
        sem, cnt = slot
        if cnt > 0 and self.seen[q].get(sem, 0) < cnt:
            self.E[q].wait_ge(sem, cnt)
            self.seen[q][sem] = cnt
        if indirect is None:
            ins = self.E[q].dma_start(out=out, in_=in_, **kw)
        else:
            ins = self.E[q].indirect_dma_start(out=out, in_=in_, **indirect)
        slot[1] = cnt + 16
        ins.then_inc(sem, 16)
        self._commit((sem, slot[1]), rk, wk)
        self.ninst += 1
        return ins

    def barrier(self):
        evs = {}
        for e in self.E:
            if self.ecnt[e] > 0:
                evs[self.esem[e]] = self.ecnt[e]
        for q, pool in self.dpool.items():
            for sem, cnt in pool:
                if cnt > 0:
                    evs[sem] = cnt
        for e in self.E:
            for sem, val in evs.items():
                if self.seen[e].get(sem, 0) >= val:
                    continue
                self.E[e].wait_ge(sem, val)
                self.seen[e][sem] = val
        self.W, self.R = {}, {}

    def finish(self):
        self.barrier()


def host_consts():
    c = {}
    c["ident_f"] = np.eye(128, dtype=np.float32)
    c["ident_b"] = np.eye(128).astype(ml_dtypes.bfloat16)
    s = np.arange(128)[:, None]
    t = np.arange(128)[None, :]
    c["mask_f"] = (s <= t).astype(np.float32)
    c["mask_b"] = (s >= t).astype(np.float32)
    c["tri_b"] = (s < t).astype(ml_dtypes.bfloat16)
    c["ones_b"] = np.ones((128, 128), ml_dtypes.bfloat16)
    c["ones_f"] = np.ones((128, 128), np.float32)
    facS = np.ones((4, 8), np.float32)
    facE = np.ones((4, 8), np.float32)
    for g, w in enumerate((2, 4, 8, 16)):
        half = w // 2
        for tt in range(min(half, 8)):
            facS[g, tt] = w / (tt + half)
        for j in range(8):
            i = 7 - j
            if i < half - 1:
                facE[g, j] = w / (half + 1 + i)
    c["facS"] = np.broadcast_to(facS[None], (128, 4, 8)).copy()
    c["facE"] = np.broadcast_to(facE[None], (128, 4, 8)).copy()
    rows = LAT // 64
    t_row = np.repeat(np.arange(rows, dtype=np.float32), 64)
    t_col = np.tile(np.arange(64, dtype=np.float32), rows)
    inv = (1.0 / (10000.0 ** (np.arange(8, dtype=np.float32) / 8))).astype(np.float32)
    ang = np.stack([t_row[:, None] * inv, t_col[:, None] * inv], axis=1).astype(np.float32)
    cos = np.cos(ang).reshape(LAT, 16).astype(np.float32)
    sin = np.sin(ang).reshape(LAT, 16).astype(np.float32)
    for l_ in range(2):
        c[f"slotbase{l_}"] = np.broadcast_to((np.arange(32, dtype=np.float32) * CAPS[l_])[None], (128, 32)).copy()
    c["pidx"] = np.arange(128, dtype=np.float32).reshape(128, 1).copy()
    c["rope_cos"] = cos.reshape(16, 128, 16).transpose(1, 0, 2).copy()
    c["rope_sin"] = sin.reshape(16, 128, 16).transpose(1, 0, 2).copy()
    return c


CONST_SPECS = {
    "ident_f": ([128, 128], F32), "ident_b": ([128, 128], BF16), "mask_f": ([128, 128], F32),
    "mask_b": ([128, 128], F32), "tri_b": ([128, 128], BF16), "ones_b": ([128, 128], BF16),
    "ones_f": ([128, 128], F32), "facS": ([128, 4, 8], F32), "facE": ([128, 4, 8], F32),
    "slotbase0": ([128, 32], F32), "slotbase1": ([128, 32], F32), "pidx": ([128, 1], F32),
    "rope_cos": ([128, 16, 16], F32), "rope_sin": ([128, 16, 16], F32),
}

IN_SPECS = {
    "x": lambda ns: [ns, LAT, D], "c": lambda ns: [ns, D], "ctx": lambda ns: [ns, CTX, D], "c_ctx": lambda ns: [D],
    "ada_w": lambda ns: [2, D, 6 * D], "ada_b": lambda ns: [2, 6 * D], "ev_w_in": lambda ns: [1, D, 3072],
    "hgrn_lb_logits": lambda ns: [3, 2, 512], "hgrn_norm_w": lambda ns: [1, 128], "pool_w": lambda ns: [1, 4, 128, 128],
    "pool_scale": lambda ns: [1, 512], "ev_w_out": lambda ns: [1, D, D], "od_w_in": lambda ns: [1, D, 1696],
    "conv_dw_w": lambda ns: [1, 31, 512], "conv_dw_b": lambda ns: [1, 512], "conv_ln_w": lambda ns: [1, 512],
    "conv_ln_b": lambda ns: [1, 512], "mla_q_a_norm": lambda ns: [1, 384], "mla_w_uq": lambda ns: [1, 384, 768],
    "mla_kv_a_norm": lambda ns: [1, 256], "mla_w_ukv": lambda ns: [1, 256, 1024], "mla_q_norm": lambda ns: [1, 96],
    "mla_k_norm": lambda ns: [1, 96], "od_w_out": lambda ns: [1, D, D], "moe_router_w": lambda ns: [2, D, 32],
    "moe_router_b": lambda ns: [2, 32], "moe_w1": lambda ns: [2, 32, D, 2 * D], "moe_b1": lambda ns: [2, 32, 2 * D],
    "moe_w2": lambda ns: [2, 32, D, D], "moe_b2": lambda ns: [2, 32, D],
}


class StopBuild(Exception):
    pass


class K:
    def __init__(self, ns, stop_after=None, dbg=(), skip_inputs=()):
        self.ns = ns
        self.stop_after = stop_after
        self.dbg = set(dbg)
        nc = self.nc = bass.Bass("TRN2", target_bir_lowering=False)
        self.S = Sched(nc)
        self.I = {k: nc.dram_tensor(k, f(ns), F32, kind="ExternalInput").ap() for k, f in IN_SPECS.items() if k not in skip_inputs}
        self.C = {k: nc.dram_tensor("k_" + k, sh, dt, kind="ExternalInput").ap() for k, (sh, dt) in CONST_SPECS.items()}
        self.out = nc.dram_tensor("out", [ns, LAT, D], F32, kind="ExternalOutput").ap()
        self.T0 = ns * SEQ
        self.T1 = ns * LAT
        self.MOD = nc.dram_tensor("MOD", [10, 6 * D], F32).ap()
        self.H1 = nc.dram_tensor("H1", [self.T0, D], F32).ap()
        self.H2 = nc.dram_tensor("H2", [self.T0, D], F32).ap()
        self.M = nc.dram_tensor("M", [self.T0 + 128, D], BF16).ap()
        self.SLOT = nc.dram_tensor("SLOT", [NSLOTMAX + 128, 2], I32).ap()
        self.YB = nc.dram_tensor("YB", [NSLOTMAX + 128, D], BF16).ap()
        self.dbg_out = {}
        self._stack = []

    def sb(self, name, shape, dt):
        self._uid = getattr(self, "_uid", 0) + 1
        cm = self.nc.sbuf_tensor(f"{name}_{self._uid}", shape, dt)
        t = cm.__enter__()
        self._stack.append(cm)
        return t

    def ps(self, name, shape, dt):
        cm = self.nc.psum_tensor(name, shape, dt)
        t = cm.__enter__()
        self.S.psum.add(name)
        self._stack.append(cm)
        return t

    def mark(self):
        return len(self._stack)

    def release(self, mark):
        self.S.barrier()
        while len(self._stack) > mark:
            self._stack.pop().__exit__(None, None, None)

    def mm(self, out, lhsT, rhs, start=True, stop=True, reads=None, writes=None):
        nc = self.nc
        return self.S.op("pe", lambda: nc.tensor.matmul(out, lhsT, rhs, start=start, stop=stop),
                         reads=reads if reads is not None else [lhsT, rhs], writes=writes if writes is not None else [out])

    def tr(self, out, in_, ident, reads=None, writes=None):
        nc = self.nc
        return self.S.op("pe", lambda: nc.tensor.transpose(out, in_, ident),
                         reads=reads if reads is not None else [in_, ident], writes=writes if writes is not None else [out])

    def act(self, out, in_, func, bias=None, scale=None, accum_out=None, reads=None, writes=None):
        nc = self.nc
        kw = {}
        rd = [in_]
        wr = [out]
        if bias is not None:
            kw["bias"] = bias
            if not isinstance(bias, (int, float)):
                rd.append(bias)
        if scale is not None:
            kw["scale"] = scale
            if not isinstance(scale, (int, float)):
                rd.append(scale)
        if accum_out is not None:
            kw["accum_out"] = accum_out
            wr.append(accum_out)
        return self.S.op("act", lambda: nc.scalar.activation(out, in_, func, **kw),
                         reads=reads if reads is not None else rd, writes=writes if writes is not None else wr)

    def v(self, e, name, *args, reads, writes, **kw):
        eng = self.S.E[e]
        return self.S.op(e, lambda: getattr(eng, name)(*args, **kw), reads=reads, writes=writes)

    def tt(self, e, out, in0, in1, op, reads=None, writes=None):
        return self.v(e, "tensor_tensor", out, in0, in1, op, reads=reads if reads is not None else [in0, in1],
                      writes=writes if writes is not None else [out])

    def ts(self, e, out, in0, s1, s2, op0, op1=None, reads=None, writes=None, accum_out=None):
        rd = [in0] + [s for s in (s1, s2) if s is not None and not isinstance(s, (int, float))]
        kw = {}
        if op1 is not None:
            kw["op1"] = op1
        wr = [out]
        if accum_out is not None:
            kw["accum_out"] = accum_out
            wr.append(accum_out)
        return self.v(e, "tensor_scalar", out, in0, s1, s2, op0, reads=reads if reads is not None else rd,
                      writes=writes if writes is not None else wr, **kw)

    def stt(self, e, out, in0, scalar, in1, op0, op1, reads=None, writes=None):
        rd = [in0, in1] + ([] if isinstance(scalar, (int, float)) else [scalar])
        return self.v(e, "scalar_tensor_tensor", out, in0, scalar, in1, op0, op1,
                      reads=reads if reads is not None else rd, writes=writes if writes is not None else [out])

    def copy(self, e, out, in_, reads=None, writes=None):
        if e == "act":
            return self.act(out, in_, AF.Copy, reads=reads, writes=writes)
        return self.v(e, "tensor_copy", out, in_, reads=reads if reads is not None else [in_],
                      writes=writes if writes is not None else [out])

    def memset(self, e, ap, val):
        return self.v(e, "memset", ap, val, reads=[], writes=[ap])

    def dma(self, q, out, in_, **kw):
        return self.S.dma(q, out, in_, **kw)

    def rstd_from_ss(self, rstd, ss, n):
        self.ts("dve", rstd, ss, 1.0 / n, EPS, ALU.mult, ALU.add)
        self.act(rstd, rstd, AF.Sqrt)
        self.v("dve", "reciprocal", rstd, rstd, reads=[rstd], writes=[rstd])

    def build(self):
        nc, S, I, C = self.nc, self.S, self.I, self.C
        ns = self.ns
        self.cst = {}
        for k in ("ident_f", "ident_b", "mask_f", "mask_b", "tri_b", "ones_b", "ones_f", "slotbase0", "slotbase1", "pidx"):
            sh, dt = CONST_SPECS[k]
            t = self.sb("c_" + k, sh, dt)
            self.dma("sp", t[:], C[k][:, :])
            self.cst[k] = t
        self.PS = [self.ps(f"ps{i}", [128, 512], F32) for i in range(6)]
        self.PB = [self.ps(f"pb{i}", [128, 1024], BF16) for i in range(2)]
        self.small = self.sb("small", [128, 64], F32)

        self.phase_adaln()
        if self.stop_after == "adaln":
            return self.end()
        mkr = self.mark()
        self.route_init(0)
        try:
            self.layer0()
        except StopBuild:
            return self.end()
        if self.stop_after == "mixer0":
            return self.end()
        if self.stop_after and self.stop_after.startswith("l1_"):
            self.S.barrier()
            self.dma("sp", self.H2, self.H1)
        else:
            self.experts(0)
            self.combine(0)
        self.release(mkr)
        if self.stop_after == "layer0":
            return self.end()
        mkr = self.mark()
        self.route_init(1)
        try:
            self.layer1()
        except StopBuild:
            return self.end()
        self.experts(1)
        self.combine(1)
        self.release(mkr)
        return self.end()

    def end(self):
        for name in ("MOD", "H1", "H2", "M"):
            if name in self.dbg:
                src = getattr(self, name)
                d = self.dbg_dump(name, list(src.shape), src.dtype)
                self.S.barrier()
                self.dma("sp", d, src)
        self.S.finish()
        return self.nc

    def dbg_dump(self, name, shape, dt=F32):
        t = self.nc.dram_tensor("dbg_" + name, shape, dt, kind="ExternalOutput").ap()
        self.dbg_out[name] = t
        return t

    def phase_adaln(self):
        nc, S, I, C = self.nc, self.S, self.I, self.C
        ns = self.ns
        mk = self.mark()
        cs = self.sb("cs", [128, 8, 8], F32)
        s5 = self.sb("s5", [128, 8, 8], F32)
        self.memset("dve", cs[:], 0.0)
        for b in range(ns):
            self.dma("sp", cs[:, :, b], I["c"][b].rearrange("(kc p) -> p kc", p=128), allow_slow_non_contiguous=True)
        self.dma("sp", cs[:, :, ns], I["c_ctx"].rearrange("(kc p) -> p kc", p=128), allow_slow_non_contiguous=True)
        self.act(s5[:], cs[:], AF.Silu)
        nr = ns + 1
        brow = self.sb("brow", [1, 6 * D], F32)
        wa = [self.sb(f"wa{i}", [128, 8, 512], F32) for i in range(2)]
        mo = [self.sb(f"mo{i}", [8, 512], F32) for i in range(2)]
        onesf = self.cst["ones_f"]
        it = 0
        for layer in range(2):
            self.dma("sp", brow[:], I["ada_b"][layer:layer + 1, :])
            for nb in range(12):
                w = wa[it % 2]
                self.dma("sp" if it % 2 == 0 else "pool", w[:],
                         I["ada_w"][layer][:, nb * 512:(nb + 1) * 512].rearrange("(kc p) n -> p kc n", p=128))
                ps = self.PS[it % 2]
                for kc in range(8):
                    self.mm(ps[0:nr, :], s5[:, kc, 0:nr], w[:, kc, :], start=(kc == 0), stop=False)
                self.mm(ps[0:nr, :], onesf[0:1, 0:nr], brow[0:1, nb * 512:(nb + 1) * 512], start=False, stop=True)
                m = mo[it % 2]
                self.copy("dve", m[0:nr, :], ps[0:nr, :])
                self.dma("sp", self.MOD[layer * 5:layer * 5 + ns, nb * 512:(nb + 1) * 512], m[0:ns, :],
                         writes=[(self.MOD, (layer, nb, 0))])
                self.dma("sp", self.MOD[layer * 5 + 4:layer * 5 + 5, nb * 512:(nb + 1) * 512], m[ns:ns + 1, :],
                         writes=[(self.MOD, (layer, nb, 1))])
                it += 1
        self.release(mk)

    def modrow(self, dst, layer, row, q):
        src = self.MOD[layer * 5 + row:layer * 5 + row + 1, q * D:(q + 1) * D].partition_broadcast(128)
        self.dma("sp", dst, src, reads=[self.MOD])

    def modulate_tile(self, u, ht, sc1p, sh, junk, ss, rstd):
        self.act(junk, ht, AF.Square, accum_out=ss)
        self.rstd_from_ss(rstd, ss, D)
        self.stt("dve", u, ht, rstd, sc1p, ALU.mult, ALU.mult)
        self.tt("pool", u, u, sh, ALU.add)

    def modulate_pipe(self, uT, srcs, hts, us, junk, mr, read_keys=None):
        n = len(srcs)
        st = self.sb("mp_st", [128, 2 * n], F32)
        cst = self.cst

        def stats(i):
            ht = hts[i % 3]
            kw = {} if read_keys is None else {"reads": read_keys}
            self.dma("sp", ht[:], srcs[i], **kw)
            self.act(junk[:], ht[:], AF.Square, accum_out=st[:, i:i + 1], writes=[junk, (st, i)])
            self.ts("dve", st[:, n + i:n + i + 1], st[:, i:i + 1], 1.0 / D, EPS, ALU.mult, ALU.add, reads=[(st, i)], writes=[(st, n + i)])
            self.act(st[:, n + i:n + i + 1], st[:, n + i:n + i + 1], AF.Sqrt, reads=[(st, n + i)], writes=[(st, n + i)])
            self.v("dve", "reciprocal", st[:, n + i:n + i + 1], st[:, n + i:n + i + 1], reads=[(st, n + i)], writes=[(st, n + i)])

        def apply(i):
            ht, u = hts[i % 3], us[i % 2]
            o = 2 if i < 2 else 0
            self.stt("dve", u[:], ht[:], st[:, n + i:n + i + 1], mr[:, o, :], ALU.mult, ALU.mult, reads=[ht, (st, n + i), mr], writes=[u])
            self.tt("pool", u[:], u[:], mr[:, o + 1, :], ALU.add)
            self.transpose_tile_to(uT, i * 128, u, 8, cst["ident_f"], self.PS[0], self.PS[1])

        stats(0)
        for i in range(n):
            if i + 1 < n:
                stats(i + 1)
            apply(i)

    def transpose_tile_to(self, dstT, col0, src, nkc, ident_f, psA, psB, dt_note=None):
        for kc in range(nkc):
            ps = psA if kc < 4 else psB
            self.tr(ps[:, (kc % 4) * 128:(kc % 4 + 1) * 128], src[:, kc * 128:(kc + 1) * 128], ident_f[:])
        n0 = min(4, nkc)
        self.copy("act", dstT[:, 0:n0, col0:col0 + 128], psA[:, 0:n0 * 128].rearrange("p (k c) -> p k c", c=128))
        if nkc > 4:
            self.copy("dve", dstT[:, 4:nkc, col0:col0 + 128], psB[:, 0:(nkc - 4) * 128].rearrange("p (k c) -> p k c", c=128))

    def layer0(self):
        nc, S, I, C = self.nc, self.S, self.I, self.C
        ns = self.ns
        cst = self.cst
        mk0 = self.mark()
        lg = self.sb("lg", [128, 3, 8], F32)
        lb = self.sb("lb", [128, 8], F32)
        oml = self.sb("oml", [128, 8], F32)
        for l in range(3):
            self.dma("sp", lg[:, l, :].rearrange("p (d h) -> p d h", d=2),
                     I["hgrn_lb_logits"][l].rearrange("d (h p) -> p d h", p=128), allow_slow_non_contiguous=True)
        self.act(lg[:], lg[:], AF.Exp)
        self.tt("dve", lb[:], lg[:, 0, :], lg[:, 1, :], ALU.add)
        self.tt("dve", lb[:], lb[:], lg[:, 2, :], ALU.add)
        self.v("dve", "reciprocal", lb[:], lb[:], reads=[lb], writes=[lb])
        self.tt("dve", lb[:], lb[:], lg[:, 0, :], ALU.mult)
        self.ts("dve", oml[:], lb[:], -1.0, 1.0, ALU.mult, ALU.add)
        nwb = self.sb("nwb", [128, 128], F32)
        self.dma("sp", nwb[:], I["hgrn_norm_w"][0:1, :].partition_broadcast(128))
        pscale = self.sb("pscale", [128, 4], F32)
        self.dma("sp", pscale[:], I["pool_scale"][0].rearrange("(g p) -> p g", p=128), allow_slow_non_contiguous=True)
        poolw = self.sb("poolw", [128, 4, 128], BF16)
        for g in range(4):
            self.dma("pool", poolw[:, g, :], I["pool_w"][0, g])
        facS = self.sb("facS", [128, 4, 8], F32)
        facE = self.sb("facE", [128, 4, 8], F32)
        self.dma("sp", facS[:], C["facS"])
        self.dma("sp", facE[:], C["facE"])
        wr = self.sb("wr", [128, 8, 32], F32)
        self.dma("sp", wr[:], I["moe_router_w"][0].rearrange("(kc p) e -> p kc e", p=128))
        brr = self.sb("brr", [1, 32], F32)
        self.dma("sp", brr[:], I["moe_router_b"][0:1, :])
        uT = self.sb("uT", [128, 8, SEQ], BF16)
        mixT = self.sb("mixT", [128, 8, SEQ], BF16)
        w_in = I["ev_w_in"][0]
        for b in range(ns):
            mk = self.mark()
            mr = self.sb("mr", [128, 4, D], F32)
            self.modrow(mr[:, 0, :], 0, b, 1)
            self.modrow(mr[:, 1, :], 0, b, 0)
            self.modrow(mr[:, 2, :], 0, 4, 1)
            self.modrow(mr[:, 3, :], 0, 4, 0)
            self.ts("dve", mr[:, 0, :], mr[:, 0, :], 1.0, None, ALU.add)
            self.ts("dve", mr[:, 2, :], mr[:, 2, :], 1.0, None, ALU.add)
            hts = [self.sb(f"ht{i}", [128, D], F32) for i in range(3)]
            us = [self.sb(f"u{i}", [128, D], F32) for i in range(2)]
            junk = self.sb("junk", [128, D], F32)
            srcs = [I["ctx"][b, i * 128:(i + 1) * 128, :] if i < 2 else I["x"][b, (i - 2) * 128:(i - 1) * 128, :] for i in range(NT)]
            self.modulate_pipe(uT, srcs, hts, us, junk, mr)
            if "uT" in self.dbg and b == 0:
                d = self.dbg_dump("uT", [128, 8, SEQ], BF16)
                self.dma("sp", d[:, :, :], uT[:])
            self.release(mk)
            if self.stop_after == "l0_p0":
                raise StopBuild()
            mk = self.mark()
            W2 = SEQ + 64
            q32 = self.sb("q32", [128, W2], F32)
            sg = self.sb("sg", [128, W2], F32)
            kin = self.sb("kin", [128, W2], F32)
            A = self.sb("A", [128, W2], F32)
            oacc = self.sb("oacc", [128, NT, 128], F32)
            v16 = self.sb("v16", [128, NT, 128], BF16)
            whs = [self.sb(f"wh{i}", [128, 8, 640], BF16) for i in range(2)]
            S32 = self.sb("S32", [128, 128], F32)
            S16 = self.sb("S16", [128, 128], BF16)
            RING = 3
            btall = self.sb("btall", [128, NT, 8], F32)
            decs = [self.sb(f"dec{i}", [128, 1], F32) for i in range(RING)]
            Pks = [[self.sb(f"Pk{r_}{i}", [128, 128], F32) for i in range(5)] for r_ in range(RING)]
            kiA = [self.sb(f"kiA{i}", [128, 128], BF16) for i in range(RING)]
            kiB = [self.sb(f"kiB{i}", [128, 128], BF16) for i in range(RING)]
            kiC = [self.sb(f"kiC{i}", [128, 128], BF16) for i in range(RING)]
            qdx = [self.sb(f"qdx{i}", [128, 64], BF16) for i in range(RING)]
            S16s = [self.sb(f"S16{i}", [128, 128], BF16) for i in range(2)]
            qd = [self.sb(f"qd{i}", [128, 128], BF16) for i in range(RING)]
            qdS = [self.sb(f"qdS{i}", [128, 128], BF16) for i in range(RING)]
            ke = [self.sb(f"ke{i}", [128, 128], BF16) for i in range(RING)]
            att16 = [self.sb(f"att{i}", [128, 128], BF16) for i in range(2)]
            ket16 = [self.sb(f"ket{i}", [128, 128], BF16) for i in range(2)]
            rrs = [self.sb(f"rr{i}", [128, 128], F32) for i in range(2)]
            rdo = self.sb("rdo", [128, 2 * NT], F32)
            zero1 = self.sb("zero1", [128, 1], F32)
            self.memset("dve", zero1[:], 0.0)
            groups = [(g * 512, min(512, SEQ - g * 512)) for g in range((SEQ + 511) // 512)]
            for h in range(4):
                wh = whs[h % 2]
                for j, base in enumerate((0, 512, 1024, 1536, 2048)):
                    c0 = base + h * 128
                    self.dma("pool", wh[:, :, j * 128:(j + 1) * 128],
                             w_in[:, c0:c0 + 128].rearrange("(kc p) n -> p kc n", p=128))
                for gi, (g0, gn) in enumerate(groups):
                    ps = self.PS[gi % 2]
                    for kc in range(8):
                        self.mm(ps[:, 0:gn], wh[:, kc, 0:128], uT[:, kc, g0:g0 + gn], start=(kc == 0), stop=(kc == 7))
                    self.act(q32[:, g0:g0 + gn], ps[:, 0:gn], AF.Silu)
                for i in range(NT):
                    ps = self.PS[2 + i % 2]
                    for kc in range(8):
                        self.mm(ps[:, 0:128], uT[:, kc, i * 128:(i + 1) * 128], wh[:, kc, 384:512], start=(kc == 0), stop=(kc == 7))
                    self.copy("dve", v16[:, i, :], ps[:, 0:128])
                for di in range(2):
                    for gi, (g0, gn) in enumerate(groups):
                        ps = self.PS[gi % 2]
                        for kc in range(8):
                            self.mm(ps[:, 0:gn], wh[:, kc, (1 + di) * 128:(2 + di) * 128], uT[:, kc, g0:g0 + gn],
                                    start=(kc == 0), stop=(kc == 7))
                        self.act(sg[:, g0:g0 + gn], ps[:, 0:gn], AF.Sigmoid)
                    lbc = lb[:, di * 4 + h:di * 4 + h + 1]
                    omc = oml[:, di * 4 + h:di * 4 + h + 1]
                    self.ts("dve", sg[:, 0:SEQ], sg[:, 0:SEQ], omc, lbc, ALU.mult, ALU.add)
                    self.ts("pool", kin[:, 0:SEQ], sg[:, 0:SEQ], -1.0, 1.0, ALU.mult, ALU.add)
                    self.act(sg[:, 0:SEQ], sg[:, 0:SEQ], AF.Ln)
                    self.S.op("dve", lambda: nc.vector.tensor_tensor_scan(A[:, 0:SEQ], sg[:, 0:SEQ], sg[:, 0:SEQ], 0.0, ALU.add, ALU.add),
                              reads=[sg], writes=[A])
                    if di == 1:
                        self.stt("dve", sg[:, 0:SEQ], sg[:, 0:SEQ], -2.0, A[:, 0:SEQ], ALU.mult, ALU.add)
                    AA = A if di == 0 else sg
                    order = list(range(NT)) if di == 0 else [1, 0] + list(range(NT - 1, 1, -1))
                    self.memset("dve", S32[:], 0.0)
                    self.memset("pool", S16s[0][:], 0.0)
                    self.memset("pool", S16s[1][:], 0.0)
                    for t_ in kiA + kiB + kiC:
                        self.memset("pool", t_[:], 0.0)
                    mask = cst["mask_f"] if di == 0 else cst["mask_b"]
                    lo, hi = slice(0, 64), slice(64, 128)
                    AAv = AA[:, 0:SEQ].rearrange("p (t c) -> p t c", c=128)
                    Av = A[:, 0:SEQ].rearrange("p (t c) -> p t c", c=128)
                    xcol = 63 if di == 0 else 64
                    for bc, (src_, sgn) in enumerate(((AAv[:, :, 31], -0.5), (AAv[:, :, 31], 0.5), (AAv[:, :, 95], -0.5), (AAv[:, :, 95], 0.5),
                                                     (AAv[:, :, xcol], -0.5), (AAv[:, :, xcol], 0.5))):
                        self.ts("dve", btall[:, :, bc], src_, sgn, None, ALU.mult, reads=[AA], writes=[btall])
                    self.memset("dve", btall[:, 0:1, 6], 0.0)
                    self.ts("dve", btall[:, 1:NT, 6], Av[:, 0:NT - 1, 127], -0.5, None, ALU.mult, reads=[A], writes=[btall])
                    self.ts("dve", btall[:, :, 7], Av[:, :, 127], 0.5, None, ALU.mult, reads=[A], writes=[btall])

                    def stageA(n, i):
                        c0, c1 = i * 128, (i + 1) * 128
                        r = n % RING
                        Pk_ = Pks[r]
                        bt_ = btall[:, i, :]
                        Alo, Ahi, Acol = AA[:, c0:c0 + 64], AA[:, c0 + 64:c1], AA[:, c0:c1]
                        if di == 0:
                            self.act(Pk_[0][:, lo], Alo, AF.Exp, bias=btall[:, i, 0:1], scale=0.5)
                            self.act(Pk_[0][:, hi], Ahi, AF.Exp, bias=btall[:, i, 2:3], scale=0.5)
                            self.act(Pk_[1][:, lo], Alo, AF.Exp, bias=btall[:, i, 1:2], scale=-0.5)
                            self.act(Pk_[1][:, hi], Ahi, AF.Exp, bias=btall[:, i, 3:4], scale=-0.5)
                            self.act(Pk_[2][:], Acol, AF.Exp, bias=btall[:, i, 6:7], scale=0.5)
                            self.act(Pk_[3][:], Acol, AF.Exp, bias=btall[:, i, 7:8], scale=-0.5)
                            self.act(Pk_[4][:, lo], Ahi, AF.Exp, bias=btall[:, i, 4:5], scale=0.5)
                            self.act(Pk_[4][:, hi], Alo, AF.Exp, bias=btall[:, i, 5:6], scale=-0.5)
                            qx_cols, kx_cols = hi, lo
                        else:
                            self.act(Pk_[0][:, lo], Alo, AF.Exp, bias=btall[:, i, 1:2], scale=-0.5)
                            self.act(Pk_[0][:, hi], Ahi, AF.Exp, bias=btall[:, i, 3:4], scale=-0.5)
                            self.act(Pk_[1][:, lo], Alo, AF.Exp, bias=btall[:, i, 0:1], scale=0.5)
                            self.act(Pk_[1][:, hi], Ahi, AF.Exp, bias=btall[:, i, 2:3], scale=0.5)
                            self.act(Pk_[2][:], Acol, AF.Exp, bias=btall[:, i, 7:8], scale=-0.5)
                            self.act(Pk_[3][:], Acol, AF.Exp, bias=btall[:, i, 6:7], scale=0.5)
                            self.act(Pk_[4][:, lo], Alo, AF.Exp, bias=btall[:, i, 5:6], scale=-0.5)
                            self.act(Pk_[4][:, hi], Ahi, AF.Exp, bias=btall[:, i, 4:5], scale=0.5)
                            qx_cols, kx_cols = lo, hi
                        qcs = slice(c0 + qx_cols.start, c0 + qx_cols.stop)
                        kcs = slice(c0 + kx_cols.start, c0 + kx_cols.stop)
                        self.tt("dve", qd[r][:], q32[:, c0:c1], Pk_[0][:], ALU.mult)
                        self.tt("pool", kiA[r][:, lo], kin[:, c0:c0 + 64], Pk_[1][:, lo], ALU.mult)
                        self.tt("pool", kiB[r][:, hi], kin[:, c0 + 64:c1], Pk_[1][:, hi], ALU.mult)
                        self.tt("dve", qdS[r][:], q32[:, c0:c1], Pk_[2][:], ALU.mult)
                        self.tt("pool", ke[r][:], kin[:, c0:c1], Pk_[3][:], ALU.mult)
                        self.tt("dve", qdx[r][:], q32[:, qcs], Pk_[4][:, lo], ALU.mult)
                        self.tt("pool", kiC[r][:, kx_cols], kin[:, kcs], Pk_[4][:, hi], ALU.mult)
                        self.copy("pool", decs[r][:], Pk_[2][:, 127:128] if di == 0 else Pk_[2][:, 0:1])

                    def stageB(n, i):
                        r = n % RING
                        p = n % 2
                        pa, po, pS = self.PS[0 + p], self.PS[2 + p], self.PS[4 + p]
                        pbt = self.PB[p]
                        if di == 0:
                            self.mm(pa[:, 0:64], kiA[r][:], qd[r][:, lo], start=True, stop=True)
                            self.mm(pa[:, 64:128], kiB[r][:], qd[r][:, hi], start=True, stop=False)
                            self.mm(pa[:, 64:128], kiC[r][:], qdx[r][:], start=False, stop=True)
                        else:
                            self.mm(pa[:, 0:64], kiA[r][:], qd[r][:, lo], start=True, stop=False)
                            self.mm(pa[:, 0:64], kiC[r][:], qdx[r][:], start=False, stop=True)
                            self.mm(pa[:, 64:128], kiB[r][:], qd[r][:, hi], start=True, stop=True)
                        self.tr(pbt[:, 0:128], ke[r][:], cst["ident_b"][:])
                        self.tt("dve", att16[p][:], pa[:, 0:128], mask[:], ALU.mult)
                        self.copy("act", ket16[p][:], pbt[:, 0:128])
                        self.mm(pS[:, 0:128], ket16[p][:], v16[:, i, :])
                        self.mm(po[:, 0:128], att16[p][:], v16[:, i, :], start=True, stop=False)
                        self.mm(po[:, 0:128], qdS[r][:], S16s[(n + 1) % 2][:], start=False, stop=True)
                        self.stt("dve", S32[:], S32[:], decs[r][:, 0:1], pS[:, 0:128], ALU.mult, ALU.add)
                        self.copy("act", S16s[n % 2][:], S32[:])
                        if di == 0:
                            self.copy("act", oacc[:, i, :], po[:, 0:128])
                        else:
                            self.tt("dve", oacc[:, i, :], oacc[:, i, :], po[:, 0:128], ALU.add)

                    LOOK = 2
                    for n in range(min(LOOK, NT)):
                        stageA(n, order[n])
                    for n, i in enumerate(order):
                        if n + LOOK < NT:
                            stageA(n + LOOK, order[n + LOOK])
                        stageB(n, i)
                ogall, rrall = sg, kin
                for i in range(NT):
                    ps = self.PS[i % 4]
                    for kc in range(8):
                        self.mm(ps[:, 0:128], uT[:, kc, i * 128:(i + 1) * 128], wh[:, kc, 512:640], start=(kc == 0), stop=(kc == 7))
                    self.act(ogall[:, i * 128:(i + 1) * 128], ps[:, 0:128], AF.Silu, writes=[(ogall, i)])
                    self.act(rrall[:, i * 128:(i + 1) * 128], oacc[:, i, :], AF.Square, accum_out=rdo[:, i:i + 1],
                             writes=[(rrall, i), (rdo, i)])
                self.ts("dve", rdo[:, NT:2 * NT], rdo[:, 0:NT], 1.0 / 128, EPS, ALU.mult, ALU.add, reads=[rdo], writes=[rdo])
                self.act(rdo[:, NT:2 * NT], rdo[:, NT:2 * NT], AF.Sqrt, reads=[rdo], writes=[rdo])
                self.v("dve", "reciprocal", rdo[:, NT:2 * NT], rdo[:, NT:2 * NT], reads=[rdo], writes=[rdo])
                self.tt("dve", rrall[:, 0:SEQ], oacc[:].rearrange("p t c -> p (t c)"), ogall[:, 0:SEQ], ALU.mult,
                        reads=[oacc, ogall], writes=[rrall])
                for i in range(NT):
                    r_ = rrs[i % 2]
                    self.stt("dve", r_[:], rrall[:, i * 128:(i + 1) * 128], rdo[:, NT + i:NT + i + 1], nwb[:], ALU.mult, ALU.mult,
                             reads=[rrall, rdo, nwb], writes=[r_])
                    pt = self.PS[4 + i % 2]
                    self.tr(pt[:, 0:128], r_[:], cst["ident_f"][:])
                    self.copy("act", mixT[:, h, i * 128:(i + 1) * 128], pt[:, 0:128])
            if self.stop_after == "l0_p1":
                raise StopBuild()
            wp = whs[0]
            self.dma("pool", wp[:, :, 0:512], w_in[:, 2560:3072].rearrange("(kc p) n -> p kc n", p=128))
            OFFC, OFFL = 16, 16 + CTX + 16
            WB = OFFL + LAT + 16
            d16 = self.sb("d16", [128, SEQ], BF16)
            for g in range(4):
                w = 2 << g
                half = w // 2
                PBf, T1, T2 = q32, kin, A
                self.memset("pool", PBf[:, 0:WB], 0.0)
                for gi, (g0, gn) in enumerate(groups):
                    ps = self.PS[gi % 2]
                    for kc in range(8):
                        self.mm(ps[:, 0:gn], wp[:, kc, g * 128:(g + 1) * 128], uT[:, kc, g0:g0 + gn], start=(kc == 0), stop=(kc == 7))
                    a0, a1 = g0, g0 + gn
                    if a0 < CTX:
                        n_c = min(a1, CTX) - a0
                        self.copy("act", PBf[:, OFFC + a0:OFFC + a0 + n_c], ps[:, 0:n_c])
                        if a1 > CTX:
                            self.copy("act", PBf[:, OFFL:OFFL + (a1 - CTX)], ps[:, n_c:gn])
                    else:
                        self.copy("act", PBf[:, OFFL + a0 - CTX:OFFL + a1 - CTX], ps[:, 0:gn])
                cur = PBf
                step = 1
                tmp = [T1, T2]
                ti = 0
                width = WB
                while step < w:
                    nxt = tmp[ti % 2]
                    ti += 1
                    width2 = width - step
                    self.tt("dve", nxt[:, 0:width2], cur[:, 0:width2], cur[:, step:step + width2], ALU.add)
                    cur, width, step = nxt, width2, step * 2
                dst = tmp[ti % 2]
                for off, L, tcol in ((OFFC, CTX, 0), (OFFL, LAT, CTX)):
                    sw = cur[:, off - half:off - half + L]
                    self.tt("pool", sw[:, 0:8], sw[:, 0:8], facS[:, g, :], ALU.mult, reads=[cur, facS], writes=[cur])
                    self.tt("pool", sw[:, L - 8:L], sw[:, L - 8:L], facE[:, g, :], ALU.mult, reads=[cur, facE], writes=[cur])
                    self.stt("dve", d16[:, tcol:tcol + L], sw, 1.0 / w, PBf[:, off:off + L], ALU.mult, ALU.subtract,
                             reads=[cur, PBf], writes=[d16])
                for gi, (g0, gn) in enumerate(groups):
                    ps = self.PS[2 + gi % 2]
                    self.mm(ps[:, 0:gn], poolw[:, g, :], d16[:, g0:g0 + gn])
                    self.ts("dve", mixT[:, 4 + g, g0:g0 + gn], ps[:, 0:gn], pscale[:, g:g + 1], None, ALU.mult)
            if "mixT" in self.dbg and b == 0:
                d = self.dbg_dump("mixT", [128, 8, SEQ], BF16)
                self.dma("sp", d[:, :, :], mixT[:])
            self.release(mk)
            if self.stop_after == "l0_p3":
                raise StopBuild()
            self.post_mixer(0, b, mixT, NT, wr, brr)
        self.release(mk0)

    def rms_rows(self, out, src, n, gain_b, sq_junk, c0):
        sm = self.small
        self.act(sq_junk, src, AF.Square, accum_out=sm[:, c0:c0 + 1])
        self.rstd_from_ss(sm[:, c0 + 1:c0 + 2], sm[:, c0:c0 + 1], n)
        self.stt("dve", out, src, sm[:, c0 + 1:c0 + 2], gain_b, ALU.mult, ALU.mult)

    def head_norm(self, xf, gain_b, tmp, hs, rs):
        self.tt("dve", tmp[:], xf[:], xf[:], ALU.mult)
        self.v("dve", "reduce_sum", hs[:], tmp[:], AX.X, reads=[tmp], writes=[hs])
        self.ts("dve", rs[:], hs[:], 1.0 / 96, EPS, ALU.mult, ALU.add)
        self.act(rs[:], rs[:], AF.Sqrt)
        self.v("dve", "reciprocal", rs[:], rs[:], reads=[rs], writes=[rs])
        self.tt("dve", xf[:], xf[:], rs[:].unsqueeze(2).to_broadcast([128, 8, 96]), ALU.mult, reads=[xf, rs], writes=[xf])
        self.tt("dve", xf[:], xf[:], gain_b[:].unsqueeze(1).to_broadcast([128, 8, 96]), ALU.mult, reads=[xf, gain_b], writes=[xf])

    def rope_all(self, xo, xf, cosr, sinr, tt_):
        s5 = xf[:, :, 64:96].rearrange("p h (a t i) -> p h a t i", a=2, t=2)
        d5 = xo[:, :, 64:96].rearrange("p h (a t i) -> p h a t i", a=2, t=2)
        cb = cosr.rearrange("p (a i) -> p a i", a=2).unsqueeze(1).to_broadcast([128, 8, 2, 8])
        sb_ = sinr.rearrange("p (a i) -> p a i", a=2).unsqueeze(1).to_broadcast([128, 8, 2, 8])
        t = [x[:].rearrange("p h (a i) -> p h a i", a=2) for x in tt_]
        x1, x2 = s5[:, :, :, 0, :], s5[:, :, :, 1, :]
        self.tt("dve", t[0], x1, cb, ALU.mult, reads=[xf, cosr], writes=[tt_[0]])
        self.tt("dve", t[1], x2, sb_, ALU.mult, reads=[xf, sinr], writes=[tt_[1]])
        self.tt("dve", t[2], x2, cb, ALU.mult, reads=[xf, cosr], writes=[tt_[2]])
        self.tt("dve", t[3], x1, sb_, ALU.mult, reads=[xf, sinr], writes=[tt_[3]])
        self.tt("dve", d5[:, :, :, 0, :], t[0], t[1], ALU.subtract, reads=[tt_[0], tt_[1]], writes=[xo])
        self.tt("dve", d5[:, :, :, 1, :], t[2], t[3], ALU.add, reads=[tt_[2], tt_[3]], writes=[xo])

    def rope(self, dst, src, cosr, sinr, t1, t2, eng):
        s4 = src.rearrange("p (a h i) -> p a h i", a=2, h=2)
        d4 = dst.rearrange("p (a h i) -> p a h i", a=2, h=2)
        c3 = cosr.rearrange("p (a i) -> p a i", a=2)
        s3 = sinr.rearrange("p (a i) -> p a i", a=2)
        a3 = t1.rearrange("p (a i) -> p a i", a=2)
        b3 = t2.rearrange("p (a i) -> p a i", a=2)
        x1, x2 = s4[:, :, 0, :], s4[:, :, 1, :]
        self.tt(eng, a3, x1, c3, ALU.mult, reads=[src, cosr], writes=[t1])
        self.tt(eng, b3, x2, s3, ALU.mult, reads=[src, sinr], writes=[t2])
        self.tt(eng, d4[:, :, 0, :], a3, b3, ALU.subtract, reads=[t1, t2], writes=[dst])
        self.tt(eng, a3, x2, c3, ALU.mult, reads=[src, cosr], writes=[t1])
        self.tt(eng, b3, x1, s3, ALU.mult, reads=[src, sinr], writes=[t2])
        self.tt(eng, d4[:, :, 1, :], a3, b3, ALU.add, reads=[t1, t2], writes=[dst])

    def layer1(self):
        nc, S, I, C = self.nc, self.S, self.I, self.C
        ns, cst = self.ns, self.cst
        mk0 = self.mark()
        w_in = I["od_w_in"][0]
        NL = LAT // 128
        wr = self.sb("wr1", [128, 8, 32], F32)
        self.dma("sp", wr[:], I["moe_router_w"][1].rearrange("(kc p) e -> p kc e", p=128))
        brr = self.sb("brr1", [1, 32], F32)
        self.dma("sp", brr[:], I["moe_router_b"][1:2, :])
        rcos = self.sb("rcos", [128, 16, 16], F32)
        rsin = self.sb("rsin", [128, 16, 16], F32)
        self.dma("sp", rcos[:], C["rope_cos"])
        self.dma("sp", rsin[:], C["rope_sin"])
        dww = self.sb("dww", [128, 4, 31], F32)
        dwr = self.sb("dwr", [32, 512], F32)
        self.dma("sp", dwr[0:31, :], I["conv_dw_w"][0])
        for cc in range(4):
            self.tr(self.PS[0][:, cc * 32:cc * 32 + 31], dwr[0:31, cc * 128:(cc + 1) * 128], self.cst["ident_f"][0:31, 0:31])
            self.copy("dve", dww[:, cc, :], self.PS[0][:, cc * 32:cc * 32 + 31])
        cpar = self.sb("cpar", [128, 3, 4], F32)
        for j, nm in enumerate(("conv_dw_b", "conv_ln_w", "conv_ln_b")):
            self.dma("sp", cpar[:, j, :], I[nm][0].rearrange("(c p) -> p c", p=128), allow_slow_non_contiguous=True)
        qan = self.sb("qan", [128, 384], F32)
        kvan = self.sb("kvan", [128, 256], F32)
        qnb = self.sb("qnb", [128, 96], F32)
        knb = self.sb("knb", [128, 96], F32)
        self.dma("sp", qan[:], I["mla_q_a_norm"][0:1, :].partition_broadcast(128))
        self.dma("sp", kvan[:], I["mla_kv_a_norm"][0:1, :].partition_broadcast(128))
        self.dma("sp", qnb[:], I["mla_q_norm"][0:1, :].partition_broadcast(128))
        self.dma("sp", knb[:], I["mla_k_norm"][0:1, :].partition_broadcast(128))
        wq = self.sb("wq", [128, 8, 384], BF16)
        wkv = self.sb("wkv", [128, 8, 288], BF16)
        wuq = self.sb("wuq", [128, 3, 768], BF16)
        wukv = self.sb("wukv", [128, 2, 1024], BF16)
        self.dma("pool", wq[:], w_in[:, 1024:1408].rearrange("(kc p) n -> p kc n", p=128))
        self.dma("pool", wkv[:], w_in[:, 1408:1696].rearrange("(kc p) n -> p kc n", p=128))
        self.dma("pool", wuq[:], I["mla_w_uq"][0].rearrange("(kc p) n -> p kc n", p=128))
        self.dma("pool", wukv[:], I["mla_w_ukv"][0].rearrange("(kc p) n -> p kc n", p=128))
        mixA = self.sb("mixA", [128, 4, LAT], BF16)
        sm = self.small
        SC = 96 ** -0.5
        for b in range(ns):
            mkb = self.mark()

            def make_uT(per_tile=None):
                uT = self.sb("uT1", [128, 8, SEQ], BF16) if per_tile is None else None
                mk = self.mark()
                uTts = [self.sb(f"uTt{i}", [128, 8, 128], BF16) for i in range(2)] if per_tile is not None else None
                mr = self.sb("mr", [128, 4, D], F32)
                self.modrow(mr[:, 0, :], 1, b, 1)
                self.modrow(mr[:, 1, :], 1, b, 0)
                self.modrow(mr[:, 2, :], 1, 4, 1)
                self.modrow(mr[:, 3, :], 1, 4, 0)
                self.ts("dve", mr[:, 0, :], mr[:, 0, :], 1.0, None, ALU.add)
                self.ts("dve", mr[:, 2, :], mr[:, 2, :], 1.0, None, ALU.add)
                if per_tile is None:
                    hts = [self.sb(f"ht{i}", [128, D], F32) for i in range(3)]
                    us = [self.sb(f"u{i}", [128, D], F32) for i in range(2)]
                    junk_ = self.sb("junkp", [128, D], F32)
                    srcs = [self.H2[b * SEQ + i * 128:b * SEQ + (i + 1) * 128, :] for i in range(NT)]
                    self.modulate_pipe(uT, srcs, hts, us, junk_, mr, read_keys=[self.H2])
                    self.release(mk)
                    return uT
                hts = [self.sb(f"ht{i}", [128, D], F32) for i in range(2)]
                us = [self.sb(f"u{i}", [128, D], F32) for i in range(1 if per_tile is not None else 2)]
                for i in range(NT):
                    ht, u = hts[i % 2], us[i % len(us)]
                    self.dma("sp", ht[:], self.H2[b * SEQ + i * 128:b * SEQ + (i + 1) * 128, :], reads=[self.H2])
                    o = 2 if i < 2 else 0
                    self.modulate_tile(u[:], ht[:], mr[:, o, :], mr[:, o + 1, :], u[:], sm[:, 0:1], sm[:, 1:2])
                    if per_tile is None:
                        self.transpose_tile_to(uT, i * 128, u, 8, cst["ident_f"], self.PS[0], self.PS[1])
                    else:
                        self.transpose_tile_to(uTts[i % 2], 0, u, 8, cst["ident_f"], self.PS[0], self.PS[1])
                        per_tile(i, uTts[i % 2])
                self.release(mk)
                return uT

            mkA = self.mark()
            uT = make_uT()
            mk = self.mark()
            wc = self.sb("wc", [128, 8, 1024], BF16)
            self.dma("pool", wc[:], w_in[:, 0:1024].rearrange("(kc p) n -> p kc n", p=128))
            hb = self.sb("hb", [128, LAT + 32], F32)
            cv = [self.sb(f"cv{i}", [128, LAT], F32) for i in range(4)]
            sgt = [self.sb(f"sgt{i}", [128, 512], F32) for i in range(2)]
            self.memset("pool", hb[:], 0.0)
            for cc in range(4):
                for tg in range(4):
                    pv, pg = self.PS[(tg % 2) * 2], self.PS[(tg % 2) * 2 + 1]
                    cols = slice(CTX + tg * 512, CTX + (tg + 1) * 512)
                    for kc in range(8):
                        self.mm(pv[:], wc[:, kc, cc * 128:(cc + 1) * 128], uT[:, kc, cols], start=(kc == 0), stop=(kc == 7))
                    for kc in range(8):
                        self.mm(pg[:], wc[:, kc, 512 + cc * 128:512 + (cc + 1) * 128], uT[:, kc, cols], start=(kc == 0), stop=(kc == 7))
                    st = sgt[tg % 2]
                    self.act(st[:], pg[:], AF.Sigmoid)
                    self.tt("dve", hb[:, 15 + tg * 512:15 + (tg + 1) * 512], pv[:], st[:], ALU.mult)
                NH = 4
                HW_ = LAT // NH
                for j in range(31):
                    for hf in range(NH):
                        o0 = hf * HW_
                        acc = cv[cc][:, o0:o0 + HW_]
                        if j == 0:
                            self.ts("dve", acc, hb[:, o0:o0 + HW_], dww[:, cc, 0:1], cpar[:, 0, cc:cc + 1], ALU.mult, ALU.add,
                                    reads=[hb, dww, cpar], writes=[(cv[cc], hf)])
                        else:
                            self.stt("dve", acc, hb[:, o0 + j:o0 + j + HW_], dww[:, cc, j:j + 1], acc, ALU.mult, ALU.add,
                                     reads=[hb, dww, (cv[cc], hf)], writes=[(cv[cc], hf)])
            mean = self.sb("lnm", [128, 512], F32)
            rstd = self.sb("lnr", [128, 512], F32)
            sq = self.sb("lnsq", [128, 512], F32)
            xn = [self.sb(f"lnx{i}", [128, 512], F32) for i in range(2)]
            for tg in range(4):
                cols = slice(tg * 512, (tg + 1) * 512)
                ps_s, ps_q = self.PS[4], self.PS[5]
                for cc in range(4):
                    self.mm(ps_s[:], cst["ones_f"][:], cv[cc][:, cols], start=(cc == 0), stop=(cc == 3))
                for cc in range(4):
                    self.act(sq[:], cv[cc][:, cols], AF.Square)
                    self.mm(ps_q[:], cst["ones_f"][:], sq[:], start=(cc == 0), stop=(cc == 3))
                self.ts("dve", mean[:], ps_s[:], 1.0 / 512, None, ALU.mult)
                self.tt("dve", rstd[:], mean[:], mean[:], ALU.mult)
                self.stt("dve", rstd[:], ps_q[:], 1.0 / 512, rstd[:], ALU.mult, ALU.subtract)
                self.ts("dve", rstd[:], rstd[:], EPS, None, ALU.add)
                self.act(rstd[:], rstd[:], AF.Sqrt)
                self.v("dve", "reciprocal", rstd[:], rstd[:], reads=[rstd], writes=[rstd])
                for cc in range(4):
                    x = xn[cc % 2]
                    self.tt("pool", x[:], cv[cc][:, cols], mean[:], ALU.subtract)
                    self.tt("dve", x[:], x[:], rstd[:], ALU.mult)
                    self.act(mixA[:, cc, cols], x[:], AF.Silu, bias=cpar[:, 2, cc:cc + 1], scale=cpar[:, 1, cc:cc + 1])
            self.release(mkA)
            if self.stop_after == "l1_conv":
                raise StopBuild()
            mixB = self.sb("mixB", [128, 4, LAT], BF16)
            mkq = self.mark()
            qT = self.sb("qT", [128, 8, LAT], BF16)
            kT = self.sb("kT", [128, 8, SEQ], BF16)
            vaug = self.sb("vaug", [128, NT, 8, 68], BF16)
            mk = self.mark()
            self.memset("pool", vaug[:], 1.0)
            cn = self.sb("cn", [128, 384], F32)
            cnT = self.sb("cnT", [128, 3, 128], BF16)
            xf = self.sb("xf", [128, 8, 96], F32)
            xo = self.sb("xo", [128, 8, 96], F32)
            tmp = self.sb("hn_tmp", [128, 8, 96], F32)
            hs = self.sb("hn_hs", [128, 8], F32)
            rs = self.sb("hn_rs", [128, 8], F32)
            rtt = [self.sb(f"rtt{i}", [128, 8, 16], F32) for i in range(4)]
            krr = self.sb("krr", [128, 32], F32)
            krg = self.sb("krg", [128, 32], F32)
            junk2 = self.sb("junk2", [128, 384], F32)
            def proj_tile(i, uTt):
                import os as _os
                _stg = float(_os.environ.get("KSTG", "9"))
                if _stg <= 0:
                    return
                lat_i = i - 2
                tok = slice(i * 128, (i + 1) * 128)
                pk = self.PS[2]
                for kc in range(8):
                    self.mm(pk[:, 0:288], uTt[:, kc, :], wkv[:, kc, :], start=(kc == 0), stop=(kc == 7))
                if _stg <= 0.1:
                    return
                self.rms_rows(cn[:, 0:256], pk[:, 0:256], 256, kvan[:], junk2[:, 0:256], 10)
                if _stg <= 0.2:
                    return
                self.tt("dve", krg[:], pk[:, 256:288], knb[:, 64:96], ALU.mult)
                if _stg <= 0.3:
                    return
                pt = self.PS[3]
                for kc in range(2):
                    self.tr(pt[:, kc * 128:(kc + 1) * 128], cn[:, kc * 128:(kc + 1) * 128], cst["ident_f"][:])
                self.copy("act", cnT[:, 0:2, :], pt[:, 0:256].rearrange("p (k c) -> p k c", c=128))
                if _stg <= 0.4:
                    return
                pkv = [self.PS[4], self.PS[5]]
                for half in range(2):
                    for kc in range(2):
                        self.mm(pkv[half][:], cnT[:, kc, :], wukv[:, kc, half * 512:(half + 1) * 512], start=(kc == 0), stop=(kc == 1))
                    v4 = pkv[half][:, :].rearrange("p (h d) -> p h d", d=128)
                    if _stg <= 0.5:
                        continue
                    if _stg != 0.56:
                        self.copy("act", xf[:, half * 4:(half + 1) * 4, 0:64], v4[:, :, 0:64])
                    if _stg != 0.55:
                        self.copy("act" if _stg == 0.57 else "dve", vaug[:, i, half * 4:(half + 1) * 4, 0:64], v4[:, :, 64:128])
                if _stg <= 0.6:
                    return
                self.copy("dve", xf[:, :, 64:96], pk[:, 256:288].unsqueeze(1).to_broadcast([128, 8, 32]), reads=[pk], writes=[xf])
                if _stg <= 1:
                    return
                self.head_norm(xf, knb, tmp, hs, rs)
                if _stg <= 2:
                    return
                if lat_i >= 0:
                    self.rope_all(xo, xf, rcos[:, lat_i, :], rsin[:, lat_i, :], rtt)
                    self.copy("act", xo[:, :, 0:64], xf[:, :, 0:64])
                    src = xo
                else:
                    src = xf
                pa, pb_ = self.PS[4], self.PS[5]
                for h in range(8):
                    pp = pa if h < 4 else pb_
                    self.tr(pp[0:96, (h % 4) * 128:(h % 4 + 1) * 128], src[:, h, :], cst["ident_f"][:])
                self.copy("act", kT[0:96, 0:4, tok], pa[0:96, :].rearrange("p (k c) -> p k c", c=128))
                self.copy("dve", kT[0:96, 4:8, tok], pb_[0:96, :].rearrange("p (k c) -> p k c", c=128))
                if lat_i < 0 or _stg <= 3:
                    return
                ltok = slice(lat_i * 128, (lat_i + 1) * 128)
                pq = self.PS[2]
                for kc in range(8):
                    self.mm(pq[:, 0:384], uTt[:, kc, :], wq[:, kc, :], start=(kc == 0), stop=(kc == 7))
                if _stg <= 3.1:
                    return
                self.rms_rows(cn[:, 0:384], pq[:, 0:384], 384, qan[:], junk2[:, 0:384], 12)
                if _stg <= 3.2:
                    return
                pt = self.PS[3]
                for kc in range(3):
                    self.tr(pt[:, kc * 128:(kc + 1) * 128], cn[:, kc * 128:(kc + 1) * 128], cst["ident_f"][:])
                self.copy("act", cnT[:, 0:3, :], pt[:, 0:384].rearrange("p (k c) -> p k c", c=128))
                if _stg <= 3.3:
                    return
                pq1, pq2 = self.PS[4], self.PS[5]
                for kc in range(3):
                    self.mm(pq1[:, 0:384], cnT[:, kc, :], wuq[:, kc, 0:384], start=(kc == 0), stop=(kc == 2))
                for kc in range(3):
                    self.mm(pq2[:, 0:384], cnT[:, kc, :], wuq[:, kc, 384:768], start=(kc == 0), stop=(kc == 2))
                if _stg <= 3.4:
                    return
                self.copy("act", xf[:, 0:4, :], pq1[:, 0:384].rearrange("p (h d) -> p h d", d=96))
                self.copy("dve", xf[:, 4:8, :], pq2[:, 0:384].rearrange("p (h d) -> p h d", d=96))
                if _stg <= 3.5:
                    return
                self.head_norm(xf, qnb, tmp, hs, rs)
                if _stg <= 3.6:
                    return
                self.rope_all(xo, xf, rcos[:, lat_i, :], rsin[:, lat_i, :], rtt)
                if _stg <= 3.7:
                    return
                self.copy("act", xo[:, :, 0:64], xf[:, :, 0:64])
                for h in range(8):
                    pp = pa if h < 4 else pb_
                    self.tr(pp[0:96, (h % 4) * 128:(h % 4 + 1) * 128], xo[:, h, :], cst["ident_f"][:])
                if _stg <= 3.8:
                    return
                self.copy("act", qT[0:96, 0:4, ltok], pa[0:96, :].rearrange("p (k c) -> p k c", c=128))
                self.copy("dve", qT[0:96, 4:8, ltok], pb_[0:96, :].rearrange("p (k c) -> p k c", c=128))

            make_uT(per_tile=proj_tile)
            self.release(mk)
            if self.stop_after == "l1_proj":
                raise StopBuild()
            mk = self.mark()
            attn = self.sb("attn", [128, NL, 512], BF16)
            PT = [self.sb(f"PT{i}", [128, 512], BF16) for i in range(4)]
            rden = self.sb("rden", [128, 4], F32)
            n = 0
            for h in range(8):
                for qg in range(4):
                    qcols = slice(qg * 512, (qg + 1) * 512)
                    po = self.PS[4 + (h * 4 + qg) % 2]
                    def s_mm(kt_, n_):
                        ps_ = self.PS[n_ % 4]
                        self.mm(ps_[:], kT[0:96, h, kt_ * 128:(kt_ + 1) * 128], qT[0:96, h, qcols])
                        self.act(PT[n_ % 4][:], ps_[:], AF.Exp, scale=SC)

                    s_mm(0, n)
                    s_mm(1, n + 1)
                    for kt in range(NT):
                        if kt + 2 < NT:
                            s_mm(kt + 2, n + 2)
                        p_ = PT[n % 4]
                        for qt in range(4):
                            self.mm(po[:, qt * 68:(qt + 1) * 68], p_[:, qt * 128:(qt + 1) * 128], vaug[:, kt, h, :],
                                    start=(kt == 0), stop=(kt == NT - 1))
                        n += 1
                    for qt in range(4):
                        self.v("dve", "reciprocal", rden[:, qt:qt + 1], po[:, qt * 68 + 64:qt * 68 + 65], reads=[po], writes=[rden])
                        self.ts("dve", attn[:, qg * 4 + qt, h * 64:(h + 1) * 64], po[:, qt * 68:qt * 68 + 64], rden[:, qt:qt + 1], None, ALU.mult,
                                reads=[po, rden], writes=[(attn, qg * 4 + qt)])
            for i in range(NL):
                pb = self.PB[i % 2]
                for c in range(4):
                    self.tr(pb[:, c * 128:(c + 1) * 128], attn[:, i, c * 128:(c + 1) * 128], cst["ident_b"][:])
                self.copy("act" if i % 2 == 0 else "dve", mixB[:, :, i * 128:(i + 1) * 128], pb[:, 0:512].rearrange("p (k c) -> p k c", c=128))
            self.release(mkq)
            if self.stop_after == "l1_attn":
                raise StopBuild()
            self.post_mixer(1, b, [mixA, mixB], NL, wr, brr)
            self.release(mkb)
        self.release(mk0)

    def post_mixer(self, layer, b, mixT, ntile, wr, brr):
        nc, S, I, C = self.nc, self.S, self.I, self.C
        cst = self.cst
        mk = self.mark()
        w_out = I["ev_w_out"][0] if layer == 0 else I["od_w_out"][0]
        wo = self.sb("wo", [128, 8, D], BF16)
        self.dma("pool", wo[:], w_out.rearrange("(kc p) n -> p kc n", p=128))
        mr = self.sb("mr2", [128, 6, D], F32)
        self.modrow(mr[:, 0, :], layer, b, 2)
        self.modrow(mr[:, 1, :], layer, b, 4)
        self.modrow(mr[:, 2, :], layer, b, 3)
        self.ts("dve", mr[:, 1, :], mr[:, 1, :], 1.0, None, ALU.add)
        if layer == 0:
            self.modrow(mr[:, 3, :], layer, 4, 2)
            self.modrow(mr[:, 4, :], layer, 4, 4)
            self.modrow(mr[:, 5, :], layer, 4, 3)
            self.ts("dve", mr[:, 4, :], mr[:, 4, :], 1.0, None, ALU.add)
        hts = [self.sb(f"pht{i}", [128, D], F32) for i in range(2)]
        h1s = [self.sb(f"ph1{i}", [128, D], F32) for i in range(2)]
        ms = [self.sb(f"pm{i}", [128, D], F32) for i in range(2)]
        m16 = [self.sb(f"pm16{i}", [128, D], BF16) for i in range(2)]
        mT = self.sb("pmT", [128, 8, 128], F32)
        junk = self.sb("pjunk", [128, D], F32)
        smf = self.sb("smf", [128, 8], F32)

        def front(i):
            if layer == 0:
                isctx = i < 2
                src = I["ctx"][b, i * 128:(i + 1) * 128, :] if isctx else I["x"][b, (i - 2) * 128:(i - 1) * 128, :]
                grow = b * SEQ + i * 128
            else:
                isctx = False
                grow = b * LAT + i * 128
                src = self.H2[b * SEQ + CTX + i * 128:b * SEQ + CTX + (i + 1) * 128, :]
            hdst = self.H1
            o = 3 if isctx else 0
            ht, h1, m, mb = hts[i % 2], h1s[i % 2], ms[i % 2], m16[i % 2]
            self.dma("sp", ht[:], src)
            p0, p1 = self.PS[0], self.PS[1]
            for half, ps in enumerate((p0, p1)):
                for kc in range(8):
                    mx = mixT[kc // 4] if isinstance(mixT, (list, tuple)) else mixT
                    kcc = kc % 4 if isinstance(mixT, (list, tuple)) else kc
                    self.mm(ps[:], mx[:, kcc, i * 128:(i + 1) * 128], wo[:, kc, half * 512:(half + 1) * 512],
                            start=(kc == 0), stop=(kc == 7))
                self.tt("dve", h1[:, half * 512:(half + 1) * 512], ps[:], mr[:, o, half * 512:(half + 1) * 512], ALU.mult)
            self.tt("pool", h1[:], h1[:], ht[:], ALU.add)
            self.dma("sp", hdst[grow:grow + 128, :], h1[:], writes=[(hdst, grow)])
            self.modulate_tile(m[:], h1[:], mr[:, o + 1, :], mr[:, o + 2, :], junk[:], smf[:, 0:1], smf[:, 1:2])
            self.copy("act", mb[:], m[:])
            self.dma("sp", self.M[grow:grow + 128, :], mb[:], writes=[(self.M, grow)])
            self.transpose_tile_to(mT, 0, m, 8, cst["ident_f"], self.PS[2], self.PS[3])
            pl = self.PS[4 + i % 2]
            for kc in range(8):
                self.mm(pl[:, 0:32], mT[:, kc, :], wr[:, kc, :], start=(kc == 0), stop=False)
            self.mm(pl[:, 0:32], cst["ones_f"][0:1, :], brr[0:1, :], start=False, stop=True)
            return grow // 128, pl

        pend = front(0)
        for i in range(ntile):
            cur = pend
            if i + 1 < ntile:
                pend = front(i + 1)
            self.route_tile(layer, cur[0], cur[1])
        self.release(mk)

    def route_init(self, layer):
        ns = self.ns
        T = self.T0 if layer == 0 else self.T1
        self.rT = T
        CAP = self.CAP = CAPS[layer]
        NSLOT = self.NSLOT = NE * CAP
        ntt = T // 128
        self.cntb = self.sb(f"cntb{layer}", [128, NE], F32)
        self.smr = self.sb(f"smr{layer}", [128, 16], F32)
        self.DEST = self.sb(f"DEST{layer}", [128, ntt, 4], I32)
        self.GATE = self.sb(f"GATE{layer}", [128, ntt, 4], F32)
        self.rt = {n: self.sb(f"rt_{n}{layer}", sh, dt) for n, sh, dt in (
            ("L", [128, NE], F32), ("t8", [128, 8], F32), ("e4", [128, 4], F32), ("mask", [128, NE], F32),
            ("m16", [128, NE], BF16), ("rf", [128, NE], F32), ("rfs", [128, NE], F32), ("tmp", [128, NE], F32),
            ("rk", [128, 4], F32), ("dk", [128, 4], F32), ("ok", [128, 4], F32), ("tok", [128, 2], F32),
            ("toki", [128, 2], I32), ("trash", [128, 1], F32), ("fill", [128, (NSLOT + 128) // 64], I32),
            ("z16", [128, D], BF16))}
        rt = self.rt
        self.memset("dve", self.cntb[:], 0.0)
        self.ts("dve", rt["trash"][:], self.cst["pidx"][:], float(NSLOT), None, ALU.add)
        self.memset("dve", rt["fill"][:], int(T))
        self.dma("sp", self.SLOT[0:NSLOT + 128, :].rearrange("(p j) two -> p (j two)", p=128), rt["fill"][:])
        self.memset("pool", rt["z16"][:], 0.0)
        self.dma("sp", self.M[T:T + 128, :], rt["z16"][:], writes=[(self.M, "trash")])
        self.dma("sp", self.YB[NSLOT:NSLOT + 128, :], rt["z16"][:], writes=[(self.YB, "trash")])

    def route_tile(self, layer, gt, pl):
        nc, rt, cst = self.nc, self.rt, self.cst
        CAP, NSLOT = self.CAP, self.NSLOT
        L, t8 = rt["L"], rt["t8"]
        self.copy("dve", L[:], pl[:, 0:NE])
        self.v("dve", "max", t8[:], L[:], reads=[L], writes=[t8])
        sm = self.smr
        self.ts("dve", sm[:, 8:9], t8[:, 0:1], -1.0, None, ALU.mult)
        self.act(rt["e4"][:], t8[:, 0:4], AF.Exp, bias=sm[:, 8:9], scale=1.0)
        self.v("dve", "reduce_sum", sm[:, 9:10], rt["e4"][:], AX.X, reads=[rt["e4"]], writes=[sm])
        self.v("dve", "reciprocal", sm[:, 9:10], sm[:, 9:10], reads=[sm], writes=[sm])
        self.ts("dve", rt["e4"][:], rt["e4"][:], sm[:, 9:10], None, ALU.mult)
        self.ts("dve", rt["mask"][:], L[:], t8[:, 3:4], None, ALU.is_ge)
        self.copy("dve", rt["m16"][:], rt["mask"][:])
        pr = pl
        self.mm(pr[:, 128:128 + NE], cst["tri_b"][:], rt["m16"][:])
        self.mm(pr[:, 64:64 + NE], cst["ones_b"][:], rt["m16"][:])
        self.tt("dve", rt["rf"][:], pr[:, 128:128 + NE], self.cntb[:], ALU.add)
        self.tt("dve", self.cntb[:], self.cntb[:], pr[:, 64:64 + NE], ALU.add)
        self.tt("dve", rt["rfs"][:], rt["rf"][:], cst[f"slotbase{layer}"][:], ALU.add)
        for k in range(4):
            self.stt("dve", rt["tmp"][:], L[:], t8[:, k:k + 1], rt["rf"][:], ALU.is_equal, ALU.mult)
            self.v("dve", "reduce_sum", rt["rk"][:, k:k + 1], rt["tmp"][:], AX.X, reads=[rt["tmp"]], writes=[rt["rk"]])
            self.stt("dve", rt["tmp"][:], L[:], t8[:, k:k + 1], rt["rfs"][:], ALU.is_equal, ALU.mult)
            self.v("dve", "reduce_sum", rt["dk"][:, k:k + 1], rt["tmp"][:], AX.X, reads=[rt["tmp"]], writes=[rt["dk"]])
        self.ts("dve", rt["ok"][:], rt["rk"][:], float(CAP), None, ALU.is_lt)
        self.stt("dve", rt["dk"][:], rt["dk"][:], rt["trash"][:, 0:1], rt["ok"][:], ALU.subtract, ALU.mult)
        self.ts("dve", rt["dk"][:], rt["dk"][:], rt["trash"][:, 0:1], None, ALU.add)
        self.tt("dve", self.GATE[:, gt, :], rt["e4"][:], rt["ok"][:], ALU.mult, writes=[(self.GATE, gt)])
        self.copy("dve", self.DEST[:, gt, :], rt["dk"][:], writes=[(self.DEST, gt)])
        self.ts("dve", rt["tok"][:, 0:1], cst["pidx"][:], float(gt * 128), None, ALU.add)
        self.copy("dve", rt["tok"][:, 1:2], rt["tok"][:, 0:1])
        self.copy("dve", rt["toki"][:], rt["tok"][:])
        for k in range(4):
            self.dma("pool", self.SLOT[:, :], rt["toki"][:, :], reads=[rt["toki"], (self.DEST, gt)], writes=[(self.SLOT, (gt, k))],
                     indirect=dict(out_offset=bass.IndirectOffsetOnAxis(ap=self.DEST[:, gt, k:k + 1], axis=0), in_offset=None))

    def experts(self, layer):
        nc, I, cst = self.nc, self.I, self.cst
        CAP, NSLOT = self.CAP, self.NSLOT
        mk = self.mark()
        w1s = [self.sb(f"w1s{i}", [128, 8, 2 * D], BF16) for i in range(2)]
        w2s = [self.sb(f"w2s{i}", [128, 8, D], BF16) for i in range(2)]
        b1s = [self.sb(f"b1s{i}", [128, 16], F32) for i in range(2)]
        b2s = [self.sb(f"b2s{i}", [128, D], F32) for i in range(2)]
        idx = [[self.sb(f"idx{s_}{i}", [128, 2], I32) for i in range(4)] for s_ in range(2)]
        xg = [[self.sb(f"xg{s_}{i}", [128, D], BF16) for i in range(4)] for s_ in range(2)]
        xT = [self.sb(f"xT{i}", [128, 8, 512], BF16) for i in range(2)]
        aT = [self.sb(f"aT{i}", [128, 8, 512], BF16) for i in range(2)]
        glc = [self.sb(f"glc{i}", [128, 512], F32) for i in range(2)]
        sig = [self.sb(f"sig{i}", [128, 512], F32) for i in range(2)]
        lin = [self.sb(f"lin{i}", [128, 512], F32) for i in range(2)]
        yt = [self.sb(f"yt{i}", [128, D], BF16) for i in range(2)]
        for xs in xg:
            for x in xs:
                self.memset("dve", x[:], 0.0)
        NG = CAP // 512
        items = [(e, g) for e in range(NE) for g in range(NG)]

        def load_weights(e):
            w1, w2, b1, b2 = w1s[e % 2], w2s[e % 2], b1s[e % 2], b2s[e % 2]
            src1 = I["moe_w1"][layer, e].rearrange("(kc p) n -> p kc n", p=128)
            for q in range(4):
                self.dma("pool", w1[:, 2 * q:2 * q + 2, :], src1[:, 2 * q:2 * q + 2, :], writes=[(w1, q)])
            src2 = I["moe_w2"][layer, e].rearrange("(kc p) n -> p kc n", p=128)
            for q in range(2):
                self.dma("pool", w2[:, 4 * q:4 * q + 4, :], src2[:, 4 * q:4 * q + 4, :], writes=[(w2, q)])
            self.dma("sp", b1[:], I["moe_b1"][layer, e].rearrange("(c p) -> p c", p=128), allow_slow_non_contiguous=True)
            self.dma("sp", b2[:], I["moe_b2"][layer, e:e + 1, :].partition_broadcast(128))
            self.ts("dve", b1[:, 8:16], b1[:, 8:16], 1.0, None, ALU.add)

        def load_tokens(k):
            e, grp = items[k]
            st = k % 2
            for j in range(4):
                r0 = e * CAP + (grp * 4 + j) * 128
                self.dma("sp", idx[st][j][:], self.SLOT[r0:r0 + 128, :], reads=[self.SLOT])
                self.dma("pool", xg[st][j][:], self.M[:, :], reads=[self.M, idx[st][j]],
                         indirect=dict(out_offset=None, in_offset=bass.IndirectOffsetOnAxis(ap=idx[st][j][:, 0:1], axis=0)))

        def stageT(k):
            e, grp = items[k]
            st = k % 2
            x_t = xT[k % 2]
            for j in range(4):
                pb = self.PB[j % 2]
                for kc in range(8):
                    self.tr(pb[:, kc * 128:(kc + 1) * 128], xg[st][j][:, kc * 128:(kc + 1) * 128], cst["ident_b"][:])
                self.copy("act", x_t[:, :, j * 128:(j + 1) * 128], pb[:, :].rearrange("p (k c) -> p k c", c=128))

        def stageH(k):
            e, grp = items[k]
            w1, b1 = w1s[e % 2], b1s[e % 2]
            x_t, a_t = xT[k % 2], aT[k % 2]
            for jc in range(8):
                pg, plin = self.PS[(jc % 2) * 2], self.PS[(jc % 2) * 2 + 1]
                for kc in range(8):
                    self.mm(pg[:], w1[:, kc, jc * 128:(jc + 1) * 128], x_t[:, kc, :], start=(kc == 0), stop=(kc == 7))
                for kc in range(8):
                    self.mm(plin[:], w1[:, kc, D + jc * 128:D + (jc + 1) * 128], x_t[:, kc, :], start=(kc == 0), stop=(kc == 7))
                g_, s_, l_ = glc[jc % 2], sig[jc % 2], lin[jc % 2]
                self.ts("dve", g_[:], pg[:], b1[:, jc:jc + 1], 7.0, ALU.add, ALU.min)
                self.act(s_[:], g_[:], AF.Sigmoid, scale=1.702)
                self.ts("dve", l_[:], plin[:], b1[:, 8 + jc:9 + jc], 8.0, ALU.add, ALU.min)
                self.tt("dve", g_[:], g_[:], s_[:], ALU.mult)
                self.stt("dve", a_t[:, jc, :], l_[:], -6.0, g_[:], ALU.max, ALU.mult)

        def stageY(k):
            e, grp = items[k]
            w2, b2 = w2s[e % 2], b2s[e % 2]
            a_t = aT[k % 2]
            for jt in range(4):
                y = yt[jt % 2]
                for half in range(2):
                    ps = self.PS[4 + half]
                    for jc in range(8):
                        self.mm(ps[:], a_t[:, jc, jt * 128:(jt + 1) * 128], w2[:, jc, half * 512:(half + 1) * 512],
                                start=(jc == 0), stop=(jc == 7))
                    self.tt("dve", y[:, half * 512:(half + 1) * 512], ps[:], b2[:, half * 512:(half + 1) * 512], ALU.add)
                r0 = e * CAP + (grp * 4 + jt) * 128
                self.dma("sp", self.YB[r0:r0 + 128, :], y[:], writes=[(self.YB, r0)])

        load_weights(0)
        load_tokens(0)
        if len(items) > 1:
            load_tokens(1)
        stageT(0)
        for k in range(len(items)):
            e, grp = items[k]
            if grp == 0 and e + 1 < NE:
                load_weights(e + 1)
            if k + 2 < len(items):
                load_tokens(k + 2)
            stageH(k)
            if k + 1 < len(items):
                stageT(k + 1)
            stageY(k)
        self.release(mk)

    def combine(self, layer):
        nc, I, cst = self.nc, self.I, self.cst
        ns = self.ns
        mk = self.mark()
        g2 = self.sb("g2", [128, 2, D], F32)
        yk = [self.sb(f"yk{i}", [128, D], BF16) for i in range(4)]
        acc = [self.sb(f"acc{i}", [128, D], F32) for i in range(2)]
        h1t = [self.sb(f"h1t{i}", [128, D], F32) for i in range(2)]
        if layer == 0:
            self.modrow(g2[:, 1, :], 0, 4, 5)
        nt_seq = NT if layer == 0 else LAT // 128
        for b in range(ns):
            self.modrow(g2[:, 0, :], layer, b, 5)
            for i in range(nt_seq):
                gt = b * nt_seq + i
                grow = gt * 128
                a, h = acc[gt % 2], h1t[gt % 2]
                self.dma("sp", h[:], self.H1[grow:grow + 128, :], reads=[self.H1])
                for k in range(4):
                    self.dma("pool", yk[k][:], self.YB[:, :], reads=[self.YB, (self.DEST, gt)],
                             indirect=dict(out_offset=None, in_offset=bass.IndirectOffsetOnAxis(ap=self.DEST[:, gt, k:k + 1], axis=0)))
                    if k == 0:
                        self.ts("dve", a[:], yk[0][:], self.GATE[:, gt, 0:1], None, ALU.mult, reads=[yk[0], (self.GATE, gt)])
                    else:
                        self.stt("dve", a[:], yk[k][:], self.GATE[:, gt, k:k + 1], a[:], ALU.mult, ALU.add,
                                 reads=[yk[k], a, (self.GATE, gt)])
                gsel = 1 if (layer == 0 and i < 2) else 0
                self.tt("pool", a[:], a[:], g2[:, gsel, :], ALU.mult)
                self.tt("dve", a[:], a[:], h[:], ALU.add)
                if layer == 0:
                    self.dma("sp", self.H2[grow:grow + 128, :], a[:], writes=[(self.H2, grow)])
                else:
                    self.dma("sp", self.out[b, i * 128:(i + 1) * 128, :], a[:], writes=[("out", grow)])
        self.release(mk)


def build_program(ns, **kw):
    k = K(ns, **kw)
    k.build()
    return k


_CACHE = {}


def kernel(**inputs):
    ncores, ns = 8, 4
    if "k" not in _CACHE:
        _CACHE["k"] = build_program(ns)
    k = _CACHE["k"]
    consts = {"k_" + n: v for n, v in host_consts().items()}
    shared = {n: np.ascontiguousarray(np.asarray(inputs[n], dtype=np.float32)) for n in IN_SPECS if n not in ("x", "c", "ctx")}
    in_maps = []
    for c in range(ncores):
        m = dict(shared)
        for n in ("x", "c", "ctx"):
            m[n] = np.ascontiguousarray(np.asarray(inputs[n], dtype=np.float32)[c * ns:(c + 1) * ns])
        m.update(consts)
        in_maps.append(m)
    res = run_bass_kernel_spmd(k.nc, in_maps, core_ids=list(range(ncores)))
    return np.concatenate([r["out"] for r in res.results], axis=0).astype(np.float32)
```

```python
import numpy as np
import ml_dtypes
import concourse.bass as bass
import concourse.mybir as mybir
from concourse.bass_utils import run_bass_kernel_spmd

F32 = mybir.dt.float32
BF16 = mybir.dt.bfloat16
I32 = mybir.dt.int32
AF = mybir.ActivationFunctionType
ALU = mybir.AluOpType
AX = mybir.AxisListType

D = 1024
LAT = 2048
CTX = 256
SEQ = LAT + CTX
NT = SEQ // 128
EPS = 1e-6
NE = 32
CAPS = (2560, 2048)
CAPMAX = max(CAPS)
NSLOTMAX = NE * CAPMAX


class Sched:
    EPOCH = 30000

    def __init__(self, nc):
        self.nc = nc
        self.E = {"pe": nc.tensor, "dve": nc.vector, "act": nc.scalar, "pool": nc.gpsimd, "sp": nc.sync}
        self.nsem = 0
        self.esem, self.ecnt = {}, {}
        self.pesems = set()
        for e in self.E:
            self._new_epoch(e)
        self.seen = {e: {} for e in self.E}
        self.W, self.R = {}, {}
        self.dpool = {q: [] for q in ("sp", "pool", "act")}
        self.dnext = {q: 0 for q in self.dpool}
        self.NPOOL = {"sp": 24, "pool": 24, "act": 4}
        self.ninst = 0
        self.psum = set()

    def _sem(self, name):
        self.nsem += 1
        return self.nc.semaphore(f"{name}{self.nsem}").__enter__()

    def _new_epoch(self, e):
        import os as _os
        self.skip_same = _os.environ.get("KSAME", "1") == "0"
        if not hasattr(self, "own"):
            self.own = {}
        self.esem[e] = self._sem("e" + e)
        self.own.setdefault(e, set()).add(self.esem[e])
        self.ecnt[e] = 0
        if e == "pe":
            self.pesems.add(self.esem[e])

    @staticmethod
    def _nm(x):
        if isinstance(x, str):
            return x
        t = getattr(x, "tensor", None)
        return t.name if t is not None else x.name

    @staticmethod
    def _key(x):
        if isinstance(x, tuple):
            return (Sched._nm(x[0]), x[1])
        return (Sched._nm(x), None)

    def _collect(self, table, key, evs):
        name, sub = key
        t = table.get(name)
        if not t:
            return
        subs = t.keys() if sub is None else [s for s in (sub, None) if s in t]
        for s in subs:
            for sem, val in t[s].items():
                if evs.get(sem, 0) < val:
                    evs[sem] = val

    def _deps(self, e, rk, wk):
        evs = {}
        for k in rk:
            self._collect(self.W, k, evs)
        for k in wk:
            self._collect(self.W, k, evs)
            self._collect(self.R, k, evs)
        for sem, val in evs.items():
            if e == "pe" and sem in self.pesems:
                continue
            if self.skip_same and sem in self.own.get(e, ()):
                continue
            if self.seen[e].get(sem, 0) >= val:
                continue
            self.E[e].wait_ge(sem, val)
            self.seen[e][sem] = val

    def _commit(self, ev, rk, wk):
        sem, val = ev
        for name, sub in rk:
            d = self.R.setdefault(name, {}).setdefault(sub, {})
            if d.get(sem, 0) < val:
                d[sem] = val
        for name, sub in wk:
            if sub is None:
                self.W[name] = {None: {sem: val}}
                self.R[name] = {}
            else:
                self.W.setdefault(name, {})[sub] = {sem: val}
                self.R.setdefault(name, {}).pop(sub, None)

    def op(self, e, fn, reads=(), writes=()):
        rk = [self._key(x) for x in reads]
        wk = [self._key(x) for x in writes]
        pr = [(k[0], None) for k in rk if k[0] in self.psum]
        if pr:
            rk = [k for k in rk if k[0] not in self.psum]
            wk = wk + pr
        wk = [((k[0], None) if k[0] in self.psum else k) for k in wk]
        self._deps(e, rk, wk)
        if self.ecnt[e] >= self.EPOCH:
            self._new_epoch(e)
        ins = fn()
        self.ecnt[e] += 1
        ins.then_inc(self.esem[e], 1)
        self._commit((self.esem[e], self.ecnt[e]), rk, wk)
        self.ninst += 1
        return ins

    def dma(self, q, out, in_, reads=None, writes=None, indirect=None, **kw):
        rk = [self._key(x) for x in (reads if reads is not None else [in_])]
        wk = [self._key(x) for x in (writes if writes is not None else [out])]
        self._deps(q, rk, wk)
        pool = self.dpool[q]
        if len(pool) < self.NPOOL[q]:
            pool.append([self._sem("d" + q), 0])
            slot = pool[-1]
        else:
            slot = pool[self.dnext[q] % len(pool)]
            self.dnext[q] += 1
            if slot[1] >= 60000:
                if self.seen[q].get(slot[0], 0) < slot[1]:
                    self.E[q].wait_ge(slot[0], slot[1])
                slot[0], slot[1] = self._sem("d" + q), 0
        sem, cnt = slot
        if cnt > 0 and self.seen[q].get(sem, 0) < cnt:
            self.E[q].wait_ge(sem, cnt)
            self.seen[q][sem] = cnt
        if indirect is None:
            ins = self.E[q].dma_start(out=out, in_=in_, **kw)
        else:
            ins = self.E[q].indirect_dma_start(out=out, in_=in_, **indirect)
        slot[1] = cnt + 16
        ins.then_inc(sem, 16)
        self._commit((sem, slot[1]), rk, wk)
        self.ninst += 1
        return ins

    def barrier(self):
        evs = {}
        for e in self.E:
            if self.ecnt[e] > 0:
                evs[self.esem[e]] = self.ecnt[e]
        for q, pool in self.dpool.items():
            for sem, cnt in pool:
                if cnt > 0:
                    evs[sem] = cnt
        for e in self.E:
            for sem, val in evs.items():
                if self.seen[e].get(sem, 0) >= val:
                    continue
                self.E[e].wait_ge(sem, val)
                self.seen[e][sem] = val
        self.W, self.R = {}, {}

    def finish(self):
        self.barrier()


def host_consts():
    c = {}
    c["ident_f"] = np.eye(128, dtype=np.float32)
    c["ident_b"] = np.eye(128).astype(ml_dtypes.bfloat16)
    s = np.arange(128)[:, None]
    t = np.arange(128)[None, :]
    c["mask_f"] = (s <= t).astype(np.float32)
    c["mask_b"] = (s >= t).astype(np.float32)
    c["tri_b"] = (s < t).astype(ml_dtypes.bfloat16)
    c["ones_b"] = np.ones((128, 128), ml_dtypes.bfloat16)
    c["ones_f"] = np.ones((128, 128), np.float32)
    facS = np.ones((4, 8), np.float32)
    facE = np.ones((4, 8), np.float32)
    for g, w in enumerate((2, 4, 8, 16)):
        half = w // 2
        for tt in range(min(half, 8)):
            facS[g, tt] = w / (tt + half)
        for j in range(8):
            i = 7 - j
            if i < half - 1:
                facE[g, j] = w / (half + 1 + i)
    c["facS"] = np.broadcast_to(facS[None], (128, 4, 8)).copy()
    c["facE"] = np.broadcast_to(facE[None], (128, 4, 8)).copy()
    rows = LAT // 64
    t_row = np.repeat(np.arange(rows, dtype=np.float32), 64)
    t_col = np.tile(np.arange(64, dtype=np.float32), rows)
    inv = (1.0 / (10000.0 ** (np.arange(8, dtype=np.float32) / 8))).astype(np.float32)
    ang = np.stack([t_row[:, None] * inv, t_col[:, None] * inv], axis=1).astype(np.float32)
    cos = np.cos(ang).reshape(LAT, 16).astype(np.float32)
    sin = np.sin(ang).reshape(LAT, 16).astype(np.float32)
    for l_ in range(2):
        c[f"slotbase{l_}"] = np.broadcast_to((np.arange(32, dtype=np.float32) * CAPS[l_])[None], (128, 32)).copy()
    c["pidx"] = np.arange(128, dtype=np.float32).reshape(128, 1).copy()
    c["rope_cos"] = cos.reshape(16, 128, 16).transpose(1, 0, 2).copy()
    c["rope_sin"] = sin.reshape(16, 128, 16).transpose(1, 0, 2).copy()
    return c


CONST_SPECS = {
    "ident_f": ([128, 128], F32), "ident_b": ([128, 128], BF16), "mask_f": ([128, 128], F32),
    "mask_b": ([128, 128], F32), "tri_b": ([128, 128], BF16), "ones_b": ([128, 128], BF16),
    "ones_f": ([128, 128], F32), "facS": ([128, 4, 8], F32), "facE": ([128, 4, 8], F32),
    "slotbase0": ([128, 32], F32), "slotbase1": ([128, 32], F32), "pidx": ([128, 1], F32),
    "rope_cos": ([128, 16, 16], F32), "rope_sin": ([128, 16, 16], F32),
}

IN_SPECS = {
    "x": lambda ns: [ns, LAT, D], "c": lambda ns: [ns, D], "ctx": lambda ns: [ns, CTX, D], "c_ctx": lambda ns: [D],
    "ada_w": lambda ns: [2, D, 6 * D], "ada_b": lambda ns: [2, 6 * D], "ev_w_in": lambda ns: [1, D, 3072],
    "hgrn_lb_logits": lambda ns: [3, 2, 512], "hgrn_norm_w": lambda ns: [1, 128], "pool_w": lambda ns: [1, 4, 128, 128],
    "pool_scale": lambda ns: [1, 512], "ev_w_out": lambda ns: [1, D, D], "od_w_in": lambda ns: [1, D, 1696],
    "conv_dw_w": lambda ns: [1, 31, 512], "conv_dw_b": lambda ns: [1, 512], "conv_ln_w": lambda ns: [1, 512],
    "conv_ln_b": lambda ns: [1, 512], "mla_q_a_norm": lambda ns: [1, 384], "mla_w_uq": lambda ns: [1, 384, 768],
    "mla_kv_a_norm": lambda ns: [1, 256], "mla_w_ukv": lambda ns: [1, 256, 1024], "mla_q_norm": lambda ns: [1, 96],
    "mla_k_norm": lambda ns: [1, 96], "od_w_out": lambda ns: [1, D, D], "moe_router_w": lambda ns: [2, D, 32],
    "moe_router_b": lambda ns: [2, 32], "moe_w1": lambda ns: [2, 32, D, 2 * D], "moe_b1": lambda ns: [2, 32, 2 * D],
    "moe_w2": lambda ns: [2, 32, D, D], "moe_b2": lambda ns: [2, 32, D],
}


class StopBuild(Exception):
    pass


class K:
    def __init__(self, ns, stop_after=None, dbg=(), skip_inputs=()):
        self.ns = ns
        self.stop_after = stop_after
        self.dbg = set(dbg)
        nc = self.nc = bass.Bass("TRN2", target_bir_lowering=False)
        self.S = Sched(nc)
        self.I = {k: nc.dram_tensor(k, f(ns), F32, kind="ExternalInput").ap() for k, f in IN_SPECS.items() if k not in skip_inputs}
        self.C = {k: nc.dram_tensor("k_" + k, sh, dt, kind="ExternalInput").ap() for k, (sh, dt) in CONST_SPECS.items()}
        self.out = nc.dram_tensor("out", [ns, LAT, D], F32, kind="ExternalOutput").ap()
        self.T0 = ns * SEQ
        self.T1 = ns * LAT
        self.MOD = nc.dram_tensor("MOD", [10, 6 * D], F32).ap()
        self.H1 = nc.dram_tensor("H1", [self.T0, D], F32).ap()
        self.H2 = nc.dram_tensor("H2", [self.T0, D], F32).ap()
        self.M = nc.dram_tensor("M", [self.T0 + 128, D], BF16).ap()
        self.SLOT = nc.dram_tensor("SLOT", [NSLOTMAX + 128, 2], I32).ap()
        self.YB = nc.dram_tensor("YB", [NSLOTMAX + 128, D], BF16).ap()
        self.dbg_out = {}
        self._stack = []

    def sb(self, name, shape, dt):
        self._uid = getattr(self, "_uid", 0) + 1
        cm = self.nc.sbuf_tensor(f"{name}_{self._uid}", shape, dt)
        t = cm.__enter__()
        self._stack.append(cm)
        return t

    def ps(self, name, shape, dt):
        cm = self.nc.psum_tensor(name, shape, dt)
        t = cm.__enter__()
        self.S.psum.add(name)
        self._stack.append(cm)
        return t

    def mark(self):
        return len(self._stack)

    def release(self, mark):
        self.S.barrier()
        while len(self._stack) > mark:
            self._stack.pop().__exit__(None, None, None)

    def mm(self, out, lhsT, rhs, start=True, stop=True, reads=None, writes=None):
        nc = self.nc
        return self.S.op("pe", lambda: nc.tensor.matmul(out, lhsT, rhs, start=start, stop=stop),
                         reads=reads if reads is not None else [lhsT, rhs], writes=writes if writes is not None else [out])

    def tr(self, out, in_, ident, reads=None, writes=None):
        nc = self.nc
        return self.S.op("pe", lambda: nc.tensor.transpose(out, in_, ident),
                         reads=reads if reads is not None else [in_, ident], writes=writes if writes is not None else [out])

    def act(self, out, in_, func, bias=None, scale=None, accum_out=None, reads=None, writes=None):
        nc = self.nc
        kw = {}
        rd = [in_]
        wr = [out]
        if bias is not None:
            kw["bias"] = bias
            if not isinstance(bias, (int, float)):
                rd.append(bias)
        if scale is not None:
            kw["scale"] = scale
            if not isinstance(scale, (int, float)):
                rd.append(scale)
        if accum_out is not None:
            kw["accum_out"] = accum_out
            wr.append(accum_out)
        return self.S.op("act", lambda: nc.scalar.activation(out, in_, func, **kw),
                         reads=reads if reads is not None else rd, writes=writes if writes is not None else wr)

    def v(self, e, name, *args, reads, writes, **kw):
        eng = self.S.E[e]
        return self.S.op(e, lambda: getattr(eng, name)(*args, **kw), reads=reads, writes=writes)

    def tt(self, e, out, in0, in1, op, reads=None, writes=None):
        return self.v(e, "tensor_tensor", out, in0, in1, op, reads=reads if reads is not None else [in0, in1],
                      writes=writes if writes is not None else [out])

    def ts(self, e, out, in0, s1, s2, op0, op1=None, reads=None, writes=None, accum_out=None):
        rd = [in0] + [s for s in (s1, s2) if s is not None and not isinstance(s, (int, float))]
        kw = {}
        if op1 is not None:
            kw["op1"] = op1
        wr = [out]
        if accum_out is not None:
            kw["accum_out"] = accum_out
            wr.append(accum_out)
        return self.v(e, "tensor_scalar", out, in0, s1, s2, op0, reads=reads if reads is not None else rd,
                      writes=writes if writes is not None else wr, **kw)

    def stt(self, e, out, in0, scalar, in1, op0, op1, reads=None, writes=None):
        rd = [in0, in1] + ([] if isinstance(scalar, (int, float)) else [scalar])
        return self.v(e, "scalar_tensor_tensor", out, in0, scalar, in1, op0, op1,
                      reads=reads if reads is not None else rd, writes=writes if writes is not None else [out])

    def copy(self, e, out, in_, reads=None, writes=None):
        if e == "act":
            return self.act(out, in_, AF.Copy, reads=reads, writes=writes)
        return self.v(e, "tensor_copy", out, in_, reads=reads if reads is not None else [in_],
                      writes=writes if writes is not None else [out])

    def memset(self, e, ap, val):
        return self.v(e, "memset", ap, val, reads=[], writes=[ap])

    def dma(self, q, out, in_, **kw):
        return self.S.dma(q, out, in_, **kw)

    def rstd_from_ss(self, rstd, ss, n):
        self.ts("dve", rstd, ss, 1.0 / n, EPS, ALU.mult, ALU.add)
        self.act(rstd, rstd, AF.Sqrt)
        self.v("dve", "reciprocal", rstd, rstd, reads=[rstd], writes=[rstd])

    def build(self):
        nc, S, I, C = self.nc, self.S, self.I, self.C
        ns = self.ns
        self.cst = {}
        for k in ("ident_f", "ident_b", "mask_f", "mask_b", "tri_b", "ones_b", "ones_f", "slotbase0", "slotbase1", "pidx"):
            sh, dt = CONST_SPECS[k]
            t = self.sb("c_" + k, sh, dt)
            self.dma("sp", t[:], C[k][:, :])
            self.cst[k] = t
        self.PS = [self.ps(f"ps{i}", [128, 512], F32) for i in range(6)]
        self.PB = [self.ps(f"pb{i}", [128, 1024], BF16) for i in range(2)]
        self.small = self.sb("small", [128, 64], F32)

        self.phase_adaln()
        if self.stop_after == "adaln":
            return self.end()
        mkr = self.mark()
        self.route_init(0)
        try:
            self.layer0()
        except StopBuild:
            return self.end()
        if self.stop_after == "mixer0":
            return self.end()
        if self.stop_after and self.stop_after.startswith("l1_"):
            self.S.barrier()
            self.dma("sp", self.H2, self.H1)
        else:
            self.experts(0)
            self.combine(0)
        self.release(mkr)
        if self.stop_after == "layer0":
            return self.end()
        mkr = self.mark()
        self.route_init(1)
        try:
            self.layer1()
        except StopBuild:
            return self.end()
        self.experts(1)
        self.combine(1)
        self.release(mkr)
        return self.end()

    def end(self):
        for name in ("MOD", "H1", "H2", "M"):
            if name in self.dbg:
                src = getattr(self, name)
                d = self.dbg_dump(name, list(src.shape), src.dtype)
                self.S.barrier()
                self.dma("sp", d, src)
        self.S.finish()
        return self.nc

    def dbg_dump(self, name, shape, dt=F32):
        t = self.nc.dram_tensor("dbg_" + name, shape, dt, kind="ExternalOutput").ap()
        self.dbg_out[name] = t
        return t

    def phase_adaln(self):
        nc, S, I, C = self.nc, self.S, self.I, self.C
        ns = self.ns
        mk = self.mark()
        cs = self.sb("cs", [128, 8, 8], F32)
        s5 = self.sb("s5", [128, 8, 8], F32)
        self.memset("dve", cs[:], 0.0)
        for b in range(ns):
            self.dma("sp", cs[:, :, b], I["c"][b].rearrange("(kc p) -> p kc", p=128), allow_slow_non_contiguous=True)
        self.dma("sp", cs[:, :, ns], I["c_ctx"].rearrange("(kc p) -> p kc", p=128), allow_slow_non_contiguous=True)
        self.act(s5[:], cs[:], AF.Silu)
        nr = ns + 1
        brow = self.sb("brow", [1, 6 * D], F32)
        wa = [self.sb(f"wa{i}", [128, 8, 512], F32) for i in range(2)]
        mo = [self.sb(f"mo{i}", [8, 512], F32) for i in range(2)]
        onesf = self.cst["ones_f"]
        it = 0
        for layer in range(2):
            self.dma("sp", brow[:], I["ada_b"][layer:layer + 1, :])
            for nb in range(12):
                w = wa[it % 2]
                self.dma("sp" if it % 2 == 0 else "pool", w[:],
                         I["ada_w"][layer][:, nb * 512:(nb + 1) * 512].rearrange("(kc p) n -> p kc n", p=128))
                ps = self.PS[it % 2]
                for kc in range(8):
                    self.mm(ps[0:nr, :], s5[:, kc, 0:nr], w[:, kc, :], start=(kc == 0), stop=False)
                self.mm(ps[0:nr, :], onesf[0:1, 0:nr], brow[0:1, nb * 512:(nb + 1) * 512], start=False, stop=True)
                m = mo[it % 2]
                self.copy("dve", m[0:nr, :], ps[0:nr, :])
                self.dma("sp", self.MOD[layer * 5:layer * 5 + ns, nb * 512:(nb + 1) * 512], m[0:ns, :],
                         writes=[(self.MOD, (layer, nb, 0))])
                self.dma("sp", self.MOD[layer * 5 + 4:layer * 5 + 5, nb * 512:(nb + 1) * 512], m[ns:ns + 1, :],
                         writes=[(self.MOD, (layer, nb, 1))])
                it += 1
        self.release(mk)

    def modrow(self, dst, layer, row, q):
        src = self.MOD[layer * 5 + row:layer * 5 + row + 1, q * D:(q + 1) * D].partition_broadcast(128)
        self.dma("sp", dst, src, reads=[self.MOD])

    def modulate_tile(self, u, ht, sc1p, sh, junk, ss, rstd):
        self.act(junk, ht, AF.Square, accum_out=ss)
        self.rstd_from_ss(rstd, ss, D)
        self.stt("dve", u, ht, rstd, sc1p, ALU.mult, ALU.mult)
        self.tt("pool", u, u, sh, ALU.add)

    def modulate_pipe(self, uT, srcs, hts, us, junk, mr, read_keys=None):
        n = len(srcs)
        st = self.sb("mp_st", [128, 2 * n], F32)
        cst = self.cst

        def stats(i):
            ht = hts[i % 3]
            kw = {} if read_keys is None else {"reads": read_keys}
            self.dma("sp", ht[:], srcs[i], **kw)
            self.act(junk[:], ht[:], AF.Square, accum_out=st[:, i:i + 1], writes=[junk, (st, i)])
            self.ts("dve", st[:, n + i:n + i + 1], st[:, i:i + 1], 1.0 / D, EPS, ALU.mult, ALU.add, reads=[(st, i)], writes=[(st, n + i)])
            self.act(st[:, n + i:n + i + 1], st[:, n + i:n + i + 1], AF.Sqrt, reads=[(st, n + i)], writes=[(st, n + i)])
            self.v("dve", "reciprocal", st[:, n + i:n + i + 1], st[:, n + i:n + i + 1], reads=[(st, n + i)], writes=[(st, n + i)])

        def apply(i):
            ht, u = hts[i % 3], us[i % 2]
            o = 2 if i < 2 else 0
            self.stt("dve", u[:], ht[:], st[:, n + i:n + i + 1], mr[:, o, :], ALU.mult, ALU.mult, reads=[ht, (st, n + i), mr], writes=[u])
            self.tt("pool", u[:], u[:], mr[:, o + 1, :], ALU.add)
            self.transpose_tile_to(uT, i * 128, u, 8, cst["ident_f"], self.PS[0], self.PS[1])

        stats(0)
        for i in range(n):
            if i + 1 < n:
                stats(i + 1)
            apply(i)

    def transpose_tile_to(self, dstT, col0, src, nkc, ident_f, psA, psB, dt_note=None):
        for kc in range(nkc):
            ps = psA if kc < 4 else psB
            self.tr(ps[:, (kc % 4) * 128:(kc % 4 + 1) * 128], src[:, kc * 128:(kc + 1) * 128], ident_f[:])
        n0 = min(4, nkc)
        self.copy("act", dstT[:, 0:n0, col0:col0 + 128], psA[:, 0:n0 * 128].rearrange("p (k c) -> p k c", c=128))
        if nkc > 4:
            self.copy("dve", dstT[:, 4:nkc, col0:col0 + 128], psB[:, 0:(nkc - 4) * 128].rearrange("p (k c) -> p k c", c=128))

    def layer0(self):
        nc, S, I, C = self.nc, self.S, self.I, self.C
        ns = self.ns
        cst = self.cst
        mk0 = self.mark()
        lg = self.sb("lg", [128, 3, 8], F32)
        lb = self.sb("lb", [128, 8], F32)
        oml = self.sb("oml", [128, 8], F32)
        for l in range(3):
            self.dma("sp", lg[:, l, :].rearrange("p (d h) -> p d h", d=2),
                     I["hgrn_lb_logits"][l].rearrange("d (h p) -> p d h", p=128), allow_slow_non_contiguous=True)
        self.act(lg[:], lg[:], AF.Exp)
        self.tt("dve", lb[:], lg[:, 0, :], lg[:, 1, :], ALU.add)
        self.tt("dve", lb[:], lb[:], lg[:, 2, :], ALU.add)
        self.v("dve", "reciprocal", lb[:], lb[:], reads=[lb], writes=[lb])
        self.tt("dve", lb[:], lb[:], lg[:, 0, :], ALU.mult)
        self.ts("dve", oml[:], lb[:], -1.0, 1.0, ALU.mult, ALU.add)
        nwb = self.sb("nwb", [128, 128], F32)
        self.dma("sp", nwb[:], I["hgrn_norm_w"][0:1, :].partition_broadcast(128))
        pscale = self.sb("pscale", [128, 4], F32)
        self.dma("sp", pscale[:], I["pool_scale"][0].rearrange("(g p) -> p g", p=128), allow_slow_non_contiguous=True)
        poolw = self.sb("poolw", [128, 4, 128], BF16)
        for g in range(4):
            self.dma("pool", poolw[:, g, :], I["pool_w"][0, g])
        facS = self.sb("facS", [128, 4, 8], F32)
        facE = self.sb("facE", [128, 4, 8], F32)
        self.dma("sp", facS[:], C["facS"])
        self.dma("sp", facE[:], C["facE"])
        wr = self.sb("wr", [128, 8, 32], F32)
        self.dma("sp", wr[:], I["moe_router_w"][0].rearrange("(kc p) e -> p kc e", p=128))
        brr = self.sb("brr", [1, 32], F32)
        self.dma("sp", brr[:], I["moe_router_b"][0:1, :])
        uT = self.sb("uT", [128, 8, SEQ], BF16)
        mixT = self.sb("mixT", [128, 8, SEQ], BF16)
        w_in = I["ev_w_in"][0]
        for b in range(ns):
            mk = self.mark()
            mr = self.sb("mr", [128, 4, D], F32)
            self.modrow(mr[:, 0, :], 0, b, 1)
            self.modrow(mr[:, 1, :], 0, b, 0)
            self.modrow(mr[:, 2, :], 0, 4, 1)
            self.modrow(mr[:, 3, :], 0, 4, 0)
            self.ts("dve", mr[:, 0, :], mr[:, 0, :], 1.0, None, ALU.add)
            self.ts("dve", mr[:, 2, :], mr[:, 2, :], 1.0, None, ALU.add)
            hts = [self.sb(f"ht{i}", [128, D], F32) for i in range(3)]
            us = [self.sb(f"u{i}", [128, D], F32) for i in range(2)]
            junk = self.sb("junk", [128, D], F32)
            srcs = [I["ctx"][b, i * 128:(i + 1) * 128, :] if i < 2 else I["x"][b, (i - 2) * 128:(i - 1) * 128, :] for i in range(NT)]
            self.modulate_pipe(uT, srcs, hts, us, junk, mr)
            if "uT" in self.dbg and b == 0:
                d = self.dbg_dump("uT", [128, 8, SEQ], BF16)
                self.dma("sp", d[:, :, :], uT[:])
            self.release(mk)
            if self.stop_after == "l0_p0":
                raise StopBuild()
            mk = self.mark()
            W2 = SEQ + 64
            q32 = self.sb("q32", [128, W2], F32)
            sg = self.sb("sg", [128, W2], F32)
            kin = self.sb("kin", [128, W2], F32)
            A = self.sb("A", [128, W2], F32)
            oacc = self.sb("oacc", [128, NT, 128], F32)
            v16 = self.sb("v16", [128, NT, 128], BF16)
            whs = [self.sb(f"wh{i}", [128, 8, 640], BF16) for i in range(2)]
            S32 = self.sb("S32", [128, 128], F32)
            S16 = self.sb("S16", [128, 128], BF16)
            RING = 3
            btall = self.sb("btall", [128, NT, 8], F32)
            decs = [self.sb(f"dec{i}", [128, 1], F32) for i in range(RING)]
            Pks = [[self.sb(f"Pk{r_}{i}", [128, 128], F32) for i in range(5)] for r_ in range(RING)]
            kiA = [self.sb(f"kiA{i}", [128, 128], BF16) for i in range(RING)]
            kiB = [self.sb(f"kiB{i}", [128, 128], BF16) for i in range(RING)]
            kiC = [self.sb(f"kiC{i}", [128, 128], BF16) for i in range(RING)]
            qdx = [self.sb(f"qdx{i}", [128, 64], BF16) for i in range(RING)]
            S16s = [self.sb(f"S16{i}", [128, 128], BF16) for i in range(2)]
            qd = [self.sb(f"qd{i}", [128, 128], BF16) for i in range(RING)]
            qdS = [self.sb(f"qdS{i}", [128, 128], BF16) for i in range(RING)]
            ke = [self.sb(f"ke{i}", [128, 128], BF16) for i in range(RING)]
            att16 = [self.sb(f"att{i}", [128, 128], BF16) for i in range(2)]
            ket16 = [self.sb(f"ket{i}", [128, 128], BF16) for i in range(2)]
            rrs = [self.sb(f"rr{i}", [128, 128], F32) for i in range(2)]
            rdo = self.sb("rdo", [128, 2 * NT], F32)
            zero1 = self.sb("zero1", [128, 1], F32)
            self.memset("dve", zero1[:], 0.0)
            groups = [(g * 512, min(512, SEQ - g * 512)) for g in range((SEQ + 511) // 512)]
            for h in range(4):
                wh = whs[h % 2]
                for j, base in enumerate((0, 512, 1024, 1536, 2048)):
                    c0 = base + h * 128
                    self.dma("pool", wh[:, :, j * 128:(j + 1) * 128],
                             w_in[:, c0:c0 + 128].rearrange("(kc p) n -> p kc n", p=128))
                for gi, (g0, gn) in enumerate(groups):
                    ps = self.PS[gi % 2]
                    for kc in range(8):
                        self.mm(ps[:, 0:gn], wh[:, kc, 0:128], uT[:, kc, g0:g0 + gn], start=(kc == 0), stop=(kc == 7))
                    self.act(q32[:, g0:g0 + gn], ps[:, 0:gn], AF.Silu)
                for i in range(NT):
                    ps = self.PS[2 + i % 2]
                    for kc in range(8):
                        self.mm(ps[:, 0:128], uT[:, kc, i * 128:(i + 1) * 128], wh[:, kc, 384:512], start=(kc == 0), stop=(kc == 7))
                    self.copy("dve", v16[:, i, :], ps[:, 0:128])
                for di in range(2):
                    for gi, (g0, gn) in enumerate(groups):
                        ps = self.PS[gi % 2]
                        for kc in range(8):
                            self.mm(ps[:, 0:gn], wh[:, kc, (1 + di) * 128:(2 + di) * 128], uT[:, kc, g0:g0 + gn],
                                    start=(kc == 0), stop=(kc == 7))
                        self.act(sg[:, g0:g0 + gn], ps[:, 0:gn], AF.Sigmoid)
                    lbc = lb[:, di * 4 + h:di * 4 + h + 1]
                    omc = oml[:, di * 4 + h:di * 4 + h + 1]
                    self.ts("dve", sg[:, 0:SEQ], sg[:, 0:SEQ], omc, lbc, ALU.mult, ALU.add)
                    self.ts("pool", kin[:, 0:SEQ], sg[:, 0:SEQ], -1.0, 1.0, ALU.mult, ALU.add)
                    self.act(sg[:, 0:SEQ], sg[:, 0:SEQ], AF.Ln)
                    self.S.op("dve", lambda: nc.vector.tensor_tensor_scan(A[:, 0:SEQ], sg[:, 0:SEQ], sg[:, 0:SEQ], 0.0, ALU.add, ALU.add),
                              reads=[sg], writes=[A])
                    if di == 1:
                        self.stt("dve", sg[:, 0:SEQ], sg[:, 0:SEQ], -2.0, A[:, 0:SEQ], ALU.mult, ALU.add)
                    AA = A if di == 0 else sg
                    order = list(range(NT)) if di == 0 else [1, 0] + list(range(NT - 1, 1, -1))
                    self.memset("dve", S32[:], 0.0)
                    self.memset("pool", S16s[0][:], 0.0)
                    self.memset("pool", S16s[1][:], 0.0)
                    for t_ in kiA + kiB + kiC:
                        self.memset("pool", t_[:], 0.0)
                    mask = cst["mask_f"] if di == 0 else cst["mask_b"]
                    lo, hi = slice(0, 64), slice(64, 128)
                    AAv = AA[:, 0:SEQ].rearrange("p (t c) -> p t c", c=128)
                    Av = A[:, 0:SEQ].rearrange("p (t c) -> p t c", c=128)
                    xcol = 63 if di == 0 else 64
                    for bc, (src_, sgn) in enumerate(((AAv[:, :, 31], -0.5), (AAv[:, :, 31], 0.5), (AAv[:, :, 95], -0.5), (AAv[:, :, 95], 0.5),
                                                     (AAv[:, :, xcol], -0.5), (AAv[:, :, xcol], 0.5))):
                        self.ts("dve", btall[:, :, bc], src_, sgn, None, ALU.mult, reads=[AA], writes=[btall])
                    self.memset("dve", btall[:, 0:1, 6], 0.0)
                    self.ts("dve", btall[:, 1:NT, 6], Av[:, 0:NT - 1, 127], -0.5, None, ALU.mult, reads=[A], writes=[btall])
                    self.ts("dve", btall[:, :, 7], Av[:, :, 127], 0.5, None, ALU.mult, reads=[A], writes=[btall])

                    def stageA(n, i):
                        c0, c1 = i * 128, (i + 1) * 128
                        r = n % RING
                        Pk_ = Pks[r]
                        bt_ = btall[:, i, :]
                        Alo, Ahi, Acol = AA[:, c0:c0 + 64], AA[:, c0 + 64:c1], AA[:, c0:c1]
                        if di == 0:
                            self.act(Pk_[0][:, lo], Alo, AF.Exp, bias=btall[:, i, 0:1], scale=0.5)
                            self.act(Pk_[0][:, hi], Ahi, AF.Exp, bias=btall[:, i, 2:3], scale=0.5)
                            self.act(Pk_[1][:, lo], Alo, AF.Exp, bias=btall[:, i, 1:2], scale=-0.5)
                            self.act(Pk_[1][:, hi], Ahi, AF.Exp, bias=btall[:, i, 3:4], scale=-0.5)
                            self.act(Pk_[2][:], Acol, AF.Exp, bias=btall[:, i, 6:7], scale=0.5)
                            self.act(Pk_[3][:], Acol, AF.Exp, bias=btall[:, i, 7:8], scale=-0.5)
                            self.act(Pk_[4][:, lo], Ahi, AF.Exp, bias=btall[:, i, 4:5], scale=0.5)
                            self.act(Pk_[4][:, hi], Alo, AF.Exp, bias=btall[:, i, 5:6], scale=-0.5)
                            qx_cols, kx_cols = hi, lo
                        else:
                            self.act(Pk_[0][:, lo], Alo, AF.Exp, bias=btall[:, i, 1:2], scale=-0.5)
                            self.act(Pk_[0][:, hi], Ahi, AF.Exp, bias=btall[:, i, 3:4], scale=-0.5)
                            self.act(Pk_[1][:, lo], Alo, AF.Exp, bias=btall[:, i, 0:1], scale=0.5)
                            self.act(Pk_[1][:, hi], Ahi, AF.Exp, bias=btall[:, i, 2:3], scale=0.5)
                            self.act(Pk_[2][:], Acol, AF.Exp, bias=btall[:, i, 7:8], scale=-0.5)
                            self.act(Pk_[3][:], Acol, AF.Exp, bias=btall[:, i, 6:7], scale=0.5)
                            self.act(Pk_[4][:, lo], Alo, AF.Exp, bias=btall[:, i, 5:6], scale=-0.5)
                            self.act(Pk_[4][:, hi], Ahi, AF.Exp, bias=btall[:, i, 4:5], scale=0.5)
                            qx_cols, kx_cols = lo, hi
                        qcs = slice(c0 + qx_cols.start, c0 + qx_cols.stop)
                        kcs = slice(c0 + kx_cols.start, c0 + kx_cols.stop)
                        self.tt("dve", qd[r][:], q32[:, c0:c1], Pk_[0][:], ALU.mult)
                        self.tt("pool", kiA[r][:, lo], kin[:, c0:c0 + 64], Pk_[1][:, lo], ALU.mult)
                        self.tt("pool", kiB[r][:, hi], kin[:, c0 + 64:c1], Pk_[1][:, hi], ALU.mult)
                        self.tt("dve", qdS[r][:], q32[:, c0:c1], Pk_[2][:], ALU.mult)
                        self.tt("pool", ke[r][:], kin[:, c0:c1], Pk_[3][:], ALU.mult)
                        self.tt("dve", qdx[r][:], q32[:, qcs], Pk_[4][:, lo], ALU.mult)
                        self.tt("pool", kiC[r][:, kx_cols], kin[:, kcs], Pk_[4][:, hi], ALU.mult)
                        self.copy("pool", decs[r][:], Pk_[2][:, 127:128] if di == 0 else Pk_[2][:, 0:1])

                    def stageB(n, i):
                        r = n % RING
                        p = n % 2
                        pa, po, pS = self.PS[0 + p], self.PS[2 + p], self.PS[4 + p]
                        pbt = self.PB[p]
                        if di == 0:
                            self.mm(pa[:, 0:64], kiA[r][:], qd[r][:, lo], start=True, stop=True)
                            self.mm(pa[:, 64:128], kiB[r][:], qd[r][:, hi], start=True, stop=False)
                            self.mm(pa[:, 64:128], kiC[r][:], qdx[r][:], start=False, stop=True)
                        else:
                            self.mm(pa[:, 0:64], kiA[r][:], qd[r][:, lo], start=True, stop=False)
                            self.mm(pa[:, 0:64], kiC[r][:], qdx[r][:], start=False, stop=True)
                            self.mm(pa[:, 64:128], kiB[r][:], qd[r][:, hi], start=True, stop=True)
                        self.tr(pbt[:, 0:128], ke[r][:], cst["ident_b"][:])
                        self.tt("dve", att16[p][:], pa[:, 0:128], mask[:], ALU.mult)
                        self.copy("act", ket16[p][:], pbt[:, 0:128])
                        self.mm(pS[:, 0:128], ket16[p][:], v16[:, i, :])
                        self.mm(po[:, 0:128], att16[p][:], v16[:, i, :], start=True, stop=False)
                        self.mm(po[:, 0:128], qdS[r][:], S16s[(n + 1) % 2][:], start=False, stop=True)
                        self.stt("dve", S32[:], S32[:], decs[r][:, 0:1], pS[:, 0:128], ALU.mult, ALU.add)
                        self.copy("act", S16s[n % 2][:], S32[:])
                        if di == 0:
                            self.copy("act", oacc[:, i, :], po[:, 0:128])
                        else:
                            self.tt("dve", oacc[:, i, :], oacc[:, i, :], po[:, 0:128], ALU.add)

                    LOOK = 2
                    for n in range(min(LOOK, NT)):
                        stageA(n, order[n])
                    for n, i in enumerate(order):
                        if n + LOOK < NT:
                            stageA(n + LOOK, order[n + LOOK])
                        stageB(n, i)
                ogall, rrall = sg, kin
                for i in range(NT):
                    ps = self.PS[i % 4]
                    for kc in range(8):
                        self.mm(ps[:, 0:128], uT[:, kc, i * 128:(i + 1) * 128], wh[:, kc, 512:640], start=(kc == 0), stop=(kc == 7))
                    self.act(ogall[:, i * 128:(i + 1) * 128], ps[:, 0:128], AF.Silu, writes=[(ogall, i)])
                    self.act(rrall[:, i * 128:(i + 1) * 128], oacc[:, i, :], AF.Square, accum_out=rdo[:, i:i + 1],
                             writes=[(rrall, i), (rdo, i)])
                self.ts("dve", rdo[:, NT:2 * NT], rdo[:, 0:NT], 1.0 / 128, EPS, ALU.mult, ALU.add, reads=[rdo], writes=[rdo])
                self.act(rdo[:, NT:2 * NT], rdo[:, NT:2 * NT], AF.Sqrt, reads=[rdo], writes=[rdo])
                self.v("dve", "reciprocal", rdo[:, NT:2 * NT], rdo[:, NT:2 * NT], reads=[rdo], writes=[rdo])
                self.tt("dve", rrall[:, 0:SEQ], oacc[:].rearrange("p t c -> p (t c)"), ogall[:, 0:SEQ], ALU.mult,
                        reads=[oacc, ogall], writes=[rrall])
                for i in range(NT):
                    r_ = rrs[i % 2]
                    self.stt("dve", r_[:], rrall[:, i * 128:(i + 1) * 128], rdo[:, NT + i:NT + i + 1], nwb[:], ALU.mult, ALU.mult,
                             reads=[rrall, rdo, nwb], writes=[r_])
                    pt = self.PS[4 + i % 2]
                    self.tr(pt[:, 0:128], r_[:], cst["ident_f"][:])
                    self.copy("act", mixT[:, h, i * 128:(i + 1) * 128], pt[:, 0:128])
            if self.stop_after == "l0_p1":
                raise StopBuild()
            wp = whs[0]
            self.dma("pool", wp[:, :, 0:512], w_in[:, 2560:3072].rearrange("(kc p) n -> p kc n", p=128))
            OFFC, OFFL = 16, 16 + CTX + 16
            WB = OFFL + LAT + 16
            d16 = self.sb("d16", [128, SEQ], BF16)
            for g in range(4):
                w = 2 << g
                half = w // 2
                PBf, T1, T2 = q32, kin, A
                self.memset("pool", PBf[:, 0:WB], 0.0)
                for gi, (g0, gn) in enumerate(groups):
                    ps = self.PS[gi % 2]
                    for kc in range(8):
                        self.mm(ps[:, 0:gn], wp[:, kc, g * 128:(g + 1) * 128], uT[:, kc, g0:g0 + gn], start=(kc == 0), stop=(kc == 7))
                    a0, a1 = g0, g0 + gn
                    if a0 < CTX:
                        n_c = min(a1, CTX) - a0
                        self.copy("act", PBf[:, OFFC + a0:OFFC + a0 + n_c], ps[:, 0:n_c])
                        if a1 > CTX:
                            self.copy("act", PBf[:, OFFL:OFFL + (a1 - CTX)], ps[:, n_c:gn])
                    else:
                        self.copy("act", PBf[:, OFFL + a0 - CTX:OFFL + a1 - CTX], ps[:, 0:gn])
                cur = PBf
                step = 1
                tmp = [T1, T2]
                ti = 0
                width = WB
                while step < w:
                    nxt = tmp[ti % 2]
                    ti += 1
                    width2 = width - step
                    self.tt("dve", nxt[:, 0:width2], cur[:, 0:width2], cur[:, step:step + width2], ALU.add)
                    cur, width, step = nxt, width2, step * 2
                dst = tmp[ti % 2]
                for off, L, tcol in ((OFFC, CTX, 0), (OFFL, LAT, CTX)):
                    sw = cur[:, off - half:off - half + L]
                    self.tt("pool", sw[:, 0:8], sw[:, 0:8], facS[:, g, :], ALU.mult, reads=[cur, facS], writes=[cur])
                    self.tt("pool", sw[:, L - 8:L], sw[:, L - 8:L], facE[:, g, :], ALU.mult, reads=[cur, facE], writes=[cur])
                    self.stt("dve", d16[:, tcol:tcol + L], sw, 1.0 / w, PBf[:, off:off + L], ALU.mult, ALU.subtract,
                             reads=[cur, PBf], writes=[d16])
                for gi, (g0, gn) in enumerate(groups):
                    ps = self.PS[2 + gi % 2]
                    self.mm(ps[:, 0:gn], poolw[:, g, :], d16[:, g0:g0 + gn])
                    self.ts("dve", mixT[:, 4 + g, g0:g0 + gn], ps[:, 0:gn], pscale[:, g:g + 1], None, ALU.mult)
            if "mixT" in self.dbg and b == 0:
                d = self.dbg_dump("mixT", [128, 8, SEQ], BF16)
                self.dma("sp", d[:, :, :], mixT[:])
            self.release(mk)
            if self.stop_after == "l0_p3":
                raise StopBuild()
            self.post_mixer(0, b, mixT, NT, wr, brr)
        self.release(mk0)

    def rms_rows(self, out, src, n, gain_b, sq_junk, c0):
        sm = self.small
        self.act(sq_junk, src, AF.Square, accum_out=sm[:, c0:c0 + 1])
        self.rstd_from_ss(sm[:, c0 + 1:c0 + 2], sm[:, c0:c0 + 1], n)
        self.stt("dve", out, src, sm[:, c0 + 1:c0 + 2], gain_b, ALU.mult, ALU.mult)

    def head_norm(self, xf, gain_b, tmp, hs, rs):
        self.tt("dve", tmp[:], xf[:], xf[:], ALU.mult)
        self.v("dve", "reduce_sum", hs[:], tmp[:], AX.X, reads=[tmp], writes=[hs])
        self.ts("dve", rs[:], hs[:], 1.0 / 96, EPS, ALU.mult, ALU.add)
        self.act(rs[:], rs[:], AF.Sqrt)
        self.v("dve", "reciprocal", rs[:], rs[:], reads=[rs], writes=[rs])
        self.tt("dve", xf[:], xf[:], rs[:].unsqueeze(2).to_broadcast([128, 8, 96]), ALU.mult, reads=[xf, rs], writes=[xf])
        self.tt("dve", xf[:], xf[:], gain_b[:].unsqueeze(1).to_broadcast([128, 8, 96]), ALU.mult, reads=[xf, gain_b], writes=[xf])

    def rope_all(self, xo, xf, cosr, sinr, tt_):
        s5 = xf[:, :, 64:96].rearrange("p h (a t i) -> p h a t i", a=2, t=2)
        d5 = xo[:, :, 64:96].rearrange("p h (a t i) -> p h a t i", a=2, t=2)
        cb = cosr.rearrange("p (a i) -> p a i", a=2).unsqueeze(1).to_broadcast([128, 8, 2, 8])
        sb_ = sinr.rearrange("p (a i) -> p a i", a=2).unsqueeze(1).to_broadcast([128, 8, 2, 8])
        t = [x[:].rearrange("p h (a i) -> p h a i", a=2) for x in tt_]
        x1, x2 = s5[:, :, :, 0, :], s5[:, :, :, 1, :]
        self.tt("dve", t[0], x1, cb, ALU.mult, reads=[xf, cosr], writes=[tt_[0]])
        self.tt("dve", t[1], x2, sb_, ALU.mult, reads=[xf, sinr], writes=[tt_[1]])
        self.tt("dve", t[2], x2, cb, ALU.mult, reads=[xf, cosr], writes=[tt_[2]])
        self.tt("dve", t[3], x1, sb_, ALU.mult, reads=[xf, sinr], writes=[tt_[3]])
        self.tt("dve", d5[:, :, :, 0, :], t[0], t[1], ALU.subtract, reads=[tt_[0], tt_[1]], writes=[xo])
        self.tt("dve", d5[:, :, :, 1, :], t[2], t[3], ALU.add, reads=[tt_[2], tt_[3]], writes=[xo])

    def rope(self, dst, src, cosr, sinr, t1, t2, eng):
        s4 = src.rearrange("p (a h i) -> p a h i", a=2, h=2)
        d4 = dst.rearrange("p (a h i) -> p a h i", a=2, h=2)
        c3 = cosr.rearrange("p (a i) -> p a i", a=2)
        s3 = sinr.rearrange("p (a i) -> p a i", a=2)
        a3 = t1.rearrange("p (a i) -> p a i", a=2)
        b3 = t2.rearrange("p (a i) -> p a i", a=2)
        x1, x2 = s4[:, :, 0, :], s4[:, :, 1, :]
        self.tt(eng, a3, x1, c3, ALU.mult, reads=[src, cosr], writes=[t1])
        self.tt(eng, b3, x2, s3, ALU.mult, reads=[src, sinr], writes=[t2])
        self.tt(eng, d4[:, :, 0, :], a3, b3, ALU.subtract, reads=[t1, t2], writes=[dst])
        self.tt(eng, a3, x2, c3, ALU.mult, reads=[src, cosr], writes=[t1])
        self.tt(eng, b3, x1, s3, ALU.mult, reads=[src, sinr], writes=[t2])
        self.tt(eng, d4[:, :, 1, :], a3, b3, ALU.add, reads=[t1, t2], writes=[dst])

    def layer1(self):
        nc, S, I, C = self.nc, self.S, self.I, self.C
        ns, cst = self.ns, self.cst
        mk0 = self.mark()
        w_in = I["od_w_in"][0]
        NL = LAT // 128
        wr = self.sb("wr1", [128, 8, 32], F32)
        self.dma("sp", wr[:], I["moe_router_w"][1].rearrange("(kc p) e -> p kc e", p=128))
        brr = self.sb("brr1", [1, 32], F32)
        self.dma("sp", brr[:], I["moe_router_b"][1:2, :])
        rcos = self.sb("rcos", [128, 16, 16], F32)
        rsin = self.sb("rsin", [128, 16, 16], F32)
        self.dma("sp", rcos[:], C["rope_cos"])
        self.dma("sp", rsin[:], C["rope_sin"])
        dww = self.sb("dww", [128, 4, 31], F32)
        dwr = self.sb("dwr", [32, 512], F32)
        self.dma("sp", dwr[0:31, :], I["conv_dw_w"][0])
        for cc in range(4):
            self.tr(self.PS[0][:, cc * 32:cc * 32 + 31], dwr[0:31, cc * 128:(cc + 1) * 128], self.cst["ident_f"][0:31, 0:31])
            self.copy("dve", dww[:, cc, :], self.PS[0][:, cc * 32:cc * 32 + 31])
        cpar = self.sb("cpar", [128, 3, 4], F32)
        for j, nm in enumerate(("conv_dw_b", "conv_ln_w", "conv_ln_b")):
            self.dma("sp", cpar[:, j, :], I[nm][0].rearrange("(c p) -> p c", p=128), allow_slow_non_contiguous=True)
        qan = self.sb("qan", [128, 384], F32)
        kvan = self.sb("kvan", [128, 256], F32)
        qnb = self.sb("qnb", [128, 96], F32)
        knb = self.sb("knb", [128, 96], F32)
        self.dma("sp", qan[:], I["mla_q_a_norm"][0:1, :].partition_broadcast(128))
        self.dma("sp", kvan[:], I["mla_kv_a_norm"][0:1, :].partition_broadcast(128))
        self.dma("sp", qnb[:], I["mla_q_norm"][0:1, :].partition_broadcast(128))
        self.dma("sp", knb[:], I["mla_k_norm"][0:1, :].partition_broadcast(128))
        wq = self.sb("wq", [128, 8, 384], BF16)
        wkv = self.sb("wkv", [128, 8, 288], BF16)
        wuq = self.sb("wuq", [128, 3, 768], BF16)
        wukv = self.sb("wukv", [128, 2, 1024], BF16)
        self.dma("pool", wq[:], w_in[:, 1024:1408].rearrange("(kc p) n -> p kc n", p=128))
        self.dma("pool", wkv[:], w_in[:, 1408:1696].rearrange("(kc p) n -> p kc n", p=128))
        self.dma("pool", wuq[:], I["mla_w_uq"][0].rearrange("(kc p) n -> p kc n", p=128))
        self.dma("pool", wukv[:], I["mla_w_ukv"][0].rearrange("(kc p) n -> p kc n", p=128))
        mixA = self.sb("mixA", [128, 4, LAT], BF16)
        sm = self.small
        SC = 96 ** -0.5
        for b in range(ns):
            mkb = self.mark()

            def make_uT(per_tile=None):
                uT = self.sb("uT1", [128, 8, SEQ], BF16) if per_tile is None else None
                mk = self.mark()
                uTts = [self.sb(f"uTt{i}", [128, 8, 128], BF16) for i in range(2)] if per_tile is not None else None
                mr = self.sb("mr", [128, 4, D], F32)
                self.modrow(mr[:, 0, :], 1, b, 1)
                self.modrow(mr[:, 1, :], 1, b, 0)
                self.modrow(mr[:, 2, :], 1, 4, 1)
                self.modrow(mr[:, 3, :], 1, 4, 0)
                self.ts("dve", mr[:, 0, :], mr[:, 0, :], 1.0, None, ALU.add)
                self.ts("dve", mr[:, 2, :], mr[:, 2, :], 1.0, None, ALU.add)
                if per_tile is None:
                    hts = [self.sb(f"ht{i}", [128, D], F32) for i in range(3)]
                    us = [self.sb(f"u{i}", [128, D], F32) for i in range(2)]
                    junk_ = self.sb("junkp", [128, D], F32)
                    srcs = [self.H2[b * SEQ + i * 128:b * SEQ + (i + 1) * 128, :] for i in range(NT)]
                    self.modulate_pipe(uT, srcs, hts, us, junk_, mr, read_keys=[self.H2])
                    self.release(mk)
                    return uT
                hts = [self.sb(f"ht{i}", [128, D], F32) for i in range(2)]
                us = [self.sb(f"u{i}", [128, D], F32) for i in range(1 if per_tile is not None else 2)]
                for i in range(NT):
                    ht, u = hts[i % 2], us[i % len(us)]
                    self.dma("sp", ht[:], self.H2[b * SEQ + i * 128:b * SEQ + (i + 1) * 128, :], reads=[self.H2])
                    o = 2 if i < 2 else 0
                    self.modulate_tile(u[:], ht[:], mr[:, o, :], mr[:, o + 1, :], u[:], sm[:, 0:1], sm[:, 1:2])
                    if per_tile is None:
                        self.transpose_tile_to(uT, i * 128, u, 8, cst["ident_f"], self.PS[0], self.PS[1])
                    else:
                        self.transpose_tile_to(uTts[i % 2], 0, u, 8, cst["ident_f"], self.PS[0], self.PS[1])
                        per_tile(i, uTts[i % 2])
                self.release(mk)
                return uT

            mkA = self.mark()
            uT = make_uT()
            mk = self.mark()
            wc = self.sb("wc", [128, 8, 1024], BF16)
            self.dma("pool", wc[:], w_in[:, 0:1024].rearrange("(kc p) n -> p kc n", p=128))
            hb = self.sb("hb", [128, LAT + 32], F32)
            cv = [self.sb(f"cv{i}", [128, LAT], F32) for i in range(4)]
            sgt = [self.sb(f"sgt{i}", [128, 512], F32) for i in range(2)]
            self.memset("pool", hb[:], 0.0)
            for cc in range(4):
                for tg in range(4):
                    pv, pg = self.PS[(tg % 2) * 2], self.PS[(tg % 2) * 2 + 1]
                    cols = slice(CTX + tg * 512, CTX + (tg + 1) * 512)
                    for kc in range(8):
                        self.mm(pv[:], wc[:, kc, cc * 128:(cc + 1) * 128], uT[:, kc, cols], start=(kc == 0), stop=(kc == 7))
                    for kc in range(8):
                        self.mm(pg[:], wc[:, kc, 512 + cc * 128:512 + (cc + 1) * 128], uT[:, kc, cols], start=(kc == 0), stop=(kc == 7))
                    st = sgt[tg % 2]
                    self.act(st[:], pg[:], AF.Sigmoid)
                    self.tt("dve", hb[:, 15 + tg * 512:15 + (tg + 1) * 512], pv[:], st[:], ALU.mult)
                NH = 4
                HW_ = LAT // NH
                for j in range(31):
                    for hf in range(NH):
                        o0 = hf * HW_
                        acc = cv[cc][:, o0:o0 + HW_]
                        if j == 0:
                            self.ts("dve", acc, hb[:, o0:o0 + HW_], dww[:, cc, 0:1], cpar[:, 0, cc:cc + 1], ALU.mult, ALU.add,
                                    reads=[hb, dww, cpar], writes=[(cv[cc], hf)])
                        else:
                            self.stt("dve", acc, hb[:, o0 + j:o0 + j + HW_], dww[:, cc, j:j + 1], acc, ALU.mult, ALU.add,
                                     reads=[hb, dww, (cv[cc], hf)], writes=[(cv[cc], hf)])
            mean = self.sb("lnm", [128, 512], F32)
            rstd = self.sb("lnr", [128, 512], F32)
            sq = self.sb("lnsq", [128, 512], F32)
            xn = [self.sb(f"lnx{i}", [128, 512], F32) for i in range(2)]
            for tg in range(4):
                cols = slice(tg * 512, (tg + 1) * 512)
                ps_s, ps_q = self.PS[4], self.PS[5]
                for cc in range(4):
                    self.mm(ps_s[:], cst["ones_f"][:], cv[cc][:, cols], start=(cc == 0), stop=(cc == 3))
                for cc in range(4):
                    self.act(sq[:], cv[cc][:, cols], AF.Square)
                    self.mm(ps_q[:], cst["ones_f"][:], sq[:], start=(cc == 0), stop=(cc == 3))
                self.ts("dve", mean[:], ps_s[:], 1.0 / 512, None, ALU.mult)
                self.tt("dve", rstd[:], mean[:], mean[:], ALU.mult)
                self.stt("dve", rstd[:], ps_q[:], 1.0 / 512, rstd[:], ALU.mult, ALU.subtract)
                self.ts("dve", rstd[:], rstd[:], EPS, None, ALU.add)
                self.act(rstd[:], rstd[:], AF.Sqrt)
                self.v("dve", "reciprocal", rstd[:], rstd[:], reads=[rstd], writes=[rstd])
                for cc in range(4):
                    x = xn[cc % 2]
                    self.tt("pool", x[:], cv[cc][:, cols], mean[:], ALU.subtract)
                    self.tt("dve", x[:], x[:], rstd[:], ALU.mult)
                    self.act(mixA[:, cc, cols], x[:], AF.Silu, bias=cpar[:, 2, cc:cc + 1], scale=cpar[:, 1, cc:cc + 1])
            self.release(mkA)
            if self.stop_after == "l1_conv":
                raise StopBuild()
            mixB = self.sb("mixB", [128, 4, LAT], BF16)
            mkq = self.mark()
            qT = self.sb("qT", [128, 8, LAT], BF16)
            kT = self.sb("kT", [128, 8, SEQ], BF16)
            vaug = self.sb("vaug", [128, NT, 8, 68], BF16)
            mk = self.mark()
            self.memset("pool", vaug[:], 1.0)
            cn = self.sb("cn", [128, 384], F32)
            cnT = self.sb("cnT", [128, 3, 128], BF16)
            xf = self.sb("xf", [128, 8, 96], F32)
            xo = self.sb("xo", [128, 8, 96], F32)
            tmp = self.sb("hn_tmp", [128, 8, 96], F32)
            hs = self.sb("hn_hs", [128, 8], F32)
            rs = self.sb("hn_rs", [128, 8], F32)
            rtt = [self.sb(f"rtt{i}", [128, 8, 16], F32) for i in range(4)]
            krr = self.sb("krr", [128, 32], F32)
            krg = self.sb("krg", [128, 32], F32)
            junk2 = self.sb("junk2", [128, 384], F32)
            def proj_tile(i, uTt):
                import os as _os
                _stg = float(_os.environ.get("KSTG", "9"))
                if _stg <= 0:
                    return
                lat_i = i - 2
                tok = slice(i * 128, (i + 1) * 128)
                pk = self.PS[2]
                for kc in range(8):
                    self.mm(pk[:, 0:288], uTt[:, kc, :], wkv[:, kc, :], start=(kc == 0), stop=(kc == 7))
                if _stg <= 0.1:
                    return
                self.rms_rows(cn[:, 0:256], pk[:, 0:256], 256, kvan[:], junk2[:, 0:256], 10)
                if _stg <= 0.2:
                    return
                self.tt("dve", krg[:], pk[:, 256:288], knb[:, 64:96], ALU.mult)
                if _stg <= 0.3:
                    return
                pt = self.PS[3]
                for kc in range(2):
                    self.tr(pt[:, kc * 128:(kc + 1) * 128], cn[:, kc * 128:(kc + 1) * 128], cst["ident_f"][:])
                self.copy("act", cnT[:, 0:2, :], pt[:, 0:256].rearrange("p (k c) -> p k c", c=128))
                if _stg <= 0.4:
                    return
                pkv = [self.PS[4], self.PS[5]]
                for half in range(2):
                    for kc in range(2):
                        self.mm(pkv[half][:], cnT[:, kc, :], wukv[:, kc, half * 512:(half + 1) * 512], start=(kc == 0), stop=(kc == 1))
                    v4 = pkv[half][:, :].rearrange("p (h d) -> p h d", d=128)
                    if _stg <= 0.5:
                        continue
                    if _stg != 0.56:
                        self.copy("act", xf[:, half * 4:(half + 1) * 4, 0:64], v4[:, :, 0:64])
                    if _stg != 0.55:
                        self.copy("act" if _stg == 0.57 else "dve", vaug[:, i, half * 4:(half + 1) * 4, 0:64], v4[:, :, 64:128])
                if _stg <= 0.6:
                    return
                self.copy("dve", xf[:, :, 64:96], pk[:, 256:288].unsqueeze(1).to_broadcast([128, 8, 32]), reads=[pk], writes=[xf])
                if _stg <= 1:
                    return
                self.head_norm(xf, knb, tmp, hs, rs)
                if _stg <= 2:
                    return
                if lat_i >= 0:
                    self.rope_all(xo, xf, rcos[:, lat_i, :], rsin[:, lat_i, :], rtt)
                    self.copy("act", xo[:, :, 0:64], xf[:, :, 0:64])
                    src = xo
                else:
                    src = xf
                pa, pb_ = self.PS[4], self.PS[5]
                for h in range(8):
                    pp = pa if h < 4 else pb_
                    self.tr(pp[0:96, (h % 4) * 128:(h % 4 + 1) * 128], src[:, h, :], cst["ident_f"][:])
                self.copy("act", kT[0:96, 0:4, tok], pa[0:96, :].rearrange("p (k c) -> p k c", c=128))
                self.copy("dve", kT[0:96, 4:8, tok], pb_[0:96, :].rearrange("p (k c) -> p k c", c=128))
                if lat_i < 0 or _stg <= 3:
                    return
                ltok = slice(lat_i * 128, (lat_i + 1) * 128)
                pq = self.PS[2]
                for kc in range(8):
                    self.mm(pq[:, 0:384], uTt[:, kc, :], wq[:, kc, :], start=(kc == 0), stop=(kc == 7))
                if _stg <= 3.1:
                    return
                self.rms_rows(cn[:, 0:384], pq[:, 0:384], 384, qan[:], junk2[:, 0:384], 12)
                if _stg <= 3.2:
                    return
                pt = self.PS[3]
                for kc in range(3):
                    self.tr(pt[:, kc * 128:(kc + 1) * 128], cn[:, kc * 128:(kc + 1) * 128], cst["ident_f"][:])
                self.copy("act", cnT[:, 0:3, :], pt[:, 0:384].rearrange("p (k c) -> p k c", c=128))
                if _stg <= 3.3:
                    return
                pq1, pq2 = self.PS[4], self.PS[5]
                for kc in range(3):
                    self.mm(pq1[:, 0:384], cnT[:, kc, :], wuq[:, kc, 0:384], start=(kc == 0), stop=(kc == 2))
                for kc in range(3):
                    self.mm(pq2[:, 0:384], cnT[:, kc, :], wuq[:, kc, 384:768], start=(kc == 0), stop=(kc == 2))
                if _stg <= 3.4:
                    return
                self.copy("act", xf[:, 0:4, :], pq1[:, 0:384].rearrange("p (h d) -> p h d", d=96))
                self.copy("dve", xf[:, 4:8, :], pq2[:, 0:384].rearrange("p (h d) -> p h d", d=96))
                if _stg <= 3.5:
                    return
                self.head_norm(xf, qnb, tmp, hs, rs)
                if _stg <= 3.6:
                    return
                self.rope_all(xo, xf, rcos[:, lat_i, :], rsin[:, lat_i, :], rtt)
                if _stg <= 3.7:
                    return
                self.copy("act", xo[:, :, 0:64], xf[:, :, 0:64])
                for h in range(8):
                    pp = pa if h < 4 else pb_
                    self.tr(pp[0:96, (h % 4) * 128:(h % 4 + 1) * 128], xo[:, h, :], cst["ident_f"][:])
                if _stg <= 3.8:
                    return
                self.copy("act", qT[0:96, 0:4, ltok], pa[0:96, :].rearrange("p (k c) -> p k c", c=128))
                self.copy("dve", qT[0:96, 4:8, ltok], pb_[0:96, :].rearrange("p (k c) -> p k c", c=128))

            make_uT(per_tile=proj_tile)
            self.release(mk)
            if self.stop_after == "l1_proj":
                raise StopBuild()
            mk = self.mark()
            attn = self.sb("attn", [128, NL, 512], BF16)
            PT = [self.sb(f"PT{i}", [128, 512], BF16) for i in range(4)]
            rden = self.sb("rden", [128, 4], F32)
            n = 0
            for h in range(8):
                for qg in range(4):
                    qcols = slice(qg * 512, (qg + 1) * 512)
                    po = self.PS[4 + (h * 4 + qg) % 2]
                    def s_mm(kt_, n_):
                        ps_ = self.PS[n_ % 4]
                        self.mm(ps_[:], kT[0:96, h, kt_ * 128:(kt_ + 1) * 128], qT[0:96, h, qcols])
                        self.act(PT[n_ % 4][:], ps_[:], AF.Exp, scale=SC)

                    s_mm(0, n)
                    s_mm(1, n + 1)
                    for kt in range(NT):
                        if kt + 2 < NT:
                            s_mm(kt + 2, n + 2)
                        p_ = PT[n % 4]
                        for qt in range(4):
                            self.mm(po[:, qt * 68:(qt + 1) * 68], p_[:, qt * 128:(qt + 1) * 128], vaug[:, kt, h, :],
                                    start=(kt == 0), stop=(kt == NT - 1))
                        n += 1
                    for qt in range(4):
                        self.v("dve", "reciprocal", rden[:, qt:qt + 1], po[:, qt * 68 + 64:qt * 68 + 65], reads=[po], writes=[rden])
                        self.ts("dve", attn[:, qg * 4 + qt, h * 64:(h + 1) * 64], po[:, qt * 68:qt * 68 + 64], rden[:, qt:qt + 1], None, ALU.mult,
                                reads=[po, rden], writes=[(attn, qg * 4 + qt)])
            for i in range(NL):
                pb = self.PB[i % 2]
                for c in range(4):
                    self.tr(pb[:, c * 128:(c + 1) * 128], attn[:, i, c * 128:(c + 1) * 128], cst["ident_b"][:])
                self.copy("act" if i % 2 == 0 else "dve", mixB[:, :, i * 128:(i + 1) * 128], pb[:, 0:512].rearrange("p (k c) -> p k c", c=128))
            self.release(mkq)
            if self.stop_after == "l1_attn":
                raise StopBuild()
            self.post_mixer(1, b, [mixA, mixB], NL, wr, brr)
            self.release(mkb)
        self.release(mk0)

    def post_mixer(self, layer, b, mixT, ntile, wr, brr):
        nc, S, I, C = self.nc, self.S, self.I, self.C
        cst = self.cst
        mk = self.mark()
        w_out = I["ev_w_out"][0] if layer == 0 else I["od_w_out"][0]
        wo = self.sb("wo", [128, 8, D], BF16)
        self.dma("pool", wo[:], w_out.rearrange("(kc p) n -> p kc n", p=128))
        mr = self.sb("mr2", [128, 6, D], F32)
        self.modrow(mr[:, 0, :], layer, b, 2)
        self.modrow(mr[:, 1, :], layer, b, 4)
        self.modrow(mr[:, 2, :], layer, b, 3)
        self.ts("dve", mr[:, 1, :], mr[:, 1, :], 1.0, None, ALU.add)
        if layer == 0:
            self.modrow(mr[:, 3, :], layer, 4, 2)
            self.modrow(mr[:, 4, :], layer, 4, 4)
            self.modrow(mr[:, 5, :], layer, 4, 3)
            self.ts("dve", mr[:, 4, :], mr[:, 4, :], 1.0, None, ALU.add)
        hts = [self.sb(f"pht{i}", [128, D], F32) for i in range(2)]
        h1s = [self.sb(f"ph1{i}", [128, D], F32) for i in range(2)]
        ms = [self.sb(f"pm{i}", [128, D], F32) for i in range(2)]
        m16 = [self.sb(f"pm16{i}", [128, D], BF16) for i in range(2)]
        mT = self.sb("pmT", [128, 8, 128], F32)
        junk = self.sb("pjunk", [128, D], F32)
        smf = self.sb("smf", [128, 8], F32)

        def front(i):
            if layer == 0:
                isctx = i < 2
                src = I["ctx"][b, i * 128:(i + 1) * 128, :] if isctx else I["x"][b, (i - 2) * 128:(i - 1) * 128, :]
                grow = b * SEQ + i * 128
            else:
                isctx = False
                grow = b * LAT + i * 128
                src = self.H2[b * SEQ + CTX + i * 128:b * SEQ + CTX + (i + 1) * 128, :]
            hdst = self.H1
            o = 3 if isctx else 0
            ht, h1, m, mb = hts[i % 2], h1s[i % 2], ms[i % 2], m16[i % 2]
            self.dma("sp", ht[:], src)
            p0, p1 = self.PS[0], self.PS[1]
            for half, ps in enumerate((p0, p1)):
                for kc in range(8):
                    mx = mixT[kc // 4] if isinstance(mixT, (list, tuple)) else mixT
                    kcc = kc % 4 if isinstance(mixT, (list, tuple)) else kc
                    self.mm(ps[:], mx[:, kcc, i * 128:(i + 1) * 128], wo[:, kc, half * 512:(half + 1) * 512],
                            start=(kc == 0), stop=(kc == 7))
                self.tt("dve", h1[:, half * 512:(half + 1) * 512], ps[:], mr[:, o, half * 512:(half + 1) * 512], ALU.mult)
            self.tt("pool", h1[:], h1[:], ht[:], ALU.add)
            self.dma("sp", hdst[grow:grow + 128, :], h1[:], writes=[(hdst, grow)])
            self.modulate_tile(m[:], h1[:], mr[:, o + 1, :], mr[:, o + 2, :], junk[:], smf[:, 0:1], smf[:, 1:2])
            self.copy("act", mb[:], m[:])
            self.dma("sp", self.M[grow:grow + 128, :], mb[:], writes=[(self.M, grow)])
            self.transpose_tile_to(mT, 0, m, 8, cst["ident_f"], self.PS[2], self.PS[3])
            pl = self.PS[4 + i % 2]
            for kc in range(8):
                self.mm(pl[:, 0:32], mT[:, kc, :], wr[:, kc, :], start=(kc == 0), stop=False)
            self.mm(pl[:, 0:32], cst["ones_f"][0:1, :], brr[0:1, :], start=False, stop=True)
            return grow // 128, pl

        pend = front(0)
        for i in range(ntile):
            cur = pend
            if i + 1 < ntile:
                pend = front(i + 1)
            self.route_tile(layer, cur[0], cur[1])
        self.release(mk)

    def route_init(self, layer):
        ns = self.ns
        T = self.T0 if layer == 0 else self.T1
        self.rT = T
        CAP = self.CAP = CAPS[layer]
        NSLOT = self.NSLOT = NE * CAP
        ntt = T // 128
        self.cntb = self.sb(f"cntb{layer}", [128, NE], F32)
        self.smr = self.sb(f"smr{layer}", [128, 16], F32)
        self.DEST = self.sb(f"DEST{layer}", [128, ntt, 4], I32)
        self.GATE = self.sb(f"GATE{layer}", [128, ntt, 4], F32)
        self.rt = {n: self.sb(f"rt_{n}{layer}", sh, dt) for n, sh, dt in (
            ("L", [128, NE], F32), ("t8", [128, 8], F32), ("e4", [128, 4], F32), ("mask", [128, NE], F32),
            ("m16", [128, NE], BF16), ("rf", [128, NE], F32), ("rfs", [128, NE], F32), ("tmp", [128, NE], F32),
            ("rk", [128, 4], F32), ("dk", [128, 4], F32), ("ok", [128, 4], F32), ("tok", [128, 2], F32),
            ("toki", [128, 2], I32), ("trash", [128, 1], F32), ("fill", [128, (NSLOT + 128) // 64], I32),
            ("z16", [128, D], BF16))}
        rt = self.rt
        self.memset("dve", self.cntb[:], 0.0)
        self.ts("dve", rt["trash"][:], self.cst["pidx"][:], float(NSLOT), None, ALU.add)
        self.memset("dve", rt["fill"][:], int(T))
        self.dma("sp", self.SLOT[0:NSLOT + 128, :].rearrange("(p j) two -> p (j two)", p=128), rt["fill"][:])
        self.memset("pool", rt["z16"][:], 0.0)
        self.dma("sp", self.M[T:T + 128, :], rt["z16"][:], writes=[(self.M, "trash")])
        self.dma("sp", self.YB[NSLOT:NSLOT + 128, :], rt["z16"][:], writes=[(self.YB, "trash")])

    def route_tile(self, layer, gt, pl):
        nc, rt, cst = self.nc, self.rt, self.cst
        CAP, NSLOT = self.CAP, self.NSLOT
        L, t8 = rt["L"], rt["t8"]
        self.copy("dve", L[:], pl[:, 0:NE])
        self.v("dve", "max", t8[:], L[:], reads=[L], writes=[t8])
        sm = self.smr
        self.ts("dve", sm[:, 8:9], t8[:, 0:1], -1.0, None, ALU.mult)
        self.act(rt["e4"][:], t8[:, 0:4], AF.Exp, bias=sm[:, 8:9], scale=1.0)
        self.v("dve", "reduce_sum", sm[:, 9:10], rt["e4"][:], AX.X, reads=[rt["e4"]], writes=[sm])
        self.v("dve", "reciprocal", sm[:, 9:10], sm[:, 9:10], reads=[sm], writes=[sm])
        self.ts("dve", rt["e4"][:], rt["e4"][:], sm[:, 9:10], None, ALU.mult)
        self.ts("dve", rt["mask"][:], L[:], t8[:, 3:4], None, ALU.is_ge)
        self.copy("dve", rt["m16"][:], rt["mask"][:])
        pr = pl
        self.mm(pr[:, 128:128 + NE], cst["tri_b"][:], rt["m16"][:])
        self.mm(pr[:, 64:64 + NE], cst["ones_b"][:], rt["m16"][:])
        self.tt("dve", rt["rf"][:], pr[:, 128:128 + NE], self.cntb[:], ALU.add)
        self.tt("dve", self.cntb[:], self.cntb[:], pr[:, 64:64 + NE], ALU.add)
        self.tt("dve", rt["rfs"][:], rt["rf"][:], cst[f"slotbase{layer}"][:], ALU.add)
        for k in range(4):
            self.stt("dve", rt["tmp"][:], L[:], t8[:, k:k + 1], rt["rf"][:], ALU.is_equal, ALU.mult)
            self.v("dve", "reduce_sum", rt["rk"][:, k:k + 1], rt["tmp"][:], AX.X, reads=[rt["tmp"]], writes=[rt["rk"]])
            self.stt("dve", rt["tmp"][:], L[:], t8[:, k:k + 1], rt["rfs"][:], ALU.is_equal, ALU.mult)
            self.v("dve", "reduce_sum", rt["dk"][:, k:k + 1], rt["tmp"][:], AX.X, reads=[rt["tmp"]], writes=[rt["dk"]])
        self.ts("dve", rt["ok"][:], rt["rk"][:], float(CAP), None, ALU.is_lt)
        self.stt("dve", rt["dk"][:], rt["dk"][:], rt["trash"][:, 0:1], rt["ok"][:], ALU.subtract, ALU.mult)
        self.ts("dve", rt["dk"][:], rt["dk"][:], rt["trash"][:, 0:1], None, ALU.add)
        self.tt("dve", self.GATE[:, gt, :], rt["e4"][:], rt["ok"][:], ALU.mult, writes=[(self.GATE, gt)])
        self.copy("dve", self.DEST[:, gt, :], rt["dk"][:], writes=[(self.DEST, gt)])
        self.ts("dve", rt["tok"][:, 0:1], cst["pidx"][:], float(gt * 128), None, ALU.add)
        self.copy("dve", rt["tok"][:, 1:2], rt["tok"][:, 0:1])
        self.copy("dve", rt["toki"][:], rt["tok"][:])
        for k in range(4):
            self.dma("pool", self.SLOT[:, :], rt["toki"][:, :], reads=[rt["toki"], (self.DEST, gt)], writes=[(self.SLOT, (gt, k))],
                     indirect=dict(out_offset=bass.IndirectOffsetOnAxis(ap=self.DEST[:, gt, k:k + 1], axis=0), in_offset=None))

    def experts(self, layer):
        nc, I, cst = self.nc, self.I, self.cst
        CAP, NSLOT = self.CAP, self.NSLOT
        mk = self.mark()
        w1s = [self.sb(f"w1s{i}", [128, 8, 2 * D], BF16) for i in range(2)]
        w2s = [self.sb(f"w2s{i}", [128, 8, D], BF16) for i in range(2)]
        b1s = [self.sb(f"b1s{i}", [128, 16], F32) for i in range(2)]
        b2s = [self.sb(f"b2s{i}", [128, D], F32) for i in range(2)]
        idx = [[self.sb(f"idx{s_}{i}", [128, 2], I32) for i in range(4)] for s_ in range(2)]
        xg = [[self.sb(f"xg{s_}{i}", [128, D], BF16) for i in range(4)] for s_ in range(2)]
        xT = [self.sb(f"xT{i}", [128, 8, 512], BF16) for i in range(2)]
        aT = [self.sb(f"aT{i}", [128, 8, 512], BF16) for i in range(2)]
        glc = [self.sb(f"glc{i}", [128, 512], F32) for i in range(2)]
        sig = [self.sb(f"sig{i}", [128, 512], F32) for i in range(2)]
        lin = [self.sb(f"lin{i}", [128, 512], F32) for i in range(2)]
        yt = [self.sb(f"yt{i}", [128, D], BF16) for i in range(2)]
        for xs in xg:
            for x in xs:
                self.memset("dve", x[:], 0.0)
        NG = CAP // 512
        items = [(e, g) for e in range(NE) for g in range(NG)]

        def load_weights(e):
            w1, w2, b1, b2 = w1s[e % 2], w2s[e % 2], b1s[e % 2], b2s[e % 2]
            src1 = I["moe_w1"][layer, e].rearrange("(kc p) n -> p kc n", p=128)
            for q in range(4):
                self.dma("pool", w1[:, 2 * q:2 * q + 2, :], src1[:, 2 * q:2 * q + 2, :], writes=[(w1, q)])
            src2 = I["moe_w2"][layer, e].rearrange("(kc p) n -> p kc n", p=128)
            for q in range(2):
                self.dma("pool", w2[:, 4 * q:4 * q + 4, :], src2[:, 4 * q:4 * q + 4, :], writes=[(w2, q)])
            self.dma("sp", b1[:], I["moe_b1"][layer, e].rearrange("(c p) -> p c", p=128), allow_slow_non_contiguous=True)
            self.dma("sp", b2[:], I["moe_b2"][layer, e:e + 1, :].partition_broadcast(128))
            self.ts("dve", b1[:, 8:16], b1[:, 8:16], 1.0, None, ALU.add)

        def load_tokens(k):
            e, grp = items[k]
            st = k % 2
            for j in range(4):
                r0 = e * CAP + (grp * 4 + j) * 128
                self.dma("sp", idx[st][j][:], self.SLOT[r0:r0 + 128, :], reads=[self.SLOT])
                self.dma("pool", xg[st][j][:], self.M[:, :], reads=[self.M, idx[st][j]],
                         indirect=dict(out_offset=None, in_offset=bass.IndirectOffsetOnAxis(ap=idx[st][j][:, 0:1], axis=0)))

        def stageT(k):
            e, grp = items[k]
            st = k % 2
            x_t = xT[k % 2]
            for j in range(4):
                pb = self.PB[j % 2]
                for kc in range(8):
                    self.tr(pb[:, kc * 128:(kc + 1) * 128], xg[st][j][:, kc * 128:(kc + 1) * 128], cst["ident_b"][:])
                self.copy("act", x_t[:, :, j * 128:(j + 1) * 128], pb[:, :].rearrange("p (k c) -> p k c", c=128))

        def stageH(k):
            e, grp = items[k]
            w1, b1 = w1s[e % 2], b1s[e % 2]
            x_t, a_t = xT[k % 2], aT[k % 2]
            for jc in range(8):
                pg, plin = self.PS[(jc % 2) * 2], self.PS[(jc % 2) * 2 + 1]
                for kc in range(8):
                    self.mm(pg[:], w1[:, kc, jc * 128:(jc + 1) * 128], x_t[:, kc, :], start=(kc == 0), stop=(kc == 7))
                for kc in range(8):
                    self.mm(plin[:], w1[:, kc, D + jc * 128:D + (jc + 1) * 128], x_t[:, kc, :], start=(kc == 0), stop=(kc == 7))
                g_, s_, l_ = glc[jc % 2], sig[jc % 2], lin[jc % 2]
                self.ts("dve", g_[:], pg[:], b1[:, jc:jc + 1], 7.0, ALU.add, ALU.min)
                self.act(s_[:], g_[:], AF.Sigmoid, scale=1.702)
                self.ts("dve", l_[:], plin[:], b1[:, 8 + jc:9 + jc], 8.0, ALU.add, ALU.min)
                self.tt("dve", g_[:], g_[:], s_[:], ALU.mult)
                self.stt("dve", a_t[:, jc, :], l_[:], -6.0, g_[:], ALU.max, ALU.mult)

        def stageY(k):
            e, grp = items[k]
            w2, b2 = w2s[e % 2], b2s[e % 2]
            a_t = aT[k % 2]
            for jt in range(4):
                y = yt[jt % 2]
                for half in range(2):
                    ps = self.PS[4 + half]
                    for jc in range(8):
                        self.mm(ps[:], a_t[:, jc, jt * 128:(jt + 1) * 128], w2[:, jc, half * 512:(half + 1) * 512],
                                start=(jc == 0), stop=(jc == 7))
                    self.tt("dve", y[:, half * 512:(half + 1) * 512], ps[:], b2[:, half * 512:(half + 1) * 512], ALU.add)
                r0 = e * CAP + (grp * 4 + jt) * 128
                self.dma("sp", self.YB[r0:r0 + 128, :], y[:], writes=[(self.YB, r0)])

        load_weights(0)
        load_tokens(0)
        if len(items) > 1:
            load_tokens(1)
        stageT(0)
        for k in range(len(items)):
            e, grp = items[k]
            if grp == 0 and e + 1 < NE:
                load_weights(e + 1)
            if k + 2 < len(items):
                load_tokens(k + 2)
            stageH(k)
            if k + 1 < len(items):
                stageT(k + 1)
            stageY(k)
        self.release(mk)

    def combine(self, layer):
        nc, I, cst = self.nc, self.I, self.cst
        ns = self.ns
        mk = self.mark()
        g2 = self.sb("g2", [128, 2, D], F32)
        yk = [self.sb(f"yk{i}", [128, D], BF16) for i in range(4)]
        acc = [self.sb(f"acc{i}", [128, D], F32) for i in range(2)]
        h1t = [self.sb(f"h1t{i}", [128, D], F32) for i in range(2)]
        if layer == 0:
            self.modrow(g2[:, 1, :], 0, 4, 5)
        nt_seq = NT if layer == 0 else LAT // 128
        for b in range(ns):
            self.modrow(g2[:, 0, :], layer, b, 5)
            for i in range(nt_seq):
                gt = b * nt_seq + i
                grow = gt * 128
                a, h = acc[gt % 2], h1t[gt % 2]
                self.dma("sp", h[:], self.H1[grow:grow + 128, :], reads=[self.H1])
                for k in range(4):
                    self.dma("pool", yk[k][:], self.YB[:, :], reads=[self.YB, (self.DEST, gt)],
                             indirect=dict(out_offset=None, in_offset=bass.IndirectOffsetOnAxis(ap=self.DEST[:, gt, k:k + 1], axis=0)))
                    if k == 0:
                        self.act(a[:], yk[0][:], AF.Copy, scale=self.GATE[:, gt, 0:1], reads=[yk[0], (self.GATE, gt)], writes=[a])
                    else:
                        self.stt("dve", a[:], yk[k][:], self.GATE[:, gt, k:k + 1], a[:], ALU.mult, ALU.add,
                                 reads=[yk[k], a, (self.GATE, gt)])
                gsel = 1 if (layer == 0 and i < 2) else 0
                self.tt("pool", a[:], a[:], g2[:, gsel, :], ALU.mult)
                self.tt("dve", a[:], a[:], h[:], ALU.add)
                if layer == 0:
                    self.dma("sp", self.H2[grow:grow + 128, :], a[:], writes=[(self.H2, grow)])
                else:
                    self.dma("sp", self.out[b, i * 128:(i + 1) * 128, :], a[:], writes=[("out", grow)])
        self.release(mk)


def build_program(ns, **kw):
    k = K(ns, **kw)
    k.build()
    return k


_CACHE = {}


def kernel(**inputs):
    ncores, ns = 8, 4
    if "k" not in _CACHE:
        _CACHE["k"] = build_program(ns)
    k = _CACHE["k"]
    consts = {"k_" + n: v for n, v in host_consts().items()}
    shared = {n: np.ascontiguousarray(np.asarray(inputs[n], dtype=np.float32)) for n in IN_SPECS if n not in ("x", "c", "ctx")}
    in_maps = []
    for c in range(ncores):
        m = dict(shared)
        for n in ("x", "c", "ctx"):
            m[n] = np.ascontiguousarray(np.asarray(inputs[n], dtype=np.float32)[c * ns:(c + 1) * ns])
        m.update(consts)
        in_maps.append(m)
    res = run_bass_kernel_spmd(k.nc, in_maps, core_ids=list(range(ncores)))
    return np.concatenate([r["out"] for r in res.results], axis=0).astype(np.float32)
```
